# Optimizing a Trainium2 kernel written in Bass

```python
import math
import jax, jax.numpy as jnp
from jax import lax
import numpy as np

D_MODEL = 1024
BATCH = 8
SEQ = 4096
DEPTH = 4

GRID_W = 64
CTX_LEN = 256
CHUNK = 128

GM_HEADS = 4
GM_HD = 64
GM_W = GM_HEADS * GM_HD

ATT_HEADS = 8
KV_HEADS = 2
HEAD_DIM = 64
Q_PER_KV = ATT_HEADS // KV_HEADS
ATT_W = ATT_HEADS * HEAD_DIM
KV_W = KV_HEADS * HEAD_DIM
WINDOW = 128
ROPE_BASE = 10000.0

SSM_HEADS = 4
SSM_HD = 64
SSM_W = SSM_HEADS * SSM_HD
SSM_GROUPS = 2
SSM_STATE = 128
BC_W = SSM_GROUPS * SSM_STATE
CONV_K = 5
XBC_W = SSM_W + 2 * BC_W

MIX_W = GM_W + ATT_W + SSM_W
IN_SIZES = (GM_W, GM_W, ATT_W, KV_W, KV_W, SSM_W, XBC_W, 2 * SSM_HEADS)
IN_W = 2 * GM_W + ATT_W + 2 * KV_W + SSM_W + XBC_W + 2 * SSM_HEADS

D_FF = 2816
N_EXPERTS = 8
TOP_K = 2
N_DENSE = (DEPTH + 1) // 2
N_MOE = DEPTH // 2
EPS = 1e-6

kernel_name = "hybrid_parallel_mixer_dit_trunk"


def rms_norm(x, g):
    x32 = x.astype(jnp.float32)
    y = x32 * lax.rsqrt(jnp.mean(jnp.square(x32), axis=-1, keepdims=True) + EPS)
    return (y * g.astype(jnp.float32)).astype(x.dtype)


def rope_1d(x, pos):
    nf = x.shape[-1] // 2
    freqs = ROPE_BASE ** (-jnp.arange(nf, dtype=jnp.float32) / nf)
    ang = pos.astype(jnp.float32)[:, None] * freqs[None, :]
    cos = jnp.cos(ang)[:, None, :]
    sin = jnp.sin(ang)[:, None, :]
    x32 = x.astype(jnp.float32)
    x1, x2 = x32[..., :nf], x32[..., nf:]
    return jnp.concatenate([x1 * cos - x2 * sin, x2 * cos + x1 * sin], axis=-1).astype(x.dtype)


def axial_rope(x):
    seq_len = x.shape[1]
    rows = seq_len // GRID_W
    row = jnp.repeat(jnp.arange(rows), GRID_W)
    col = jnp.tile(jnp.arange(GRID_W), rows)
    half = HEAD_DIM // 2
    return jnp.concatenate([rope_1d(x[..., :half], row), rope_1d(x[..., half:], col)], axis=-1)


def chunk_gmlp(u, v, g_v, w_s, b_s):
    b, t, _ = u.shape
    u = jax.nn.gelu(u)
    vh = rms_norm(jax.nn.gelu(v).reshape(b, t // CHUNK, CHUNK, GM_HEADS, GM_HD), g_v.reshape(GM_HEADS, GM_HD))
    s = jnp.einsum('hij,bcjhd->bcihd', w_s, vh) + b_s.T[:, :, None]
    return u * s.reshape(b, t, GM_W)


def sink_softmax(logits, sink):
    s = jnp.broadcast_to(sink.astype(jnp.float32)[None, :, :, None, None], logits.shape[:-1] + (1,))
    return jax.nn.softmax(jnp.concatenate([logits, s], axis=-1), axis=-1)[..., :-1]


def window_attention(q, k, v, kc, vc, sink):
    b, l = q.shape[0], q.shape[1]
    nb = l // WINDOW
    scale = HEAD_DIM ** -0.5
    pad = ((0, 0), (WINDOW, WINDOW), (0, 0), (0, 0))

    def band(t):
        tp = jnp.pad(t, pad).reshape(b, nb + 2, WINDOW, KV_HEADS, HEAD_DIM)
        return jnp.concatenate([tp[:, :-2], tp[:, 1:-1], tp[:, 2:]], axis=2)

    qb = jnp.moveaxis(q.reshape(b, nb, WINDOW, KV_HEADS, Q_PER_KV, HEAD_DIM), 1, 0)
    kb = jnp.moveaxis(band(k), 1, 0)
    vb = jnp.moveaxis(band(v), 1, 0)
    qi = jnp.arange(WINDOW)[:, None]
    ki = jnp.arange(3 * WINDOW)[None, :]
    rel = ki - qi

    def block(args):
        qn, kn, vn, n = args
        j = n * WINDOW + ki - WINDOW
        valid = (rel >= 0) & (rel <= 2 * WINDOW) & (j >= 0) & (j < l)
        s_loc = jnp.einsum('bqkgd,bskd->bkgqs', qn, kn).astype(jnp.float32) * scale
        s_loc = jnp.where(valid, s_loc, -jnp.inf)
        s_ctx = jnp.einsum('bqkgd,bskd->bkgqs', qn, kc).astype(jnp.float32) * scale
        p = sink_softmax(jnp.concatenate([s_loc, s_ctx], axis=-1), sink).astype(vn.dtype)
        return (jnp.einsum('bkgqs,bskd->bqkgd', p[..., :3 * WINDOW], vn)
                + jnp.einsum('bkgqs,bskd->bqkgd', p[..., 3 * WINDOW:], vc))

    out = lax.map(block, (qb, kb, vb, jnp.arange(nb)))
    return jnp.moveaxis(out, 0, 1).reshape(b, l, ATT_W)


def context_attention(qc, kc, vc, sink):
    b, cl = qc.shape[0], qc.shape[1]
    s = jnp.einsum('bqkgd,bskd->bkgqs', qc, kc).astype(jnp.float32) * (HEAD_DIM ** -0.5)
    p = sink_softmax(s, sink).astype(vc.dtype)
    return jnp.einsum('bkgqs,bskd->bqkgd', p, vc).reshape(b, cl, ATT_W)


def dw_conv(x, w, bias):
    y = lax.conv_general_dilated(x, w[:, None, :], window_strides=(1,),
                                 padding=[(CONV_K // 2, CONV_K // 2)],
                                 dimension_numbers=('NWC', 'WIO', 'NWC'),
                                 feature_group_count=x.shape[-1])
    return y + bias


def ssd_scan(xh, dt, a, bm, cm, s0, want_y):
    b, t, nh, hp = xh.shape
    ns = bm.shape[-1]
    nc = t // CHUNK
    xq = xh.reshape(b, nc, CHUNK, nh, hp)
    dq = dt.reshape(b, nc, CHUNK, nh)
    bq = bm.reshape(b, nc, CHUNK, nh, ns)
    cq = cm.reshape(b, nc, CHUNK, nh, ns)
    a_cum = jnp.cumsum(dq * a, axis=2)
    a_last = a_cum[:, :, -1]
    w_end = jnp.exp(a_last[:, :, None] - a_cum) * dq
    states = jnp.einsum('bcjhn,bcjh,bcjhp->bchpn', bq, w_end, xq)

    def step(s, inp):
        st, al = inp
        return jnp.exp(al)[:, :, None, None] * s + st, s

    s_fin, s_in = lax.scan(step, s0, (jnp.moveaxis(states, 1, 0), jnp.moveaxis(a_last, 1, 0)))
    if not want_y:
        return None, s_fin
    s_in = jnp.moveaxis(s_in, 0, 1)
    tri = jnp.tril(jnp.ones((CHUNK, CHUNK), dtype=bool))[None, None, :, :, None]
    seg = jnp.exp(jnp.where(tri, a_cum[:, :, :, None, :] - a_cum[:, :, None, :, :], -jnp.inf))
    mix = jnp.einsum('bcihn,bcjhn->bcijh', cq, bq) * seg * dq[:, :, None]
    y = (jnp.einsum('bcijh,bcjhp->bcihp', mix, xq)
         + jnp.einsum('bcihn,bchpn->bcihp', cq, s_in) * jnp.exp(a_cum)[..., None])
    return y.reshape(b, t, nh, hp), s_fin


def ssd_direction(xh, dt, a, bm, cm, s0, want_y, reverse):
    if reverse:
        xh, dt, bm, cm = (jnp.flip(t, axis=1) for t in (xh, dt, bm, cm))
    y, s = ssd_scan(xh, dt, a, bm, cm, s0, want_y)
    if reverse and y is not None:
        y = jnp.flip(y, axis=1)
    return y, s


def ssd_prep(xbc, dt_raw, conv_w, conv_b):
    b, t, _ = xbc.shape
    xbc = jax.nn.silu(dw_conv(xbc, conv_w, conv_b)).astype(jnp.float32)
    xs, bm, cm = jnp.split(xbc, [SSM_W, SSM_W + BC_W], axis=-1)
    rep = SSM_HEADS // SSM_GROUPS
    xh = xs.reshape(b, t, SSM_HEADS, SSM_HD)
    bm = jnp.repeat(bm.reshape(b, t, SSM_GROUPS, SSM_STATE), rep, axis=2)
    cm = jnp.repeat(cm.reshape(b, t, SSM_GROUPS, SSM_STATE), rep, axis=2)
    return xh, bm, cm, dt_raw.reshape(b, t, 2, SSM_HEADS).astype(jnp.float32)


def ssd_mixer(z, xbc, dt_raw, z_c, xbc_c, dt_raw_c, conv_w, conv_b, dt_bias, a_log, d_skip, norm_g, need_ctx):
    xh, bm, cm, dtr = ssd_prep(xbc, dt_raw, conv_w, conv_b)
    xhc, bmc, cmc, dtrc = ssd_prep(xbc_c, dt_raw_c, conv_w, conv_b)
    dsk = d_skip.astype(jnp.float32)[:, None]
    y_lat = dsk * xh
    y_ctx = dsk * xhc
    s0 = jnp.zeros((xh.shape[0], SSM_HEADS, SSM_HD, SSM_STATE), jnp.float32)
    for d in range(2):
        a = -jnp.exp(a_log[d].astype(jnp.float32))
        bias = dt_bias[d].astype(jnp.float32)
        dt = jax.nn.softplus(dtr[:, :, d] + bias)
        dtc = jax.nn.softplus(dtrc[:, :, d] + bias)
        yc, sc = ssd_direction(xhc, dtc, a, bmc, cmc, s0, need_ctx, d == 1)
        yl, _ = ssd_direction(xh, dt, a, bm, cm, sc, True, d == 1)
        y_lat = y_lat + yl
        if need_ctx:
            y_ctx = y_ctx + yc

    def gate_norm(y, zz):
        b, t = zz.shape[0], zz.shape[1]
        return rms_norm(y.reshape(b, t, SSM_W) * jax.nn.silu(zz.astype(jnp.float32)), norm_g).astype(zz.dtype)

    out_c = gate_norm(y_ctx, z_c) if need_ctx else None
    return gate_norm(y_lat, z), out_c


def swiglu(h, wg, wu, wd):
    return (jax.nn.silu(h @ wg) * (h @ wu)) @ wd


def moe_ffn(h, w_r, wg, wu, wd):
    logits = (h @ w_r).astype(jnp.float32)
    top_v, top_i = lax.top_k(logits, TOP_K)
    gates = jax.nn.softmax(top_v, axis=-1)
    combine = jnp.sum(jax.nn.one_hot(top_i, N_EXPERTS, dtype=jnp.float32) * gates[..., None], axis=-2).astype(h.dtype)
    out = jnp.zeros_like(h)
    for e in range(N_EXPERTS):
        out = out + combine[..., e:e + 1] * swiglu(h, wg[e], wu[e], wd[e])
    return out


def qk_heads(t, n_heads, g):
    return rms_norm(t.reshape(t.shape[0], t.shape[1], n_heads, HEAD_DIM), g)


def setup_inputs(seed: int = 0) -> dict:
    key = jax.random.key(seed)
    ks = jax.random.split(key, 32)
    f32 = jnp.float32

    def nrm(k, shape, scale):
        return jax.random.normal(k, shape, f32) * scale

    def gain(k, shape):
        return 1.0 + 0.02 * jax.random.normal(k, shape, f32)

    dt0 = jnp.exp(jax.random.uniform(ks[20], (DEPTH, 2, SSM_HEADS), f32, math.log(1e-3), math.log(1e-1)))
    return {
        "x": nrm(ks[0], (BATCH, SEQ, D_MODEL), 1.0),
        "c": nrm(ks[1], (BATCH, D_MODEL), 1.0),
        "ctx": nrm(ks[2], (BATCH, CTX_LEN, D_MODEL), 1.0),
        "c_ctx": nrm(ks[3], (D_MODEL,), 1.0),
        "w_mod": nrm(ks[4], (DEPTH, D_MODEL, 6 * D_MODEL), 0.5 * D_MODEL ** -0.5),
        "b_mod": nrm(ks[5], (DEPTH, 6 * D_MODEL), 0.02),
        "norm1_g": gain(ks[6], (DEPTH, D_MODEL)),
        "norm2_g": gain(ks[7], (DEPTH, D_MODEL)),
        "w_in": nrm(ks[8], (DEPTH, D_MODEL, IN_W), D_MODEL ** -0.5),
        "w_out": nrm(ks[9], (DEPTH, MIX_W, D_MODEL), MIX_W ** -0.5),
        "gm_v_g": gain(ks[10], (DEPTH, GM_W)),
        "gm_ws": nrm(ks[11], (DEPTH, GM_HEADS, CHUNK, CHUNK), 0.5 * CHUNK ** -0.5),
        "gm_bs": gain(ks[12], (DEPTH, GM_HEADS, CHUNK)),
        "att_q_g": gain(ks[13], (DEPTH, HEAD_DIM)),
        "att_k_g": gain(ks[14], (DEPTH, HEAD_DIM)),
        "att_sink": nrm(ks[15], (DEPTH, ATT_HEADS), 0.5),
        "ssm_conv_w": nrm(ks[16], (DEPTH, CONV_K, XBC_W), CONV_K ** -0.5),
        "ssm_conv_b": nrm(ks[17], (DEPTH, XBC_W), 0.02),
        "ssm_dt_bias": dt0 + jnp.log(-jnp.expm1(-dt0)),
        "ssm_a_log": jnp.log(jax.random.uniform(ks[18], (DEPTH, 2, SSM_HEADS), f32, 1.0, 16.0)),
        "ssm_d": 1.0 + 0.1 * jax.random.normal(ks[19], (DEPTH, SSM_HEADS), f32),
        "ssm_norm_g": gain(ks[21], (DEPTH, SSM_W)),
        "ffn_w_gate": nrm(ks[22], (N_DENSE, D_MODEL, D_FF), D_MODEL ** -0.5),
        "ffn_w_up": nrm(ks[23], (N_DENSE, D_MODEL, D_FF), D_MODEL ** -0.5),
        "ffn_w_down": nrm(ks[24], (N_DENSE, D_FF, D_MODEL), D_FF ** -0.5),
        "moe_router": nrm(ks[25], (N_MOE, D_MODEL, N_EXPERTS), D_MODEL ** -0.5),
        "moe_w_gate": nrm(ks[26], (N_MOE, N_EXPERTS, D_MODEL, D_FF), D_MODEL ** -0.5),
        "moe_w_up": nrm(ks[27], (N_MOE, N_EXPERTS, D_MODEL, D_FF), D_MODEL ** -0.5),
        "moe_w_down": nrm(ks[28], (N_MOE, N_EXPERTS, D_FF, D_MODEL), D_FF ** -0.5),
    }


def reference(x, c, ctx, c_ctx, w_mod, b_mod, norm1_g, norm2_g, w_in, w_out, gm_v_g, gm_ws, gm_bs,
              att_q_g, att_k_g, att_sink, ssm_conv_w, ssm_conv_b, ssm_dt_bias, ssm_a_log, ssm_d,
              ssm_norm_g, ffn_w_gate, ffn_w_up, ffn_w_down, moe_router, moe_w_gate, moe_w_up, moe_w_down):
    b, l = x.shape[0], x.shape[1]
    cl = ctx.shape[1]
    split_idx = [int(v) for v in np.cumsum(IN_SIZES)[:-1]]
    c_silu = jax.nn.silu(c)
    cc_silu = jax.nn.silu(c_ctx)
    xc = ctx
    for i in range(DEPTH):
        need_ctx = i < DEPTH - 1
        mod = jnp.split((c_silu @ w_mod[i] + b_mod[i])[:, None, :], 6, axis=-1)
        modc = jnp.split(cc_silu @ w_mod[i] + b_mod[i], 6, axis=-1)

        h = rms_norm(x, norm1_g[i]) * (1 + mod[1]) + mod[0]
        hc = rms_norm(xc, norm1_g[i]) * (1 + modc[1]) + modc[0]
        gu, gv, pq, pk, pv, pz, pxbc, pdt = jnp.split(h @ w_in[i], split_idx, axis=-1)
        cu, cv, cq, ck, cvv, cz, cxbc, cdt = jnp.split(hc @ w_in[i], split_idx, axis=-1)

        gm = chunk_gmlp(gu, gv, gm_v_g[i], gm_ws[i], gm_bs[i])

        sink = att_sink[i].reshape(KV_HEADS, Q_PER_KV)
        q = axial_rope(qk_heads(pq, ATT_HEADS, att_q_g[i])).reshape(b, l, KV_HEADS, Q_PER_KV, HEAD_DIM)
        k = axial_rope(qk_heads(pk, KV_HEADS, att_k_g[i]))
        v = pv.reshape(b, l, KV_HEADS, HEAD_DIM)
        kc = qk_heads(ck, KV_HEADS, att_k_g[i])
        vc = cvv.reshape(b, cl, KV_HEADS, HEAD_DIM)
        att = window_attention(q, k, v, kc, vc, sink)

        ssm, ssm_c = ssd_mixer(pz, pxbc, pdt, cz, cxbc, cdt, ssm_conv_w[i], ssm_conv_b[i], ssm_dt_bias[i],
                               ssm_a_log[i], ssm_d[i], ssm_norm_g[i], need_ctx)

        x = x + mod[2] * (jnp.concatenate([gm, att, ssm], axis=-1) @ w_out[i])
        if need_ctx:
            gm_c = chunk_gmlp(cu, cv, gm_v_g[i], gm_ws[i], gm_bs[i])
            qc = qk_heads(cq, ATT_HEADS, att_q_g[i]).reshape(b, cl, KV_HEADS, Q_PER_KV, HEAD_DIM)
            att_c = context_attention(qc, kc, vc, sink)
            xc = xc + modc[2] * (jnp.concatenate([gm_c, att_c, ssm_c], axis=-1) @ w_out[i])

        hf = rms_norm(x, norm2_g[i]) * (1 + mod[4]) + mod[3]
        if need_ctx:
            hfc = rms_norm(xc, norm2_g[i]) * (1 + modc[4]) + modc[3]
            tokens = jnp.concatenate([hfc, hf], axis=1)
        else:
            tokens = hf
        j = i // 2
        if i % 2 == 0:
            f = swiglu(tokens, ffn_w_gate[j], ffn_w_up[j], ffn_w_down[j])
        else:
            f = moe_ffn(tokens, moe_router[j], moe_w_gate[j], moe_w_up[j], moe_w_down[j])
        if need_ctx:
            xc = xc + modc[5] * f[:, :cl]
            f = f[:, cl:]
        x = x + mod[5] * f
    return x
```

```python
import contextlib
import numpy as np
import concourse.bass as bass
import concourse.mybir as mybir
from concourse.bass_utils import run_bass_kernel_spmd

F32, BF16 = mybir.dt.float32, mybir.dt.bfloat16
AF = mybir.ActivationFunctionType
ALU = mybir.AluOpType
AX = mybir.AxisListType

D = 1024
L = 4096
CL = 256
NT = L + CL
NTILE = NT // 128
DEPTH = 4
DFF = 2816
NEXP = 8
EPS = 1e-6
FGROUPS = [(0, 6), (6, 6), (12, 5), (17, 5)]
BLOCKS = [(0, 256, True)] + [(256 + 512 * i, 512, False) for i in range(8)]


SIM_FRESH_POOL = False


class KB:
    def __init__(self, nc, stack):
        self.nc = nc
        self.stack = stack
        self.eng = {"pe": nc.tensor, "dve": nc.vector, "act": nc.scalar, "pool": nc.gpsimd, "sp": nc.sync}
        self.esem = {e: stack.enter_context(nc.semaphore("es_" + e)) for e in self.eng}
        self.ecnt = {e: 0 for e in self.eng}
        self.seen = {e: {} for e in self.eng}
        self.lw = {}
        self.rd = {}
        self.dsem = {}
        self.free = []
        self.nsem = 0
        self.dead = False

    def _wait(self, E, tok):
        sem, val, _ = tok
        sid = id(sem)
        if self.seen[E].get(sid, 0) >= val:
            return
        self.eng[E].wait_ge(sem, val)
        self.seen[E][sid] = val

    def _deps(self, E, reads, writes):
        for k in reads:
            w = self.lw.get(k)
            if w is not None:
                if w[2] == E and E == "pe":
                    continue
                self._wait(E, w)
        for k in writes:
            w = self.lw.get(k)
            if w is not None and w[2] != E:
                self._wait(E, w)
            for r in self.rd.get(k, {}).values():
                if r[2] != E:
                    self._wait(E, r)

    def _record(self, tok, reads, writes):
        for k in writes:
            self.lw[k] = tok
            self.rd[k] = {}
        for k in reads:
            self.rd.setdefault(k, {})[id(tok[0])] = tok

    def op(self, E, fn, reads=(), writes=()):
        if self.dead:
            return None
        self._deps(E, reads, writes)
        inst = fn()
        self.ecnt[E] += 1
        inst.then_inc(self.esem[E], 1)
        tok = (self.esem[E], self.ecnt[E], E)
        self._record(tok, reads, writes)
        return tok

    def dma(self, E, out, in_, reads=(), writes=(), semkey=None):
        if self.dead:
            return None
        if SIM_FRESH_POOL and E == "pool":
            self.nsem += 1
            semkey = ("__fresh", self.nsem)
            self.dsem[semkey] = [self.stack.enter_context(self.nc.semaphore("dp%d" % self.nsem)), 0]
        if semkey not in self.dsem:
            if self.free:
                self.dsem[semkey] = self.free.pop()
            else:
                self.nsem += 1
                self.dsem[semkey] = [self.stack.enter_context(self.nc.semaphore("ds%d" % self.nsem)), 0]
        ent = self.dsem[semkey]
        self._deps(E, reads, writes)
        if ent[1] > 0:
            self._wait(E, (ent[0], ent[1], "dma"))
        ent[1] += 16
        self.eng[E].dma_start(out=out, in_=in_).then_inc(ent[0], 16)
        tok = (ent[0], ent[1], "dma:" + str(semkey))
        self._record(tok, reads, writes)
        return tok

    def barrier(self):
        if self.dead:
            return
        for E in self.eng:
            for Fn in self.eng:
                if Fn != E and self.ecnt[Fn] > 0:
                    self._wait(E, (self.esem[Fn], self.ecnt[Fn], Fn))
            for ent in self.dsem.values():
                if ent[1] > 0:
                    self._wait(E, (ent[0], ent[1], "dma"))
        self.lw = {}
        self.rd = {}
        self.free.extend(v for k, v in self.dsem.items() if not (isinstance(k, tuple) and k and k[0] == "__fresh"))
        self.dsem = {}


class _Stop(Exception):
    pass


def build_program(nlayers=DEPTH, debug=False, stop=None):
    nc = bass.Bass("TRN2", target_bir_lowering=False)
    stack = contextlib.ExitStack()
    with stack:
        try:
            _emit(nc, stack, nlayers, debug, stop)
        except _Stop:
            pass
    return nc


def _emit(nc, stack, nlayers, debug, stop=None):
    kb = KB(nc, stack)
    _bar = kb.barrier
    _phase = [0]

    def barrier_named():
        _bar()
        _phase[0] += 1
        if stop is not None and _phase[0] >= stop:
            kb.dead = True

    kb.barrier = barrier_named

    def din(name, shape, dt=F32):
        return nc.dram_tensor(name, list(shape), dt, kind="ExternalInput").ap()

    def dscr(name, shape, dt):
        kind = "ExternalOutput" if debug else None
        if kind:
            return nc.dram_tensor(name, list(shape), dt, kind=kind).ap()
        return nc.dram_tensor(name, list(shape), dt).ap()

    xT0 = din("xT0", [D, NT])
    cs_in = din("cs", [128, 8, 2])
    w_mod = din("w_mod", [DEPTH, D, 6 * D])
    bmodT = din("bmodT", [128, DEPTH, 48])
    g1T = din("g1T", [128, DEPTH, 8])
    g2T = din("g2T", [128, DEPTH, 8])
    w_in = din("w_in", [DEPTH, D, 2312])
    w_out = din("w_out", [DEPTH, D, D])
    gvg = din("gvg", [DEPTH, 256])
    wsT = din("wsT", [DEPTH, 4, 128, 128])
    gbs = din("gbs", [128, DEPTH, 2, 128])
    qgT = din("qgT", [128, DEPTH])
    kgT = din("kgT", [128, DEPTH])
    sinkL = din("sinkL", [128, DEPTH, 2, 2])
    convw = din("convw", [128, DEPTH, 6, 5])
    convb = din("convb", [128, DEPTH, 6])
    dtb = din("dtb", [DEPTH, 8])
    alog = din("alog", [DEPTH, 8])
    dskE = din("dskE", [DEPTH, 256])
    sng = din("sng", [DEPTH, 256])
    ffn_g = din("ffn_g", [2, D, DFF])
    ffn_u = din("ffn_u", [2, D, DFF])
    ffn_d = din("ffn_d", [2, DFF, D])
    moe_r = din("moe_r", [2, D, NEXP])
    _small = nlayers < 2
    moe_g = din("moe_g", [2, NEXP, D, DFF] if not _small else [2, NEXP, 128, 768])
    moe_u = din("moe_u", [2, NEXP, D, DFF] if not _small else [2, NEXP, 128, 768])
    moe_d = din("moe_d", [2, NEXP, DFF, D] if not _small else [2, NEXP, 128, 768])
    c_ident = din("c_ident", [128, 128])
    c_bones = din("c_bones", [128, 128])
    c_perm = din("c_perm", [128, 128])
    c_uf = din("c_uf", [128, 128])
    c_ub = din("c_ub", [128, 128])
    c_negf = din("c_negf", [128, 512])
    c_negb = din("c_negb", [128, 512])
    c_ropeC = din("c_ropeC", [128, L])
    c_ropeS = din("c_ropeS", [128, L])

    outT = nc.dram_tensor("outT", [D, L], F32, kind="ExternalOutput").ap()

    R = dscr("R", [D, NT], F32)
    GU = dscr("GU", [256, NT], BF16)
    GV = dscr("GV", [NT, 256], BF16)
    QT = dscr("QT", [512, NT], BF16)
    KT = dscr("KT", [256, NT], BF16)
    V = dscr("V", [NT, 128], BF16)
    ZS = dscr("ZS", [NT, 256], BF16)
    XBC = dscr("XBC", [768, NT], BF16)
    DT = dscr("DT", [NT, 16], F32)
    MIX = dscr("MIX", [D, NT], BF16)
    HT = dscr("HT", [D, NT], BF16)
    COMBT = dscr("COMBT", [NEXP, NT], F32)

    _uniq = [0]

    def sb(st, name, shape, dt):
        _uniq[0] += 1
        return st.enter_context(nc.sbuf_tensor("%s_u%d" % (name, _uniq[0]), list(shape), dt))

    PS = [stack.enter_context(nc.psum_tensor("ps%d" % i, [128, 512], F32)) for i in range(8)]

    ident_bf = sb(stack, "ident_bf", [128, 128], BF16)
    ident_f = sb(stack, "ident_f", [128, 128], F32)
    ones_bf = sb(stack, "ones_bf", [128, 128], BF16)
    bones_bf = sb(stack, "bones_bf", [128, 128], BF16)
    perm_bf = sb(stack, "perm_bf", [128, 128], BF16)
    uf_bf = sb(stack, "uf_bf", [128, 128], BF16)
    ub_bf = sb(stack, "ub_bf", [128, 128], BF16)
    ones_f = sb(stack, "ones_f", [128, 64], F32)
    eps_t = sb(stack, "eps_t", [128, 1], F32)
    one_t = sb(stack, "one_t", [128, 1], F32)
    mods = sb(stack, "mods", [128, DEPTH, 48, 2], F32)
    G1 = sb(stack, "G1", [128, DEPTH, 8, 2], F32)
    G2 = sb(stack, "G2", [128, DEPTH, 8, 2], F32)
    g1s = sb(stack, "g1s", [128, DEPTH, 8], F32)
    g2s = sb(stack, "g2s", [128, DEPTH, 8], F32)
    bms = sb(stack, "bms", [128, DEPTH, 48], F32)
    css = sb(stack, "css", [128, 8, 2], F32)
    qgs = sb(stack, "qgs", [128, DEPTH], F32)
    kgs = sb(stack, "kgs", [128, DEPTH], F32)
    sinkE = sb(stack, "sinkE", [128, DEPTH, 2, 2], F32)
    cws = sb(stack, "cws", [128, DEPTH, 6, 5], F32)
    cbs = sb(stack, "cbs", [128, DEPTH, 6], F32)

    sp, pool = "sp", "pool"
    kb.dma(pool, ident_bf[:], c_ident, writes=["ident_bf"], semkey="c0")
    kb.dma(sp, ident_f[:], c_ident, writes=["ident_f"], semkey="c1")
    kb.dma(pool, bones_bf[:], c_bones, writes=["bones_bf"], semkey="c2")
    kb.dma(pool, perm_bf[:], c_perm, writes=["perm_bf"], semkey="c3")
    kb.dma(pool, uf_bf[:], c_uf, writes=["uf_bf"], semkey="c4")
    kb.dma(pool, ub_bf[:], c_ub, writes=["ub_bf"], semkey="c5")
    kb.dma(sp, g1s[:], g1T, writes=["g1s"], semkey="c6")
    kb.dma(sp, g2s[:], g2T, writes=["g2s"], semkey="c7")
    kb.dma(sp, bms[:], bmodT, writes=["bms"], semkey="c8")
    kb.dma(sp, css[:], cs_in, writes=["css"], semkey="c9")
    kb.dma(sp, qgs[:], qgT, writes=["qgs"], semkey="c10")
    kb.dma(sp, kgs[:], kgT, writes=["kgs"], semkey="c11")
    kb.dma(sp, sinkE[:], sinkL, writes=["sinkE"], semkey="c12")
    kb.dma(sp, cws[:], convw, writes=["cws"], semkey="c13")
    kb.dma(sp, cbs[:], convb, writes=["cbs"], semkey="c14")
    kb.op("dve", lambda: nc.vector.memset(ones_bf[:], 1.0), writes=["ones_bf"])
    kb.op("dve", lambda: nc.vector.memset(ones_f[:], 1.0), writes=["ones_f"])
    kb.op("dve", lambda: nc.vector.memset(eps_t[:], EPS), writes=["eps_t"])
    kb.op("dve", lambda: nc.vector.memset(one_t[:], 1.0), writes=["one_t"])
    kb.op("act", lambda: nc.scalar.activation(out=css[:], in_=css[:], func=AF.Silu), reads=["css"], writes=["css"])
    kb.op("act", lambda: nc.scalar.activation(out=sinkE[:], in_=sinkE[:], func=AF.Exp), reads=["sinkE"], writes=["sinkE"])

    with contextlib.ExitStack() as st:
        wm = [sb(st, "wm%d" % i, [128, 8, 512], F32) for i in range(2)]
        it = 0
        for l in range(nlayers):
            for gidx in range(12):
                s = it % 2
                it += 1
                kb.dma(sp, wm[s][:], w_mod[l].rearrange("(k p) n -> p k n", p=128)[:, :, gidx * 512:(gidx + 1) * 512],
                       writes=[("wm", s)], semkey=("wm", s))
                for jj in range(4):
                    j = gidx * 4 + jj
                    for k in range(8):
                        kb.op("pe", lambda k=k, jj=jj, j=j, s=s: nc.tensor.matmul(
                            PS[0][:, j * 2:j * 2 + 2], lhsT=wm[s][:, k, jj * 128:(jj + 1) * 128], rhs=css[:, k, :],
                            start=(k == 0), stop=(k == 7)), reads=[("wm", s), "css"], writes=["ps0"])
            kb.op("dve", lambda l=l: nc.vector.tensor_tensor(
                out=mods[:, l, :, :], in0=PS[0][:, 0:96].rearrange("p (a b) -> p a b", b=2),
                in1=bms[:, l, :].unsqueeze(2).broadcast_to([128, 48, 2]), op=ALU.add),
                reads=["ps0", "bms"], writes=["mods"])
            for (Gt, gs, off) in ((G1, g1s, 8), (G2, g2s, 32)):
                kb.op("dve", lambda Gt=Gt, off=off, l=l: nc.vector.tensor_scalar(
                    out=Gt[:, l, :, :], in0=mods[:, l, off:off + 8, :], scalar1=1.0, scalar2=None, op0=ALU.add),
                    reads=["mods"], writes=["G"])
                kb.op("dve", lambda Gt=Gt, gs=gs, l=l: nc.vector.tensor_tensor(
                    out=Gt[:, l, :, :], in0=Gt[:, l, :, :], in1=gs[:, l, :].unsqueeze(2).broadcast_to([128, 8, 2]),
                    op=ALU.mult), reads=["G", "g1s", "g2s"], writes=["G"])
        kb.barrier()

    def rmsnorm_block(st_tiles, xt, T, Gt, l, shift_off, which, out_tile, out_key, hf=None):
        sq, rstd, tmp = st_tiles
        for k in range(8):
            s = k % 2
            kb.op("act", lambda k=k, s=s: nc.scalar.activation(out=sq[s][:, :T], in_=xt[:, k, :T], func=AF.Square),
                  reads=["xt"], writes=[("sq", s)])
            kb.op("pe", lambda k=k, s=s: nc.tensor.matmul(PS[7][:, :T], lhsT=ones_bf[:], rhs=sq[s][:, :T],
                                                          start=(k == 0), stop=(k == 7)),
                  reads=[("sq", s), "ones_bf"], writes=["ps7"])
        kb.op("act", lambda: nc.scalar.activation(out=rstd[:, :T], in_=PS[7][:, :T], func=AF.Sqrt,
                                                  bias=eps_t[:], scale=1.0 / D), reads=["ps7", "eps_t"], writes=["rstd"])
        kb.op("dve", lambda: nc.vector.reciprocal(out=rstd[:, :T], in_=rstd[:, :T]), reads=["rstd"], writes=["rstd"])
        for k in range(8):
            s = k % 2
            kb.op("dve", lambda k=k, s=s: nc.vector.scalar_tensor_tensor(
                out=tmp[s][:, :T], in0=xt[:, k, :T], scalar=Gt[:, l, k, which:which + 1], in1=rstd[:, :T],
                op0=ALU.mult, op1=ALU.mult), reads=["xt", "G", "rstd"], writes=[("tmp", s)])
            if hf is None:
                kb.op("act", lambda k=k, s=s: nc.scalar.activation(
                    out=out_tile[:, k, :T], in_=tmp[s][:, :T], func=AF.Identity,
                    bias=mods[:, l, shift_off + k, which:which + 1], scale=1.0),
                    reads=[("tmp", s), "mods"], writes=[out_key])
            else:
                kb.op("act", lambda k=k, s=s: nc.scalar.activation(
                    out=hf[:, k, :T], in_=tmp[s][:, :T], func=AF.Identity,
                    bias=mods[:, l, shift_off + k, which:which + 1], scale=1.0),
                    reads=[("tmp", s), "mods"], writes=["hf"])
                kb.op("pool", lambda k=k: nc.gpsimd.tensor_copy(out=out_tile[:, k, :T], in_=hf[:, k, :T]),
                      reads=["hf"], writes=[out_key])

    for l in range(nlayers):
        need_ctx = l < DEPTH - 1
        is_moe = (l % 2 == 1)
        jl = l // 2
        xsrc = xT0 if l == 0 else R

        with contextlib.ExitStack() as st:
            Wfm = sb(st, "Wfm", [128, 8, 1792], BF16)
            Wtm = sb(st, "Wtm", [128, 8, 648], BF16)
            xts = [sb(st, "xt%d" % i, [128, 8, 512], F32) for i in range(2)]
            hTs = [sb(st, "hT%d" % i, [128, 8, 512], BF16) for i in range(2)]
            sq = [sb(st, "sq%d" % i, [128, 512], BF16) for i in range(2)]
            tmp = [sb(st, "tmp%d" % i, [128, 512], F32) for i in range(2)]
            rstd = sb(st, "rstd", [128, 512], F32)
            stgF = [sb(st, "stgF%d" % i, [128, 14, 512], BF16) for i in range(2)]
            stgGV = [sb(st, "stgGV%d" % i, [128, 4, 256], BF16) for i in range(2)]
            stgV = [sb(st, "stgV%d" % i, [128, 4, 128], BF16) for i in range(2)]
            stgZ = [sb(st, "stgZ%d" % i, [128, 4, 256], BF16) for i in range(2)]
            stgDT = [sb(st, "stgDT%d" % i, [128, 4, 16], F32) for i in range(2)]
            ropeC = sb(st, "ropeC", [128, L], F32)
            ropeS = sb(st, "ropeS", [128, L], F32)
            gvB = sb(st, "gvB", [128, 256], F32)
            dtbB = sb(st, "dtbB", [128, 8], F32)
            aB = sb(st, "aB", [128, 8], F32)
            qsq = [sb(st, "qsq%d" % i, [128, 512], BF16) for i in range(2)]
            qrn = [sb(st, "qrn%d" % i, [128, 512], F32) for i in range(2)]
            qn = [sb(st, "qn%d" % i, [128, 512], BF16) for i in range(2)]
            qt1 = [sb(st, "qt1%d" % i, [128, 512], F32) for i in range(2)]
            qt2 = [sb(st, "qt2%d" % i, [128, 512], F32) for i in range(2)]
            gl = [sb(st, "gl%d" % i, [128, 256], F32) for i in range(2)]
            gsq = [sb(st, "gsq%d" % i, [128, 256], F32) for i in range(2)]
            gss = [sb(st, "gss%d" % i, [128, 4], F32) for i in range(2)]
            dtx = [sb(st, "dtx%d" % i, [128, 8], F32) for i in range(2)]

            wv = w_in[l].rearrange("(k p) n -> p k n", p=128)
            kb.dma(pool, Wfm[:, :, 0:256], wv[:, :, 0:256], writes=["Wfm"], semkey="wf0")
            kb.dma(pool, Wfm[:, :, 256:768], wv[:, :, 512:1024], writes=["Wfm"], semkey="wf1")
            for kvh in range(2):
                for dup in range(2):
                    c0 = 768 + kvh * 128 + dup * 64
                    kb.dma(pool, Wfm[:, :, c0:c0 + 64], wv[:, :, 1024 + kvh * 64:1024 + (kvh + 1) * 64],
                           writes=["Wfm"], semkey="wf2_%d%d" % (kvh, dup))
            kb.dma(pool, Wfm[:, :, 1024:1792], wv[:, :, 1536:2304], writes=["Wfm"], semkey="wf3")
            kb.dma(pool, Wtm[:, :, 0:256], wv[:, :, 256:512], writes=["Wtm"], semkey="wt0")
            kb.dma(pool, Wtm[:, :, 256:384], wv[:, :, 1152:1280], writes=["Wtm"], semkey="wt1")
            kb.dma(pool, Wtm[:, :, 384:392], wv[:, :, 2304:2312], writes=["Wtm"], semkey="wt2")
            kb.dma(pool, Wtm[:, :, 392:648], wv[:, :, 1280:1536], writes=["Wtm"], semkey="wt3")
            kb.dma(sp, ropeC[:], c_ropeC, writes=["ropeC"], semkey="rc")
            kb.dma(sp, ropeS[:], c_ropeS, writes=["ropeS"], semkey="rs")
            kb.dma(sp, gvB[:], gvg[l].partition_broadcast(128), writes=["gvB"], semkey="gvB")
            kb.dma(sp, dtbB[:], dtb[l].partition_broadcast(128), writes=["dtbB"], semkey="dtbB")
            kb.dma(sp, aB[:], alog[l].partition_broadcast(128), writes=["aB"], semkey="aB")
            kb.op("act", lambda: nc.scalar.activation(out=aB[:], in_=aB[:], func=AF.Exp), reads=["aB"], writes=["aB"])
            kb.op("dve", lambda: nc.vector.tensor_scalar(out=aB[:], in0=aB[:], scalar1=-1.0, scalar2=None, op0=ALU.mult),
                  reads=["aB"], writes=["aB"])

            bank_rot = [0]

            def next_bank():
                b = bank_rot[0] % 5
                bank_rot[0] += 1
                return b

            for bi, (t0, T, isctx) in enumerate(BLOCKS):
                s = bi % 2
                which = 1 if isctx else 0
                xt, hT = xts[s], hTs[s]
                kb.dma(sp, xt[:, :, :T], xsrc.rearrange("(k p) t -> p k t", p=128)[:, :, t0:t0 + T],
                       writes=[("xt", s)], semkey=("xt", s))
                for k in range(8):
                    s2 = k % 2
                    kb.op("act", lambda k=k, s2=s2: nc.scalar.activation(out=sq[s2][:, :T], in_=xt[:, k, :T], func=AF.Square),
                          reads=[("xt", s)], writes=[("sq", s2)])
                    kb.op("pe", lambda k=k, s2=s2: nc.tensor.matmul(PS[7][:, :T], lhsT=ones_bf[:], rhs=sq[s2][:, :T],
                                                                    start=(k == 0), stop=(k == 7)),
                          reads=[("sq", s2), "ones_bf"], writes=["ps7"])
                kb.op("act", lambda: nc.scalar.activation(out=rstd[:, :T], in_=PS[7][:, :T], func=AF.Sqrt,
                                                          bias=eps_t[:], scale=1.0 / D),
                      reads=["ps7", "eps_t"], writes=["rstd"])
                kb.op("dve", lambda: nc.vector.reciprocal(out=rstd[:, :T], in_=rstd[:, :T]), reads=["rstd"], writes=["rstd"])
                for k in range(8):
                    s2 = k % 2
                    kb.op("dve", lambda k=k, s2=s2: nc.vector.scalar_tensor_tensor(
                        out=tmp[s2][:, :T], in0=xt[:, k, :T], scalar=G1[:, l, k, which:which + 1], in1=rstd[:, :T],
                        op0=ALU.mult, op1=ALU.mult), reads=[("xt", s), "G", "rstd"], writes=[("tmp", s2)])
                    kb.op("act", lambda k=k, s2=s2: nc.scalar.activation(
                        out=hT[:, k, :T], in_=tmp[s2][:, :T], func=AF.Identity,
                        bias=mods[:, l, k, which:which + 1], scale=1.0),
                        reads=[("tmp", s2), "mods"], writes=[("hT", s)])

                sF = stgF[s]
                for ci in range(14):
                    b = next_bank()
                    pk = "ps%d" % b
                    for k in range(8):
                        kb.op("pe", lambda k=k, ci=ci, b=b: nc.tensor.matmul(
                            PS[b][:, :T], lhsT=Wfm[:, k, ci * 128:(ci + 1) * 128], rhs=hT[:, k, :T],
                            start=(k == 0), stop=(k == 7)), reads=["Wfm", ("hT", s)], writes=[pk])
                    ok = ("stgF", s, ci)
                    if ci < 2:
                        kb.op("act", lambda ci=ci, b=b: nc.scalar.activation(out=sF[:, ci, :T], in_=PS[b][:, :T],
                                                                             func=AF.Gelu_apprx_tanh),
                              reads=[pk], writes=[ok])
                    elif ci >= 8:
                        kb.op("pool" if False else "dve", lambda ci=ci, b=b: nc.vector.tensor_copy(out=sF[:, ci, :T], in_=PS[b][:, :T]),
                              reads=[pk], writes=[ok])
                    else:
                        qs = ci % 2
                        gcol = qgs if ci < 6 else kgs
                        kb.op("act", lambda b=b, qs=qs: nc.scalar.activation(out=qsq[qs][:, :T], in_=PS[b][:, :T], func=AF.Square),
                              reads=[pk], writes=[("qsq", qs)])
                        kb.op("pe", lambda qs=qs: nc.tensor.matmul(PS[5][:, :T], lhsT=bones_bf[:], rhs=qsq[qs][:, :T],
                                                                  start=True, stop=True),
                              reads=[("qsq", qs), "bones_bf"], writes=["ps5"])
                        kb.op("act", lambda qs=qs: nc.scalar.activation(out=qrn[qs][:, :T], in_=PS[5][:, :T], func=AF.Sqrt,
                                                                        bias=eps_t[:], scale=1.0 / 64),
                              reads=["ps5", "eps_t"], writes=[("qrn", qs)])
                        kb.op("dve", lambda qs=qs: nc.vector.reciprocal(out=qrn[qs][:, :T], in_=qrn[qs][:, :T]),
                              reads=[("qrn", qs)], writes=[("qrn", qs)])
                        if isctx:
                            kb.op("dve", lambda b=b, qs=qs, ci=ci, gcol=gcol: nc.vector.scalar_tensor_tensor(
                                out=sF[:, ci, :T], in0=PS[b][:, :T], scalar=gcol[:, l:l + 1], in1=qrn[qs][:, :T],
                                op0=ALU.mult, op1=ALU.mult), reads=[pk, ("qrn", qs), "qgs", "kgs"], writes=[ok])
                        else:
                            lt0 = t0 - CL
                            kb.op("dve", lambda b=b, qs=qs, gcol=gcol: nc.vector.scalar_tensor_tensor(
                                out=qn[qs][:, :T], in0=PS[b][:, :T], scalar=gcol[:, l:l + 1], in1=qrn[qs][:, :T],
                                op0=ALU.mult, op1=ALU.mult), reads=[pk, ("qrn", qs), "qgs", "kgs"], writes=[("qn", qs)])
                            kb.op("pe", lambda qs=qs: nc.tensor.matmul(PS[6][:, :T], lhsT=perm_bf[:], rhs=qn[qs][:, :T],
                                                                      start=True, stop=True),
                                  reads=[("qn", qs), "perm_bf"], writes=["ps6"])
                            kb.op("pool", lambda qs=qs, lt0=lt0: nc.gpsimd.tensor_tensor(
                                out=qt1[qs][:, :T], in0=qn[qs][:, :T], in1=ropeC[:, lt0:lt0 + T], op=ALU.mult),
                                reads=[("qn", qs), "ropeC"], writes=[("qt1", qs)])
                            kb.op("dve", lambda qs=qs, lt0=lt0: nc.vector.tensor_tensor(
                                out=qt2[qs][:, :T], in0=PS[6][:, :T], in1=ropeS[:, lt0:lt0 + T], op=ALU.mult),
                                reads=["ps6", "ropeS"], writes=[("qt2", qs)])
                            kb.op("pool", lambda qs=qs, ci=ci: nc.gpsimd.tensor_tensor(
                                out=sF[:, ci, :T], in0=qt1[qs][:, :T], in1=qt2[qs][:, :T], op=ALU.add),
                                reads=[("qt1", qs), ("qt2", qs)], writes=[ok])
                kb.dma(sp, GU.rearrange("(c p) t -> p c t", p=128)[:, :, t0:t0 + T], sF[:, 0:2, :T],
                       reads=[("stgF", s, ci) for ci in (0, 1)], semkey=("oGU", s))
                kb.dma(sp, QT.rearrange("(c p) t -> p c t", p=128)[:, :, t0:t0 + T], sF[:, 2:6, :T],
                       reads=[("stgF", s, ci) for ci in (2, 3, 4, 5)], semkey=("oQT", s))
                kb.dma(sp, KT.rearrange("(c p) t -> p c t", p=128)[:, :, t0:t0 + T], sF[:, 6:8, :T],
                       reads=[("stgF", s, ci) for ci in (6, 7)], semkey=("oKT", s))
                kb.dma(sp, XBC.rearrange("(c p) t -> p c t", p=128)[:, :, t0:t0 + T], sF[:, 8:14, :T],
                       reads=[("stgF", s, ci) for ci in range(8, 14)], semkey=("oXB", s))

                nsub = T // 128
                for sub in range(nsub):
                    ss_ = sub % 2
                    ba = next_bank()
                    bb = next_bank()
                    pka, pkb = "ps%d" % ba, "ps%d" % bb
                    for k in range(8):
                        kb.op("pe", lambda k=k, sub=sub, ba=ba: nc.tensor.matmul(
                            PS[ba][:, 0:392], lhsT=hT[:, k, sub * 128:(sub + 1) * 128], rhs=Wtm[:, k, 0:392],
                            start=(k == 0), stop=(k == 7)), reads=["Wtm", ("hT", s)], writes=[pka])
                    for k in range(8):
                        kb.op("pe", lambda k=k, sub=sub, bb=bb: nc.tensor.matmul(
                            PS[bb][:, 0:256], lhsT=hT[:, k, sub * 128:(sub + 1) * 128], rhs=Wtm[:, k, 392:648],
                            start=(k == 0), stop=(k == 7)), reads=["Wtm", ("hT", s)], writes=[pkb])
                    kb.op("act", lambda ba=ba, ss_=ss_: nc.scalar.activation(out=gl[ss_][:], in_=PS[ba][:, 0:256],
                                                                             func=AF.Gelu_apprx_tanh),
                          reads=[pka], writes=[("gl", ss_)])
                    kb.op("pool", lambda ss_=ss_: nc.gpsimd.tensor_tensor(out=gsq[ss_][:], in0=gl[ss_][:], in1=gl[ss_][:], op=ALU.mult),
                          reads=[("gl", ss_)], writes=[("gsq", ss_)])
                    kb.op("dve", lambda ss_=ss_: nc.vector.tensor_reduce(
                        out=gss[ss_][:], in_=gsq[ss_][:].rearrange("p (a b) -> p a b", b=64), axis=AX.X, op=ALU.add),
                        reads=[("gsq", ss_)], writes=[("gss", ss_)])
                    kb.op("act", lambda ss_=ss_: nc.scalar.activation(out=gss[ss_][:], in_=gss[ss_][:], func=AF.Sqrt,
                                                                      bias=eps_t[:], scale=1.0 / 64),
                          reads=[("gss", ss_), "eps_t"], writes=[("gss", ss_)])
                    kb.op("dve", lambda ss_=ss_: nc.vector.reciprocal(out=gss[ss_][:], in_=gss[ss_][:]),
                          reads=[("gss", ss_)], writes=[("gss", ss_)])
                    kb.op("dve", lambda ss_=ss_: nc.vector.tensor_tensor(
                        out=gsq[ss_][:].rearrange("p (a b) -> p a b", b=64), in0=gl[ss_][:].rearrange("p (a b) -> p a b", b=64),
                        in1=gss[ss_][:].unsqueeze(2).broadcast_to([128, 4, 64]), op=ALU.mult),
                        reads=[("gl", ss_), ("gss", ss_)], writes=[("gsq", ss_)])
                    kb.op("pool", lambda ss_=ss_, sub=sub: nc.gpsimd.tensor_tensor(
                        out=stgGV[s][:, sub, :], in0=gsq[ss_][:], in1=gvB[:], op=ALU.mult),
                        reads=[("gsq", ss_), "gvB"], writes=[("stgGV", s)])
                    kb.op("act", lambda ba=ba, sub=sub: nc.scalar.copy(out=stgV[s][:, sub, :], in_=PS[ba][:, 256:384]),
                          reads=[pka], writes=[("stgV", s)])
                    kb.op("dve", lambda ba=ba, ss_=ss_: nc.vector.tensor_tensor(out=dtx[ss_][:], in0=PS[ba][:, 384:392], in1=dtbB[:],
                                                                               op=ALU.add),
                          reads=[pka, "dtbB"], writes=[("dtx", ss_)])
                    kb.op("act", lambda ss_=ss_: nc.scalar.activation(out=dtx[ss_][:], in_=dtx[ss_][:], func=AF.Exp),
                          reads=[("dtx", ss_)], writes=[("dtx", ss_)])
                    kb.op("act", lambda ss_=ss_, sub=sub: nc.scalar.activation(out=stgDT[s][:, sub, 0:8], in_=dtx[ss_][:], func=AF.Ln,
                                                                               bias=one_t[:], scale=1.0),
                          reads=[("dtx", ss_), "one_t"], writes=[("stgDT", s)])
                    kb.op("dve", lambda sub=sub: nc.vector.tensor_tensor(out=stgDT[s][:, sub, 8:16], in0=stgDT[s][:, sub, 0:8],
                                                                         in1=aB[:], op=ALU.mult),
                          reads=[("stgDT", s), "aB"], writes=[("stgDT", s)])
                    kb.op("act", lambda bb=bb, sub=sub: nc.scalar.activation(out=stgZ[s][:, sub, :], in_=PS[bb][:, 0:256], func=AF.Silu),
                          reads=[pkb], writes=[("stgZ", s)])
                kb.dma(sp, GV[t0:t0 + T, :].rearrange("(s p) c -> p s c", p=128), stgGV[s][:, 0:nsub, :],
                       reads=[("stgGV", s)], semkey=("oGV", s))
                kb.dma(sp, V[t0:t0 + T, :].rearrange("(s p) c -> p s c", p=128), stgV[s][:, 0:nsub, :],
                       reads=[("stgV", s)], semkey=("oV", s))
                kb.dma(sp, ZS[t0:t0 + T, :].rearrange("(s p) c -> p s c", p=128), stgZ[s][:, 0:nsub, :],
                       reads=[("stgZ", s)], semkey=("oZS", s))
                kb.dma(sp, DT[t0:t0 + T, :].rearrange("(s p) c -> p s c", p=128), stgDT[s][:, 0:nsub, :],
                       reads=[("stgDT", s)], semkey=("oDT", s))
            kb.barrier()

        with contextlib.ExitStack() as st:
            wsb = sb(st, "wsb", [128, 4, 128], BF16)
            bsr = sb(st, "bsr", [128, 2, 128], F32)
            gtmp = [sb(st, "gtmp%d" % i, [128, 2, 128], F32) for i in range(2)]
            guT = [sb(st, "guT%d" % i, [128, 2, 512], BF16) for i in range(2)]
            vh = [sb(st, "vh%d" % i, [128, 4, 256], BF16) for i in range(2)]
            stg = [sb(st, "gstg%d" % i, [128, 2, 512], BF16) for i in range(2)]
            kb.dma(pool, wsb[:], wsT[l].rearrange("h j i -> j h i"), writes=["wsb"], semkey="wsb")
            kb.dma(sp, bsr[:], gbs[:, l, :, :], writes=["bsr"], semkey="bsr")
            blks = [b for b in BLOCKS if (need_ctx or not b[2])]
            for bi, (t0, T, isctx) in enumerate(blks):
                s = bi % 2
                nsub = T // 128
                kb.dma(sp, guT[s][:, :, :T], GU.rearrange("(c p) t -> p c t", p=128)[:, :, t0:t0 + T],
                       writes=[("guT", s)], semkey=("guT", s))
                kb.dma(sp, vh[s][:, 0:nsub, :], GV[t0:t0 + T, :].rearrange("(s p) c -> p s c", p=128),
                       writes=[("vh", s)], semkey=("vh", s))
                for sub in range(nsub):
                    b = sub % 4
                    pk = "ps%d" % b
                    for j in range(2):
                        for hh in range(2):
                            h = 2 * j + hh
                            kb.op("pe", lambda b=b, j=j, hh=hh, h=h, sub=sub: nc.tensor.matmul(
                                PS[b][hh * 64:(hh + 1) * 64, j * 128:(j + 1) * 128], lhsT=vh[s][:, sub, h * 64:(h + 1) * 64],
                                rhs=wsb[:, h, :], start=True, stop=True), reads=[("vh", s), "wsb"], writes=[pk])
                    g2 = sub % 2
                    kb.op("dve", lambda b=b, g2=g2: nc.vector.tensor_tensor(
                        out=gtmp[g2][:], in0=PS[b][:, 0:256].rearrange("p (a b) -> p a b", b=128),
                        in1=bsr[:], op=ALU.add), reads=[pk, "bsr"], writes=[("gtmp", g2)])
                    kb.op("pool", lambda sub=sub, g2=g2: nc.gpsimd.tensor_tensor(
                        out=stg[s][:, :, sub * 128:(sub + 1) * 128], in0=gtmp[g2][:],
                        in1=guT[s][:, :, sub * 128:(sub + 1) * 128], op=ALU.mult),
                        reads=[("gtmp", g2), ("guT", s)], writes=[("gstg", s)])
                kb.dma(sp, MIX.rearrange("(c p) t -> p c t", p=128)[:, 0:2, t0:t0 + T], stg[s][:, :, :T],
                       reads=[("gstg", s)], semkey=("oMIXg", s))
            kb.barrier()

        with contextlib.ExitStack() as st:
            QTs = sb(st, "QTs", [128, 4, NT], BF16)
            KTz = sb(st, "KTz", [128, 2, 2, NT], BF16)
            Vs = sb(st, "Vs", [128, NTILE, 128], BF16)
            onesv = sb(st, "onesv", [128, 64], BF16)
            Es = [sb(st, "E%d" % i, [128, 512], BF16) for i in range(4)]
            den = [sb(st, "den%d" % i, [128, 256], F32) for i in range(2)]
            stg = [sb(st, "astg%d" % i, [128, 4, 512], BF16) for i in range(2)]
            kb.op("dve", lambda: nc.vector.memset(onesv[:], 1.0), writes=["onesv"])
            for c4 in range(4):
                kb.dma(sp, QTs[:, c4, :], QT[c4 * 128:(c4 + 1) * 128, :], writes=["QTs"], semkey=("QTs", c4))
            kb.op("pool", lambda: nc.gpsimd.memset(KTz[:], 0.0), writes=["KTs"])
            for c2 in range(2):
                for half in range(2):
                    kb.dma(sp, KTz[half * 64:(half + 1) * 64, c2, half, :],
                           KT[c2 * 128 + half * 64:c2 * 128 + (half + 1) * 64, :], writes=["KTs"], semkey=("KTs", c2, half))
            for c8 in range(0, NTILE, 6):
                c9 = min(NTILE, c8 + 6)
                kb.dma(sp, Vs[:, c8:c9, :], V[c8 * 128:c9 * 128, :].rearrange("(s p) c -> p s c", p=128),
                       writes=["Vs"], semkey=("Vs", c8))
            srot = [0]
            od = [0]
            blks = [b for b in BLOCKS if (need_ctx or not b[2])]
            for bi, (t0, T, isctx) in enumerate(blks):
                s = bi % 2
                nsub = T // 128
                for sub in range(nsub):
                    tq = t0 // 128 + sub
                    if isctx:
                        kts = [(0, None), (1, None)]
                    else:
                        n = tq - 2
                        kts = [(0, None), (1, None)]
                        if n > 0:
                            kts.append((tq - 1, ub_bf))
                        kts.append((tq, None))
                        if n < 31:
                            kts.append((tq + 1, uf_bf))
                    for kv in range(2):
                        ob = 4 + (od[0] % 2)
                        db = 6 + (od[0] % 2)
                        od[0] += 1
                        okey, dkey = "ps%d" % ob, "ps%d" % db
                        slots = {}

                        def emitS(i, kv=kv, tq=tq, kts=kts, slots=slots):
                            kt, msk = kts[i]
                            sl = srot[0] % 4
                            srot[0] += 1
                            slots[i] = sl
                            pk = "ps%d" % sl
                            for half in range(2):
                                kb.op("pe", lambda half=half, sl=sl, kt=kt: nc.tensor.matmul(
                                    PS[sl][:, half * 256:(half + 1) * 256].rearrange("p (a b) -> p a b", b=128),
                                    lhsT=KTz[:, kv, half, kt * 128:(kt + 1) * 128],
                                    rhs=QTs[:, 2 * kv:2 * kv + 2, tq * 128:(tq + 1) * 128],
                                    start=True, stop=True), reads=["KTs", "QTs"], writes=[pk])
                            kb.op("act", lambda sl=sl: nc.scalar.activation(out=Es[sl][:], in_=PS[sl][:], func=AF.Exp, scale=0.125),
                                  reads=[pk], writes=[("E", sl)])
                            if msk is not None:
                                kb.op("pool", lambda sl=sl, msk=msk: nc.gpsimd.tensor_tensor(
                                    out=Es[sl][:].rearrange("p (a b) -> p a b", b=128),
                                    in0=Es[sl][:].rearrange("p (a b) -> p a b", b=128),
                                    in1=msk[:].unsqueeze(1).broadcast_to([128, 4, 128]), op=ALU.mult),
                                    reads=[("E", sl), "uf_bf", "ub_bf"], writes=[("E", sl)])

                        def emitPV(i, kv=kv, kts=kts, slots=slots, ob=ob, db=db, okey=okey, dkey=dkey):
                            kt, _ = kts[i]
                            sl = slots[i]
                            first, last = (i == 0), (i == len(kts) - 1)
                            for half in range(2):
                                kb.op("pe", lambda half=half, sl=sl, kt=kt: nc.tensor.matmul(
                                    PS[ob][half * 64:(half + 1) * 64, 0:256], lhsT=Vs[:, kt, kv * 64:(kv + 1) * 64],
                                    rhs=Es[sl][:, half * 256:(half + 1) * 256], start=first, stop=last),
                                    reads=["Vs", ("E", sl)], writes=[okey])
                                kb.op("pe", lambda half=half, sl=sl: nc.tensor.matmul(
                                    PS[db][half * 64:(half + 1) * 64, 0:256], lhsT=onesv[:],
                                    rhs=Es[sl][:, half * 256:(half + 1) * 256], start=first, stop=last),
                                    reads=["onesv", ("E", sl)], writes=[dkey])

                        nk = len(kts)
                        emitS(0)
                        if nk > 1:
                            emitS(1)
                        for i in range(nk):
                            emitPV(i)
                            if i + 2 < nk:
                                emitS(i + 2)
                        dn = den[kv]
                        kb.op("dve", lambda db=db, dn=dn, kv=kv: nc.vector.tensor_tensor(
                            out=dn[:].rearrange("p (a b) -> p a b", b=128),
                            in0=PS[db][:, 0:256].rearrange("p (a b) -> p a b", b=128),
                            in1=sinkE[:, l, kv, :].unsqueeze(2).broadcast_to([128, 2, 128]), op=ALU.add),
                            reads=[dkey, "sinkE"], writes=[("den", kv)])
                        kb.op("dve", lambda dn=dn: nc.vector.reciprocal(out=dn[:], in_=dn[:]),
                              reads=[("den", kv)], writes=[("den", kv)])
                        kb.op("dve", lambda ob=ob, dn=dn, kv=kv, sub=sub: nc.vector.tensor_tensor(
                            out=stg[s][:, 2 * kv:2 * kv + 2, sub * 128:(sub + 1) * 128],
                            in0=PS[ob][:, 0:256].rearrange("p (a b) -> p a b", b=128),
                            in1=dn[:].rearrange("p (a b) -> p a b", b=128), op=ALU.mult),
                            reads=[okey, ("den", kv)], writes=[("astg", s)])
                kb.dma(sp, MIX.rearrange("(c p) t -> p c t", p=128)[:, 2:6, t0:t0 + T], stg[s][:, :, :T],
                       reads=[("astg", s)], semkey=("oMIXa", s))
            kb.barrier()

        with contextlib.ExitStack() as st:
            XC = sb(st, "XC", [128, 6, NT], BF16)
            Xtm = sb(st, "Xtm", [128, NTILE, 256], BF16)
            Btm = sb(st, "Btm", [128, NTILE, 256], BF16)
            Ytm = sb(st, "Ytm", [128, NTILE, 256], F32)
            DTs = sb(st, "DTs", [128, NTILE, 16], F32)
            ZSs = sb(st, "ZSs", [128, NTILE, 256], BF16)
            Sf = sb(st, "Sf", [128, 256], F32)
            Sb_ = sb(st, "Sb", [128, 256], BF16)
            negf = sb(st, "negf", [128, 512], BF16)
            negb = sb(st, "negb", [128, 512], BF16)
            dskB = sb(st, "dskB", [128, 256], F32)
            ngB = sb(st, "ngB", [128, 256], F32)
            XB = [sb(st, "XB%d" % i, [128, 6, 516], BF16) for i in range(2)]
            cacc = [sb(st, "cacc%d" % i, [128, 512], F32) for i in range(2)]
            rhsA = [sb(st, "rhsA%d" % i, [128, 512], BF16) for i in range(2)]
            adtb = [sb(st, "adtb%d" % i, [128, 4], BF16) for i in range(2)]
            col = [sb(st, "col%d" % i, [128, 4], F32) for i in range(2)]
            Dm = [sb(st, "Dm%d" % i, [128, 512], F32) for i in range(2)]
            Lm = [sb(st, "Lm%d" % i, [128, 512], BF16) for i in range(2)]
            MT = [sb(st, "MT%d" % i, [128, 512], BF16) for i in range(2)]
            sm = [sb(st, "sm%d" % i, [128, 16], F32) for i in range(2)]
            xw = [sb(st, "xw%d" % i, [128, 256], BF16) for i in range(2)]
            xdt = [sb(st, "xdt%d" % i, [128, 256], BF16) for i in range(2)]
            yt1 = [sb(st, "yt1%d" % i, [128, 256], F32) for i in range(2)]
            yt2 = [sb(st, "yt2%d" % i, [128, 256], F32) for i in range(2)]
            tS = sb(st, "tS", [128, 256], F32)
            yz = [sb(st, "yz%d" % i, [128, 256], F32) for i in range(2)]
            yjunk = sb(st, "yjunk", [128, 256], F32)
            yss = [sb(st, "yss%d" % i, [128, 1], F32) for i in range(2)]
            yo = [sb(st, "yo%d" % i, [128, 256], BF16) for i in range(2)]
            stg = [sb(st, "sstg%d" % i, [128, 2, 512], BF16) for i in range(2)]

            kb.dma(pool, negf[:], c_negf, writes=["negf"], semkey="negf")
            kb.dma(pool, negb[:], c_negb, writes=["negb"], semkey="negb")
            kb.dma(sp, dskB[:], dskE[l].partition_broadcast(128), writes=["dskB"], semkey="dskB")
            kb.dma(sp, ngB[:], sng[l].partition_broadcast(128), writes=["ngB"], semkey="ngB")
            for c8 in range(0, NTILE, 6):
                c9 = min(NTILE, c8 + 6)
                kb.dma(sp, DTs[:, c8:c9, :], DT[c8 * 128:c9 * 128, :].rearrange("(s p) c -> p s c", p=128),
                       writes=["DTs"], semkey=("DTs", c8))
                kb.dma(sp, ZSs[:, c8:c9, :], ZS[c8 * 128:c9 * 128, :].rearrange("(s p) c -> p s c", p=128),
                       writes=["ZSs"], semkey=("ZSs", c8))

            for bi, (t0, T, isctx) in enumerate(BLOCKS):
                s = bi % 2
                seg0, seg1 = (0, CL) if isctx else (CL, NT)
                lo, hi = max(t0 - 2, seg0), min(t0 + T + 2, seg1)
                xb = XB[s]
                wr = [("XB", s)]
                if lo > t0 - 2:
                    kb.op("pool", lambda xb=xb: nc.gpsimd.memset(xb[:, :, 0:2], 0.0), writes=wr)
                if hi < t0 + T + 2:
                    kb.op("pool", lambda xb=xb, T=T: nc.gpsimd.memset(xb[:, :, T + 2:T + 4], 0.0), writes=wr)
                kb.dma(sp, xb[:, :, lo - (t0 - 2):hi - (t0 - 2)], XBC.rearrange("(c p) t -> p c t", p=128)[:, :, lo:hi],
                       writes=wr, semkey=("XB", s))
                for j in range(6):
                    a = cacc[j % 2]
                    ak = ("cacc", j % 2)
                    kb.op("dve", lambda j=j, a=a, xb=xb, T=T: nc.vector.tensor_scalar(
                        out=a[:, :T], in0=xb[:, j, 0:T], scalar1=cws[:, l, j, 0:1], scalar2=None, op0=ALU.mult),
                        reads=wr + ["cws"], writes=[ak])
                    for k in range(1, 5):
                        kb.op("dve", lambda j=j, a=a, xb=xb, T=T, k=k: nc.vector.scalar_tensor_tensor(
                            out=a[:, :T], in0=xb[:, j, k:k + T], scalar=cws[:, l, j, k:k + 1], in1=a[:, :T],
                            op0=ALU.mult, op1=ALU.add), reads=wr + ["cws", ak], writes=[ak])
                    kb.op("act", lambda j=j, a=a, T=T, t0=t0: nc.scalar.activation(
                        out=XC[:, j, t0:t0 + T], in_=a[:, :T], func=AF.Silu, bias=cbs[:, l, j:j + 1], scale=1.0),
                        reads=[ak, "cbs"], writes=[("XC", bi)])
            kb.barrier()

            for c in range(NTILE):
                b = 6 + (c % 2)
                pk = "ps%d" % b
                psb = PS[b][:].bitcast(BF16)
                for q4, j in enumerate((0, 1, 2, 3)):
                    kb.op("pe", lambda q4=q4, j=j, c=c, psb=psb: nc.tensor.transpose(
                        out=psb[:, q4 * 128:(q4 + 1) * 128], in_=XC[:, j, c * 128:(c + 1) * 128], identity=ident_bf[:]),
                        reads=["XC", "ident_bf"], writes=[pk])
                kb.op("act", lambda c=c, psb=psb: nc.scalar.copy(out=Xtm[:, c, :], in_=psb[:, 0:256]), reads=[pk], writes=[("Xtm", c)])
                kb.op("act", lambda c=c, psb=psb: nc.scalar.copy(out=Btm[:, c, :], in_=psb[:, 256:512]), reads=[pk], writes=[("Btm", c)])
                kb.op("pool", lambda c=c: nc.gpsimd.tensor_tensor(out=Ytm[:, c, :], in0=Xtm[:, c, :], in1=dskB[:], op=ALU.mult),
                      reads=[("Xtm", c), "dskB"], writes=[("Ytm", c)])

            kb.barrier()
            it = [0]
            for d in range(2):
                order = list(range(NTILE)) if d == 0 else [1, 0] + list(range(NTILE - 1, 1, -1))
                U = uf_bf if d == 0 else ub_bf
                NEG = negf if d == 0 else negb
                last = 127 if d == 0 else 0
                kb.op("dve", lambda: nc.vector.memset(Sf[:], 0.0), reads=["Sb"], writes=["Sf"])
                kb.op("pool", lambda: nc.gpsimd.memset(Sb_[:], 0.0), writes=["Sb"])
                for c in order:
                    want_y = need_ctx or c >= 2
                    if c == 2 and d == 0:
                        pass
                    s = it[0] % 2
                    it[0] += 1
                    rb = s
                    rk = "ps%d" % rb
                    dt4 = DTs[:, c, d * 4:(d + 1) * 4]
                    adt4 = DTs[:, c, 8 + d * 4:12 + d * 4]
                    kb.op("dve", lambda s=s, U=U, adt4=adt4: nc.vector.tensor_tensor(
                        out=rhsA[s][:].rearrange("p (a b) -> p a b", b=128),
                        in0=U[:].unsqueeze(1).broadcast_to([128, 4, 128]),
                        in1=adt4.unsqueeze(2).broadcast_to([128, 4, 128]), op=ALU.mult),
                        reads=["DTs", "uf_bf", "ub_bf"], writes=[("rhsA", s)])
                    kb.op("act", lambda s=s, adt4=adt4: nc.scalar.copy(out=adtb[s][:], in_=adt4), reads=["DTs"], writes=[("adtb", s)])
                    kb.op("pe", lambda s=s, rb=rb: nc.tensor.matmul(PS[rb][:], lhsT=ones_bf[:], rhs=rhsA[s][:], start=True, stop=False),
                          reads=[("rhsA", s), "ones_bf"], writes=[rk])
                    kb.op("pe", lambda rb=rb, NEG=NEG: nc.tensor.matmul(PS[rb][:], lhsT=ident_bf[:], rhs=NEG[:], start=False, stop=True),
                          reads=["negf", "negb", "ident_bf"], writes=[rk])
                    kb.op("pe", lambda s=s, U=U: nc.tensor.matmul(PS[2][:, 256:260], lhsT=U[:], rhs=adtb[s][:], start=True, stop=True),
                          reads=[("adtb", s), "uf_bf", "ub_bf"], writes=["ps2"])
                    for g in range(2):
                        kb.op("pe", lambda g=g, c=c: nc.tensor.matmul(
                            PS[2][:, g * 128:(g + 1) * 128], lhsT=XC[:, 2 + g, c * 128:(c + 1) * 128],
                            rhs=XC[:, 4 + g, c * 128:(c + 1) * 128], start=True, stop=True), reads=["XC"], writes=["ps2"])
                    kb.op("act", lambda s=s: nc.scalar.copy(out=col[s][:], in_=PS[2][:, 256:260]), reads=["ps2"], writes=[("col", s)])
                    kb.op("dve", lambda s=s, rb=rb: nc.vector.tensor_tensor(
                        out=Dm[s][:].rearrange("p (a b) -> p a b", b=128), in0=PS[rb][:].rearrange("p (a b) -> p a b", b=128),
                        in1=col[s][:].unsqueeze(2).broadcast_to([128, 4, 128]), op=ALU.subtract),
                        reads=[rk, ("col", s)], writes=[("Dm", s)])
                    kb.op("act", lambda s=s: nc.scalar.activation(out=Lm[s][:], in_=Dm[s][:], func=AF.Exp),
                          reads=[("Dm", s)], writes=[("Lm", s)])
                    kb.op("dve", lambda s=s: nc.vector.tensor_tensor(
                        out=MT[s][:].rearrange("p (g h i) -> p g h i", g=2, h=2),
                        in0=Lm[s][:].rearrange("p (g h i) -> p g h i", g=2, h=2),
                        in1=PS[2][:, 0:256].rearrange("p (g i) -> p g i", g=2).unsqueeze(2).broadcast_to([128, 2, 2, 128]),
                        op=ALU.mult), reads=[("Lm", s), "ps2"], writes=[("MT", s)])
                    tot = PS[rb][:].rearrange("p (a b) -> p a b", b=128)[:, :, last]
                    smk = ("sm", s)
                    kb.op("dve", lambda s=s, tot=tot: nc.vector.tensor_tensor(out=sm[s][:, 0:4], in0=tot, in1=col[s][:], op=ALU.subtract),
                          reads=[rk, ("col", s)], writes=[smk])
                    kb.op("act", lambda s=s: nc.scalar.activation(out=sm[s][:, 0:4], in_=sm[s][:, 0:4], func=AF.Exp), reads=[smk], writes=[smk])
                    kb.op("dve", lambda s=s, dt4=dt4: nc.vector.tensor_tensor(out=sm[s][:, 0:4], in0=sm[s][:, 0:4], in1=dt4, op=ALU.mult),
                          reads=[smk, "DTs"], writes=[smk])
                    kb.op("act", lambda s=s: nc.scalar.activation(out=sm[s][:, 4:8], in_=col[s][:], func=AF.Exp), reads=[("col", s)], writes=[smk])
                    kb.op("act", lambda s=s, tot=tot: nc.scalar.activation(out=sm[s][:, 8:12], in_=tot, func=AF.Exp), reads=[rk], writes=[smk])
                    kb.op("dve", lambda s=s, c=c: nc.vector.tensor_tensor(
                        out=xw[s][:].rearrange("p (a b) -> p a b", b=64), in0=Xtm[:, c, :].rearrange("p (a b) -> p a b", b=64),
                        in1=sm[s][:, 0:4].unsqueeze(2).broadcast_to([128, 4, 64]), op=ALU.mult),
                        reads=[("Xtm", c), smk], writes=[("xw", s)])
                    if want_y:
                        kb.op("pool", lambda s=s, c=c, dt4=dt4: nc.gpsimd.tensor_tensor(
                            out=xdt[s][:].rearrange("p (a b) -> p a b", b=64), in0=Xtm[:, c, :].rearrange("p (a b) -> p a b", b=64),
                            in1=dt4.unsqueeze(2).broadcast_to([128, 4, 64]), op=ALU.mult),
                            reads=[("Xtm", c), "DTs"], writes=[("xdt", s)])
                        for h in range(4):
                            kb.op("pe", lambda s=s, h=h: nc.tensor.matmul(
                                PS[3][:, h * 64:(h + 1) * 64], lhsT=MT[s][:, h * 128:(h + 1) * 128], rhs=xdt[s][:, h * 64:(h + 1) * 64],
                                start=True, stop=True), reads=[("MT", s), ("xdt", s)], writes=["ps3"])
                        for g in range(2):
                            kb.op("pe", lambda g=g, c=c: nc.tensor.matmul(
                                PS[4][:, g * 128:(g + 1) * 128], lhsT=XC[:, 4 + g, c * 128:(c + 1) * 128],
                                rhs=Sb_[:, g * 128:(g + 1) * 128], start=True, stop=True), reads=["XC", "Sb"], writes=["ps4"])
                        kb.op("dve", lambda s=s: nc.vector.tensor_tensor(
                            out=yt1[s][:].rearrange("p (a b) -> p a b", b=64), in0=PS[4][:, 0:256].rearrange("p (a b) -> p a b", b=64),
                            in1=sm[s][:, 4:8].unsqueeze(2).broadcast_to([128, 4, 64]), op=ALU.mult),
                            reads=["ps4", smk], writes=[("yt1", s)])
                        kb.op("dve", lambda s=s: nc.vector.tensor_tensor(out=yt2[s][:], in0=PS[3][:, 0:256], in1=yt1[s][:], op=ALU.add),
                              reads=["ps3", ("yt1", s)], writes=[("yt2", s)])
                        kb.op("pool", lambda s=s, c=c: nc.gpsimd.tensor_tensor(out=Ytm[:, c, :], in0=Ytm[:, c, :], in1=yt2[s][:], op=ALU.add),
                              reads=[("yt2", s), ("Ytm", c)], writes=[("Ytm", c)])
                    for g in range(2):
                        kb.op("pe", lambda g=g, c=c, s=s: nc.tensor.matmul(
                            PS[5][:, g * 128:(g + 1) * 128], lhsT=Btm[:, c, g * 128:(g + 1) * 128],
                            rhs=xw[s][:, g * 128:(g + 1) * 128], start=True, stop=True), reads=[("Btm", c), ("xw", s)], writes=["ps5"])
                    kb.op("dve", lambda s=s: nc.vector.tensor_tensor(
                        out=tS[:].rearrange("p (a b) -> p a b", b=64), in0=Sf[:].rearrange("p (a b) -> p a b", b=64),
                        in1=sm[s][:, 8:12].unsqueeze(2).broadcast_to([128, 4, 64]), op=ALU.mult),
                        reads=["Sf", smk], writes=["tS"])
                    kb.op("dve", lambda: nc.vector.tensor_tensor(out=Sf[:], in0=PS[5][:, 0:256], in1=tS[:], op=ALU.add),
                          reads=["ps5", "tS"], writes=["Sf"])
                    kb.op("act", lambda: nc.scalar.copy(out=Sb_[:], in_=Sf[:]), reads=["Sf"], writes=["Sb"])

            kb.barrier()
            blks = [b for b in BLOCKS if (need_ctx or not b[2])]
            for bi, (t0, T, isctx) in enumerate(blks):
                sg = bi % 2
                nsub = T // 128
                for sub in range(nsub):
                    c = t0 // 128 + sub
                    s = sub % 2
                    kb.op("pool", lambda c=c, s=s: nc.gpsimd.tensor_tensor(out=yz[s][:], in0=Ytm[:, c, :], in1=ZSs[:, c, :], op=ALU.mult),
                          reads=[("Ytm", c), "ZSs"], writes=[("yz", s)])
                    kb.op("act", lambda s=s: nc.scalar.activation(out=yjunk[:], in_=yz[s][:], func=AF.Square, accum_out=yss[s][:]),
                          reads=[("yz", s)], writes=["yjunk", ("yss", s)])
                    kb.op("act", lambda s=s: nc.scalar.activation(out=yss[s][:], in_=yss[s][:], func=AF.Sqrt, bias=eps_t[:], scale=1.0 / 256),
                          reads=[("yss", s), "eps_t"], writes=[("yss", s)])
                    kb.op("dve", lambda s=s: nc.vector.reciprocal(out=yss[s][:], in_=yss[s][:]), reads=[("yss", s)], writes=[("yss", s)])
                    kb.op("dve", lambda s=s: nc.vector.scalar_tensor_tensor(
                        out=yo[s][:], in0=yz[s][:], scalar=yss[s][:, 0:1], in1=ngB[:], op0=ALU.mult, op1=ALU.mult),
                        reads=[("yz", s), ("yss", s), "ngB"], writes=[("yo", s)])
                    b = 6 + s
                    pk = "ps%d" % b
                    psb = PS[b][:].bitcast(BF16)
                    for j in range(2):
                        kb.op("pe", lambda j=j, s=s, psb=psb: nc.tensor.transpose(
                            out=psb[:, j * 128:(j + 1) * 128], in_=yo[s][:, j * 128:(j + 1) * 128], identity=ident_bf[:]),
                            reads=[("yo", s), "ident_bf"], writes=[pk])
                    kb.op("act", lambda sub=sub, psb=psb, sg=sg: nc.scalar.copy(
                        out=stg[sg][:, :, sub * 128:(sub + 1) * 128], in_=psb[:, 0:256].rearrange("p (a b) -> p a b", b=128)),
                        reads=[pk], writes=[("sstg", sg)])
                kb.dma(sp, MIX.rearrange("(c p) t -> p c t", p=128)[:, 6:8, t0:t0 + T], stg[sg][:, :, :T],
                       reads=[("sstg", sg)], semkey=("oMIXs", sg))
            kb.barrier()

        with contextlib.ExitStack() as st:
            Wo = sb(st, "Wo", [128, 8, D], BF16)
            mix = [sb(st, "mix%d" % i, [128, 8, 512], BF16) for i in range(2)]
            xts = [sb(st, "xt%d" % i, [128, 8, 512], F32) for i in range(2)]
            hTs = [sb(st, "hTs%d" % i, [128, 8, 512], BF16) for i in range(2)]
            sq = [sb(st, "sq%d" % i, [128, 512], BF16) for i in range(2)]
            tmp = [sb(st, "tmp%d" % i, [128, 512], F32) for i in range(2)]
            rstd = sb(st, "rstd", [128, 512], F32)
            if is_moe:
                hf = sb(st, "hf", [128, 8, 512], F32)
                wr_ = sb(st, "wr", [128, 8, NEXP], F32)
                lg = [sb(st, "lg%d" % i, [128, 8], F32) for i in range(2)]
                mx = [sb(st, "mx%d" % i, [128, 8], F32) for i in range(2)]
                ee = [sb(st, "ee%d" % i, [128, 8], F32) for i in range(2)]
                mk = [sb(st, "mk%d" % i, [128, 8], F32) for i in range(2)]
                r2 = [sb(st, "r2%d" % i, [128, 1], F32) for i in range(2)]
                cmb = [sb(st, "cmb%d" % i, [128, 8], F32) for i in range(2)]
                cstg = [sb(st, "cstg%d" % i, [8, 512], F32) for i in range(2)]
                kb.dma(sp, wr_[:], moe_r[jl].rearrange("(k p) e -> p k e", p=128), writes=["wr"], semkey="wr")
            kb.dma(pool, Wo[:], w_out[l].rearrange("(k p) n -> p k n", p=128), writes=["Wo"], semkey="Wo")
            blks = [b for b in BLOCKS if (need_ctx or not b[2])]
            brot = [0]
            for bi, (t0, T, isctx) in enumerate(blks):
                s = bi % 2
                which = 1 if isctx else 0
                xt = xts[s]
                kb.dma(sp, mix[s][:, :, :T], MIX.rearrange("(c p) t -> p c t", p=128)[:, :, t0:t0 + T],
                       writes=[("mix", s)], semkey=("mix", s))
                kb.dma(sp, xt[:, :, :T], xsrc.rearrange("(k p) t -> p k t", p=128)[:, :, t0:t0 + T],
                       writes=["xt"], semkey=("xt3", s))
                for co in range(8):
                    b = brot[0] % 4
                    brot[0] += 1
                    pk = "ps%d" % b
                    for k in range(8):
                        kb.op("pe", lambda k=k, co=co, b=b: nc.tensor.matmul(
                            PS[b][:, :T], lhsT=Wo[:, k, co * 128:(co + 1) * 128], rhs=mix[s][:, k, :T],
                            start=(k == 0), stop=(k == 7)), reads=["Wo", ("mix", s)], writes=[pk])
                    kb.op("dve", lambda co=co, b=b: nc.vector.scalar_tensor_tensor(
                        out=xt[:, co, :T], in0=PS[b][:, :T], scalar=mods[:, l, 16 + co, which:which + 1], in1=xt[:, co, :T],
                        op0=ALU.mult, op1=ALU.add), reads=[pk, "mods", "xt"], writes=["xt"])
                kb.dma(sp, R.rearrange("(k p) t -> p k t", p=128)[:, :, t0:t0 + T], xt[:, :, :T],
                       reads=["xt"], semkey=("oR3", s))
                rmsnorm_block((sq, rstd, tmp), xt, T, G2, l, 24, which, hTs[s], ("hTs", s), hf=(hf if is_moe else None))
                kb.dma(sp, HT.rearrange("(k p) t -> p k t", p=128)[:, :, t0:t0 + T], hTs[s][:, :, :T],
                       reads=[("hTs", s)], semkey=("oHT", s))
                if is_moe:
                    nsub = T // 128
                    for sub in range(nsub):
                        s2 = sub % 2
                        for k in range(8):
                            kb.op("pe", lambda k=k, sub=sub: nc.tensor.matmul(
                                PS[4][:, 0:8], lhsT=hf[:, k, sub * 128:(sub + 1) * 128], rhs=wr_[:, k, :],
                                start=(k == 0), stop=(k == 7)), reads=["hf", "wr"], writes=["ps4"])
                        kb.op("act", lambda s2=s2: nc.scalar.copy(out=lg[s2][:], in_=PS[4][:, 0:8]), reads=["ps4"], writes=[("lg", s2)])
                        kb.op("dve", lambda s2=s2: nc.vector.max(out=mx[s2][:], in_=lg[s2][:]), reads=[("lg", s2)], writes=[("mx", s2)])
                        kb.op("dve", lambda s2=s2: nc.vector.tensor_scalar(out=ee[s2][:], in0=lg[s2][:], scalar1=mx[s2][:, 0:1], scalar2=None,
                                                                          op0=ALU.subtract), reads=[("lg", s2), ("mx", s2)], writes=[("ee", s2)])
                        kb.op("act", lambda s2=s2: nc.scalar.activation(out=ee[s2][:], in_=ee[s2][:], func=AF.Exp), reads=[("ee", s2)], writes=[("ee", s2)])
                        kb.op("dve", lambda s2=s2: nc.vector.tensor_tensor(out=r2[s2][:], in0=mx[s2][:, 1:2], in1=mx[s2][:, 0:1], op=ALU.subtract),
                              reads=[("mx", s2)], writes=[("r2", s2)])
                        kb.op("act", lambda s2=s2: nc.scalar.activation(out=r2[s2][:], in_=r2[s2][:], func=AF.Exp), reads=[("r2", s2)], writes=[("r2", s2)])
                        kb.op("dve", lambda s2=s2: nc.vector.tensor_scalar(out=r2[s2][:], in0=r2[s2][:], scalar1=1.0, scalar2=None, op0=ALU.add),
                              reads=[("r2", s2)], writes=[("r2", s2)])
                        kb.op("dve", lambda s2=s2: nc.vector.reciprocal(out=r2[s2][:], in_=r2[s2][:]), reads=[("r2", s2)], writes=[("r2", s2)])
                        kb.op("dve", lambda s2=s2: nc.vector.tensor_scalar(out=mk[s2][:], in0=lg[s2][:], scalar1=mx[s2][:, 1:2], scalar2=None,
                                                                          op0=ALU.is_ge), reads=[("lg", s2), ("mx", s2)], writes=[("mk", s2)])
                        kb.op("dve", lambda s2=s2: nc.vector.scalar_tensor_tensor(
                            out=cmb[s2][:], in0=ee[s2][:], scalar=r2[s2][:, 0:1], in1=mk[s2][:], op0=ALU.mult, op1=ALU.mult),
                            reads=[("ee", s2), ("r2", s2), ("mk", s2)], writes=[("cmb", s2)])
                        kb.op("pe", lambda s2=s2, sub=sub: nc.tensor.transpose(
                            out=PS[5][0:8, sub * 128:(sub + 1) * 128], in_=cmb[s2][:], identity=ident_f[:]),
                            reads=[("cmb", s2), "ident_f"], writes=["ps5"])
                    kb.op("act", lambda s=s, T=T: nc.scalar.copy(out=cstg[s][:, :T], in_=PS[5][0:8, :T]), reads=["ps5"], writes=[("cstg", s)])
                    kb.dma(sp, COMBT[:, t0:t0 + T], cstg[s][:, :T], reads=[("cstg", s)], semkey=("oCB", s))
            kb.barrier()

        with contextlib.ExitStack() as st:
            Wg = [sb(st, "Wg%d" % i, [128, 8, 768], BF16) for i in range(2)]
            Wu = [sb(st, "Wu%d" % i, [128, 8, 768], BF16) for i in range(2)]
            Wd = [sb(st, "Wd%d" % i, [128, 6, D], BF16) for i in range(2)]
            xts = [sb(st, "xt%d" % i, [128, 8, 512], F32) for i in range(2)]
            hTs = [sb(st, "hTs%d" % i, [128, 8, 512], BF16) for i in range(2)]
            aT = [sb(st, "aT%d" % i, [128, 6, 512], BF16) for i in range(2)]
            sgs = [sb(st, "sg%d" % i, [128, 512], F32) for i in range(2)]
            cb = [sb(st, "cb%d" % i, [128, 512], F32) for i in range(2)]
            t4 = [sb(st, "t4%d" % i, [128, 512], F32) for i in range(2)]
            blks = [b for b in BLOCKS if (need_ctx or not b[2])]
            passes = [(e, g) for e in range(NEXP if is_moe else 1) for g in range(4)]
            final_layer = (l == nlayers - 1)

            def load_w(pi):
                e, g = passes[pi]
                f0, nf = FGROUPS[g]
                s = pi % 2
                if is_moe:
                    gsrc, usrc, dsrc = moe_g[jl, e], moe_u[jl, e], moe_d[jl, e]
                else:
                    gsrc, usrc, dsrc = ffn_g[jl], ffn_u[jl], ffn_d[jl]
                kb.dma(pool, Wg[s][:, :, 0:nf * 128], gsrc.rearrange("(k p) f -> p k f", p=128)[:, :, f0 * 128:(f0 + nf) * 128],
                       writes=[("Wg", s)], semkey=("Wg", s))
                kb.dma(pool, Wu[s][:, :, 0:nf * 128], usrc.rearrange("(k p) f -> p k f", p=128)[:, :, f0 * 128:(f0 + nf) * 128],
                       writes=[("Wu", s)], semkey=("Wu", s))
                kb.dma(pool, Wd[s][:, 0:nf, :], dsrc[f0 * 128:(f0 + nf) * 128, :].rearrange("(c p) d -> p c d", p=128),
                       writes=[("Wd", s)], semkey=("Wd", s))

            load_w(0)
            it = 0
            gub = [0]
            dbk = [0]
            for pi, (e, g) in enumerate(passes):
                if pi + 1 < len(passes):
                    load_w(pi + 1)
                f0, nf = FGROUPS[g]
                ws = pi % 2
                last_pass = (pi == len(passes) - 1)
                for bi, (t0, T, isctx) in enumerate(blks):
                    s = it % 2
                    it += 1
                    which = 1 if isctx else 0
                    xt, hT = xts[s], hTs[s]
                    kb.dma(sp, hT[:, :, :T], HT.rearrange("(k p) t -> p k t", p=128)[:, :, t0:t0 + T],
                           writes=[("hT", s)], semkey=("hT4", s))
                    kb.dma(sp, xt[:, :, :T], R.rearrange("(k p) t -> p k t", p=128)[:, :, t0:t0 + T],
                           reads=[("R", bi)], writes=[("xt", s)], semkey=("xt4", s))
                    if is_moe:
                        kb.dma(sp, cb[s][:, :T], COMBT[e, t0:t0 + T].partition_broadcast(128), writes=[("cb", s)], semkey=("cb", s))
                    for fc in range(nf):
                        bg = (gub[0] % 2) * 2
                        gub[0] += 1
                        bu = bg + 1
                        gk, uk = "ps%d" % bg, "ps%d" % bu
                        for k in range(8):
                            kb.op("pe", lambda k=k, fc=fc, bg=bg: nc.tensor.matmul(
                                PS[bg][:, :T], lhsT=Wg[ws][:, k, fc * 128:(fc + 1) * 128], rhs=hT[:, k, :T],
                                start=(k == 0), stop=(k == 7)), reads=[("Wg", ws), ("hT", s)], writes=[gk])
                        for k in range(8):
                            kb.op("pe", lambda k=k, fc=fc, bu=bu: nc.tensor.matmul(
                                PS[bu][:, :T], lhsT=Wu[ws][:, k, fc * 128:(fc + 1) * 128], rhs=hT[:, k, :T],
                                start=(k == 0), stop=(k == 7)), reads=[("Wu", ws), ("hT", s)], writes=[uk])
                        sgi = fc % 2
                        kb.op("act", lambda bg=bg, sgi=sgi: nc.scalar.activation(out=sgs[sgi][:, :T], in_=PS[bg][:, :T], func=AF.Silu),
                              reads=[gk], writes=[("sg", sgi)])
                        kb.op("dve", lambda bu=bu, sgi=sgi, fc=fc: nc.vector.tensor_tensor(
                            out=aT[s][:, fc, :T], in0=sgs[sgi][:, :T], in1=PS[bu][:, :T], op=ALU.mult),
                            reads=[uk, ("sg", sgi)], writes=[("aT", s)])
                    for co in range(8):
                        bd = 4 + (dbk[0] % 3)
                        dbk[0] += 1
                        dk = "ps%d" % bd
                        for fc in range(nf):
                            kb.op("pe", lambda fc=fc, co=co, bd=bd: nc.tensor.matmul(
                                PS[bd][:, :T], lhsT=Wd[ws][:, fc, co * 128:(co + 1) * 128], rhs=aT[s][:, fc, :T],
                                start=(fc == 0), stop=(fc == nf - 1)), reads=[("Wd", ws), ("aT", s)], writes=[dk])
                        if is_moe:
                            ti = co % 2
                            kb.op("dve", lambda co=co, bd=bd, ti=ti: nc.vector.scalar_tensor_tensor(
                                out=t4[ti][:, :T], in0=PS[bd][:, :T], scalar=mods[:, l, 40 + co, which:which + 1], in1=cb[s][:, :T],
                                op0=ALU.mult, op1=ALU.mult), reads=[dk, "mods", ("cb", s)], writes=[("t4", ti)])
                            kb.op("pool", lambda co=co, ti=ti: nc.gpsimd.tensor_tensor(
                                out=xt[:, co, :T], in0=xt[:, co, :T], in1=t4[ti][:, :T], op=ALU.add),
                                reads=[("t4", ti), ("xt", s)], writes=[("xt", s)])
                        else:
                            kb.op("dve", lambda co=co, bd=bd: nc.vector.scalar_tensor_tensor(
                                out=xt[:, co, :T], in0=PS[bd][:, :T], scalar=mods[:, l, 40 + co, which:which + 1], in1=xt[:, co, :T],
                                op0=ALU.mult, op1=ALU.add), reads=[dk, "mods", ("xt", s)], writes=[("xt", s)])
                    if last_pass and final_layer:
                        if not isctx:
                            kb.dma(sp, outT.rearrange("(k p) t -> p k t", p=128)[:, :, t0 - CL:t0 - CL + T], xt[:, :, :T],
                                   reads=[("xt", s)], semkey=("oR4", s))
                    else:
                        kb.dma(sp, R.rearrange("(k p) t -> p k t", p=128)[:, :, t0:t0 + T], xt[:, :, :T],
                               reads=[("xt", s)], writes=[("R", bi)], semkey=("oR4", s))
            kb.barrier()

    return nc


def _consts():
    c = {}
    c["c_ident"] = np.eye(128, dtype=np.float32)
    bo = np.zeros((128, 128), np.float32)
    bo[:64, :64] = 1
    bo[64:, 64:] = 1
    c["c_bones"] = bo
    P = np.zeros((128, 128), np.float32)
    for m in range(128):
        partner = m + 16 if (m % 32) < 16 else m - 16
        P[partner, m] = 1
    c["c_perm"] = P
    t = np.arange(128)
    uf = (t[:, None] <= t[None, :]).astype(np.float32)
    ub = (t[:, None] >= t[None, :]).astype(np.float32)
    c["c_uf"], c["c_ub"] = uf, ub
    c["c_negf"] = np.tile((uf - 1.0) * 30000.0, (1, 4)).astype(np.float32)
    c["c_negb"] = np.tile((ub - 1.0) * 30000.0, (1, 4)).astype(np.float32)
    pos = np.arange(L)
    row, colp = pos // 64, pos % 64
    freqs = (10000.0 ** (-np.arange(16, dtype=np.float32) / 16)).astype(np.float32)
    C = np.zeros((128, L), np.float32)
    S = np.zeros((128, L), np.float32)
    for p in range(128):
        dd = p % 64
        pp = row if dd < 32 else colp
        ang = pp.astype(np.float32) * freqs[dd % 16]
        C[p] = np.cos(ang)
        S[p] = np.sin(ang) * (-1.0 if (dd % 32) < 16 else 1.0)
    c["c_ropeC"], c["c_ropeS"] = C, S
    return c


def _prep_shared(inp):
    f = np.float32
    a = lambda v: np.ascontiguousarray(np.asarray(v, dtype=f))
    sh = {}
    sh["w_mod"] = a(inp["w_mod"])
    sh["bmodT"] = a(np.asarray(inp["b_mod"]).reshape(DEPTH, 48, 128).transpose(2, 0, 1))
    sh["g1T"] = a(np.asarray(inp["norm1_g"]).reshape(DEPTH, 8, 128).transpose(2, 0, 1))
    sh["g2T"] = a(np.asarray(inp["norm2_g"]).reshape(DEPTH, 8, 128).transpose(2, 0, 1))
    sh["w_in"] = a(inp["w_in"])
    sh["w_out"] = a(inp["w_out"])
    sh["gvg"] = a(inp["gm_v_g"])
    sh["wsT"] = a(np.asarray(inp["gm_ws"]).transpose(0, 1, 3, 2))
    p = np.arange(128)
    bsv = np.asarray(inp["gm_bs"])
    gb = np.zeros((128, DEPTH, 2, 128), f)
    for j in range(2):
        gb[:, :, j, :] = bsv[:, 2 * j + (p // 64), :].transpose(1, 0, 2)
    sh["gbs"] = gb
    sh["qgT"] = a(np.asarray(inp["att_q_g"])[:, p % 64].T)
    sh["kgT"] = a(np.asarray(inp["att_k_g"])[:, p % 64].T)
    sk = np.asarray(inp["att_sink"])
    sl = np.zeros((128, DEPTH, 2, 2), f)
    for kv in range(2):
        for ti in range(2):
            sl[:, :, kv, ti] = sk[:, 4 * kv + 2 * ti + (p // 64)].T
    sh["sinkL"] = sl
    sh["convw"] = a(np.asarray(inp["ssm_conv_w"]).reshape(DEPTH, 5, 6, 128).transpose(3, 0, 2, 1))
    sh["convb"] = a(np.asarray(inp["ssm_conv_b"]).reshape(DEPTH, 6, 128).transpose(2, 0, 1))
    sh["dtb"] = a(np.asarray(inp["ssm_dt_bias"]).reshape(DEPTH, 8))
    sh["alog"] = a(np.asarray(inp["ssm_a_log"]).reshape(DEPTH, 8))
    sh["dskE"] = a(np.repeat(np.asarray(inp["ssm_d"]), 64, axis=1))
    sh["sng"] = a(inp["ssm_norm_g"])
    sh["ffn_g"] = a(inp["ffn_w_gate"])
    sh["ffn_u"] = a(inp["ffn_w_up"])
    sh["ffn_d"] = a(inp["ffn_w_down"])
    sh["moe_r"] = a(inp["moe_router"])
    sh["moe_g"] = a(inp["moe_w_gate"])
    sh["moe_u"] = a(inp["moe_w_up"])
    sh["moe_d"] = a(inp["moe_w_down"])
    sh.update(_consts())
    return sh


def _prep_core(inp, b):
    f = np.float32
    x = np.asarray(inp["x"][b], dtype=f)
    ctx = np.asarray(inp["ctx"][b], dtype=f)
    xT0 = np.ascontiguousarray(np.concatenate([ctx.T, x.T], axis=1))
    c = np.asarray(inp["c"][b], dtype=f).reshape(8, 128).T
    cc = np.asarray(inp["c_ctx"], dtype=f).reshape(8, 128).T
    cs = np.ascontiguousarray(np.stack([c, cc], axis=-1))
    return {"xT0": xT0, "cs": cs}


_NC_CACHE = {}


def kernel(**inputs):
    if "nc" not in _NC_CACHE:
        _NC_CACHE["nc"] = build_program()
    nc = _NC_CACHE["nc"]
    sh = _prep_shared(inputs)
    in_maps = []
    for b in range(8):
        m = dict(sh)
        m.update(_prep_core(inputs, b))
        in_maps.append(m)
    res = run_bass_kernel_spmd(nc, in_maps, core_ids=list(range(8)))
    out = np.stack([np.ascontiguousarray(r["outT"].T) for r in res.results], axis=0)
    return out.astype(np.float32)
```

```python
import contextlib
import numpy as np
import concourse.bass as bass
import concourse.mybir as mybir
from concourse.bass_utils import run_bass_kernel_spmd

F32, BF16 = mybir.dt.float32, mybir.dt.bfloat16
AF = mybir.ActivationFunctionType
ALU = mybir.AluOpType
AX = mybir.AxisListType

D = 1024
L = 4096
CL = 256
NT = L + CL
NTILE = NT // 128
DEPTH = 4
DFF = 2816
NEXP = 8
EPS = 1e-6
FGROUPS = [(0, 6), (6, 6), (12, 5), (17, 5)]
BLOCKS = [(0, 256, True)] + [(256 + 512 * i, 512, False) for i in range(8)]


SIM_FRESH_POOL = False


class KB:
    def __init__(self, nc, stack):
        self.nc = nc
        self.stack = stack
        self.eng = {"pe": nc.tensor, "dve": nc.vector, "act": nc.scalar, "pool": nc.gpsimd, "sp": nc.sync}
        self.esem = {e: stack.enter_context(nc.semaphore("es_" + e)) for e in self.eng}
        self.ecnt = {e: 0 for e in self.eng}
        self.seen = {e: {} for e in self.eng}
        self.lw = {}
        self.rd = {}
        self.dsem = {}
        self.free = []
        self.nsem = 0
        self.dead = False

    def _wait(self, E, tok):
        sem, val, _ = tok
        sid = id(sem)
        if self.seen[E].get(sid, 0) >= val:
            return
        self.eng[E].wait_ge(sem, val)
        self.seen[E][sid] = val

    def _deps(self, E, reads, writes):
        for k in reads:
            w = self.lw.get(k)
            if w is not None:
                if w[2] == E and E == "pe":
                    continue
                self._wait(E, w)
        for k in writes:
            w = self.lw.get(k)
            if w is not None and w[2] != E:
                self._wait(E, w)
            for r in self.rd.get(k, {}).values():
                if r[2] != E:
                    self._wait(E, r)

    def _record(self, tok, reads, writes):
        for k in writes:
            self.lw[k] = tok
            self.rd[k] = {}
        for k in reads:
            self.rd.setdefault(k, {})[id(tok[0])] = tok

    def op(self, E, fn, reads=(), writes=()):
        if self.dead:
            return None
        self._deps(E, reads, writes)
        inst = fn()
        self.ecnt[E] += 1
        inst.then_inc(self.esem[E], 1)
        tok = (self.esem[E], self.ecnt[E], E)
        self._record(tok, reads, writes)
        return tok

    def dma(self, E, out, in_, reads=(), writes=(), semkey=None):
        if self.dead:
            return None
        if SIM_FRESH_POOL and E == "pool":
            self.nsem += 1
            semkey = ("__fresh", self.nsem)
            self.dsem[semkey] = [self.stack.enter_context(self.nc.semaphore("dp%d" % self.nsem)), 0]
        if semkey not in self.dsem:
            if self.free:
                self.dsem[semkey] = self.free.pop()
            else:
                self.nsem += 1
                self.dsem[semkey] = [self.stack.enter_context(self.nc.semaphore("ds%d" % self.nsem)), 0]
        ent = self.dsem[semkey]
        self._deps(E, reads, writes)
        if ent[1] > 0:
            self._wait(E, (ent[0], ent[1], "dma"))
        ent[1] += 16
        self.eng[E].dma_start(out=out, in_=in_).then_inc(ent[0], 16)
        tok = (ent[0], ent[1], "dma:" + str(semkey))
        self._record(tok, reads, writes)
        return tok

    def barrier(self):
        if self.dead:
            return
        for E in self.eng:
            for Fn in self.eng:
                if Fn != E and self.ecnt[Fn] > 0:
                    self._wait(E, (self.esem[Fn], self.ecnt[Fn], Fn))
            for ent in self.dsem.values():
                if ent[1] > 0:
                    self._wait(E, (ent[0], ent[1], "dma"))
        self.lw = {}
        self.rd = {}
        self.free.extend(v for k, v in self.dsem.items() if not (isinstance(k, tuple) and k and k[0] == "__fresh"))
        self.dsem = {}


class _Stop(Exception):
    pass


def build_program(nlayers=DEPTH, debug=False, stop=None):
    nc = bass.Bass("TRN2", target_bir_lowering=False)
    stack = contextlib.ExitStack()
    with stack:
        try:
            _emit(nc, stack, nlayers, debug, stop)
        except _Stop:
            pass
    return nc


def _emit(nc, stack, nlayers, debug, stop=None):
    kb = KB(nc, stack)
    _bar = kb.barrier
    _phase = [0]

    def barrier_named():
        _bar()
        _phase[0] += 1
        if stop is not None and _phase[0] >= stop:
            kb.dead = True

    kb.barrier = barrier_named

    def din(name, shape, dt=F32):
        return nc.dram_tensor(name, list(shape), dt, kind="ExternalInput").ap()

    def dscr(name, shape, dt):
        kind = "ExternalOutput" if debug else None
        if kind:
            return nc.dram_tensor(name, list(shape), dt, kind=kind).ap()
        return nc.dram_tensor(name, list(shape), dt).ap()

    xT0 = din("xT0", [D, NT])
    cs_in = din("cs", [128, 8, 2])
    w_mod = din("w_mod", [DEPTH, D, 6 * D])
    bmodT = din("bmodT", [128, DEPTH, 48])
    g1T = din("g1T", [128, DEPTH, 8])
    g2T = din("g2T", [128, DEPTH, 8])
    w_in = din("w_in", [DEPTH, D, 2312])
    w_out = din("w_out", [DEPTH, D, D])
    gvg = din("gvg", [DEPTH, 256])
    wsT = din("wsT", [DEPTH, 4, 128, 128])
    gbs = din("gbs", [128, DEPTH, 2, 128])
    qgT = din("qgT", [128, DEPTH])
    kgT = din("kgT", [128, DEPTH])
    sinkL = din("sinkL", [128, DEPTH, 2, 2])
    convw = din("convw", [128, DEPTH, 6, 5])
    convb = din("convb", [128, DEPTH, 6])
    dtb = din("dtb", [DEPTH, 8])
    alog = din("alog", [DEPTH, 8])
    dskE = din("dskE", [DEPTH, 256])
    sng = din("sng", [DEPTH, 256])
    ffn_g = din("ffn_g", [2, D, DFF])
    ffn_u = din("ffn_u", [2, D, DFF])
    ffn_d = din("ffn_d", [2, DFF, D])
    moe_r = din("moe_r", [2, D, NEXP])
    _small = nlayers < 2
    moe_g = din("moe_g", [2, NEXP, D, DFF] if not _small else [2, NEXP, 128, 768])
    moe_u = din("moe_u", [2, NEXP, D, DFF] if not _small else [2, NEXP, 128, 768])
    moe_d = din("moe_d", [2, NEXP, DFF, D] if not _small else [2, NEXP, 128, 768])
    c_ident = din("c_ident", [128, 128])
    c_bones = din("c_bones", [128, 128])
    c_perm = din("c_perm", [128, 128])
    c_uf = din("c_uf", [128, 128])
    c_ub = din("c_ub", [128, 128])
    c_negf = din("c_negf", [128, 512])
    c_negb = din("c_negb", [128, 512])
    c_ropeC = din("c_ropeC", [128, L])
    c_ropeS = din("c_ropeS", [128, L])

    outT = nc.dram_tensor("outT", [D, L], F32, kind="ExternalOutput").ap()

    R = dscr("R", [D, NT], F32)
    GU = dscr("GU", [256, NT], BF16)
    GV = dscr("GV", [NT, 256], BF16)
    QT = dscr("QT", [512, NT], BF16)
    KT = dscr("KT", [256, NT], BF16)
    V = dscr("V", [NT, 128], BF16)
    ZS = dscr("ZS", [NT, 256], BF16)
    XBC = dscr("XBC", [768, NT], BF16)
    DT = dscr("DT", [NT, 16], F32)
    MIX = dscr("MIX", [D, NT], BF16)
    HT = dscr("HT", [D, NT], BF16)
    COMBT = dscr("COMBT", [NEXP, NT], F32)

    _uniq = [0]

    def sb(st, name, shape, dt):
        _uniq[0] += 1
        return st.enter_context(nc.sbuf_tensor("%s_u%d" % (name, _uniq[0]), list(shape), dt))

    PS = [stack.enter_context(nc.psum_tensor("ps%d" % i, [128, 512], F32)) for i in range(8)]

    ident_bf = sb(stack, "ident_bf", [128, 128], BF16)
    ident_f = sb(stack, "ident_f", [128, 128], F32)
    ones_bf = sb(stack, "ones_bf", [128, 128], BF16)
    bones_bf = sb(stack, "bones_bf", [128, 128], BF16)
    perm_bf = sb(stack, "perm_bf", [128, 128], BF16)
    uf_bf = sb(stack, "uf_bf", [128, 128], BF16)
    ub_bf = sb(stack, "ub_bf", [128, 128], BF16)
    ones_f = sb(stack, "ones_f", [128, 64], F32)
    eps_t = sb(stack, "eps_t", [128, 1], F32)
    one_t = sb(stack, "one_t", [128, 1], F32)
    mods = sb(stack, "mods", [128, DEPTH, 48, 2], F32)
    G1 = sb(stack, "G1", [128, DEPTH, 8, 2], F32)
    G2 = sb(stack, "G2", [128, DEPTH, 8, 2], F32)
    g1s = sb(stack, "g1s", [128, DEPTH, 8], F32)
    g2s = sb(stack, "g2s", [128, DEPTH, 8], F32)
    bms = sb(stack, "bms", [128, DEPTH, 48], F32)
    css = sb(stack, "css", [128, 8, 2], F32)
    qgs = sb(stack, "qgs", [128, DEPTH], F32)
    kgs = sb(stack, "kgs", [128, DEPTH], F32)
    sinkE = sb(stack, "sinkE", [128, DEPTH, 2, 2], F32)
    cws = sb(stack, "cws", [128, DEPTH, 6, 5], F32)
    cbs = sb(stack, "cbs", [128, DEPTH, 6], F32)

    sp, pool = "sp", "pool"
    kb.dma(pool, ident_bf[:], c_ident, writes=["ident_bf"], semkey="c0")
    kb.dma(sp, ident_f[:], c_ident, writes=["ident_f"], semkey="c1")
    kb.dma(pool, bones_bf[:], c_bones, writes=["bones_bf"], semkey="c2")
    kb.dma(pool, perm_bf[:], c_perm, writes=["perm_bf"], semkey="c3")
    kb.dma(pool, uf_bf[:], c_uf, writes=["uf_bf"], semkey="c4")
    kb.dma(pool, ub_bf[:], c_ub, writes=["ub_bf"], semkey="c5")
    kb.dma(sp, g1s[:], g1T, writes=["g1s"], semkey="c6")
    kb.dma(sp, g2s[:], g2T, writes=["g2s"], semkey="c7")
    kb.dma(sp, bms[:], bmodT, writes=["bms"], semkey="c8")
    kb.dma(sp, css[:], cs_in, writes=["css"], semkey="c9")
    kb.dma(sp, qgs[:], qgT, writes=["qgs"], semkey="c10")
    kb.dma(sp, kgs[:], kgT, writes=["kgs"], semkey="c11")
    kb.dma(sp, sinkE[:], sinkL, writes=["sinkE"], semkey="c12")
    kb.dma(sp, cws[:], convw, writes=["cws"], semkey="c13")
    kb.dma(sp, cbs[:], convb, writes=["cbs"], semkey="c14")
    kb.op("dve", lambda: nc.vector.memset(ones_bf[:], 1.0), writes=["ones_bf"])
    kb.op("dve", lambda: nc.vector.memset(ones_f[:], 1.0), writes=["ones_f"])
    kb.op("dve", lambda: nc.vector.memset(eps_t[:], EPS), writes=["eps_t"])
    kb.op("dve", lambda: nc.vector.memset(one_t[:], 1.0), writes=["one_t"])
    kb.op("act", lambda: nc.scalar.activation(out=css[:], in_=css[:], func=AF.Silu), reads=["css"], writes=["css"])
    kb.op("act", lambda: nc.scalar.activation(out=sinkE[:], in_=sinkE[:], func=AF.Exp), reads=["sinkE"], writes=["sinkE"])

    with contextlib.ExitStack() as st:
        wm = [sb(st, "wm%d" % i, [128, 8, 512], F32) for i in range(2)]
        it = 0
        for l in range(nlayers):
            for gidx in range(12):
                s = it % 2
                it += 1
                kb.dma(sp, wm[s][:], w_mod[l].rearrange("(k p) n -> p k n", p=128)[:, :, gidx * 512:(gidx + 1) * 512],
                       writes=[("wm", s)], semkey=("wm", s))
                for jj in range(4):
                    j = gidx * 4 + jj
                    for k in range(8):
                        kb.op("pe", lambda k=k, jj=jj, j=j, s=s: nc.tensor.matmul(
                            PS[0][:, j * 2:j * 2 + 2], lhsT=wm[s][:, k, jj * 128:(jj + 1) * 128], rhs=css[:, k, :],
                            start=(k == 0), stop=(k == 7)), reads=[("wm", s), "css"], writes=["ps0"])
            kb.op("dve", lambda l=l: nc.vector.tensor_tensor(
                out=mods[:, l, :, :], in0=PS[0][:, 0:96].rearrange("p (a b) -> p a b", b=2),
                in1=bms[:, l, :].unsqueeze(2).broadcast_to([128, 48, 2]), op=ALU.add),
                reads=["ps0", "bms"], writes=["mods"])
            for (Gt, gs, off) in ((G1, g1s, 8), (G2, g2s, 32)):
                kb.op("dve", lambda Gt=Gt, off=off, l=l: nc.vector.tensor_scalar(
                    out=Gt[:, l, :, :], in0=mods[:, l, off:off + 8, :], scalar1=1.0, scalar2=None, op0=ALU.add),
                    reads=["mods"], writes=["G"])
                kb.op("dve", lambda Gt=Gt, gs=gs, l=l: nc.vector.tensor_tensor(
                    out=Gt[:, l, :, :], in0=Gt[:, l, :, :], in1=gs[:, l, :].unsqueeze(2).broadcast_to([128, 8, 2]),
                    op=ALU.mult), reads=["G", "g1s", "g2s"], writes=["G"])
        kb.barrier()

    def rmsnorm_block(st_tiles, xt, T, Gt, l, shift_off, which, out_tile, out_key, hf=None, xk="xt"):
        sq, rstd, tmp = st_tiles
        for k in range(8):
            s = k % 2
            kb.op("act", lambda k=k, s=s: nc.scalar.activation(out=sq[s][:, :T], in_=xt[:, k, :T], func=AF.Square),
                  reads=[xk], writes=[("sq", s)])
            kb.op("pe", lambda k=k, s=s: nc.tensor.matmul(PS[7][:, :T], lhsT=ones_bf[:], rhs=sq[s][:, :T],
                                                          start=(k == 0), stop=(k == 7)),
                  reads=[("sq", s), "ones_bf"], writes=["ps7"])
        kb.op("act", lambda: nc.scalar.activation(out=rstd[:, :T], in_=PS[7][:, :T], func=AF.Sqrt,
                                                  bias=eps_t[:], scale=1.0 / D), reads=["ps7", "eps_t"], writes=["rstd"])
        kb.op("dve", lambda: nc.vector.reciprocal(out=rstd[:, :T], in_=rstd[:, :T]), reads=["rstd"], writes=["rstd"])
        for k in range(8):
            s = k % 2
            kb.op("dve", lambda k=k, s=s: nc.vector.scalar_tensor_tensor(
                out=tmp[s][:, :T], in0=xt[:, k, :T], scalar=Gt[:, l, k, which:which + 1], in1=rstd[:, :T],
                op0=ALU.mult, op1=ALU.mult), reads=[xk, "G", "rstd"], writes=[("tmp", s)])
            if hf is None:
                kb.op("act", lambda k=k, s=s: nc.scalar.activation(
                    out=out_tile[:, k, :T], in_=tmp[s][:, :T], func=AF.Identity,
                    bias=mods[:, l, shift_off + k, which:which + 1], scale=1.0),
                    reads=[("tmp", s), "mods"], writes=[out_key])
            else:
                kb.op("act", lambda k=k, s=s: nc.scalar.activation(
                    out=hf[:, k, :T], in_=tmp[s][:, :T], func=AF.Identity,
                    bias=mods[:, l, shift_off + k, which:which + 1], scale=1.0),
                    reads=[("tmp", s), "mods"], writes=["hf"])
                kb.op("pool", lambda k=k: nc.gpsimd.tensor_copy(out=out_tile[:, k, :T], in_=hf[:, k, :T]),
                      reads=["hf"], writes=[out_key])

    for l in range(nlayers):
        need_ctx = l < DEPTH - 1
        is_moe = (l % 2 == 1)
        jl = l // 2
        xsrc = xT0 if l == 0 else R

        with contextlib.ExitStack() as st:
            Wfm = sb(st, "Wfm", [128, 8, 1792], BF16)
            Wtm = sb(st, "Wtm", [128, 8, 648], BF16)
            xts = [sb(st, "xt%d" % i, [128, 8, 512], F32) for i in range(2)]
            hTs = [sb(st, "hT%d" % i, [128, 8, 512], BF16) for i in range(2)]
            sq = [sb(st, "sq%d" % i, [128, 512], BF16) for i in range(2)]
            tmp = [sb(st, "tmp%d" % i, [128, 512], F32) for i in range(2)]
            rstd = sb(st, "rstd", [128, 512], F32)
            stgF = [sb(st, "stgF%d" % i, [128, 14, 512], BF16) for i in range(2)]
            stgGV = [sb(st, "stgGV%d" % i, [128, 4, 256], BF16) for i in range(2)]
            stgV = [sb(st, "stgV%d" % i, [128, 4, 128], BF16) for i in range(2)]
            stgZ = [sb(st, "stgZ%d" % i, [128, 4, 256], BF16) for i in range(2)]
            stgDT = [sb(st, "stgDT%d" % i, [128, 4, 16], F32) for i in range(2)]
            ropeC = sb(st, "ropeC", [128, L], F32)
            ropeS = sb(st, "ropeS", [128, L], F32)
            gvB = sb(st, "gvB", [128, 256], F32)
            dtbB = sb(st, "dtbB", [128, 8], F32)
            aB = sb(st, "aB", [128, 8], F32)
            qsq = [sb(st, "qsq%d" % i, [128, 512], BF16) for i in range(2)]
            qrn = [sb(st, "qrn%d" % i, [128, 512], F32) for i in range(2)]
            qn = [sb(st, "qn%d" % i, [128, 512], BF16) for i in range(2)]
            qt1 = [sb(st, "qt1%d" % i, [128, 512], F32) for i in range(2)]
            qt2 = [sb(st, "qt2%d" % i, [128, 512], F32) for i in range(2)]
            gl = [sb(st, "gl%d" % i, [128, 256], F32) for i in range(2)]
            gsq = [sb(st, "gsq%d" % i, [128, 256], F32) for i in range(2)]
            gss = [sb(st, "gss%d" % i, [128, 4], F32) for i in range(2)]
            dtx = [sb(st, "dtx%d" % i, [128, 8], F32) for i in range(2)]

            wv = w_in[l].rearrange("(k p) n -> p k n", p=128)
            kb.dma(pool, Wfm[:, :, 0:256], wv[:, :, 0:256], writes=["Wfm"], semkey="wf0")
            kb.dma(pool, Wfm[:, :, 256:768], wv[:, :, 512:1024], writes=["Wfm"], semkey="wf1")
            for kvh in range(2):
                for dup in range(2):
                    c0 = 768 + kvh * 128 + dup * 64
                    kb.dma(pool, Wfm[:, :, c0:c0 + 64], wv[:, :, 1024 + kvh * 64:1024 + (kvh + 1) * 64],
                           writes=["Wfm"], semkey="wf2_%d%d" % (kvh, dup))
            kb.dma(pool, Wfm[:, :, 1024:1792], wv[:, :, 1536:2304], writes=["Wfm"], semkey="wf3")
            kb.dma(pool, Wtm[:, :, 0:256], wv[:, :, 256:512], writes=["Wtm"], semkey="wt0")
            kb.dma(pool, Wtm[:, :, 256:384], wv[:, :, 1152:1280], writes=["Wtm"], semkey="wt1")
            kb.dma(pool, Wtm[:, :, 384:392], wv[:, :, 2304:2312], writes=["Wtm"], semkey="wt2")
            kb.dma(pool, Wtm[:, :, 392:648], wv[:, :, 1280:1536], writes=["Wtm"], semkey="wt3")
            kb.dma(sp, ropeC[:], c_ropeC, writes=["ropeC"], semkey="rc")
            kb.dma(sp, ropeS[:], c_ropeS, writes=["ropeS"], semkey="rs")
            kb.dma(sp, gvB[:], gvg[l].partition_broadcast(128), writes=["gvB"], semkey="gvB")
            kb.dma(sp, dtbB[:], dtb[l].partition_broadcast(128), writes=["dtbB"], semkey="dtbB")
            kb.dma(sp, aB[:], alog[l].partition_broadcast(128), writes=["aB"], semkey="aB")
            kb.op("act", lambda: nc.scalar.activation(out=aB[:], in_=aB[:], func=AF.Exp), reads=["aB"], writes=["aB"])
            kb.op("dve", lambda: nc.vector.tensor_scalar(out=aB[:], in0=aB[:], scalar1=-1.0, scalar2=None, op0=ALU.mult),
                  reads=["aB"], writes=["aB"])

            bank_rot = [0]

            def next_bank():
                b = bank_rot[0] % 5
                bank_rot[0] += 1
                return b

            def load1(bi):
                t0, T, isctx = BLOCKS[bi]
                s = bi % 2
                kb.dma(sp, xts[s][:, :, :T], xsrc.rearrange("(k p) t -> p k t", p=128)[:, :, t0:t0 + T],
                       writes=[("xt", s)], semkey=("xt", s))

            load1(0)
            for bi, (t0, T, isctx) in enumerate(BLOCKS):
                s = bi % 2
                which = 1 if isctx else 0
                xt, hT = xts[s], hTs[s]
                if bi + 1 < len(BLOCKS):
                    load1(bi + 1)
                for k in range(8):
                    s2 = k % 2
                    kb.op("act", lambda k=k, s2=s2: nc.scalar.activation(out=sq[s2][:, :T], in_=xt[:, k, :T], func=AF.Square),
                          reads=[("xt", s)], writes=[("sq", s2)])
                    kb.op("pe", lambda k=k, s2=s2: nc.tensor.matmul(PS[7][:, :T], lhsT=ones_bf[:], rhs=sq[s2][:, :T],
                                                                    start=(k == 0), stop=(k == 7)),
                          reads=[("sq", s2), "ones_bf"], writes=["ps7"])
                kb.op("act", lambda: nc.scalar.activation(out=rstd[:, :T], in_=PS[7][:, :T], func=AF.Sqrt,
                                                          bias=eps_t[:], scale=1.0 / D),
                      reads=["ps7", "eps_t"], writes=["rstd"])
                kb.op("dve", lambda: nc.vector.reciprocal(out=rstd[:, :T], in_=rstd[:, :T]), reads=["rstd"], writes=["rstd"])
                for k in range(8):
                    s2 = k % 2
                    kb.op("dve", lambda k=k, s2=s2: nc.vector.scalar_tensor_tensor(
                        out=tmp[s2][:, :T], in0=xt[:, k, :T], scalar=G1[:, l, k, which:which + 1], in1=rstd[:, :T],
                        op0=ALU.mult, op1=ALU.mult), reads=[("xt", s), "G", "rstd"], writes=[("tmp", s2)])
                    kb.op("act", lambda k=k, s2=s2: nc.scalar.activation(
                        out=hT[:, k, :T], in_=tmp[s2][:, :T], func=AF.Identity,
                        bias=mods[:, l, k, which:which + 1], scale=1.0),
                        reads=[("tmp", s2), "mods"], writes=[("hT", s)])

                sF = stgF[s]
                for ci in range(14):
                    b = next_bank()
                    pk = "ps%d" % b
                    for k in range(8):
                        kb.op("pe", lambda k=k, ci=ci, b=b: nc.tensor.matmul(
                            PS[b][:, :T], lhsT=Wfm[:, k, ci * 128:(ci + 1) * 128], rhs=hT[:, k, :T],
                            start=(k == 0), stop=(k == 7)), reads=["Wfm", ("hT", s)], writes=[pk])
                    ok = ("stgF", s, ci)
                    if ci < 2:
                        kb.op("act", lambda ci=ci, b=b: nc.scalar.activation(out=sF[:, ci, :T], in_=PS[b][:, :T],
                                                                             func=AF.Gelu_apprx_tanh),
                              reads=[pk], writes=[ok])
                    elif ci >= 8:
                        kb.op("pool" if False else "dve", lambda ci=ci, b=b: nc.vector.tensor_copy(out=sF[:, ci, :T], in_=PS[b][:, :T]),
                              reads=[pk], writes=[ok])
                    else:
                        qs = ci % 2
                        gcol = qgs if ci < 6 else kgs
                        kb.op("act", lambda b=b, qs=qs: nc.scalar.activation(out=qsq[qs][:, :T], in_=PS[b][:, :T], func=AF.Square),
                              reads=[pk], writes=[("qsq", qs)])
                        kb.op("pe", lambda qs=qs: nc.tensor.matmul(PS[5][:, :T], lhsT=bones_bf[:], rhs=qsq[qs][:, :T],
                                                                  start=True, stop=True),
                              reads=[("qsq", qs), "bones_bf"], writes=["ps5"])
                        kb.op("act", lambda qs=qs: nc.scalar.activation(out=qrn[qs][:, :T], in_=PS[5][:, :T], func=AF.Sqrt,
                                                                        bias=eps_t[:], scale=1.0 / 64),
                              reads=["ps5", "eps_t"], writes=[("qrn", qs)])
                        kb.op("dve", lambda qs=qs: nc.vector.reciprocal(out=qrn[qs][:, :T], in_=qrn[qs][:, :T]),
                              reads=[("qrn", qs)], writes=[("qrn", qs)])
                        if isctx:
                            kb.op("dve", lambda b=b, qs=qs, ci=ci, gcol=gcol: nc.vector.scalar_tensor_tensor(
                                out=sF[:, ci, :T], in0=PS[b][:, :T], scalar=gcol[:, l:l + 1], in1=qrn[qs][:, :T],
                                op0=ALU.mult, op1=ALU.mult), reads=[pk, ("qrn", qs), "qgs", "kgs"], writes=[ok])
                        else:
                            lt0 = t0 - CL
                            kb.op("dve", lambda b=b, qs=qs, gcol=gcol: nc.vector.scalar_tensor_tensor(
                                out=qn[qs][:, :T], in0=PS[b][:, :T], scalar=gcol[:, l:l + 1], in1=qrn[qs][:, :T],
                                op0=ALU.mult, op1=ALU.mult), reads=[pk, ("qrn", qs), "qgs", "kgs"], writes=[("qn", qs)])
                            kb.op("pe", lambda qs=qs: nc.tensor.matmul(PS[6][:, :T], lhsT=perm_bf[:], rhs=qn[qs][:, :T],
                                                                      start=True, stop=True),
                                  reads=[("qn", qs), "perm_bf"], writes=["ps6"])
                            kb.op("pool", lambda qs=qs, lt0=lt0: nc.gpsimd.tensor_tensor(
                                out=qt1[qs][:, :T], in0=qn[qs][:, :T], in1=ropeC[:, lt0:lt0 + T], op=ALU.mult),
                                reads=[("qn", qs), "ropeC"], writes=[("qt1", qs)])
                            kb.op("dve", lambda qs=qs, lt0=lt0: nc.vector.tensor_tensor(
                                out=qt2[qs][:, :T], in0=PS[6][:, :T], in1=ropeS[:, lt0:lt0 + T], op=ALU.mult),
                                reads=["ps6", "ropeS"], writes=[("qt2", qs)])
                            kb.op("pool", lambda qs=qs, ci=ci: nc.gpsimd.tensor_tensor(
                                out=sF[:, ci, :T], in0=qt1[qs][:, :T], in1=qt2[qs][:, :T], op=ALU.add),
                                reads=[("qt1", qs), ("qt2", qs)], writes=[ok])
                kb.dma(sp, GU.rearrange("(c p) t -> p c t", p=128)[:, :, t0:t0 + T], sF[:, 0:2, :T],
                       reads=[("stgF", s, ci) for ci in (0, 1)], semkey=("oGU", s))
                kb.dma(sp, QT.rearrange("(c p) t -> p c t", p=128)[:, :, t0:t0 + T], sF[:, 2:6, :T],
                       reads=[("stgF", s, ci) for ci in (2, 3, 4, 5)], semkey=("oQT", s))
                kb.dma(sp, KT.rearrange("(c p) t -> p c t", p=128)[:, :, t0:t0 + T], sF[:, 6:8, :T],
                       reads=[("stgF", s, ci) for ci in (6, 7)], semkey=("oKT", s))
                kb.dma(sp, XBC.rearrange("(c p) t -> p c t", p=128)[:, :, t0:t0 + T], sF[:, 8:14, :T],
                       reads=[("stgF", s, ci) for ci in range(8, 14)], semkey=("oXB", s))

                nsub = T // 128
                for sub in range(nsub):
                    ss_ = sub % 2
                    ba = next_bank()
                    bb = next_bank()
                    pka, pkb = "ps%d" % ba, "ps%d" % bb
                    for k in range(8):
                        kb.op("pe", lambda k=k, sub=sub, ba=ba: nc.tensor.matmul(
                            PS[ba][:, 0:392], lhsT=hT[:, k, sub * 128:(sub + 1) * 128], rhs=Wtm[:, k, 0:392],
                            start=(k == 0), stop=(k == 7)), reads=["Wtm", ("hT", s)], writes=[pka])
                    for k in range(8):
                        kb.op("pe", lambda k=k, sub=sub, bb=bb: nc.tensor.matmul(
                            PS[bb][:, 0:256], lhsT=hT[:, k, sub * 128:(sub + 1) * 128], rhs=Wtm[:, k, 392:648],
                            start=(k == 0), stop=(k == 7)), reads=["Wtm", ("hT", s)], writes=[pkb])
                    kb.op("act", lambda ba=ba, ss_=ss_: nc.scalar.activation(out=gl[ss_][:], in_=PS[ba][:, 0:256],
                                                                             func=AF.Gelu_apprx_tanh),
                          reads=[pka], writes=[("gl", ss_)])
                    kb.op("pool", lambda ss_=ss_: nc.gpsimd.tensor_tensor(out=gsq[ss_][:], in0=gl[ss_][:], in1=gl[ss_][:], op=ALU.mult),
                          reads=[("gl", ss_)], writes=[("gsq", ss_)])
                    kb.op("dve", lambda ss_=ss_: nc.vector.tensor_reduce(
                        out=gss[ss_][:], in_=gsq[ss_][:].rearrange("p (a b) -> p a b", b=64), axis=AX.X, op=ALU.add),
                        reads=[("gsq", ss_)], writes=[("gss", ss_)])
                    kb.op("act", lambda ss_=ss_: nc.scalar.activation(out=gss[ss_][:], in_=gss[ss_][:], func=AF.Sqrt,
                                                                      bias=eps_t[:], scale=1.0 / 64),
                          reads=[("gss", ss_), "eps_t"], writes=[("gss", ss_)])
                    kb.op("dve", lambda ss_=ss_: nc.vector.reciprocal(out=gss[ss_][:], in_=gss[ss_][:]),
                          reads=[("gss", ss_)], writes=[("gss", ss_)])
                    kb.op("dve", lambda ss_=ss_: nc.vector.tensor_tensor(
                        out=gsq[ss_][:].rearrange("p (a b) -> p a b", b=64), in0=gl[ss_][:].rearrange("p (a b) -> p a b", b=64),
                        in1=gss[ss_][:].unsqueeze(2).broadcast_to([128, 4, 64]), op=ALU.mult),
                        reads=[("gl", ss_), ("gss", ss_)], writes=[("gsq", ss_)])
                    kb.op("pool", lambda ss_=ss_, sub=sub: nc.gpsimd.tensor_tensor(
                        out=stgGV[s][:, sub, :], in0=gsq[ss_][:], in1=gvB[:], op=ALU.mult),
                        reads=[("gsq", ss_), "gvB"], writes=[("stgGV", s)])
                    kb.op("act", lambda ba=ba, sub=sub: nc.scalar.copy(out=stgV[s][:, sub, :], in_=PS[ba][:, 256:384]),
                          reads=[pka], writes=[("stgV", s)])
                    kb.op("dve", lambda ba=ba, ss_=ss_: nc.vector.tensor_tensor(out=dtx[ss_][:], in0=PS[ba][:, 384:392], in1=dtbB[:],
                                                                               op=ALU.add),
                          reads=[pka, "dtbB"], writes=[("dtx", ss_)])
                    kb.op("act", lambda ss_=ss_: nc.scalar.activation(out=dtx[ss_][:], in_=dtx[ss_][:], func=AF.Exp),
                          reads=[("dtx", ss_)], writes=[("dtx", ss_)])
                    kb.op("act", lambda ss_=ss_, sub=sub: nc.scalar.activation(out=stgDT[s][:, sub, 0:8], in_=dtx[ss_][:], func=AF.Ln,
                                                                               bias=one_t[:], scale=1.0),
                          reads=[("dtx", ss_), "one_t"], writes=[("stgDT", s)])
                    kb.op("dve", lambda sub=sub: nc.vector.tensor_tensor(out=stgDT[s][:, sub, 8:16], in0=stgDT[s][:, sub, 0:8],
                                                                         in1=aB[:], op=ALU.mult),
                          reads=[("stgDT", s), "aB"], writes=[("stgDT", s)])
                    kb.op("act", lambda bb=bb, sub=sub: nc.scalar.activation(out=stgZ[s][:, sub, :], in_=PS[bb][:, 0:256], func=AF.Silu),
                          reads=[pkb], writes=[("stgZ", s)])
                kb.dma(sp, GV[t0:t0 + T, :].rearrange("(s p) c -> p s c", p=128), stgGV[s][:, 0:nsub, :],
                       reads=[("stgGV", s)], semkey=("oGV", s))
                kb.dma(sp, V[t0:t0 + T, :].rearrange("(s p) c -> p s c", p=128), stgV[s][:, 0:nsub, :],
                       reads=[("stgV", s)], semkey=("oV", s))
                kb.dma(sp, ZS[t0:t0 + T, :].rearrange("(s p) c -> p s c", p=128), stgZ[s][:, 0:nsub, :],
                       reads=[("stgZ", s)], semkey=("oZS", s))
                kb.dma(sp, DT[t0:t0 + T, :].rearrange("(s p) c -> p s c", p=128), stgDT[s][:, 0:nsub, :],
                       reads=[("stgDT", s)], semkey=("oDT", s))
            kb.barrier()

        with contextlib.ExitStack() as st:
            wsb = sb(st, "wsb", [128, 4, 128], BF16)
            bsr = sb(st, "bsr", [128, 2, 128], F32)
            gtmp = [sb(st, "gtmp%d" % i, [128, 2, 128], F32) for i in range(2)]
            guT = [sb(st, "guT%d" % i, [128, 2, 512], BF16) for i in range(2)]
            vh = [sb(st, "vh%d" % i, [128, 4, 256], BF16) for i in range(2)]
            stg = [sb(st, "gstg%d" % i, [128, 2, 512], BF16) for i in range(2)]
            kb.dma(pool, wsb[:], wsT[l].rearrange("h j i -> j h i"), writes=["wsb"], semkey="wsb")
            kb.dma(sp, bsr[:], gbs[:, l, :, :], writes=["bsr"], semkey="bsr")
            blks = [b for b in BLOCKS if (need_ctx or not b[2])]
            def load2a(bi):
                t0, T, isctx = blks[bi]
                s = bi % 2
                nsub = T // 128
                kb.dma(sp, guT[s][:, :, :T], GU.rearrange("(c p) t -> p c t", p=128)[:, :, t0:t0 + T],
                       writes=[("guT", s)], semkey=("guT", s))
                kb.dma(sp, vh[s][:, 0:nsub, :], GV[t0:t0 + T, :].rearrange("(s p) c -> p s c", p=128),
                       writes=[("vh", s)], semkey=("vh", s))

            load2a(0)
            for bi, (t0, T, isctx) in enumerate(blks):
                s = bi % 2
                nsub = T // 128
                if bi + 1 < len(blks):
                    load2a(bi + 1)
                for sub in range(nsub):
                    b = sub % 4
                    pk = "ps%d" % b
                    for j in range(2):
                        for hh in range(2):
                            h = 2 * j + hh
                            kb.op("pe", lambda b=b, j=j, hh=hh, h=h, sub=sub: nc.tensor.matmul(
                                PS[b][hh * 64:(hh + 1) * 64, j * 128:(j + 1) * 128], lhsT=vh[s][:, sub, h * 64:(h + 1) * 64],
                                rhs=wsb[:, h, :], start=True, stop=True), reads=[("vh", s), "wsb"], writes=[pk])
                    g2 = sub % 2
                    kb.op("dve", lambda b=b, g2=g2: nc.vector.tensor_tensor(
                        out=gtmp[g2][:], in0=PS[b][:, 0:256].rearrange("p (a b) -> p a b", b=128),
                        in1=bsr[:], op=ALU.add), reads=[pk, "bsr"], writes=[("gtmp", g2)])
                    kb.op("pool", lambda sub=sub, g2=g2: nc.gpsimd.tensor_tensor(
                        out=stg[s][:, :, sub * 128:(sub + 1) * 128], in0=gtmp[g2][:],
                        in1=guT[s][:, :, sub * 128:(sub + 1) * 128], op=ALU.mult),
                        reads=[("gtmp", g2), ("guT", s)], writes=[("gstg", s)])
                kb.dma(sp, MIX.rearrange("(c p) t -> p c t", p=128)[:, 0:2, t0:t0 + T], stg[s][:, :, :T],
                       reads=[("gstg", s)], semkey=("oMIXg", s))
            kb.barrier()

        with contextlib.ExitStack() as st:
            QTs = sb(st, "QTs", [128, 4, NT], BF16)
            KTz = sb(st, "KTz", [128, 2, 2, NT], BF16)
            Vs = sb(st, "Vs", [128, NTILE, 128], BF16)
            onesv = sb(st, "onesv", [128, 64], BF16)
            Es = [sb(st, "E%d" % i, [128, 512], BF16) for i in range(4)]
            den = [sb(st, "den%d" % i, [128, 256], F32) for i in range(2)]
            stg = [sb(st, "astg%d" % i, [128, 4, 512], BF16) for i in range(2)]
            kb.op("dve", lambda: nc.vector.memset(onesv[:], 1.0), writes=["onesv"])
            for c4 in range(4):
                kb.dma(sp, QTs[:, c4, :], QT[c4 * 128:(c4 + 1) * 128, :], writes=["QTs"], semkey=("QTs", c4))
            kb.op("pool", lambda: nc.gpsimd.memset(KTz[:], 0.0), writes=["KTs"])
            for c2 in range(2):
                for half in range(2):
                    kb.dma(sp, KTz[half * 64:(half + 1) * 64, c2, half, :],
                           KT[c2 * 128 + half * 64:c2 * 128 + (half + 1) * 64, :], writes=["KTs"], semkey=("KTs", c2, half))
            for c8 in range(0, NTILE, 6):
                c9 = min(NTILE, c8 + 6)
                kb.dma(sp, Vs[:, c8:c9, :], V[c8 * 128:c9 * 128, :].rearrange("(s p) c -> p s c", p=128),
                       writes=["Vs"], semkey=("Vs", c8))
            srot = [0]
            od = [0]
            blks = [b for b in BLOCKS if (need_ctx or not b[2])]
            for bi, (t0, T, isctx) in enumerate(blks):
                s = bi % 2
                nsub = T // 128
                for sub in range(nsub):
                    tq = t0 // 128 + sub
                    if isctx:
                        kts = [(0, None), (1, None)]
                    else:
                        n = tq - 2
                        kts = [(0, None), (1, None)]
                        if n > 0:
                            kts.append((tq - 1, ub_bf))
                        kts.append((tq, None))
                        if n < 31:
                            kts.append((tq + 1, uf_bf))
                    for kv in range(2):
                        ob = 4 + (od[0] % 2)
                        db = 6 + (od[0] % 2)
                        od[0] += 1
                        okey, dkey = "ps%d" % ob, "ps%d" % db
                        slots = {}

                        def emitS(i, kv=kv, tq=tq, kts=kts, slots=slots):
                            kt, msk = kts[i]
                            sl = srot[0] % 4
                            srot[0] += 1
                            slots[i] = sl
                            pk = "ps%d" % sl
                            for half in range(2):
                                kb.op("pe", lambda half=half, sl=sl, kt=kt: nc.tensor.matmul(
                                    PS[sl][:, half * 256:(half + 1) * 256].rearrange("p (a b) -> p a b", b=128),
                                    lhsT=KTz[:, kv, half, kt * 128:(kt + 1) * 128],
                                    rhs=QTs[:, 2 * kv:2 * kv + 2, tq * 128:(tq + 1) * 128],
                                    start=True, stop=True), reads=["KTs", "QTs"], writes=[pk])
                            kb.op("act", lambda sl=sl: nc.scalar.activation(out=Es[sl][:], in_=PS[sl][:], func=AF.Exp, scale=0.125),
                                  reads=[pk], writes=[("E", sl)])
                            if msk is not None:
                                kb.op("pool", lambda sl=sl, msk=msk: nc.gpsimd.tensor_tensor(
                                    out=Es[sl][:].rearrange("p (a b) -> p a b", b=128),
                                    in0=Es[sl][:].rearrange("p (a b) -> p a b", b=128),
                                    in1=msk[:].unsqueeze(1).broadcast_to([128, 4, 128]), op=ALU.mult),
                                    reads=[("E", sl), "uf_bf", "ub_bf"], writes=[("E", sl)])

                        def emitPV(i, kv=kv, kts=kts, slots=slots, ob=ob, db=db, okey=okey, dkey=dkey):
                            kt, _ = kts[i]
                            sl = slots[i]
                            first, last = (i == 0), (i == len(kts) - 1)
                            for half in range(2):
                                kb.op("pe", lambda half=half, sl=sl, kt=kt: nc.tensor.matmul(
                                    PS[ob][half * 64:(half + 1) * 64, 0:256], lhsT=Vs[:, kt, kv * 64:(kv + 1) * 64],
                                    rhs=Es[sl][:, half * 256:(half + 1) * 256], start=first, stop=last),
                                    reads=["Vs", ("E", sl)], writes=[okey])
                                kb.op("pe", lambda half=half, sl=sl: nc.tensor.matmul(
                                    PS[db][half * 64:(half + 1) * 64, 0:256], lhsT=onesv[:],
                                    rhs=Es[sl][:, half * 256:(half + 1) * 256], start=first, stop=last),
                                    reads=["onesv", ("E", sl)], writes=[dkey])

                        nk = len(kts)
                        emitS(0)
                        if nk > 1:
                            emitS(1)
                        for i in range(nk):
                            emitPV(i)
                            if i + 2 < nk:
                                emitS(i + 2)
                        dn = den[kv]
                        kb.op("dve", lambda db=db, dn=dn, kv=kv: nc.vector.tensor_tensor(
                            out=dn[:].rearrange("p (a b) -> p a b", b=128),
                            in0=PS[db][:, 0:256].rearrange("p (a b) -> p a b", b=128),
                            in1=sinkE[:, l, kv, :].unsqueeze(2).broadcast_to([128, 2, 128]), op=ALU.add),
                            reads=[dkey, "sinkE"], writes=[("den", kv)])
                        kb.op("dve", lambda dn=dn: nc.vector.reciprocal(out=dn[:], in_=dn[:]),
                              reads=[("den", kv)], writes=[("den", kv)])
                        kb.op("dve", lambda ob=ob, dn=dn, kv=kv, sub=sub: nc.vector.tensor_tensor(
                            out=stg[s][:, 2 * kv:2 * kv + 2, sub * 128:(sub + 1) * 128],
                            in0=PS[ob][:, 0:256].rearrange("p (a b) -> p a b", b=128),
                            in1=dn[:].rearrange("p (a b) -> p a b", b=128), op=ALU.mult),
                            reads=[okey, ("den", kv)], writes=[("astg", s)])
                kb.dma(sp, MIX.rearrange("(c p) t -> p c t", p=128)[:, 2:6, t0:t0 + T], stg[s][:, :, :T],
                       reads=[("astg", s)], semkey=("oMIXa", s))
            kb.barrier()

        with contextlib.ExitStack() as st:
            XC = sb(st, "XC", [128, 6, NT], BF16)
            Xtm = sb(st, "Xtm", [128, NTILE, 256], BF16)
            Btm = sb(st, "Btm", [128, NTILE, 256], BF16)
            Ytm = sb(st, "Ytm", [128, NTILE, 256], F32)
            DTs = sb(st, "DTs", [128, NTILE, 16], F32)
            ZSs = sb(st, "ZSs", [128, NTILE, 256], BF16)
            Sf = sb(st, "Sf", [128, 256], F32)
            Sb_ = sb(st, "Sb", [128, 256], BF16)
            negf = sb(st, "negf", [128, 512], BF16)
            negb = sb(st, "negb", [128, 512], BF16)
            dskB = sb(st, "dskB", [128, 256], F32)
            ngB = sb(st, "ngB", [128, 256], F32)
            XB = [sb(st, "XB%d" % i, [128, 6, 516], BF16) for i in range(2)]
            cacc = [sb(st, "cacc%d" % i, [128, 512], F32) for i in range(2)]
            rhsA = [sb(st, "rhsA%d" % i, [128, 512], BF16) for i in range(2)]
            adtb = [sb(st, "adtb%d" % i, [128, 4], BF16) for i in range(2)]
            col = [sb(st, "col%d" % i, [128, 4], F32) for i in range(2)]
            Dm = [sb(st, "Dm%d" % i, [128, 512], F32) for i in range(2)]
            Lm = [sb(st, "Lm%d" % i, [128, 512], BF16) for i in range(2)]
            MT = [sb(st, "MT%d" % i, [128, 512], BF16) for i in range(2)]
            sm = [sb(st, "sm%d" % i, [128, 16], F32) for i in range(2)]
            xw = [sb(st, "xw%d" % i, [128, 256], BF16) for i in range(2)]
            xdt = [sb(st, "xdt%d" % i, [128, 256], BF16) for i in range(2)]
            yt1 = [sb(st, "yt1%d" % i, [128, 256], F32) for i in range(2)]
            yt2 = [sb(st, "yt2%d" % i, [128, 256], F32) for i in range(2)]
            tS = sb(st, "tS", [128, 256], F32)
            yz = [sb(st, "yz%d" % i, [128, 256], F32) for i in range(2)]
            yjunk = sb(st, "yjunk", [128, 256], F32)
            yss = [sb(st, "yss%d" % i, [128, 1], F32) for i in range(2)]
            yo = [sb(st, "yo%d" % i, [128, 256], BF16) for i in range(2)]
            stg = [sb(st, "sstg%d" % i, [128, 2, 512], BF16) for i in range(2)]

            kb.dma(pool, negf[:], c_negf, writes=["negf"], semkey="negf")
            kb.dma(pool, negb[:], c_negb, writes=["negb"], semkey="negb")
            kb.dma(sp, dskB[:], dskE[l].partition_broadcast(128), writes=["dskB"], semkey="dskB")
            kb.dma(sp, ngB[:], sng[l].partition_broadcast(128), writes=["ngB"], semkey="ngB")
            for c8 in range(0, NTILE, 6):
                c9 = min(NTILE, c8 + 6)
                kb.dma(sp, DTs[:, c8:c9, :], DT[c8 * 128:c9 * 128, :].rearrange("(s p) c -> p s c", p=128),
                       writes=["DTs"], semkey=("DTs", c8))
                kb.dma(sp, ZSs[:, c8:c9, :], ZS[c8 * 128:c9 * 128, :].rearrange("(s p) c -> p s c", p=128),
                       writes=["ZSs"], semkey=("ZSs", c8))

            def loadxb(bi):
                t0, T, isctx = BLOCKS[bi]
                s = bi % 2
                seg0, seg1 = (0, CL) if isctx else (CL, NT)
                lo, hi = max(t0 - 2, seg0), min(t0 + T + 2, seg1)
                xb = XB[s]
                wr = [("XB", s)]
                if lo > t0 - 2:
                    kb.op("pool", lambda xb=xb: nc.gpsimd.memset(xb[:, :, 0:2], 0.0), writes=wr)
                if hi < t0 + T + 2:
                    kb.op("pool", lambda xb=xb, T=T: nc.gpsimd.memset(xb[:, :, T + 2:T + 4], 0.0), writes=wr)
                kb.dma(sp, xb[:, :, lo - (t0 - 2):hi - (t0 - 2)], XBC.rearrange("(c p) t -> p c t", p=128)[:, :, lo:hi],
                       writes=wr, semkey=("XB", s))

            loadxb(0)
            for bi, (t0, T, isctx) in enumerate(BLOCKS):
                s = bi % 2
                xb = XB[s]
                wr = [("XB", s)]
                if bi + 1 < len(BLOCKS):
                    loadxb(bi + 1)
                for j in range(6):
                    a = cacc[j % 2]
                    ak = ("cacc", j % 2)
                    kb.op("dve", lambda j=j, a=a, xb=xb, T=T: nc.vector.tensor_scalar(
                        out=a[:, :T], in0=xb[:, j, 0:T], scalar1=cws[:, l, j, 0:1], scalar2=None, op0=ALU.mult),
                        reads=wr + ["cws"], writes=[ak])
                    for k in range(1, 5):
                        kb.op("dve", lambda j=j, a=a, xb=xb, T=T, k=k: nc.vector.scalar_tensor_tensor(
                            out=a[:, :T], in0=xb[:, j, k:k + T], scalar=cws[:, l, j, k:k + 1], in1=a[:, :T],
                            op0=ALU.mult, op1=ALU.add), reads=wr + ["cws", ak], writes=[ak])
                    kb.op("act", lambda j=j, a=a, T=T, t0=t0: nc.scalar.activation(
                        out=XC[:, j, t0:t0 + T], in_=a[:, :T], func=AF.Silu, bias=cbs[:, l, j:j + 1], scale=1.0),
                        reads=[ak, "cbs"], writes=[("XC", bi)])
            kb.barrier()

            for c in range(NTILE):
                b = 6 + (c % 2)
                pk = "ps%d" % b
                psb = PS[b][:].bitcast(BF16)
                for q4, j in enumerate((0, 1, 2, 3)):
                    kb.op("pe", lambda q4=q4, j=j, c=c, psb=psb: nc.tensor.transpose(
                        out=psb[:, q4 * 128:(q4 + 1) * 128], in_=XC[:, j, c * 128:(c + 1) * 128], identity=ident_bf[:]),
                        reads=["XC", "ident_bf"], writes=[pk])
                kb.op("act", lambda c=c, psb=psb: nc.scalar.copy(out=Xtm[:, c, :], in_=psb[:, 0:256]), reads=[pk], writes=[("Xtm", c)])
                kb.op("act", lambda c=c, psb=psb: nc.scalar.copy(out=Btm[:, c, :], in_=psb[:, 256:512]), reads=[pk], writes=[("Btm", c)])
                kb.op("pool", lambda c=c: nc.gpsimd.tensor_tensor(out=Ytm[:, c, :], in0=Xtm[:, c, :], in1=dskB[:], op=ALU.mult),
                      reads=[("Xtm", c), "dskB"], writes=[("Ytm", c)])

            kb.barrier()
            it = [0]
            for d in range(2):
                order = list(range(NTILE)) if d == 0 else [1, 0] + list(range(NTILE - 1, 1, -1))
                U = uf_bf if d == 0 else ub_bf
                NEG = negf if d == 0 else negb
                last = 127 if d == 0 else 0
                kb.op("dve", lambda: nc.vector.memset(Sf[:], 0.0), reads=["Sb"], writes=["Sf"])
                kb.op("pool", lambda: nc.gpsimd.memset(Sb_[:], 0.0), writes=["Sb"])
                for c in order:
                    want_y = need_ctx or c >= 2
                    if c == 2 and d == 0:
                        pass
                    s = it[0] % 2
                    it[0] += 1
                    rb = s
                    rk = "ps%d" % rb
                    dt4 = DTs[:, c, d * 4:(d + 1) * 4]
                    adt4 = DTs[:, c, 8 + d * 4:12 + d * 4]
                    kb.op("dve", lambda s=s, U=U, adt4=adt4: nc.vector.tensor_tensor(
                        out=rhsA[s][:].rearrange("p (a b) -> p a b", b=128),
                        in0=U[:].unsqueeze(1).broadcast_to([128, 4, 128]),
                        in1=adt4.unsqueeze(2).broadcast_to([128, 4, 128]), op=ALU.mult),
                        reads=["DTs", "uf_bf", "ub_bf"], writes=[("rhsA", s)])
                    kb.op("act", lambda s=s, adt4=adt4: nc.scalar.copy(out=adtb[s][:], in_=adt4), reads=["DTs"], writes=[("adtb", s)])
                    kb.op("pe", lambda s=s, rb=rb: nc.tensor.matmul(PS[rb][:], lhsT=ones_bf[:], rhs=rhsA[s][:], start=True, stop=False),
                          reads=[("rhsA", s), "ones_bf"], writes=[rk])
                    kb.op("pe", lambda rb=rb, NEG=NEG: nc.tensor.matmul(PS[rb][:], lhsT=ident_bf[:], rhs=NEG[:], start=False, stop=True),
                          reads=["negf", "negb", "ident_bf"], writes=[rk])
                    kb.op("pe", lambda s=s, U=U: nc.tensor.matmul(PS[2][:, 256:260], lhsT=U[:], rhs=adtb[s][:], start=True, stop=True),
                          reads=[("adtb", s), "uf_bf", "ub_bf"], writes=["ps2"])
                    for g in range(2):
                        kb.op("pe", lambda g=g, c=c: nc.tensor.matmul(
                            PS[2][:, g * 128:(g + 1) * 128], lhsT=XC[:, 2 + g, c * 128:(c + 1) * 128],
                            rhs=XC[:, 4 + g, c * 128:(c + 1) * 128], start=True, stop=True), reads=["XC"], writes=["ps2"])
                    kb.op("act", lambda s=s: nc.scalar.copy(out=col[s][:], in_=PS[2][:, 256:260]), reads=["ps2"], writes=[("col", s)])
                    kb.op("dve", lambda s=s, rb=rb: nc.vector.tensor_tensor(
                        out=Dm[s][:].rearrange("p (a b) -> p a b", b=128), in0=PS[rb][:].rearrange("p (a b) -> p a b", b=128),
                        in1=col[s][:].unsqueeze(2).broadcast_to([128, 4, 128]), op=ALU.subtract),
                        reads=[rk, ("col", s)], writes=[("Dm", s)])
                    kb.op("act", lambda s=s: nc.scalar.activation(out=Lm[s][:], in_=Dm[s][:], func=AF.Exp),
                          reads=[("Dm", s)], writes=[("Lm", s)])
                    kb.op("dve", lambda s=s: nc.vector.tensor_tensor(
                        out=MT[s][:].rearrange("p (g h i) -> p g h i", g=2, h=2),
                        in0=Lm[s][:].rearrange("p (g h i) -> p g h i", g=2, h=2),
                        in1=PS[2][:, 0:256].rearrange("p (g i) -> p g i", g=2).unsqueeze(2).broadcast_to([128, 2, 2, 128]),
                        op=ALU.mult), reads=[("Lm", s), "ps2"], writes=[("MT", s)])
                    tot = PS[rb][:].rearrange("p (a b) -> p a b", b=128)[:, :, last]
                    smk = ("sm", s)
                    kb.op("dve", lambda s=s, tot=tot: nc.vector.tensor_tensor(out=sm[s][:, 0:4], in0=tot, in1=col[s][:], op=ALU.subtract),
                          reads=[rk, ("col", s)], writes=[smk])
                    kb.op("act", lambda s=s: nc.scalar.activation(out=sm[s][:, 0:4], in_=sm[s][:, 0:4], func=AF.Exp), reads=[smk], writes=[smk])
                    kb.op("dve", lambda s=s, dt4=dt4: nc.vector.tensor_tensor(out=sm[s][:, 0:4], in0=sm[s][:, 0:4], in1=dt4, op=ALU.mult),
                          reads=[smk, "DTs"], writes=[smk])
                    kb.op("act", lambda s=s: nc.scalar.activation(out=sm[s][:, 4:8], in_=col[s][:], func=AF.Exp), reads=[("col", s)], writes=[smk])
                    kb.op("act", lambda s=s, tot=tot: nc.scalar.activation(out=sm[s][:, 8:12], in_=tot, func=AF.Exp), reads=[rk], writes=[smk])
                    kb.op("dve", lambda s=s, c=c: nc.vector.tensor_tensor(
                        out=xw[s][:].rearrange("p (a b) -> p a b", b=64), in0=Xtm[:, c, :].rearrange("p (a b) -> p a b", b=64),
                        in1=sm[s][:, 0:4].unsqueeze(2).broadcast_to([128, 4, 64]), op=ALU.mult),
                        reads=[("Xtm", c), smk], writes=[("xw", s)])
                    if want_y:
                        kb.op("pool", lambda s=s, c=c, dt4=dt4: nc.gpsimd.tensor_tensor(
                            out=xdt[s][:].rearrange("p (a b) -> p a b", b=64), in0=Xtm[:, c, :].rearrange("p (a b) -> p a b", b=64),
                            in1=dt4.unsqueeze(2).broadcast_to([128, 4, 64]), op=ALU.mult),
                            reads=[("Xtm", c), "DTs"], writes=[("xdt", s)])
                        for h in range(4):
                            kb.op("pe", lambda s=s, h=h: nc.tensor.matmul(
                                PS[3][:, h * 64:(h + 1) * 64], lhsT=MT[s][:, h * 128:(h + 1) * 128], rhs=xdt[s][:, h * 64:(h + 1) * 64],
                                start=True, stop=True), reads=[("MT", s), ("xdt", s)], writes=["ps3"])
                        for g in range(2):
                            kb.op("pe", lambda g=g, c=c: nc.tensor.matmul(
                                PS[4][:, g * 128:(g + 1) * 128], lhsT=XC[:, 4 + g, c * 128:(c + 1) * 128],
                                rhs=Sb_[:, g * 128:(g + 1) * 128], start=True, stop=True), reads=["XC", "Sb"], writes=["ps4"])
                        kb.op("dve", lambda s=s: nc.vector.tensor_tensor(
                            out=yt1[s][:].rearrange("p (a b) -> p a b", b=64), in0=PS[4][:, 0:256].rearrange("p (a b) -> p a b", b=64),
                            in1=sm[s][:, 4:8].unsqueeze(2).broadcast_to([128, 4, 64]), op=ALU.mult),
                            reads=["ps4", smk], writes=[("yt1", s)])
                        kb.op("dve", lambda s=s: nc.vector.tensor_tensor(out=yt2[s][:], in0=PS[3][:, 0:256], in1=yt1[s][:], op=ALU.add),
                              reads=["ps3", ("yt1", s)], writes=[("yt2", s)])
                        kb.op("pool", lambda s=s, c=c: nc.gpsimd.tensor_tensor(out=Ytm[:, c, :], in0=Ytm[:, c, :], in1=yt2[s][:], op=ALU.add),
                              reads=[("yt2", s), ("Ytm", c)], writes=[("Ytm", c)])
                    for g in range(2):
                        kb.op("pe", lambda g=g, c=c, s=s: nc.tensor.matmul(
                            PS[5][:, g * 128:(g + 1) * 128], lhsT=Btm[:, c, g * 128:(g + 1) * 128],
                            rhs=xw[s][:, g * 128:(g + 1) * 128], start=True, stop=True), reads=[("Btm", c), ("xw", s)], writes=["ps5"])
                    kb.op("dve", lambda s=s: nc.vector.tensor_tensor(
                        out=tS[:].rearrange("p (a b) -> p a b", b=64), in0=Sf[:].rearrange("p (a b) -> p a b", b=64),
                        in1=sm[s][:, 8:12].unsqueeze(2).broadcast_to([128, 4, 64]), op=ALU.mult),
                        reads=["Sf", smk], writes=["tS"])
                    kb.op("dve", lambda: nc.vector.tensor_tensor(out=Sf[:], in0=PS[5][:, 0:256], in1=tS[:], op=ALU.add),
                          reads=["ps5", "tS"], writes=["Sf"])
                    kb.op("act", lambda: nc.scalar.copy(out=Sb_[:], in_=Sf[:]), reads=["Sf"], writes=["Sb"])

            kb.barrier()
            blks = [b for b in BLOCKS if (need_ctx or not b[2])]
            for bi, (t0, T, isctx) in enumerate(blks):
                sg = bi % 2
                nsub = T // 128
                for sub in range(nsub):
                    c = t0 // 128 + sub
                    s = sub % 2
                    kb.op("pool", lambda c=c, s=s: nc.gpsimd.tensor_tensor(out=yz[s][:], in0=Ytm[:, c, :], in1=ZSs[:, c, :], op=ALU.mult),
                          reads=[("Ytm", c), "ZSs"], writes=[("yz", s)])
                    kb.op("act", lambda s=s: nc.scalar.activation(out=yjunk[:], in_=yz[s][:], func=AF.Square, accum_out=yss[s][:]),
                          reads=[("yz", s)], writes=["yjunk", ("yss", s)])
                    kb.op("act", lambda s=s: nc.scalar.activation(out=yss[s][:], in_=yss[s][:], func=AF.Sqrt, bias=eps_t[:], scale=1.0 / 256),
                          reads=[("yss", s), "eps_t"], writes=[("yss", s)])
                    kb.op("dve", lambda s=s: nc.vector.reciprocal(out=yss[s][:], in_=yss[s][:]), reads=[("yss", s)], writes=[("yss", s)])
                    kb.op("dve", lambda s=s: nc.vector.scalar_tensor_tensor(
                        out=yo[s][:], in0=yz[s][:], scalar=yss[s][:, 0:1], in1=ngB[:], op0=ALU.mult, op1=ALU.mult),
                        reads=[("yz", s), ("yss", s), "ngB"], writes=[("yo", s)])
                    b = 6 + s
                    pk = "ps%d" % b
                    psb = PS[b][:].bitcast(BF16)
                    for j in range(2):
                        kb.op("pe", lambda j=j, s=s, psb=psb: nc.tensor.transpose(
                            out=psb[:, j * 128:(j + 1) * 128], in_=yo[s][:, j * 128:(j + 1) * 128], identity=ident_bf[:]),
                            reads=[("yo", s), "ident_bf"], writes=[pk])
                    kb.op("act", lambda sub=sub, psb=psb, sg=sg: nc.scalar.copy(
                        out=stg[sg][:, :, sub * 128:(sub + 1) * 128], in_=psb[:, 0:256].rearrange("p (a b) -> p a b", b=128)),
                        reads=[pk], writes=[("sstg", sg)])
                kb.dma(sp, MIX.rearrange("(c p) t -> p c t", p=128)[:, 6:8, t0:t0 + T], stg[sg][:, :, :T],
                       reads=[("sstg", sg)], semkey=("oMIXs", sg))
            kb.barrier()

        with contextlib.ExitStack() as st:
            Wo = sb(st, "Wo", [128, 8, D], BF16)
            mix = [sb(st, "mix%d" % i, [128, 8, 512], BF16) for i in range(2)]
            xts = [sb(st, "xt%d" % i, [128, 8, 512], F32) for i in range(2)]
            hTs = [sb(st, "hTs%d" % i, [128, 8, 512], BF16) for i in range(2)]
            sq = [sb(st, "sq%d" % i, [128, 512], BF16) for i in range(2)]
            tmp = [sb(st, "tmp%d" % i, [128, 512], F32) for i in range(2)]
            rstd = sb(st, "rstd", [128, 512], F32)
            if is_moe:
                hf = sb(st, "hf", [128, 8, 512], F32)
                wr_ = sb(st, "wr", [128, 8, NEXP], F32)
                lg = [sb(st, "lg%d" % i, [128, 8], F32) for i in range(2)]
                mx = [sb(st, "mx%d" % i, [128, 8], F32) for i in range(2)]
                ee = [sb(st, "ee%d" % i, [128, 8], F32) for i in range(2)]
                mk = [sb(st, "mk%d" % i, [128, 8], F32) for i in range(2)]
                r2 = [sb(st, "r2%d" % i, [128, 1], F32) for i in range(2)]
                cmb = [sb(st, "cmb%d" % i, [128, 8], F32) for i in range(2)]
                cstg = [sb(st, "cstg%d" % i, [8, 512], F32) for i in range(2)]
                kb.dma(sp, wr_[:], moe_r[jl].rearrange("(k p) e -> p k e", p=128), writes=["wr"], semkey="wr")
            kb.dma(pool, Wo[:], w_out[l].rearrange("(k p) n -> p k n", p=128), writes=["Wo"], semkey="Wo")
            blks = [b for b in BLOCKS if (need_ctx or not b[2])]
            brot = [0]
            def load3(bi):
                t0, T, isctx = blks[bi]
                s = bi % 2
                kb.dma(sp, mix[s][:, :, :T], MIX.rearrange("(c p) t -> p c t", p=128)[:, :, t0:t0 + T],
                       writes=[("mix", s)], semkey=("mix", s))
                kb.dma(sp, xts[s][:, :, :T], xsrc.rearrange("(k p) t -> p k t", p=128)[:, :, t0:t0 + T],
                       writes=[("xt", s)], semkey=("xt3", s))

            load3(0)
            for bi, (t0, T, isctx) in enumerate(blks):
                s = bi % 2
                which = 1 if isctx else 0
                xt = xts[s]
                if bi + 1 < len(blks):
                    load3(bi + 1)
                for co in range(8):
                    b = brot[0] % 4
                    brot[0] += 1
                    pk = "ps%d" % b
                    for k in range(8):
                        kb.op("pe", lambda k=k, co=co, b=b: nc.tensor.matmul(
                            PS[b][:, :T], lhsT=Wo[:, k, co * 128:(co + 1) * 128], rhs=mix[s][:, k, :T],
                            start=(k == 0), stop=(k == 7)), reads=["Wo", ("mix", s)], writes=[pk])
                    kb.op("dve", lambda co=co, b=b: nc.vector.scalar_tensor_tensor(
                        out=xt[:, co, :T], in0=PS[b][:, :T], scalar=mods[:, l, 16 + co, which:which + 1], in1=xt[:, co, :T],
                        op0=ALU.mult, op1=ALU.add), reads=[pk, "mods", ("xt", s)], writes=[("xt", s)])
                kb.dma(sp, R.rearrange("(k p) t -> p k t", p=128)[:, :, t0:t0 + T], xt[:, :, :T],
                       reads=[("xt", s)], semkey=("oR3", s))
                rmsnorm_block((sq, rstd, tmp), xt, T, G2, l, 24, which, hTs[s], ("hTs", s), hf=(hf if is_moe else None),
                              xk=("xt", s))
                kb.dma(sp, HT.rearrange("(k p) t -> p k t", p=128)[:, :, t0:t0 + T], hTs[s][:, :, :T],
                       reads=[("hTs", s)], semkey=("oHT", s))
                if is_moe:
                    nsub = T // 128
                    for sub in range(nsub):
                        s2 = sub % 2
                        for k in range(8):
                            kb.op("pe", lambda k=k, sub=sub: nc.tensor.matmul(
                                PS[4][:, 0:8], lhsT=hf[:, k, sub * 128:(sub + 1) * 128], rhs=wr_[:, k, :],
                                start=(k == 0), stop=(k == 7)), reads=["hf", "wr"], writes=["ps4"])
                        kb.op("act", lambda s2=s2: nc.scalar.copy(out=lg[s2][:], in_=PS[4][:, 0:8]), reads=["ps4"], writes=[("lg", s2)])
                        kb.op("dve", lambda s2=s2: nc.vector.max(out=mx[s2][:], in_=lg[s2][:]), reads=[("lg", s2)], writes=[("mx", s2)])
                        kb.op("dve", lambda s2=s2: nc.vector.tensor_scalar(out=ee[s2][:], in0=lg[s2][:], scalar1=mx[s2][:, 0:1], scalar2=None,
                                                                          op0=ALU.subtract), reads=[("lg", s2), ("mx", s2)], writes=[("ee", s2)])
                        kb.op("act", lambda s2=s2: nc.scalar.activation(out=ee[s2][:], in_=ee[s2][:], func=AF.Exp), reads=[("ee", s2)], writes=[("ee", s2)])
                        kb.op("dve", lambda s2=s2: nc.vector.tensor_tensor(out=r2[s2][:], in0=mx[s2][:, 1:2], in1=mx[s2][:, 0:1], op=ALU.subtract),
                              reads=[("mx", s2)], writes=[("r2", s2)])
                        kb.op("act", lambda s2=s2: nc.scalar.activation(out=r2[s2][:], in_=r2[s2][:], func=AF.Exp), reads=[("r2", s2)], writes=[("r2", s2)])
                        kb.op("dve", lambda s2=s2: nc.vector.tensor_scalar(out=r2[s2][:], in0=r2[s2][:], scalar1=1.0, scalar2=None, op0=ALU.add),
                              reads=[("r2", s2)], writes=[("r2", s2)])
                        kb.op("dve", lambda s2=s2: nc.vector.reciprocal(out=r2[s2][:], in_=r2[s2][:]), reads=[("r2", s2)], writes=[("r2", s2)])
                        kb.op("dve", lambda s2=s2: nc.vector.tensor_scalar(out=mk[s2][:], in0=lg[s2][:], scalar1=mx[s2][:, 1:2], scalar2=None,
                                                                          op0=ALU.is_ge), reads=[("lg", s2), ("mx", s2)], writes=[("mk", s2)])
                        kb.op("dve", lambda s2=s2: nc.vector.scalar_tensor_tensor(
                            out=cmb[s2][:], in0=ee[s2][:], scalar=r2[s2][:, 0:1], in1=mk[s2][:], op0=ALU.mult, op1=ALU.mult),
                            reads=[("ee", s2), ("r2", s2), ("mk", s2)], writes=[("cmb", s2)])
                        kb.op("pe", lambda s2=s2, sub=sub: nc.tensor.transpose(
                            out=PS[5][0:8, sub * 128:(sub + 1) * 128], in_=cmb[s2][:], identity=ident_f[:]),
                            reads=[("cmb", s2), "ident_f"], writes=["ps5"])
                    kb.op("act", lambda s=s, T=T: nc.scalar.copy(out=cstg[s][:, :T], in_=PS[5][0:8, :T]), reads=["ps5"], writes=[("cstg", s)])
                    kb.dma(sp, COMBT[:, t0:t0 + T], cstg[s][:, :T], reads=[("cstg", s)], semkey=("oCB", s))
            kb.barrier()

        with contextlib.ExitStack() as st:
            Wg = [sb(st, "Wg%d" % i, [128, 8, 768], BF16) for i in range(2)]
            Wu = [sb(st, "Wu%d" % i, [128, 8, 768], BF16) for i in range(2)]
            Wd = [sb(st, "Wd%d" % i, [128, 6, D], BF16) for i in range(2)]
            xts = [sb(st, "xt%d" % i, [128, 8, 512], F32) for i in range(2)]
            hTs = [sb(st, "hTs%d" % i, [128, 8, 512], BF16) for i in range(2)]
            aT = [sb(st, "aT%d" % i, [128, 6, 512], BF16) for i in range(2)]
            sgs = [sb(st, "sg%d" % i, [128, 512], F32) for i in range(2)]
            cb = [sb(st, "cb%d" % i, [128, 512], F32) for i in range(2)]
            t4 = [sb(st, "t4%d" % i, [128, 512], F32) for i in range(2)]
            blks = [b for b in BLOCKS if (need_ctx or not b[2])]
            passes = [(e, g) for e in range(NEXP if is_moe else 1) for g in range(4)]
            final_layer = (l == nlayers - 1)

            def load_w(pi):
                e, g = passes[pi]
                f0, nf = FGROUPS[g]
                s = pi % 2
                if is_moe:
                    gsrc, usrc, dsrc = moe_g[jl, e], moe_u[jl, e], moe_d[jl, e]
                else:
                    gsrc, usrc, dsrc = ffn_g[jl], ffn_u[jl], ffn_d[jl]
                kb.dma(pool, Wg[s][:, :, 0:nf * 128], gsrc.rearrange("(k p) f -> p k f", p=128)[:, :, f0 * 128:(f0 + nf) * 128],
                       writes=[("Wg", s)], semkey=("Wg", s))
                kb.dma(pool, Wu[s][:, :, 0:nf * 128], usrc.rearrange("(k p) f -> p k f", p=128)[:, :, f0 * 128:(f0 + nf) * 128],
                       writes=[("Wu", s)], semkey=("Wu", s))
                kb.dma(pool, Wd[s][:, 0:nf, :], dsrc[f0 * 128:(f0 + nf) * 128, :].rearrange("(c p) d -> p c d", p=128),
                       writes=[("Wd", s)], semkey=("Wd", s))

            iters = [(pi, bi) for pi in range(len(passes)) for bi in range(len(blks))]

            def load_act(n):
                pi, bi = iters[n]
                e = passes[pi][0]
                t0, T, isctx = blks[bi]
                s = n % 2
                kb.dma(sp, hTs[s][:, :, :T], HT.rearrange("(k p) t -> p k t", p=128)[:, :, t0:t0 + T],
                       writes=[("hT", s)], semkey=("hT4", s))
                kb.dma(sp, xts[s][:, :, :T], R.rearrange("(k p) t -> p k t", p=128)[:, :, t0:t0 + T],
                       reads=[("R", bi)], writes=[("xt", s)], semkey=("xt4", s))
                if is_moe:
                    kb.dma(sp, cb[s][:, :T], COMBT[e, t0:t0 + T].partition_broadcast(128), writes=[("cb", s)], semkey=("cb", s))

            load_w(0)
            load_act(0)
            it = 0
            gub = [0]
            dbk = [0]
            for pi, (e, g) in enumerate(passes):
                if pi + 1 < len(passes):
                    load_w(pi + 1)
                f0, nf = FGROUPS[g]
                ws = pi % 2
                last_pass = (pi == len(passes) - 1)
                for bi, (t0, T, isctx) in enumerate(blks):
                    s = it % 2
                    if it + 1 < len(iters):
                        load_act(it + 1)
                    it += 1
                    which = 1 if isctx else 0
                    xt, hT = xts[s], hTs[s]
                    for fc in range(nf):
                        bg = (gub[0] % 2) * 2
                        gub[0] += 1
                        bu = bg + 1
                        gk, uk = "ps%d" % bg, "ps%d" % bu
                        for k in range(8):
                            kb.op("pe", lambda k=k, fc=fc, bg=bg: nc.tensor.matmul(
                                PS[bg][:, :T], lhsT=Wg[ws][:, k, fc * 128:(fc + 1) * 128], rhs=hT[:, k, :T],
                                start=(k == 0), stop=(k == 7)), reads=[("Wg", ws), ("hT", s)], writes=[gk])
                        for k in range(8):
                            kb.op("pe", lambda k=k, fc=fc, bu=bu: nc.tensor.matmul(
                                PS[bu][:, :T], lhsT=Wu[ws][:, k, fc * 128:(fc + 1) * 128], rhs=hT[:, k, :T],
                                start=(k == 0), stop=(k == 7)), reads=[("Wu", ws), ("hT", s)], writes=[uk])
                        sgi = fc % 2
                        kb.op("act", lambda bg=bg, sgi=sgi: nc.scalar.activation(out=sgs[sgi][:, :T], in_=PS[bg][:, :T], func=AF.Silu),
                              reads=[gk], writes=[("sg", sgi)])
                        kb.op("dve", lambda bu=bu, sgi=sgi, fc=fc: nc.vector.tensor_tensor(
                            out=aT[s][:, fc, :T], in0=sgs[sgi][:, :T], in1=PS[bu][:, :T], op=ALU.mult),
                            reads=[uk, ("sg", sgi)], writes=[("aT", s)])
                    for co in range(8):
                        bd = 4 + (dbk[0] % 3)
                        dbk[0] += 1
                        dk = "ps%d" % bd
                        for fc in range(nf):
                            kb.op("pe", lambda fc=fc, co=co, bd=bd: nc.tensor.matmul(
                                PS[bd][:, :T], lhsT=Wd[ws][:, fc, co * 128:(co + 1) * 128], rhs=aT[s][:, fc, :T],
                                start=(fc == 0), stop=(fc == nf - 1)), reads=[("Wd", ws), ("aT", s)], writes=[dk])
                        if is_moe:
                            ti = co % 2
                            kb.op("dve", lambda co=co, bd=bd, ti=ti: nc.vector.scalar_tensor_tensor(
                                out=t4[ti][:, :T], in0=PS[bd][:, :T], scalar=mods[:, l, 40 + co, which:which + 1], in1=cb[s][:, :T],
                                op0=ALU.mult, op1=ALU.mult), reads=[dk, "mods", ("cb", s)], writes=[("t4", ti)])
                            kb.op("pool", lambda co=co, ti=ti: nc.gpsimd.tensor_tensor(
                                out=xt[:, co, :T], in0=xt[:, co, :T], in1=t4[ti][:, :T], op=ALU.add),
                                reads=[("t4", ti), ("xt", s)], writes=[("xt", s)])
                        else:
                            kb.op("dve", lambda co=co, bd=bd: nc.vector.scalar_tensor_tensor(
                                out=xt[:, co, :T], in0=PS[bd][:, :T], scalar=mods[:, l, 40 + co, which:which + 1], in1=xt[:, co, :T],
                                op0=ALU.mult, op1=ALU.add), reads=[dk, "mods", ("xt", s)], writes=[("xt", s)])
                    if last_pass and final_layer:
                        if not isctx:
                            kb.dma(sp, outT.rearrange("(k p) t -> p k t", p=128)[:, :, t0 - CL:t0 - CL + T], xt[:, :, :T],
                                   reads=[("xt", s)], semkey=("oR4", s))
                    else:
                        kb.dma(sp, R.rearrange("(k p) t -> p k t", p=128)[:, :, t0:t0 + T], xt[:, :, :T],
                               reads=[("xt", s)], writes=[("R", bi)], semkey=("oR4", s))
            kb.barrier()

    return nc


def _consts():
    c = {}
    c["c_ident"] = np.eye(128, dtype=np.float32)
    bo = np.zeros((128, 128), np.float32)
    bo[:64, :64] = 1
    bo[64:, 64:] = 1
    c["c_bones"] = bo
    P = np.zeros((128, 128), np.float32)
    for m in range(128):
        partner = m + 16 if (m % 32) < 16 else m - 16
        P[partner, m] = 1
    c["c_perm"] = P
    t = np.arange(128)
    uf = (t[:, None] <= t[None, :]).astype(np.float32)
    ub = (t[:, None] >= t[None, :]).astype(np.float32)
    c["c_uf"], c["c_ub"] = uf, ub
    c["c_negf"] = np.tile((uf - 1.0) * 30000.0, (1, 4)).astype(np.float32)
    c["c_negb"] = np.tile((ub - 1.0) * 30000.0, (1, 4)).astype(np.float32)
    pos = np.arange(L)
    row, colp = pos // 64, pos % 64
    freqs = (10000.0 ** (-np.arange(16, dtype=np.float32) / 16)).astype(np.float32)
    C = np.zeros((128, L), np.float32)
    S = np.zeros((128, L), np.float32)
    for p in range(128):
        dd = p % 64
        pp = row if dd < 32 else colp
        ang = pp.astype(np.float32) * freqs[dd % 16]
        C[p] = np.cos(ang)
        S[p] = np.sin(ang) * (-1.0 if (dd % 32) < 16 else 1.0)
    c["c_ropeC"], c["c_ropeS"] = C, S
    return c


def _prep_shared(inp):
    f = np.float32
    a = lambda v: np.ascontiguousarray(np.asarray(v, dtype=f))
    sh = {}
    sh["w_mod"] = a(inp["w_mod"])
    sh["bmodT"] = a(np.asarray(inp["b_mod"]).reshape(DEPTH, 48, 128).transpose(2, 0, 1))
    sh["g1T"] = a(np.asarray(inp["norm1_g"]).reshape(DEPTH, 8, 128).transpose(2, 0, 1))
    sh["g2T"] = a(np.asarray(inp["norm2_g"]).reshape(DEPTH, 8, 128).transpose(2, 0, 1))
    sh["w_in"] = a(inp["w_in"])
    sh["w_out"] = a(inp["w_out"])
    sh["gvg"] = a(inp["gm_v_g"])
    sh["wsT"] = a(np.asarray(inp["gm_ws"]).transpose(0, 1, 3, 2))
    p = np.arange(128)
    bsv = np.asarray(inp["gm_bs"])
    gb = np.zeros((128, DEPTH, 2, 128), f)
    for j in range(2):
        gb[:, :, j, :] = bsv[:, 2 * j + (p // 64), :].transpose(1, 0, 2)
    sh["gbs"] = gb
    sh["qgT"] = a(np.asarray(inp["att_q_g"])[:, p % 64].T)
    sh["kgT"] = a(np.asarray(inp["att_k_g"])[:, p % 64].T)
    sk = np.asarray(inp["att_sink"])
    sl = np.zeros((128, DEPTH, 2, 2), f)
    for kv in range(2):
        for ti in range(2):
            sl[:, :, kv, ti] = sk[:, 4 * kv + 2 * ti + (p // 64)].T
    sh["sinkL"] = sl
    sh["convw"] = a(np.asarray(inp["ssm_conv_w"]).reshape(DEPTH, 5, 6, 128).transpose(3, 0, 2, 1))
    sh["convb"] = a(np.asarray(inp["ssm_conv_b"]).reshape(DEPTH, 6, 128).transpose(2, 0, 1))
    sh["dtb"] = a(np.asarray(inp["ssm_dt_bias"]).reshape(DEPTH, 8))
    sh["alog"] = a(np.asarray(inp["ssm_a_log"]).reshape(DEPTH, 8))
    sh["dskE"] = a(np.repeat(np.asarray(inp["ssm_d"]), 64, axis=1))
    sh["sng"] = a(inp["ssm_norm_g"])
    sh["ffn_g"] = a(inp["ffn_w_gate"])
    sh["ffn_u"] = a(inp["ffn_w_up"])
    sh["ffn_d"] = a(inp["ffn_w_down"])
    sh["moe_r"] = a(inp["moe_router"])
    sh["moe_g"] = a(inp["moe_w_gate"])
    sh["moe_u"] = a(inp["moe_w_up"])
    sh["moe_d"] = a(inp["moe_w_down"])
    sh.update(_consts())
    return sh


def _prep_core(inp, b):
    f = np.float32
    x = np.asarray(inp["x"][b], dtype=f)
    ctx = np.asarray(inp["ctx"][b], dtype=f)
    xT0 = np.ascontiguousarray(np.concatenate([ctx.T, x.T], axis=1))
    c = np.asarray(inp["c"][b], dtype=f).reshape(8, 128).T
    cc = np.asarray(inp["c_ctx"], dtype=f).reshape(8, 128).T
    cs = np.ascontiguousarray(np.stack([c, cc], axis=-1))
    return {"xT0": xT0, "cs": cs}


_NC_CACHE = {}


def kernel(**inputs):
    if "nc" not in _NC_CACHE:
        _NC_CACHE["nc"] = build_program()
    nc = _NC_CACHE["nc"]
    sh = _prep_shared(inputs)
    in_maps = []
    for b in range(8):
        m = dict(sh)
        m.update(_prep_core(inputs, b))
        in_maps.append(m)
    res = run_bass_kernel_spmd(nc, in_maps, core_ids=list(range(8)))
    out = np.stack([np.ascontiguousarray(r["outT"].T) for r in res.results], axis=0)
    return out.astype(np.float32)
```

```python
import contextlib
import numpy as np
import concourse.bass as bass
import concourse.mybir as mybir
from concourse.bass_utils import run_bass_kernel_spmd

F32, BF16 = mybir.dt.float32, mybir.dt.bfloat16
AF = mybir.ActivationFunctionType
ALU = mybir.AluOpType
AX = mybir.AxisListType

D = 1024
L = 4096
CL = 256
NT = L + CL
NTILE = NT // 128
DEPTH = 4
DFF = 2816
NEXP = 8
EPS = 1e-6
FGROUPS = [(0, 6), (6, 6), (12, 5), (17, 5)]
BLOCKS = [(0, 256, True)] + [(256 + 512 * i, 512, False) for i in range(8)]


SIM_FRESH_POOL = False


class KB:
    def __init__(self, nc, stack):
        self.nc = nc
        self.stack = stack
        self.eng = {"pe": nc.tensor, "dve": nc.vector, "act": nc.scalar, "pool": nc.gpsimd, "sp": nc.sync}
        self.esem = {e: stack.enter_context(nc.semaphore("es_" + e)) for e in self.eng}
        self.ecnt = {e: 0 for e in self.eng}
        self.seen = {e: {} for e in self.eng}
        self.lw = {}
        self.rd = {}
        self.dsem = {}
        self.free = []
        self.nsem = 0
        self.dead = False

    def _wait(self, E, tok):
        sem, val, _ = tok
        sid = id(sem)
        if self.seen[E].get(sid, 0) >= val:
            return
        self.eng[E].wait_ge(sem, val)
        self.seen[E][sid] = val

    def _deps(self, E, reads, writes):
        for k in reads:
            w = self.lw.get(k)
            if w is not None:
                if w[2] == E and E == "pe":
                    continue
                self._wait(E, w)
        for k in writes:
            w = self.lw.get(k)
            if w is not None and w[2] != E:
                self._wait(E, w)
            for r in self.rd.get(k, {}).values():
                if r[2] != E:
                    self._wait(E, r)

    def _record(self, tok, reads, writes):
        for k in writes:
            self.lw[k] = tok
            self.rd[k] = {}
        for k in reads:
            self.rd.setdefault(k, {})[id(tok[0])] = tok

    def op(self, E, fn, reads=(), writes=()):
        if self.dead:
            return None
        self._deps(E, reads, writes)
        inst = fn()
        self.ecnt[E] += 1
        inst.then_inc(self.esem[E], 1)
        tok = (self.esem[E], self.ecnt[E], E)
        self._record(tok, reads, writes)
        return tok

    def dma(self, E, out, in_, reads=(), writes=(), semkey=None):
        if self.dead:
            return None
        if SIM_FRESH_POOL and E == "pool":
            self.nsem += 1
            semkey = ("__fresh", self.nsem)
            self.dsem[semkey] = [self.stack.enter_context(self.nc.semaphore("dp%d" % self.nsem)), 0]
        if semkey not in self.dsem:
            if self.free:
                self.dsem[semkey] = self.free.pop()
            else:
                self.nsem += 1
                self.dsem[semkey] = [self.stack.enter_context(self.nc.semaphore("ds%d" % self.nsem)), 0]
        ent = self.dsem[semkey]
        self._deps(E, reads, writes)
        if ent[1] > 0:
            self._wait(E, (ent[0], ent[1], "dma"))
        ent[1] += 16
        self.eng[E].dma_start(out=out, in_=in_).then_inc(ent[0], 16)
        tok = (ent[0], ent[1], "dma:" + str(semkey))
        self._record(tok, reads, writes)
        return tok

    def barrier(self):
        if self.dead:
            return
        for E in self.eng:
            for Fn in self.eng:
                if Fn != E and self.ecnt[Fn] > 0:
                    self._wait(E, (self.esem[Fn], self.ecnt[Fn], Fn))
            for ent in self.dsem.values():
                if ent[1] > 0:
                    self._wait(E, (ent[0], ent[1], "dma"))
        self.lw = {}
        self.rd = {}
        self.free.extend(v for k, v in self.dsem.items() if not (isinstance(k, tuple) and k and k[0] == "__fresh"))
        self.dsem = {}


class _Stop(Exception):
    pass


def build_program(nlayers=DEPTH, debug=False, stop=None):
    nc = bass.Bass("TRN2", target_bir_lowering=False)
    stack = contextlib.ExitStack()
    with stack:
        try:
            _emit(nc, stack, nlayers, debug, stop)
        except _Stop:
            pass
    return nc


def _emit(nc, stack, nlayers, debug, stop=None):
    kb = KB(nc, stack)
    _bar = kb.barrier
    _phase = [0]

    def barrier_named():
        _bar()
        _phase[0] += 1
        if stop is not None and _phase[0] >= stop:
            kb.dead = True

    kb.barrier = barrier_named

    def din(name, shape, dt=F32):
        return nc.dram_tensor(name, list(shape), dt, kind="ExternalInput").ap()

    def dscr(name, shape, dt):
        kind = "ExternalOutput" if debug else None
        if kind:
            return nc.dram_tensor(name, list(shape), dt, kind=kind).ap()
        return nc.dram_tensor(name, list(shape), dt).ap()

    xT0 = din("xT0", [D, NT])
    cs_in = din("cs", [128, 8, 2])
    w_mod = din("w_mod", [DEPTH, D, 6 * D])
    bmodT = din("bmodT", [128, DEPTH, 48])
    g1T = din("g1T", [128, DEPTH, 8])
    g2T = din("g2T", [128, DEPTH, 8])
    w_in = din("w_in", [DEPTH, D, 2312])
    w_out = din("w_out", [DEPTH, D, D])
    gvg = din("gvg", [DEPTH, 256])
    wsT = din("wsT", [DEPTH, 4, 128, 128])
    gbs = din("gbs", [128, DEPTH, 2, 128])
    qgT = din("qgT", [128, DEPTH])
    kgT = din("kgT", [128, DEPTH])
    sinkL = din("sinkL", [128, DEPTH, 2, 2])
    convw = din("convw", [128, DEPTH, 6, 5])
    convb = din("convb", [128, DEPTH, 6])
    dtb = din("dtb", [DEPTH, 8])
    alog = din("alog", [DEPTH, 8])
    dskE = din("dskE", [DEPTH, 256])
    sng = din("sng", [DEPTH, 256])
    ffn_g = din("ffn_g", [2, D, DFF])
    ffn_u = din("ffn_u", [2, D, DFF])
    ffn_d = din("ffn_d", [2, DFF, D])
    moe_r = din("moe_r", [2, D, NEXP])
    _small = nlayers < 2
    moe_g = din("moe_g", [2, NEXP, D, DFF] if not _small else [2, NEXP, 128, 768])
    moe_u = din("moe_u", [2, NEXP, D, DFF] if not _small else [2, NEXP, 128, 768])
    moe_d = din("moe_d", [2, NEXP, DFF, D] if not _small else [2, NEXP, 128, 768])
    c_ident = din("c_ident", [128, 128])
    c_bones = din("c_bones", [128, 128])
    c_perm = din("c_perm", [128, 128])
    c_uf = din("c_uf", [128, 128])
    c_ub = din("c_ub", [128, 128])
    c_negf = din("c_negf", [128, 512])
    c_negb = din("c_negb", [128, 512])
    c_ropeC = din("c_ropeC", [128, L])
    c_ropeS = din("c_ropeS", [128, L])

    outT = nc.dram_tensor("outT", [D, L], F32, kind="ExternalOutput").ap()

    R = dscr("R", [D, NT], F32)
    GU = dscr("GU", [256, NT], BF16)
    GV = dscr("GV", [NT, 256], BF16)
    QT = dscr("QT", [512, NT], BF16)
    KT = dscr("KT", [256, NT], BF16)
    V = dscr("V", [NT, 128], BF16)
    ZS = dscr("ZS", [NT, 256], BF16)
    XBC = dscr("XBC", [768, NT], BF16)
    DT = dscr("DT", [NT, 16], F32)
    MIX = dscr("MIX", [D, NT], BF16)
    HT = dscr("HT", [D, NT], BF16)
    COMBT = dscr("COMBT", [NEXP, NT], F32)

    _uniq = [0]

    def sb(st, name, shape, dt):
        _uniq[0] += 1
        return st.enter_context(nc.sbuf_tensor("%s_u%d" % (name, _uniq[0]), list(shape), dt))

    PS = [stack.enter_context(nc.psum_tensor("ps%d" % i, [128, 512], F32)) for i in range(8)]

    ident_bf = sb(stack, "ident_bf", [128, 128], BF16)
    ident_f = sb(stack, "ident_f", [128, 128], F32)
    ones_bf = sb(stack, "ones_bf", [128, 128], BF16)
    bones_bf = sb(stack, "bones_bf", [128, 128], BF16)
    perm_bf = sb(stack, "perm_bf", [128, 128], BF16)
    uf_bf = sb(stack, "uf_bf", [128, 128], BF16)
    ub_bf = sb(stack, "ub_bf", [128, 128], BF16)
    ones_f = sb(stack, "ones_f", [128, 64], F32)
    eps_t = sb(stack, "eps_t", [128, 1], F32)
    one_t = sb(stack, "one_t", [128, 1], F32)
    mods = sb(stack, "mods", [128, DEPTH, 48, 2], F32)
    G1 = sb(stack, "G1", [128, DEPTH, 8, 2], F32)
    G2 = sb(stack, "G2", [128, DEPTH, 8, 2], F32)
    g1s = sb(stack, "g1s", [128, DEPTH, 8], F32)
    g2s = sb(stack, "g2s", [128, DEPTH, 8], F32)
    bms = sb(stack, "bms", [128, DEPTH, 48], F32)
    css = sb(stack, "css", [128, 8, 2], F32)
    qgs = sb(stack, "qgs", [128, DEPTH], F32)
    kgs = sb(stack, "kgs", [128, DEPTH], F32)
    sinkE = sb(stack, "sinkE", [128, DEPTH, 2, 2], F32)
    cws = sb(stack, "cws", [128, DEPTH, 6, 5], F32)
    cbs = sb(stack, "cbs", [128, DEPTH, 6], F32)

    sp, pool = "sp", "pool"
    kb.dma(pool, ident_bf[:], c_ident, writes=["ident_bf"], semkey="c0")
    kb.dma(sp, ident_f[:], c_ident, writes=["ident_f"], semkey="c1")
    kb.dma(pool, bones_bf[:], c_bones, writes=["bones_bf"], semkey="c2")
    kb.dma(pool, perm_bf[:], c_perm, writes=["perm_bf"], semkey="c3")
    kb.dma(pool, uf_bf[:], c_uf, writes=["uf_bf"], semkey="c4")
    kb.dma(pool, ub_bf[:], c_ub, writes=["ub_bf"], semkey="c5")
    kb.dma(sp, g1s[:], g1T, writes=["g1s"], semkey="c6")
    kb.dma(sp, g2s[:], g2T, writes=["g2s"], semkey="c7")
    kb.dma(sp, bms[:], bmodT, writes=["bms"], semkey="c8")
    kb.dma(sp, css[:], cs_in, writes=["css"], semkey="c9")
    kb.dma(sp, qgs[:], qgT, writes=["qgs"], semkey="c10")
    kb.dma(sp, kgs[:], kgT, writes=["kgs"], semkey="c11")
    kb.dma(sp, sinkE[:], sinkL, writes=["sinkE"], semkey="c12")
    kb.dma(sp, cws[:], convw, writes=["cws"], semkey="c13")
    kb.dma(sp, cbs[:], convb, writes=["cbs"], semkey="c14")
    kb.op("dve", lambda: nc.vector.memset(ones_bf[:], 1.0), writes=["ones_bf"])
    kb.op("dve", lambda: nc.vector.memset(ones_f[:], 1.0), writes=["ones_f"])
    kb.op("dve", lambda: nc.vector.memset(eps_t[:], EPS), writes=["eps_t"])
    kb.op("dve", lambda: nc.vector.memset(one_t[:], 1.0), writes=["one_t"])
    kb.op("act", lambda: nc.scalar.activation(out=css[:], in_=css[:], func=AF.Silu), reads=["css"], writes=["css"])
    kb.op("act", lambda: nc.scalar.activation(out=sinkE[:], in_=sinkE[:], func=AF.Exp), reads=["sinkE"], writes=["sinkE"])

    with contextlib.ExitStack() as st:
        wm = [sb(st, "wm%d" % i, [128, 8, 512], F32) for i in range(2)]
        it = 0
        for l in range(nlayers):
            for gidx in range(12):
                s = it % 2
                it += 1
                kb.dma(sp, wm[s][:], w_mod[l].rearrange("(k p) n -> p k n", p=128)[:, :, gidx * 512:(gidx + 1) * 512],
                       writes=[("wm", s)], semkey=("wm", s))
                for jj in range(4):
                    j = gidx * 4 + jj
                    for k in range(8):
                        kb.op("pe", lambda k=k, jj=jj, j=j, s=s: nc.tensor.matmul(
                            PS[0][:, j * 2:j * 2 + 2], lhsT=wm[s][:, k, jj * 128:(jj + 1) * 128], rhs=css[:, k, :],
                            start=(k == 0), stop=(k == 7)), reads=[("wm", s), "css"], writes=["ps0"])
            kb.op("dve", lambda l=l: nc.vector.tensor_tensor(
                out=mods[:, l, :, :], in0=PS[0][:, 0:96].rearrange("p (a b) -> p a b", b=2),
                in1=bms[:, l, :].unsqueeze(2).broadcast_to([128, 48, 2]), op=ALU.add),
                reads=["ps0", "bms"], writes=["mods"])
            for (Gt, gs, off) in ((G1, g1s, 8), (G2, g2s, 32)):
                kb.op("dve", lambda Gt=Gt, off=off, l=l: nc.vector.tensor_scalar(
                    out=Gt[:, l, :, :], in0=mods[:, l, off:off + 8, :], scalar1=1.0, scalar2=None, op0=ALU.add),
                    reads=["mods"], writes=["G"])
                kb.op("dve", lambda Gt=Gt, gs=gs, l=l: nc.vector.tensor_tensor(
                    out=Gt[:, l, :, :], in0=Gt[:, l, :, :], in1=gs[:, l, :].unsqueeze(2).broadcast_to([128, 8, 2]),
                    op=ALU.mult), reads=["G", "g1s", "g2s"], writes=["G"])
        kb.barrier()

    def rmsnorm_block(st_tiles, xt, T, Gt, l, shift_off, which, out_tile, out_key, hf=None, xk="xt"):
        sq, rstd, tmp = st_tiles
        for k in range(8):
            s = k % 2
            kb.op("act", lambda k=k, s=s: nc.scalar.activation(out=sq[s][:, :T], in_=xt[:, k, :T], func=AF.Square),
                  reads=[xk], writes=[("sq", s)])
            kb.op("pe", lambda k=k, s=s: nc.tensor.matmul(PS[7][:, :T], lhsT=ones_bf[:], rhs=sq[s][:, :T],
                                                          start=(k == 0), stop=(k == 7)),
                  reads=[("sq", s), "ones_bf"], writes=["ps7"])
        kb.op("act", lambda: nc.scalar.activation(out=rstd[:, :T], in_=PS[7][:, :T], func=AF.Sqrt,
                                                  bias=eps_t[:], scale=1.0 / D), reads=["ps7", "eps_t"], writes=["rstd"])
        kb.op("dve", lambda: nc.vector.reciprocal(out=rstd[:, :T], in_=rstd[:, :T]), reads=["rstd"], writes=["rstd"])
        for k in range(8):
            s = k % 2
            kb.op("dve", lambda k=k, s=s: nc.vector.scalar_tensor_tensor(
                out=tmp[s][:, :T], in0=xt[:, k, :T], scalar=Gt[:, l, k, which:which + 1], in1=rstd[:, :T],
                op0=ALU.mult, op1=ALU.mult), reads=[xk, "G", "rstd"], writes=[("tmp", s)])
            if hf is None:
                kb.op("act", lambda k=k, s=s: nc.scalar.activation(
                    out=out_tile[:, k, :T], in_=tmp[s][:, :T], func=AF.Identity,
                    bias=mods[:, l, shift_off + k, which:which + 1], scale=1.0),
                    reads=[("tmp", s), "mods"], writes=[out_key])
            else:
                kb.op("act", lambda k=k, s=s: nc.scalar.activation(
                    out=hf[:, k, :T], in_=tmp[s][:, :T], func=AF.Identity,
                    bias=mods[:, l, shift_off + k, which:which + 1], scale=1.0),
                    reads=[("tmp", s), "mods"], writes=["hf"])
                kb.op("pool", lambda k=k: nc.gpsimd.tensor_copy(out=out_tile[:, k, :T], in_=hf[:, k, :T]),
                      reads=["hf"], writes=[out_key])

    for l in range(nlayers):
        need_ctx = l < DEPTH - 1
        is_moe = (l % 2 == 1)
        jl = l // 2
        xsrc = xT0 if l == 0 else R

        with contextlib.ExitStack() as st:
            Wfm = sb(st, "Wfm", [128, 8, 1792], BF16)
            Wtm = sb(st, "Wtm", [128, 8, 648], BF16)
            xts = [sb(st, "xt%d" % i, [128, 8, 512], F32) for i in range(2)]
            hTs = [sb(st, "hT%d" % i, [128, 8, 512], BF16) for i in range(2)]
            sq = [sb(st, "sq%d" % i, [128, 512], BF16) for i in range(2)]
            tmp = [sb(st, "tmp%d" % i, [128, 512], F32) for i in range(2)]
            rstd = sb(st, "rstd", [128, 512], F32)
            stgF = [sb(st, "stgF%d" % i, [128, 14, 512], BF16) for i in range(2)]
            stgGV = [sb(st, "stgGV%d" % i, [128, 4, 256], BF16) for i in range(2)]
            stgV = [sb(st, "stgV%d" % i, [128, 4, 128], BF16) for i in range(2)]
            stgZ = [sb(st, "stgZ%d" % i, [128, 4, 256], BF16) for i in range(2)]
            stgDT = [sb(st, "stgDT%d" % i, [128, 4, 16], F32) for i in range(2)]
            ropeC = sb(st, "ropeC", [128, L], F32)
            ropeS = sb(st, "ropeS", [128, L], F32)
            gvB = sb(st, "gvB", [128, 256], F32)
            dtbB = sb(st, "dtbB", [128, 8], F32)
            aB = sb(st, "aB", [128, 8], F32)
            qsq = [sb(st, "qsq%d" % i, [128, 512], BF16) for i in range(2)]
            qrn = [sb(st, "qrn%d" % i, [128, 512], F32) for i in range(2)]
            qn = [sb(st, "qn%d" % i, [128, 512], BF16) for i in range(2)]
            qt1 = [sb(st, "qt1%d" % i, [128, 512], F32) for i in range(2)]
            qt2 = [sb(st, "qt2%d" % i, [128, 512], F32) for i in range(2)]
            gl = [sb(st, "gl%d" % i, [128, 256], F32) for i in range(2)]
            gsq = [sb(st, "gsq%d" % i, [128, 256], F32) for i in range(2)]
            gss = [sb(st, "gss%d" % i, [128, 4], F32) for i in range(2)]
            dtx = [sb(st, "dtx%d" % i, [128, 8], F32) for i in range(2)]

            wv = w_in[l].rearrange("(k p) n -> p k n", p=128)
            kb.dma(pool, Wfm[:, :, 0:256], wv[:, :, 0:256], writes=["Wfm"], semkey="wf0")
            kb.dma(pool, Wfm[:, :, 256:768], wv[:, :, 512:1024], writes=["Wfm"], semkey="wf1")
            for kvh in range(2):
                for dup in range(2):
                    c0 = 768 + kvh * 128 + dup * 64
                    kb.dma(pool, Wfm[:, :, c0:c0 + 64], wv[:, :, 1024 + kvh * 64:1024 + (kvh + 1) * 64],
                           writes=["Wfm"], semkey="wf2_%d%d" % (kvh, dup))
            kb.dma(pool, Wfm[:, :, 1024:1792], wv[:, :, 1536:2304], writes=["Wfm"], semkey="wf3")
            kb.dma(pool, Wtm[:, :, 0:256], wv[:, :, 256:512], writes=["Wtm"], semkey="wt0")
            kb.dma(pool, Wtm[:, :, 256:384], wv[:, :, 1152:1280], writes=["Wtm"], semkey="wt1")
            kb.dma(pool, Wtm[:, :, 384:392], wv[:, :, 2304:2312], writes=["Wtm"], semkey="wt2")
            kb.dma(pool, Wtm[:, :, 392:648], wv[:, :, 1280:1536], writes=["Wtm"], semkey="wt3")
            kb.dma(sp, ropeC[:], c_ropeC, writes=["ropeC"], semkey="rc")
            kb.dma(sp, ropeS[:], c_ropeS, writes=["ropeS"], semkey="rs")
            kb.dma(sp, gvB[:], gvg[l].partition_broadcast(128), writes=["gvB"], semkey="gvB")
            kb.dma(sp, dtbB[:], dtb[l].partition_broadcast(128), writes=["dtbB"], semkey="dtbB")
            kb.dma(sp, aB[:], alog[l].partition_broadcast(128), writes=["aB"], semkey="aB")
            kb.op("act", lambda: nc.scalar.activation(out=aB[:], in_=aB[:], func=AF.Exp), reads=["aB"], writes=["aB"])
            kb.op("dve", lambda: nc.vector.tensor_scalar(out=aB[:], in0=aB[:], scalar1=-1.0, scalar2=None, op0=ALU.mult),
                  reads=["aB"], writes=["aB"])

            bank_rot = [0]

            def next_bank():
                b = bank_rot[0] % 5
                bank_rot[0] += 1
                return b

            def load1(bi):
                t0, T, isctx = BLOCKS[bi]
                s = bi % 2
                kb.dma(sp, xts[s][:, :, :T], xsrc.rearrange("(k p) t -> p k t", p=128)[:, :, t0:t0 + T],
                       writes=[("xt", s)], semkey=("xt", s))

            def norm1(bi):
                t0, T, isctx = BLOCKS[bi]
                s = bi % 2
                which = 1 if isctx else 0
                xt, hT = xts[s], hTs[s]
                for k in range(8):
                    s2 = k % 2
                    kb.op("act", lambda k=k, s2=s2: nc.scalar.activation(out=sq[s2][:, :T], in_=xt[:, k, :T], func=AF.Square),
                          reads=[("xt", s)], writes=[("sq", s2)])
                    kb.op("pe", lambda k=k, s2=s2: nc.tensor.matmul(PS[7][:, :T], lhsT=ones_bf[:], rhs=sq[s2][:, :T],
                                                                    start=(k == 0), stop=(k == 7)),
                          reads=[("sq", s2), "ones_bf"], writes=["ps7"])
                kb.op("act", lambda: nc.scalar.activation(out=rstd[:, :T], in_=PS[7][:, :T], func=AF.Sqrt,
                                                          bias=eps_t[:], scale=1.0 / D),
                      reads=["ps7", "eps_t"], writes=["rstd"])
                kb.op("dve", lambda: nc.vector.reciprocal(out=rstd[:, :T], in_=rstd[:, :T]), reads=["rstd"], writes=["rstd"])
                for k in range(8):
                    s2 = k % 2
                    kb.op("dve", lambda k=k, s2=s2: nc.vector.scalar_tensor_tensor(
                        out=tmp[s2][:, :T], in0=xt[:, k, :T], scalar=G1[:, l, k, which:which + 1], in1=rstd[:, :T],
                        op0=ALU.mult, op1=ALU.mult), reads=[("xt", s), "G", "rstd"], writes=[("tmp", s2)])
                    kb.op("act", lambda k=k, s2=s2: nc.scalar.activation(
                        out=hT[:, k, :T], in_=tmp[s2][:, :T], func=AF.Identity,
                        bias=mods[:, l, k, which:which + 1], scale=1.0),
                        reads=[("tmp", s2), "mods"], writes=[("hT", s)])

            load1(0)
            load1(1)
            norm1(0)
            for bi, (t0, T, isctx) in enumerate(BLOCKS):
                s = bi % 2
                which = 1 if isctx else 0
                xt, hT = xts[s], hTs[s]
                if bi + 2 < len(BLOCKS):
                    load1(bi + 2)
                sF = stgF[s]
                stageB, stageC = {}, {}

                def chunkA(ci, T=T, hT=hT, s=s, sF=sF, isctx=isctx, t0=t0):
                    b = next_bank()
                    pk = "ps%d" % b
                    for k in range(8):
                        kb.op("pe", lambda k=k: nc.tensor.matmul(
                            PS[b][:, :T], lhsT=Wfm[:, k, ci * 128:(ci + 1) * 128], rhs=hT[:, k, :T],
                            start=(k == 0), stop=(k == 7)), reads=["Wfm", ("hT", s)], writes=[pk])
                    ok = ("stgF", s, ci)
                    if ci < 2:
                        kb.op("act", lambda: nc.scalar.activation(out=sF[:, ci, :T], in_=PS[b][:, :T], func=AF.Gelu_apprx_tanh),
                              reads=[pk], writes=[ok])
                        return
                    if ci >= 8:
                        kb.op("dve", lambda: nc.vector.tensor_copy(out=sF[:, ci, :T], in_=PS[b][:, :T]), reads=[pk], writes=[ok])
                        return
                    qs = ci % 2
                    gcol = qgs if ci < 6 else kgs
                    kb.op("act", lambda: nc.scalar.activation(out=qsq[qs][:, :T], in_=PS[b][:, :T], func=AF.Square),
                          reads=[pk], writes=[("qsq", qs)])

                    def B():
                        kb.op("pe", lambda: nc.tensor.matmul(PS[5][:, :T], lhsT=bones_bf[:], rhs=qsq[qs][:, :T], start=True, stop=True),
                              reads=[("qsq", qs), "bones_bf"], writes=["ps5"])
                        kb.op("act", lambda: nc.scalar.activation(out=qrn[qs][:, :T], in_=PS[5][:, :T], func=AF.Sqrt,
                                                                  bias=eps_t[:], scale=1.0 / 64),
                              reads=["ps5", "eps_t"], writes=[("qrn", qs)])
                        kb.op("dve", lambda: nc.vector.reciprocal(out=qrn[qs][:, :T], in_=qrn[qs][:, :T]),
                              reads=[("qrn", qs)], writes=[("qrn", qs)])
                        if isctx:
                            kb.op("dve", lambda: nc.vector.scalar_tensor_tensor(
                                out=sF[:, ci, :T], in0=PS[b][:, :T], scalar=gcol[:, l:l + 1], in1=qrn[qs][:, :T],
                                op0=ALU.mult, op1=ALU.mult), reads=[pk, ("qrn", qs), "qgs", "kgs"], writes=[ok])
                        else:
                            kb.op("dve", lambda: nc.vector.scalar_tensor_tensor(
                                out=qn[qs][:, :T], in0=PS[b][:, :T], scalar=gcol[:, l:l + 1], in1=qrn[qs][:, :T],
                                op0=ALU.mult, op1=ALU.mult), reads=[pk, ("qrn", qs), "qgs", "kgs"], writes=[("qn", qs)])

                    def C():
                        lt0 = t0 - CL
                        kb.op("pe", lambda: nc.tensor.matmul(PS[6][:, :T], lhsT=perm_bf[:], rhs=qn[qs][:, :T], start=True, stop=True),
                              reads=[("qn", qs), "perm_bf"], writes=["ps6"])
                        kb.op("pool", lambda: nc.gpsimd.tensor_tensor(
                            out=qt1[qs][:, :T], in0=qn[qs][:, :T], in1=ropeC[:, lt0:lt0 + T], op=ALU.mult),
                            reads=[("qn", qs), "ropeC"], writes=[("qt1", qs)])
                        kb.op("dve", lambda: nc.vector.tensor_tensor(
                            out=qt2[qs][:, :T], in0=PS[6][:, :T], in1=ropeS[:, lt0:lt0 + T], op=ALU.mult),
                            reads=["ps6", "ropeS"], writes=[("qt2", qs)])
                        kb.op("pool", lambda: nc.gpsimd.tensor_tensor(
                            out=sF[:, ci, :T], in0=qt1[qs][:, :T], in1=qt2[qs][:, :T], op=ALU.add),
                            reads=[("qt1", qs), ("qt2", qs)], writes=[ok])

                    stageB[ci] = B
                    if not isctx:
                        stageC[ci] = C

                for ci in range(16):
                    if ci < 14:
                        chunkA(ci)
                    if ci == 9 and bi + 1 < len(BLOCKS):
                        norm1(bi + 1)
                    if ci - 1 in stageB:
                        stageB.pop(ci - 1)()
                    if ci - 2 in stageC:
                        stageC.pop(ci - 2)()
                assert not stageB and not stageC
                kb.dma(sp, GU.rearrange("(c p) t -> p c t", p=128)[:, :, t0:t0 + T], sF[:, 0:2, :T],
                       reads=[("stgF", s, ci) for ci in (0, 1)], semkey=("oGU", s))
                kb.dma(sp, QT.rearrange("(c p) t -> p c t", p=128)[:, :, t0:t0 + T], sF[:, 2:6, :T],
                       reads=[("stgF", s, ci) for ci in (2, 3, 4, 5)], semkey=("oQT", s))
                kb.dma(sp, KT.rearrange("(c p) t -> p c t", p=128)[:, :, t0:t0 + T], sF[:, 6:8, :T],
                       reads=[("stgF", s, ci) for ci in (6, 7)], semkey=("oKT", s))
                kb.dma(sp, XBC.rearrange("(c p) t -> p c t", p=128)[:, :, t0:t0 + T], sF[:, 8:14, :T],
                       reads=[("stgF", s, ci) for ci in range(8, 14)], semkey=("oXB", s))

                nsub = T // 128
                for sub in range(nsub):
                    ss_ = sub % 2
                    ba = next_bank()
                    bb = next_bank()
                    pka, pkb = "ps%d" % ba, "ps%d" % bb
                    for k in range(8):
                        kb.op("pe", lambda k=k, sub=sub, ba=ba: nc.tensor.matmul(
                            PS[ba][:, 0:392], lhsT=hT[:, k, sub * 128:(sub + 1) * 128], rhs=Wtm[:, k, 0:392],
                            start=(k == 0), stop=(k == 7)), reads=["Wtm", ("hT", s)], writes=[pka])
                    for k in range(8):
                        kb.op("pe", lambda k=k, sub=sub, bb=bb: nc.tensor.matmul(
                            PS[bb][:, 0:256], lhsT=hT[:, k, sub * 128:(sub + 1) * 128], rhs=Wtm[:, k, 392:648],
                            start=(k == 0), stop=(k == 7)), reads=["Wtm", ("hT", s)], writes=[pkb])
                    kb.op("act", lambda ba=ba, ss_=ss_: nc.scalar.activation(out=gl[ss_][:], in_=PS[ba][:, 0:256],
                                                                             func=AF.Gelu_apprx_tanh),
                          reads=[pka], writes=[("gl", ss_)])
                    kb.op("pool", lambda ss_=ss_: nc.gpsimd.tensor_tensor(out=gsq[ss_][:], in0=gl[ss_][:], in1=gl[ss_][:], op=ALU.mult),
                          reads=[("gl", ss_)], writes=[("gsq", ss_)])
                    kb.op("dve", lambda ss_=ss_: nc.vector.tensor_reduce(
                        out=gss[ss_][:], in_=gsq[ss_][:].rearrange("p (a b) -> p a b", b=64), axis=AX.X, op=ALU.add),
                        reads=[("gsq", ss_)], writes=[("gss", ss_)])
                    kb.op("act", lambda ss_=ss_: nc.scalar.activation(out=gss[ss_][:], in_=gss[ss_][:], func=AF.Sqrt,
                                                                      bias=eps_t[:], scale=1.0 / 64),
                          reads=[("gss", ss_), "eps_t"], writes=[("gss", ss_)])
                    kb.op("dve", lambda ss_=ss_: nc.vector.reciprocal(out=gss[ss_][:], in_=gss[ss_][:]),
                          reads=[("gss", ss_)], writes=[("gss", ss_)])
                    kb.op("dve", lambda ss_=ss_: nc.vector.tensor_tensor(
                        out=gsq[ss_][:].rearrange("p (a b) -> p a b", b=64), in0=gl[ss_][:].rearrange("p (a b) -> p a b", b=64),
                        in1=gss[ss_][:].unsqueeze(2).broadcast_to([128, 4, 64]), op=ALU.mult),
                        reads=[("gl", ss_), ("gss", ss_)], writes=[("gsq", ss_)])
                    kb.op("pool", lambda ss_=ss_, sub=sub: nc.gpsimd.tensor_tensor(
                        out=stgGV[s][:, sub, :], in0=gsq[ss_][:], in1=gvB[:], op=ALU.mult),
                        reads=[("gsq", ss_), "gvB"], writes=[("stgGV", s)])
                    kb.op("act", lambda ba=ba, sub=sub: nc.scalar.copy(out=stgV[s][:, sub, :], in_=PS[ba][:, 256:384]),
                          reads=[pka], writes=[("stgV", s)])
                    kb.op("dve", lambda ba=ba, ss_=ss_: nc.vector.tensor_tensor(out=dtx[ss_][:], in0=PS[ba][:, 384:392], in1=dtbB[:],
                                                                               op=ALU.add),
                          reads=[pka, "dtbB"], writes=[("dtx", ss_)])
                    kb.op("act", lambda ss_=ss_: nc.scalar.activation(out=dtx[ss_][:], in_=dtx[ss_][:], func=AF.Exp),
                          reads=[("dtx", ss_)], writes=[("dtx", ss_)])
                    kb.op("act", lambda ss_=ss_, sub=sub: nc.scalar.activation(out=stgDT[s][:, sub, 0:8], in_=dtx[ss_][:], func=AF.Ln,
                                                                               bias=one_t[:], scale=1.0),
                          reads=[("dtx", ss_), "one_t"], writes=[("stgDT", s)])
                    kb.op("dve", lambda sub=sub: nc.vector.tensor_tensor(out=stgDT[s][:, sub, 8:16], in0=stgDT[s][:, sub, 0:8],
                                                                         in1=aB[:], op=ALU.mult),
                          reads=[("stgDT", s), "aB"], writes=[("stgDT", s)])
                    kb.op("act", lambda bb=bb, sub=sub: nc.scalar.activation(out=stgZ[s][:, sub, :], in_=PS[bb][:, 0:256], func=AF.Silu),
                          reads=[pkb], writes=[("stgZ", s)])
                kb.dma(sp, GV[t0:t0 + T, :].rearrange("(s p) c -> p s c", p=128), stgGV[s][:, 0:nsub, :],
                       reads=[("stgGV", s)], semkey=("oGV", s))
                kb.dma(sp, V[t0:t0 + T, :].rearrange("(s p) c -> p s c", p=128), stgV[s][:, 0:nsub, :],
                       reads=[("stgV", s)], semkey=("oV", s))
                kb.dma(sp, ZS[t0:t0 + T, :].rearrange("(s p) c -> p s c", p=128), stgZ[s][:, 0:nsub, :],
                       reads=[("stgZ", s)], semkey=("oZS", s))
                kb.dma(sp, DT[t0:t0 + T, :].rearrange("(s p) c -> p s c", p=128), stgDT[s][:, 0:nsub, :],
                       reads=[("stgDT", s)], semkey=("oDT", s))
            kb.barrier()

        with contextlib.ExitStack() as st:
            wsb = sb(st, "wsb", [128, 4, 128], BF16)
            bsr = sb(st, "bsr", [128, 2, 128], F32)
            gtmp = [sb(st, "gtmp%d" % i, [128, 2, 128], F32) for i in range(2)]
            guT = [sb(st, "guT%d" % i, [128, 2, 512], BF16) for i in range(2)]
            vh = [sb(st, "vh%d" % i, [128, 4, 256], BF16) for i in range(2)]
            stg = [sb(st, "gstg%d" % i, [128, 2, 512], BF16) for i in range(2)]
            kb.dma(pool, wsb[:], wsT[l].rearrange("h j i -> j h i"), writes=["wsb"], semkey="wsb")
            kb.dma(sp, bsr[:], gbs[:, l, :, :], writes=["bsr"], semkey="bsr")
            blks = [b for b in BLOCKS if (need_ctx or not b[2])]
            def load2a(bi):
                t0, T, isctx = blks[bi]
                s = bi % 2
                nsub = T // 128
                kb.dma(sp, guT[s][:, :, :T], GU.rearrange("(c p) t -> p c t", p=128)[:, :, t0:t0 + T],
                       writes=[("guT", s)], semkey=("guT", s))
                kb.dma(sp, vh[s][:, 0:nsub, :], GV[t0:t0 + T, :].rearrange("(s p) c -> p s c", p=128),
                       writes=[("vh", s)], semkey=("vh", s))

            load2a(0)
            for bi, (t0, T, isctx) in enumerate(blks):
                s = bi % 2
                nsub = T // 128
                if bi + 1 < len(blks):
                    load2a(bi + 1)
                for sub in range(nsub):
                    b = sub % 4
                    pk = "ps%d" % b
                    for j in range(2):
                        for hh in range(2):
                            h = 2 * j + hh
                            kb.op("pe", lambda b=b, j=j, hh=hh, h=h, sub=sub: nc.tensor.matmul(
                                PS[b][hh * 64:(hh + 1) * 64, j * 128:(j + 1) * 128], lhsT=vh[s][:, sub, h * 64:(h + 1) * 64],
                                rhs=wsb[:, h, :], start=True, stop=True), reads=[("vh", s), "wsb"], writes=[pk])
                    g2 = sub % 2
                    kb.op("dve", lambda b=b, g2=g2: nc.vector.tensor_tensor(
                        out=gtmp[g2][:], in0=PS[b][:, 0:256].rearrange("p (a b) -> p a b", b=128),
                        in1=bsr[:], op=ALU.add), reads=[pk, "bsr"], writes=[("gtmp", g2)])
                    kb.op("pool", lambda sub=sub, g2=g2: nc.gpsimd.tensor_tensor(
                        out=stg[s][:, :, sub * 128:(sub + 1) * 128], in0=gtmp[g2][:],
                        in1=guT[s][:, :, sub * 128:(sub + 1) * 128], op=ALU.mult),
                        reads=[("gtmp", g2), ("guT", s)], writes=[("gstg", s)])
                kb.dma(sp, MIX.rearrange("(c p) t -> p c t", p=128)[:, 0:2, t0:t0 + T], stg[s][:, :, :T],
                       reads=[("gstg", s)], semkey=("oMIXg", s))
            kb.barrier()

        with contextlib.ExitStack() as st:
            QTs = sb(st, "QTs", [128, 4, NT], BF16)
            KTz = sb(st, "KTz", [128, 2, 2, NT], BF16)
            Vs = sb(st, "Vs", [128, NTILE, 128], BF16)
            onesv = sb(st, "onesv", [128, 64], BF16)
            Es = [sb(st, "E%d" % i, [128, 512], BF16) for i in range(4)]
            den = [sb(st, "den%d" % i, [128, 256], F32) for i in range(2)]
            stg = [sb(st, "astg%d" % i, [128, 4, 512], BF16) for i in range(2)]
            kb.op("dve", lambda: nc.vector.memset(onesv[:], 1.0), writes=["onesv"])
            for c4 in range(4):
                kb.dma(sp, QTs[:, c4, :], QT[c4 * 128:(c4 + 1) * 128, :], writes=["QTs"], semkey=("QTs", c4))
            kb.op("pool", lambda: nc.gpsimd.memset(KTz[:], 0.0), writes=["KTs"])
            for c2 in range(2):
                for half in range(2):
                    kb.dma(sp, KTz[half * 64:(half + 1) * 64, c2, half, :],
                           KT[c2 * 128 + half * 64:c2 * 128 + (half + 1) * 64, :], writes=["KTs"], semkey=("KTs", c2, half))
            for c8 in range(0, NTILE, 6):
                c9 = min(NTILE, c8 + 6)
                kb.dma(sp, Vs[:, c8:c9, :], V[c8 * 128:c9 * 128, :].rearrange("(s p) c -> p s c", p=128),
                       writes=["Vs"], semkey=("Vs", c8))
            srot = [0]
            od = [0]
            blks = [b for b in BLOCKS if (need_ctx or not b[2])]
            for bi, (t0, T, isctx) in enumerate(blks):
                s = bi % 2
                nsub = T // 128
                for sub in range(nsub):
                    tq = t0 // 128 + sub
                    if isctx:
                        kts = [(0, None), (1, None)]
                    else:
                        n = tq - 2
                        kts = [(0, None), (1, None)]
                        if n > 0:
                            kts.append((tq - 1, ub_bf))
                        kts.append((tq, None))
                        if n < 31:
                            kts.append((tq + 1, uf_bf))
                    for kv in range(2):
                        ob = 4 + (od[0] % 2)
                        db = 6 + (od[0] % 2)
                        od[0] += 1
                        okey, dkey = "ps%d" % ob, "ps%d" % db
                        slots = {}

                        def emitS(i, kv=kv, tq=tq, kts=kts, slots=slots):
                            kt, msk = kts[i]
                            sl = srot[0] % 4
                            srot[0] += 1
                            slots[i] = sl
                            pk = "ps%d" % sl
                            for half in range(2):
                                kb.op("pe", lambda half=half, sl=sl, kt=kt: nc.tensor.matmul(
                                    PS[sl][:, half * 256:(half + 1) * 256].rearrange("p (a b) -> p a b", b=128),
                                    lhsT=KTz[:, kv, half, kt * 128:(kt + 1) * 128],
                                    rhs=QTs[:, 2 * kv:2 * kv + 2, tq * 128:(tq + 1) * 128],
                                    start=True, stop=True), reads=["KTs", "QTs"], writes=[pk])
                            kb.op("act", lambda sl=sl: nc.scalar.activation(out=Es[sl][:], in_=PS[sl][:], func=AF.Exp, scale=0.125),
                                  reads=[pk], writes=[("E", sl)])
                            if msk is not None:
                                kb.op("pool", lambda sl=sl, msk=msk: nc.gpsimd.tensor_tensor(
                                    out=Es[sl][:].rearrange("p (a b) -> p a b", b=128),
                                    in0=Es[sl][:].rearrange("p (a b) -> p a b", b=128),
                                    in1=msk[:].unsqueeze(1).broadcast_to([128, 4, 128]), op=ALU.mult),
                                    reads=[("E", sl), "uf_bf", "ub_bf"], writes=[("E", sl)])

                        def emitPV(i, kv=kv, kts=kts, slots=slots, ob=ob, db=db, okey=okey, dkey=dkey):
                            kt, _ = kts[i]
                            sl = slots[i]
                            first, last = (i == 0), (i == len(kts) - 1)
                            for half in range(2):
                                kb.op("pe", lambda half=half, sl=sl, kt=kt: nc.tensor.matmul(
                                    PS[ob][half * 64:(half + 1) * 64, 0:256], lhsT=Vs[:, kt, kv * 64:(kv + 1) * 64],
                                    rhs=Es[sl][:, half * 256:(half + 1) * 256], start=first, stop=last),
                                    reads=["Vs", ("E", sl)], writes=[okey])
                                kb.op("pe", lambda half=half, sl=sl: nc.tensor.matmul(
                                    PS[db][half * 64:(half + 1) * 64, 0:256], lhsT=onesv[:],
                                    rhs=Es[sl][:, half * 256:(half + 1) * 256], start=first, stop=last),
                                    reads=["onesv", ("E", sl)], writes=[dkey])

                        nk = len(kts)
                        emitS(0)
                        if nk > 1:
                            emitS(1)
                        for i in range(nk):
                            emitPV(i)
                            if i + 2 < nk:
                                emitS(i + 2)
                        dn = den[kv]
                        kb.op("dve", lambda db=db, dn=dn, kv=kv: nc.vector.tensor_tensor(
                            out=dn[:].rearrange("p (a b) -> p a b", b=128),
                            in0=PS[db][:, 0:256].rearrange("p (a b) -> p a b", b=128),
                            in1=sinkE[:, l, kv, :].unsqueeze(2).broadcast_to([128, 2, 128]), op=ALU.add),
                            reads=[dkey, "sinkE"], writes=[("den", kv)])
                        kb.op("dve", lambda dn=dn: nc.vector.reciprocal(out=dn[:], in_=dn[:]),
                              reads=[("den", kv)], writes=[("den", kv)])
                        kb.op("dve", lambda ob=ob, dn=dn, kv=kv, sub=sub: nc.vector.tensor_tensor(
                            out=stg[s][:, 2 * kv:2 * kv + 2, sub * 128:(sub + 1) * 128],
                            in0=PS[ob][:, 0:256].rearrange("p (a b) -> p a b", b=128),
                            in1=dn[:].rearrange("p (a b) -> p a b", b=128), op=ALU.mult),
                            reads=[okey, ("den", kv)], writes=[("astg", s)])
                kb.dma(sp, MIX.rearrange("(c p) t -> p c t", p=128)[:, 2:6, t0:t0 + T], stg[s][:, :, :T],
                       reads=[("astg", s)], semkey=("oMIXa", s))
            kb.barrier()

        with contextlib.ExitStack() as st:
            XC = sb(st, "XC", [128, 6, NT], BF16)
            Xtm = sb(st, "Xtm", [128, NTILE, 256], BF16)
            Btm = sb(st, "Btm", [128, NTILE, 256], BF16)
            Ytm = sb(st, "Ytm", [128, NTILE, 256], F32)
            DTs = sb(st, "DTs", [128, NTILE, 16], F32)
            ZSs = sb(st, "ZSs", [128, NTILE, 256], BF16)
            Sf = sb(st, "Sf", [128, 256], F32)
            Sb_ = sb(st, "Sb", [128, 256], BF16)
            negf = sb(st, "negf", [128, 512], BF16)
            negb = sb(st, "negb", [128, 512], BF16)
            dskB = sb(st, "dskB", [128, 256], F32)
            ngB = sb(st, "ngB", [128, 256], F32)
            XB = [sb(st, "XB%d" % i, [128, 6, 516], BF16) for i in range(2)]
            cacc = [sb(st, "cacc%d" % i, [128, 512], F32) for i in range(2)]
            rhsA = [sb(st, "rhsA%d" % i, [128, 512], BF16) for i in range(2)]
            adtb = [sb(st, "adtb%d" % i, [128, 4], BF16) for i in range(2)]
            col = [sb(st, "col%d" % i, [128, 4], F32) for i in range(2)]
            Dm = [sb(st, "Dm%d" % i, [128, 512], F32) for i in range(2)]
            Lm = [sb(st, "Lm%d" % i, [128, 512], BF16) for i in range(2)]
            MT = [sb(st, "MT%d" % i, [128, 512], BF16) for i in range(2)]
            sm = [sb(st, "sm%d" % i, [128, 16], F32) for i in range(2)]
            xw = [sb(st, "xw%d" % i, [128, 256], BF16) for i in range(2)]
            xdt = [sb(st, "xdt%d" % i, [128, 256], BF16) for i in range(2)]
            yt1 = [sb(st, "yt1%d" % i, [128, 256], F32) for i in range(2)]
            yt2 = [sb(st, "yt2%d" % i, [128, 256], F32) for i in range(2)]
            tS = sb(st, "tS", [128, 256], F32)
            Sf2 = sb(st, "Sf2", [128, 256], F32)
            Sb2 = sb(st, "Sb2", [128, 256], BF16)
            tS2 = sb(st, "tS2", [128, 256], F32)
            yz = [sb(st, "yz%d" % i, [128, 256], F32) for i in range(2)]
            yjunk = sb(st, "yjunk", [128, 256], F32)
            yss = [sb(st, "yss%d" % i, [128, 1], F32) for i in range(2)]
            yo = [sb(st, "yo%d" % i, [128, 256], BF16) for i in range(2)]
            stg = [sb(st, "sstg%d" % i, [128, 2, 512], BF16) for i in range(2)]

            kb.dma(pool, negf[:], c_negf, writes=["negf"], semkey="negf")
            kb.dma(pool, negb[:], c_negb, writes=["negb"], semkey="negb")
            kb.dma(sp, dskB[:], dskE[l].partition_broadcast(128), writes=["dskB"], semkey="dskB")
            kb.dma(sp, ngB[:], sng[l].partition_broadcast(128), writes=["ngB"], semkey="ngB")
            for c8 in range(0, NTILE, 6):
                c9 = min(NTILE, c8 + 6)
                kb.dma(sp, DTs[:, c8:c9, :], DT[c8 * 128:c9 * 128, :].rearrange("(s p) c -> p s c", p=128),
                       writes=["DTs"], semkey=("DTs", c8))
                kb.dma(sp, ZSs[:, c8:c9, :], ZS[c8 * 128:c9 * 128, :].rearrange("(s p) c -> p s c", p=128),
                       writes=["ZSs"], semkey=("ZSs", c8))

            def loadxb(bi):
                t0, T, isctx = BLOCKS[bi]
                s = bi % 2
                seg0, seg1 = (0, CL) if isctx else (CL, NT)
                lo, hi = max(t0 - 2, seg0), min(t0 + T + 2, seg1)
                xb = XB[s]
                wr = [("XB", s)]
                if lo > t0 - 2:
                    kb.op("pool", lambda xb=xb: nc.gpsimd.memset(xb[:, :, 0:2], 0.0), writes=wr)
                if hi < t0 + T + 2:
                    kb.op("pool", lambda xb=xb, T=T: nc.gpsimd.memset(xb[:, :, T + 2:T + 4], 0.0), writes=wr)
                kb.dma(sp, xb[:, :, lo - (t0 - 2):hi - (t0 - 2)], XBC.rearrange("(c p) t -> p c t", p=128)[:, :, lo:hi],
                       writes=wr, semkey=("XB", s))

            loadxb(0)
            for bi, (t0, T, isctx) in enumerate(BLOCKS):
                s = bi % 2
                xb = XB[s]
                wr = [("XB", s)]
                if bi + 1 < len(BLOCKS):
                    loadxb(bi + 1)
                for j in range(6):
                    a = cacc[j % 2]
                    ak = ("cacc", j % 2)
                    kb.op("dve", lambda j=j, a=a, xb=xb, T=T: nc.vector.tensor_scalar(
                        out=a[:, :T], in0=xb[:, j, 0:T], scalar1=cws[:, l, j, 0:1], scalar2=None, op0=ALU.mult),
                        reads=wr + ["cws"], writes=[ak])
                    for k in range(1, 5):
                        kb.op("dve", lambda j=j, a=a, xb=xb, T=T, k=k: nc.vector.scalar_tensor_tensor(
                            out=a[:, :T], in0=xb[:, j, k:k + T], scalar=cws[:, l, j, k:k + 1], in1=a[:, :T],
                            op0=ALU.mult, op1=ALU.add), reads=wr + ["cws", ak], writes=[ak])
                    kb.op("act", lambda j=j, a=a, T=T, t0=t0: nc.scalar.activation(
                        out=XC[:, j, t0:t0 + T], in_=a[:, :T], func=AF.Silu, bias=cbs[:, l, j:j + 1], scale=1.0),
                        reads=[ak, "cbs"], writes=[("XC", bi)])
            kb.barrier()

            for c in range(NTILE):
                b = 6 + (c % 2)
                pk = "ps%d" % b
                psb = PS[b][:].bitcast(BF16)
                for q4, j in enumerate((0, 1, 2, 3)):
                    kb.op("pe", lambda q4=q4, j=j, c=c, psb=psb: nc.tensor.transpose(
                        out=psb[:, q4 * 128:(q4 + 1) * 128], in_=XC[:, j, c * 128:(c + 1) * 128], identity=ident_bf[:]),
                        reads=["XC", "ident_bf"], writes=[pk])
                kb.op("act", lambda c=c, psb=psb: nc.scalar.copy(out=Xtm[:, c, :], in_=psb[:, 0:256]), reads=[pk], writes=[("Xtm", c)])
                kb.op("act", lambda c=c, psb=psb: nc.scalar.copy(out=Btm[:, c, :], in_=psb[:, 256:512]), reads=[pk], writes=[("Btm", c)])
                kb.op("pool", lambda c=c: nc.gpsimd.tensor_tensor(out=Ytm[:, c, :], in0=Xtm[:, c, :], in1=dskB[:], op=ALU.mult),
                      reads=[("Xtm", c), "dskB"], writes=[("Ytm", c)])

            kb.barrier()
            it = [0]
            Sfs, Sbs, tSs = [Sf, Sf2], [Sb_, Sb2], [tS, tS2]
            for d in range(2):
                kb.op("dve", lambda d=d: nc.vector.memset(Sfs[d][:], 0.0), writes=[("Sf", d)])
                kb.op("pool", lambda d=d: nc.gpsimd.memset(Sbs[d][:], 0.0), writes=[("Sb", d)])

            def scan_step(d, c):
                U = uf_bf if d == 0 else ub_bf
                NEG = negf if d == 0 else negb
                last = 127 if d == 0 else 0
                Sfd, Sbd, tSd = Sfs[d], Sbs[d], tSs[d]
                kSf, kSb, ktS = ("Sf", d), ("Sb", d), ("tS", d)
                b2, b5 = (2, 5) if d == 0 else (6, 7)
                k2, k5 = "ps%d" % b2, "ps%d" % b5
                want_y = need_ctx or c >= 2
                s = it[0] % 2
                it[0] += 1
                rb = s
                rk = "ps%d" % rb
                dt4 = DTs[:, c, d * 4:(d + 1) * 4]
                adt4 = DTs[:, c, 8 + d * 4:12 + d * 4]
                kb.op("dve", lambda s=s, U=U, adt4=adt4: nc.vector.tensor_tensor(
                    out=rhsA[s][:].rearrange("p (a b) -> p a b", b=128),
                    in0=U[:].unsqueeze(1).broadcast_to([128, 4, 128]),
                    in1=adt4.unsqueeze(2).broadcast_to([128, 4, 128]), op=ALU.mult),
                    reads=["DTs", "uf_bf", "ub_bf"], writes=[("rhsA", s)])
                kb.op("act", lambda s=s, adt4=adt4: nc.scalar.copy(out=adtb[s][:], in_=adt4), reads=["DTs"], writes=[("adtb", s)])
                kb.op("pe", lambda s=s, rb=rb: nc.tensor.matmul(PS[rb][:], lhsT=ones_bf[:], rhs=rhsA[s][:], start=True, stop=False),
                      reads=[("rhsA", s), "ones_bf"], writes=[rk])
                kb.op("pe", lambda rb=rb, NEG=NEG: nc.tensor.matmul(PS[rb][:], lhsT=ident_bf[:], rhs=NEG[:], start=False, stop=True),
                      reads=["negf", "negb", "ident_bf"], writes=[rk])
                kb.op("pe", lambda s=s, U=U: nc.tensor.matmul(PS[b2][:, 256:260], lhsT=U[:], rhs=adtb[s][:], start=True, stop=True),
                      reads=[("adtb", s), "uf_bf", "ub_bf"], writes=[k2])
                for g in range(2):
                    kb.op("pe", lambda g=g, c=c: nc.tensor.matmul(
                        PS[b2][:, g * 128:(g + 1) * 128], lhsT=XC[:, 2 + g, c * 128:(c + 1) * 128],
                        rhs=XC[:, 4 + g, c * 128:(c + 1) * 128], start=True, stop=True), reads=["XC"], writes=[k2])
                kb.op("act", lambda s=s: nc.scalar.copy(out=col[s][:], in_=PS[b2][:, 256:260]), reads=[k2], writes=[("col", s)])
                kb.op("dve", lambda s=s, rb=rb: nc.vector.tensor_tensor(
                    out=Dm[s][:].rearrange("p (a b) -> p a b", b=128), in0=PS[rb][:].rearrange("p (a b) -> p a b", b=128),
                    in1=col[s][:].unsqueeze(2).broadcast_to([128, 4, 128]), op=ALU.subtract),
                    reads=[rk, ("col", s)], writes=[("Dm", s)])
                kb.op("act", lambda s=s: nc.scalar.activation(out=Lm[s][:], in_=Dm[s][:], func=AF.Exp),
                      reads=[("Dm", s)], writes=[("Lm", s)])
                kb.op("dve", lambda s=s: nc.vector.tensor_tensor(
                    out=MT[s][:].rearrange("p (g h i) -> p g h i", g=2, h=2),
                    in0=Lm[s][:].rearrange("p (g h i) -> p g h i", g=2, h=2),
                    in1=PS[b2][:, 0:256].rearrange("p (g i) -> p g i", g=2).unsqueeze(2).broadcast_to([128, 2, 2, 128]),
                    op=ALU.mult), reads=[("Lm", s), k2], writes=[("MT", s)])
                tot = PS[rb][:].rearrange("p (a b) -> p a b", b=128)[:, :, last]
                smk = ("sm", s)
                kb.op("dve", lambda s=s, tot=tot: nc.vector.tensor_tensor(out=sm[s][:, 0:4], in0=tot, in1=col[s][:], op=ALU.subtract),
                      reads=[rk, ("col", s)], writes=[smk])
                kb.op("act", lambda s=s: nc.scalar.activation(out=sm[s][:, 0:4], in_=sm[s][:, 0:4], func=AF.Exp), reads=[smk], writes=[smk])
                kb.op("dve", lambda s=s, dt4=dt4: nc.vector.tensor_tensor(out=sm[s][:, 0:4], in0=sm[s][:, 0:4], in1=dt4, op=ALU.mult),
                      reads=[smk, "DTs"], writes=[smk])
                kb.op("act", lambda s=s: nc.scalar.activation(out=sm[s][:, 4:8], in_=col[s][:], func=AF.Exp), reads=[("col", s)], writes=[smk])
                kb.op("act", lambda s=s, tot=tot: nc.scalar.activation(out=sm[s][:, 8:12], in_=tot, func=AF.Exp), reads=[rk], writes=[smk])
                kb.op("dve", lambda s=s, c=c: nc.vector.tensor_tensor(
                    out=xw[s][:].rearrange("p (a b) -> p a b", b=64), in0=Xtm[:, c, :].rearrange("p (a b) -> p a b", b=64),
                    in1=sm[s][:, 0:4].unsqueeze(2).broadcast_to([128, 4, 64]), op=ALU.mult),
                    reads=[("Xtm", c), smk], writes=[("xw", s)])
                if want_y:
                    kb.op("pool", lambda s=s, c=c, dt4=dt4: nc.gpsimd.tensor_tensor(
                        out=xdt[s][:].rearrange("p (a b) -> p a b", b=64), in0=Xtm[:, c, :].rearrange("p (a b) -> p a b", b=64),
                        in1=dt4.unsqueeze(2).broadcast_to([128, 4, 64]), op=ALU.mult),
                        reads=[("Xtm", c), "DTs"], writes=[("xdt", s)])
                    for h in range(4):
                        kb.op("pe", lambda s=s, h=h: nc.tensor.matmul(
                            PS[3][:, h * 64:(h + 1) * 64], lhsT=MT[s][:, h * 128:(h + 1) * 128], rhs=xdt[s][:, h * 64:(h + 1) * 64],
                            start=True, stop=True), reads=[("MT", s), ("xdt", s)], writes=["ps3"])
                    for g in range(2):
                        kb.op("pe", lambda g=g, c=c: nc.tensor.matmul(
                            PS[4][:, g * 128:(g + 1) * 128], lhsT=XC[:, 4 + g, c * 128:(c + 1) * 128],
                            rhs=Sbd[:, g * 128:(g + 1) * 128], start=True, stop=True), reads=["XC", kSb], writes=["ps4"])
                    kb.op("dve", lambda s=s: nc.vector.tensor_tensor(
                        out=yt1[s][:].rearrange("p (a b) -> p a b", b=64), in0=PS[4][:, 0:256].rearrange("p (a b) -> p a b", b=64),
                        in1=sm[s][:, 4:8].unsqueeze(2).broadcast_to([128, 4, 64]), op=ALU.mult),
                        reads=["ps4", smk], writes=[("yt1", s)])
                    kb.op("dve", lambda s=s: nc.vector.tensor_tensor(out=yt2[s][:], in0=PS[3][:, 0:256], in1=yt1[s][:], op=ALU.add),
                          reads=["ps3", ("yt1", s)], writes=[("yt2", s)])
                    kb.op("pool", lambda s=s, c=c: nc.gpsimd.tensor_tensor(out=Ytm[:, c, :], in0=Ytm[:, c, :], in1=yt2[s][:], op=ALU.add),
                          reads=[("yt2", s), ("Ytm", c)], writes=[("Ytm", c)])
                for g in range(2):
                    kb.op("pe", lambda g=g, c=c, s=s: nc.tensor.matmul(
                        PS[b5][:, g * 128:(g + 1) * 128], lhsT=Btm[:, c, g * 128:(g + 1) * 128],
                        rhs=xw[s][:, g * 128:(g + 1) * 128], start=True, stop=True), reads=[("Btm", c), ("xw", s)], writes=[k5])
                kb.op("dve", lambda s=s: nc.vector.tensor_tensor(
                    out=tSd[:].rearrange("p (a b) -> p a b", b=64), in0=Sfd[:].rearrange("p (a b) -> p a b", b=64),
                    in1=sm[s][:, 8:12].unsqueeze(2).broadcast_to([128, 4, 64]), op=ALU.mult),
                    reads=[kSf, smk], writes=[ktS])
                kb.op("dve", lambda: nc.vector.tensor_tensor(out=Sfd[:], in0=PS[b5][:, 0:256], in1=tSd[:], op=ALU.add),
                      reads=[k5, ktS], writes=[kSf])
                kb.op("act", lambda: nc.scalar.copy(out=Sbd[:], in_=Sfd[:]), reads=[kSf], writes=[kSb])

            orders = [list(range(NTILE)), [1, 0] + list(range(NTILE - 1, 1, -1))]
            for i in range(NTILE):
                for d in range(2):
                    scan_step(d, orders[d][i])
            kb.barrier()
            blks = [b for b in BLOCKS if (need_ctx or not b[2])]
            for bi, (t0, T, isctx) in enumerate(blks):
                sg = bi % 2
                nsub = T // 128
                for sub in range(nsub):
                    c = t0 // 128 + sub
                    s = sub % 2
                    kb.op("pool", lambda c=c, s=s: nc.gpsimd.tensor_tensor(out=yz[s][:], in0=Ytm[:, c, :], in1=ZSs[:, c, :], op=ALU.mult),
                          reads=[("Ytm", c), "ZSs"], writes=[("yz", s)])
                    kb.op("act", lambda s=s: nc.scalar.activation(out=yjunk[:], in_=yz[s][:], func=AF.Square, accum_out=yss[s][:]),
                          reads=[("yz", s)], writes=["yjunk", ("yss", s)])
                    kb.op("act", lambda s=s: nc.scalar.activation(out=yss[s][:], in_=yss[s][:], func=AF.Sqrt, bias=eps_t[:], scale=1.0 / 256),
                          reads=[("yss", s), "eps_t"], writes=[("yss", s)])
                    kb.op("dve", lambda s=s: nc.vector.reciprocal(out=yss[s][:], in_=yss[s][:]), reads=[("yss", s)], writes=[("yss", s)])
                    kb.op("dve", lambda s=s: nc.vector.scalar_tensor_tensor(
                        out=yo[s][:], in0=yz[s][:], scalar=yss[s][:, 0:1], in1=ngB[:], op0=ALU.mult, op1=ALU.mult),
                        reads=[("yz", s), ("yss", s), "ngB"], writes=[("yo", s)])
                    b = 6 + s
                    pk = "ps%d" % b
                    psb = PS[b][:].bitcast(BF16)
                    for j in range(2):
                        kb.op("pe", lambda j=j, s=s, psb=psb: nc.tensor.transpose(
                            out=psb[:, j * 128:(j + 1) * 128], in_=yo[s][:, j * 128:(j + 1) * 128], identity=ident_bf[:]),
                            reads=[("yo", s), "ident_bf"], writes=[pk])
                    kb.op("act", lambda sub=sub, psb=psb, sg=sg: nc.scalar.copy(
                        out=stg[sg][:, :, sub * 128:(sub + 1) * 128], in_=psb[:, 0:256].rearrange("p (a b) -> p a b", b=128)),
                        reads=[pk], writes=[("sstg", sg)])
                kb.dma(sp, MIX.rearrange("(c p) t -> p c t", p=128)[:, 6:8, t0:t0 + T], stg[sg][:, :, :T],
                       reads=[("sstg", sg)], semkey=("oMIXs", sg))
            kb.barrier()

        with contextlib.ExitStack() as st:
            Wo = sb(st, "Wo", [128, 8, D], BF16)
            mix = [sb(st, "mix%d" % i, [128, 8, 512], BF16) for i in range(2)]
            xts = [sb(st, "xt%d" % i, [128, 8, 512], F32) for i in range(2)]
            hTs = [sb(st, "hTs%d" % i, [128, 8, 512], BF16) for i in range(2)]
            sq = [sb(st, "sq%d" % i, [128, 512], BF16) for i in range(2)]
            tmp = [sb(st, "tmp%d" % i, [128, 512], F32) for i in range(2)]
            rstd = sb(st, "rstd", [128, 512], F32)
            if is_moe:
                hf = sb(st, "hf", [128, 8, 512], F32)
                wr_ = sb(st, "wr", [128, 8, NEXP], F32)
                lg = [sb(st, "lg%d" % i, [128, 8], F32) for i in range(2)]
                mx = [sb(st, "mx%d" % i, [128, 8], F32) for i in range(2)]
                ee = [sb(st, "ee%d" % i, [128, 8], F32) for i in range(2)]
                mk = [sb(st, "mk%d" % i, [128, 8], F32) for i in range(2)]
                r2 = [sb(st, "r2%d" % i, [128, 1], F32) for i in range(2)]
                cmb = [sb(st, "cmb%d" % i, [128, 8], F32) for i in range(2)]
                cstg = [sb(st, "cstg%d" % i, [8, 512], F32) for i in range(2)]
                kb.dma(sp, wr_[:], moe_r[jl].rearrange("(k p) e -> p k e", p=128), writes=["wr"], semkey="wr")
            kb.dma(pool, Wo[:], w_out[l].rearrange("(k p) n -> p k n", p=128), writes=["Wo"], semkey="Wo")
            blks = [b for b in BLOCKS if (need_ctx or not b[2])]
            brot = [0]
            def load3(bi):
                t0, T, isctx = blks[bi]
                s = bi % 2
                kb.dma(sp, mix[s][:, :, :T], MIX.rearrange("(c p) t -> p c t", p=128)[:, :, t0:t0 + T],
                       writes=[("mix", s)], semkey=("mix", s))
                kb.dma(sp, xts[s][:, :, :T], xsrc.rearrange("(k p) t -> p k t", p=128)[:, :, t0:t0 + T],
                       writes=[("xt", s)], semkey=("xt3", s))

            load3(0)
            for bi, (t0, T, isctx) in enumerate(blks):
                s = bi % 2
                which = 1 if isctx else 0
                xt = xts[s]
                if bi + 1 < len(blks):
                    load3(bi + 1)
                for co in range(8):
                    b = brot[0] % 4
                    brot[0] += 1
                    pk = "ps%d" % b
                    for k in range(8):
                        kb.op("pe", lambda k=k, co=co, b=b: nc.tensor.matmul(
                            PS[b][:, :T], lhsT=Wo[:, k, co * 128:(co + 1) * 128], rhs=mix[s][:, k, :T],
                            start=(k == 0), stop=(k == 7)), reads=["Wo", ("mix", s)], writes=[pk])
                    kb.op("dve", lambda co=co, b=b: nc.vector.scalar_tensor_tensor(
                        out=xt[:, co, :T], in0=PS[b][:, :T], scalar=mods[:, l, 16 + co, which:which + 1], in1=xt[:, co, :T],
                        op0=ALU.mult, op1=ALU.add), reads=[pk, "mods", ("xt", s)], writes=[("xt", s)])
                kb.dma(sp, R.rearrange("(k p) t -> p k t", p=128)[:, :, t0:t0 + T], xt[:, :, :T],
                       reads=[("xt", s)], semkey=("oR3", s))
                rmsnorm_block((sq, rstd, tmp), xt, T, G2, l, 24, which, hTs[s], ("hTs", s), hf=(hf if is_moe else None),
                              xk=("xt", s))
                kb.dma(sp, HT.rearrange("(k p) t -> p k t", p=128)[:, :, t0:t0 + T], hTs[s][:, :, :T],
                       reads=[("hTs", s)], semkey=("oHT", s))
                if is_moe:
                    nsub = T // 128
                    for sub in range(nsub):
                        s2 = sub % 2
                        for k in range(8):
                            kb.op("pe", lambda k=k, sub=sub: nc.tensor.matmul(
                                PS[4][:, 0:8], lhsT=hf[:, k, sub * 128:(sub + 1) * 128], rhs=wr_[:, k, :],
                                start=(k == 0), stop=(k == 7)), reads=["hf", "wr"], writes=["ps4"])
                        kb.op("act", lambda s2=s2: nc.scalar.copy(out=lg[s2][:], in_=PS[4][:, 0:8]), reads=["ps4"], writes=[("lg", s2)])
                        kb.op("dve", lambda s2=s2: nc.vector.max(out=mx[s2][:], in_=lg[s2][:]), reads=[("lg", s2)], writes=[("mx", s2)])
                        kb.op("dve", lambda s2=s2: nc.vector.tensor_scalar(out=ee[s2][:], in0=lg[s2][:], scalar1=mx[s2][:, 0:1], scalar2=None,
                                                                          op0=ALU.subtract), reads=[("lg", s2), ("mx", s2)], writes=[("ee", s2)])
                        kb.op("act", lambda s2=s2: nc.scalar.activation(out=ee[s2][:], in_=ee[s2][:], func=AF.Exp), reads=[("ee", s2)], writes=[("ee", s2)])
                        kb.op("dve", lambda s2=s2: nc.vector.tensor_tensor(out=r2[s2][:], in0=mx[s2][:, 1:2], in1=mx[s2][:, 0:1], op=ALU.subtract),
                              reads=[("mx", s2)], writes=[("r2", s2)])
                        kb.op("act", lambda s2=s2: nc.scalar.activation(out=r2[s2][:], in_=r2[s2][:], func=AF.Exp), reads=[("r2", s2)], writes=[("r2", s2)])
                        kb.op("dve", lambda s2=s2: nc.vector.tensor_scalar(out=r2[s2][:], in0=r2[s2][:], scalar1=1.0, scalar2=None, op0=ALU.add),
                              reads=[("r2", s2)], writes=[("r2", s2)])
                        kb.op("dve", lambda s2=s2: nc.vector.reciprocal(out=r2[s2][:], in_=r2[s2][:]), reads=[("r2", s2)], writes=[("r2", s2)])
                        kb.op("dve", lambda s2=s2: nc.vector.tensor_scalar(out=mk[s2][:], in0=lg[s2][:], scalar1=mx[s2][:, 1:2], scalar2=None,
                                                                          op0=ALU.is_ge), reads=[("lg", s2), ("mx", s2)], writes=[("mk", s2)])
                        kb.op("dve", lambda s2=s2: nc.vector.scalar_tensor_tensor(
                            out=cmb[s2][:], in0=ee[s2][:], scalar=r2[s2][:, 0:1], in1=mk[s2][:], op0=ALU.mult, op1=ALU.mult),
                            reads=[("ee", s2), ("r2", s2), ("mk", s2)], writes=[("cmb", s2)])
                        kb.op("pe", lambda s2=s2, sub=sub: nc.tensor.transpose(
                            out=PS[5][0:8, sub * 128:(sub + 1) * 128], in_=cmb[s2][:], identity=ident_f[:]),
                            reads=[("cmb", s2), "ident_f"], writes=["ps5"])
                    kb.op("act", lambda s=s, T=T: nc.scalar.copy(out=cstg[s][:, :T], in_=PS[5][0:8, :T]), reads=["ps5"], writes=[("cstg", s)])
                    kb.dma(sp, COMBT[:, t0:t0 + T], cstg[s][:, :T], reads=[("cstg", s)], semkey=("oCB", s))
            kb.barrier()

        with contextlib.ExitStack() as st:
            Wg = [sb(st, "Wg%d" % i, [128, 8, 768], BF16) for i in range(2)]
            Wu = [sb(st, "Wu%d" % i, [128, 8, 768], BF16) for i in range(2)]
            Wd = [sb(st, "Wd%d" % i, [128, 6, D], BF16) for i in range(2)]
            xts = [sb(st, "xt%d" % i, [128, 8, 512], F32) for i in range(2)]
            hTs = [sb(st, "hTs%d" % i, [128, 8, 512], BF16) for i in range(2)]
            aT = [sb(st, "aT%d" % i, [128, 6, 512], BF16) for i in range(2)]
            sgs = [sb(st, "sg%d" % i, [128, 512], F32) for i in range(2)]
            cb = [sb(st, "cb%d" % i, [128, 512], F32) for i in range(2)]
            t4 = [sb(st, "t4%d" % i, [128, 512], F32) for i in range(2)]
            blks = [b for b in BLOCKS if (need_ctx or not b[2])]
            passes = [(e, g) for e in range(NEXP if is_moe else 1) for g in range(4)]
            final_layer = (l == nlayers - 1)

            def load_w(pi):
                e, g = passes[pi]
                f0, nf = FGROUPS[g]
                s = pi % 2
                if is_moe:
                    gsrc, usrc, dsrc = moe_g[jl, e], moe_u[jl, e], moe_d[jl, e]
                else:
                    gsrc, usrc, dsrc = ffn_g[jl], ffn_u[jl], ffn_d[jl]
                kb.dma(pool, Wg[s][:, :, 0:nf * 128], gsrc.rearrange("(k p) f -> p k f", p=128)[:, :, f0 * 128:(f0 + nf) * 128],
                       writes=[("Wg", s)], semkey=("Wg", s))
                kb.dma(pool, Wu[s][:, :, 0:nf * 128], usrc.rearrange("(k p) f -> p k f", p=128)[:, :, f0 * 128:(f0 + nf) * 128],
                       writes=[("Wu", s)], semkey=("Wu", s))
                kb.dma(pool, Wd[s][:, 0:nf, :], dsrc[f0 * 128:(f0 + nf) * 128, :].rearrange("(c p) d -> p c d", p=128),
                       writes=[("Wd", s)], semkey=("Wd", s))

            iters = [(pi, bi) for pi in range(len(passes)) for bi in range(len(blks))]

            def load_act(n):
                pi, bi = iters[n]
                e = passes[pi][0]
                t0, T, isctx = blks[bi]
                s = n % 2
                kb.dma(sp, hTs[s][:, :, :T], HT.rearrange("(k p) t -> p k t", p=128)[:, :, t0:t0 + T],
                       writes=[("hT", s)], semkey=("hT4", s))
                kb.dma(sp, xts[s][:, :, :T], R.rearrange("(k p) t -> p k t", p=128)[:, :, t0:t0 + T],
                       reads=[("R", bi)], writes=[("xt", s)], semkey=("xt4", s))
                if is_moe:
                    kb.dma(sp, cb[s][:, :T], COMBT[e, t0:t0 + T].partition_broadcast(128), writes=[("cb", s)], semkey=("cb", s))

            load_w(0)
            load_act(0)
            it = 0
            gub = [0]
            dbk = [0]
            for pi, (e, g) in enumerate(passes):
                if pi + 1 < len(passes):
                    load_w(pi + 1)
                f0, nf = FGROUPS[g]
                ws = pi % 2
                last_pass = (pi == len(passes) - 1)
                for bi, (t0, T, isctx) in enumerate(blks):
                    s = it % 2
                    if it + 1 < len(iters):
                        load_act(it + 1)
                    it += 1
                    which = 1 if isctx else 0
                    xt, hT = xts[s], hTs[s]
                    for fc in range(nf):
                        bg = (gub[0] % 2) * 2
                        gub[0] += 1
                        bu = bg + 1
                        gk, uk = "ps%d" % bg, "ps%d" % bu
                        for k in range(8):
                            kb.op("pe", lambda k=k, fc=fc, bg=bg: nc.tensor.matmul(
                                PS[bg][:, :T], lhsT=Wg[ws][:, k, fc * 128:(fc + 1) * 128], rhs=hT[:, k, :T],
                                start=(k == 0), stop=(k == 7)), reads=[("Wg", ws), ("hT", s)], writes=[gk])
                        for k in range(8):
                            kb.op("pe", lambda k=k, fc=fc, bu=bu: nc.tensor.matmul(
                                PS[bu][:, :T], lhsT=Wu[ws][:, k, fc * 128:(fc + 1) * 128], rhs=hT[:, k, :T],
                                start=(k == 0), stop=(k == 7)), reads=[("Wu", ws), ("hT", s)], writes=[uk])
                        sgi = fc % 2
                        kb.op("act", lambda bg=bg, sgi=sgi: nc.scalar.activation(out=sgs[sgi][:, :T], in_=PS[bg][:, :T], func=AF.Silu),
                              reads=[gk], writes=[("sg", sgi)])
                        kb.op("dve", lambda bu=bu, sgi=sgi, fc=fc: nc.vector.tensor_tensor(
                            out=aT[s][:, fc, :T], in0=sgs[sgi][:, :T], in1=PS[bu][:, :T], op=ALU.mult),
                            reads=[uk, ("sg", sgi)], writes=[("aT", s)])
                    for co in range(8):
                        bd = 4 + (dbk[0] % 3)
                        dbk[0] += 1
                        dk = "ps%d" % bd
                        for fc in range(nf):
                            kb.op("pe", lambda fc=fc, co=co, bd=bd: nc.tensor.matmul(
                                PS[bd][:, :T], lhsT=Wd[ws][:, fc, co * 128:(co + 1) * 128], rhs=aT[s][:, fc, :T],
                                start=(fc == 0), stop=(fc == nf - 1)), reads=[("Wd", ws), ("aT", s)], writes=[dk])
                        if is_moe:
                            ti = co % 2
                            kb.op("dve", lambda co=co, bd=bd, ti=ti: nc.vector.scalar_tensor_tensor(
                                out=t4[ti][:, :T], in0=PS[bd][:, :T], scalar=mods[:, l, 40 + co, which:which + 1], in1=cb[s][:, :T],
                                op0=ALU.mult, op1=ALU.mult), reads=[dk, "mods", ("cb", s)], writes=[("t4", ti)])
                            kb.op("pool", lambda co=co, ti=ti: nc.gpsimd.tensor_tensor(
                                out=xt[:, co, :T], in0=xt[:, co, :T], in1=t4[ti][:, :T], op=ALU.add),
                                reads=[("t4", ti), ("xt", s)], writes=[("xt", s)])
                        else:
                            kb.op("dve", lambda co=co, bd=bd: nc.vector.scalar_tensor_tensor(
                                out=xt[:, co, :T], in0=PS[bd][:, :T], scalar=mods[:, l, 40 + co, which:which + 1], in1=xt[:, co, :T],
                                op0=ALU.mult, op1=ALU.add), reads=[dk, "mods", ("xt", s)], writes=[("xt", s)])
                    if last_pass and final_layer:
                        if not isctx:
                            kb.dma(sp, outT.rearrange("(k p) t -> p k t", p=128)[:, :, t0 - CL:t0 - CL + T], xt[:, :, :T],
                                   reads=[("xt", s)], semkey=("oR4", s))
                    else:
                        kb.dma(sp, R.rearrange("(k p) t -> p k t", p=128)[:, :, t0:t0 + T], xt[:, :, :T],
                               reads=[("xt", s)], writes=[("R", bi)], semkey=("oR4", s))
            kb.barrier()

    return nc


def _consts():
    c = {}
    c["c_ident"] = np.eye(128, dtype=np.float32)
    bo = np.zeros((128, 128), np.float32)
    bo[:64, :64] = 1
    bo[64:, 64:] = 1
    c["c_bones"] = bo
    P = np.zeros((128, 128), np.float32)
    for m in range(128):
        partner = m + 16 if (m % 32) < 16 else m - 16
        P[partner, m] = 1
    c["c_perm"] = P
    t = np.arange(128)
    uf = (t[:, None] <= t[None, :]).astype(np.float32)
    ub = (t[:, None] >= t[None, :]).astype(np.float32)
    c["c_uf"], c["c_ub"] = uf, ub
    c["c_negf"] = np.tile((uf - 1.0) * 30000.0, (1, 4)).astype(np.float32)
    c["c_negb"] = np.tile((ub - 1.0) * 30000.0, (1, 4)).astype(np.float32)
    pos = np.arange(L)
    row, colp = pos // 64, pos % 64
    freqs = (10000.0 ** (-np.arange(16, dtype=np.float32) / 16)).astype(np.float32)
    C = np.zeros((128, L), np.float32)
    S = np.zeros((128, L), np.float32)
    for p in range(128):
        dd = p % 64
        pp = row if dd < 32 else colp
        ang = pp.astype(np.float32) * freqs[dd % 16]
        C[p] = np.cos(ang)
        S[p] = np.sin(ang) * (-1.0 if (dd % 32) < 16 else 1.0)
    c["c_ropeC"], c["c_ropeS"] = C, S
    return c


def _prep_shared(inp):
    f = np.float32
    a = lambda v: np.ascontiguousarray(np.asarray(v, dtype=f))
    sh = {}
    sh["w_mod"] = a(inp["w_mod"])
    sh["bmodT"] = a(np.asarray(inp["b_mod"]).reshape(DEPTH, 48, 128).transpose(2, 0, 1))
    sh["g1T"] = a(np.asarray(inp["norm1_g"]).reshape(DEPTH, 8, 128).transpose(2, 0, 1))
    sh["g2T"] = a(np.asarray(inp["norm2_g"]).reshape(DEPTH, 8, 128).transpose(2, 0, 1))
    sh["w_in"] = a(inp["w_in"])
    sh["w_out"] = a(inp["w_out"])
    sh["gvg"] = a(inp["gm_v_g"])
    sh["wsT"] = a(np.asarray(inp["gm_ws"]).transpose(0, 1, 3, 2))
    p = np.arange(128)
    bsv = np.asarray(inp["gm_bs"])
    gb = np.zeros((128, DEPTH, 2, 128), f)
    for j in range(2):
        gb[:, :, j, :] = bsv[:, 2 * j + (p // 64), :].transpose(1, 0, 2)
    sh["gbs"] = gb
    sh["qgT"] = a(np.asarray(inp["att_q_g"])[:, p % 64].T)
    sh["kgT"] = a(np.asarray(inp["att_k_g"])[:, p % 64].T)
    sk = np.asarray(inp["att_sink"])
    sl = np.zeros((128, DEPTH, 2, 2), f)
    for kv in range(2):
        for ti in range(2):
            sl[:, :, kv, ti] = sk[:, 4 * kv + 2 * ti + (p // 64)].T
    sh["sinkL"] = sl
    sh["convw"] = a(np.asarray(inp["ssm_conv_w"]).reshape(DEPTH, 5, 6, 128).transpose(3, 0, 2, 1))
    sh["convb"] = a(np.asarray(inp["ssm_conv_b"]).reshape(DEPTH, 6, 128).transpose(2, 0, 1))
    sh["dtb"] = a(np.asarray(inp["ssm_dt_bias"]).reshape(DEPTH, 8))
    sh["alog"] = a(np.asarray(inp["ssm_a_log"]).reshape(DEPTH, 8))
    sh["dskE"] = a(np.repeat(np.asarray(inp["ssm_d"]), 64, axis=1))
    sh["sng"] = a(inp["ssm_norm_g"])
    sh["ffn_g"] = a(inp["ffn_w_gate"])
    sh["ffn_u"] = a(inp["ffn_w_up"])
    sh["ffn_d"] = a(inp["ffn_w_down"])
    sh["moe_r"] = a(inp["moe_router"])
    sh["moe_g"] = a(inp["moe_w_gate"])
    sh["moe_u"] = a(inp["moe_w_up"])
    sh["moe_d"] = a(inp["moe_w_down"])
    sh.update(_consts())
    return sh


def _prep_core(inp, b):
    f = np.float32
    x = np.asarray(inp["x"][b], dtype=f)
    ctx = np.asarray(inp["ctx"][b], dtype=f)
    xT0 = np.ascontiguousarray(np.concatenate([ctx.T, x.T], axis=1))
    c = np.asarray(inp["c"][b], dtype=f).reshape(8, 128).T
    cc = np.asarray(inp["c_ctx"], dtype=f).reshape(8, 128).T
    cs = np.ascontiguousarray(np.stack([c, cc], axis=-1))
    return {"xT0": xT0, "cs": cs}


_NC_CACHE = {}


def kernel(**inputs):
    if "nc" not in _NC_CACHE:
        _NC_CACHE["nc"] = build_program()
    nc = _NC_CACHE["nc"]
    sh = _prep_shared(inputs)
    in_maps = []
    for b in range(8):
        m = dict(sh)
        m.update(_prep_core(inputs, b))
        in_maps.append(m)
    res = run_bass_kernel_spmd(nc, in_maps, core_ids=list(range(8)))
    out = np.stack([np.ascontiguousarray(r["outT"].T) for r in res.results], axis=0)
    return out.astype(np.float32)
```

```python
import contextlib
import numpy as np
import concourse.bass as bass
import concourse.mybir as mybir
from concourse.bass_utils import run_bass_kernel_spmd

F32, BF16 = mybir.dt.float32, mybir.dt.bfloat16
AF = mybir.ActivationFunctionType
ALU = mybir.AluOpType
AX = mybir.AxisListType

D = 1024
L = 4096
CL = 256
NT = L + CL
NTILE = NT // 128
DEPTH = 4
DFF = 2816
NEXP = 8
EPS = 1e-6
FGROUPS = [(0, 6), (6, 6), (12, 5), (17, 5)]
BLOCKS = [(0, 256, True)] + [(256 + 512 * i, 512, False) for i in range(8)]


SIM_FRESH_POOL = False


class KB:
    def __init__(self, nc, stack):
        self.nc = nc
        self.stack = stack
        self.eng = {"pe": nc.tensor, "dve": nc.vector, "act": nc.scalar, "pool": nc.gpsimd, "sp": nc.sync}
        self.esem = {e: stack.enter_context(nc.semaphore("es_" + e)) for e in self.eng}
        self.ecnt = {e: 0 for e in self.eng}
        self.seen = {e: {} for e in self.eng}
        self.lw = {}
        self.rd = {}
        self.dsem = {}
        self.free = []
        self.nsem = 0
        self.dead = False

    def _wait(self, E, tok):
        sem, val, _ = tok
        sid = id(sem)
        if self.seen[E].get(sid, 0) >= val:
            return
        self.eng[E].wait_ge(sem, val)
        self.seen[E][sid] = val

    def _deps(self, E, reads, writes):
        for k in reads:
            w = self.lw.get(k)
            if w is not None:
                if w[2] == E and E == "pe":
                    continue
                self._wait(E, w)
        for k in writes:
            w = self.lw.get(k)
            if w is not None and w[2] != E:
                self._wait(E, w)
            for r in self.rd.get(k, {}).values():
                if r[2] != E:
                    self._wait(E, r)

    def _record(self, tok, reads, writes):
        for k in writes:
            self.lw[k] = tok
            self.rd[k] = {}
        for k in reads:
            self.rd.setdefault(k, {})[id(tok[0])] = tok

    def op(self, E, fn, reads=(), writes=()):
        if self.dead:
            return None
        self._deps(E, reads, writes)
        inst = fn()
        self.ecnt[E] += 1
        inst.then_inc(self.esem[E], 1)
        tok = (self.esem[E], self.ecnt[E], E)
        self._record(tok, reads, writes)
        return tok

    def dma(self, E, out, in_, reads=(), writes=(), semkey=None):
        if self.dead:
            return None
        if SIM_FRESH_POOL and E == "pool":
            self.nsem += 1
            semkey = ("__fresh", self.nsem)
            self.dsem[semkey] = [self.stack.enter_context(self.nc.semaphore("dp%d" % self.nsem)), 0]
        if semkey not in self.dsem:
            if self.free:
                self.dsem[semkey] = self.free.pop()
            else:
                self.nsem += 1
                self.dsem[semkey] = [self.stack.enter_context(self.nc.semaphore("ds%d" % self.nsem)), 0]
        ent = self.dsem[semkey]
        self._deps(E, reads, writes)
        if ent[1] > 0:
            self._wait(E, (ent[0], ent[1], "dma"))
        ent[1] += 16
        self.eng[E].dma_start(out=out, in_=in_).then_inc(ent[0], 16)
        tok = (ent[0], ent[1], "dma:" + str(semkey))
        self._record(tok, reads, writes)
        return tok

    def barrier(self):
        if self.dead:
            return
        for E in self.eng:
            for Fn in self.eng:
                if Fn != E and self.ecnt[Fn] > 0:
                    self._wait(E, (self.esem[Fn], self.ecnt[Fn], Fn))
            for ent in self.dsem.values():
                if ent[1] > 0:
                    self._wait(E, (ent[0], ent[1], "dma"))
        self.lw = {}
        self.rd = {}
        self.free.extend(v for k, v in self.dsem.items() if not (isinstance(k, tuple) and k and k[0] == "__fresh"))
        self.dsem = {}


class _Stop(Exception):
    pass


def build_program(nlayers=DEPTH, debug=False, stop=None):
    nc = bass.Bass("TRN2", target_bir_lowering=False)
    stack = contextlib.ExitStack()
    with stack:
        try:
            _emit(nc, stack, nlayers, debug, stop)
        except _Stop:
            pass
    return nc


def _emit(nc, stack, nlayers, debug, stop=None):
    kb = KB(nc, stack)
    _bar = kb.barrier
    _phase = [0]

    def barrier_named():
        _bar()
        _phase[0] += 1
        if stop is not None and _phase[0] >= stop:
            kb.dead = True

    kb.barrier = barrier_named

    def din(name, shape, dt=F32):
        return nc.dram_tensor(name, list(shape), dt, kind="ExternalInput").ap()

    def dscr(name, shape, dt):
        kind = "ExternalOutput" if debug else None
        if kind:
            return nc.dram_tensor(name, list(shape), dt, kind=kind).ap()
        return nc.dram_tensor(name, list(shape), dt).ap()

    xT0 = din("xT0", [D, NT])
    cs_in = din("cs", [128, 8, 2])
    w_mod = din("w_mod", [DEPTH, D, 6 * D])
    bmodT = din("bmodT", [128, DEPTH, 48])
    g1T = din("g1T", [128, DEPTH, 8])
    g2T = din("g2T", [128, DEPTH, 8])
    w_in = din("w_in", [DEPTH, D, 2312])
    w_out = din("w_out", [DEPTH, D, D])
    gvg = din("gvg", [DEPTH, 256])
    wsT = din("wsT", [DEPTH, 4, 128, 128])
    gbs = din("gbs", [128, DEPTH, 2, 128])
    qgT = din("qgT", [128, DEPTH])
    kgT = din("kgT", [128, DEPTH])
    sinkL = din("sinkL", [128, DEPTH, 2, 2])
    convw = din("convw", [128, DEPTH, 6, 5])
    convb = din("convb", [128, DEPTH, 6])
    dtb = din("dtb", [DEPTH, 8])
    alog = din("alog", [DEPTH, 8])
    dskE = din("dskE", [DEPTH, 256])
    sng = din("sng", [DEPTH, 256])
    ffn_g = din("ffn_g", [2, D, DFF])
    ffn_u = din("ffn_u", [2, D, DFF])
    ffn_d = din("ffn_d", [2, DFF, D])
    moe_r = din("moe_r", [2, D, NEXP])
    _small = nlayers < 2
    moe_g = din("moe_g", [2, NEXP, D, DFF] if not _small else [2, NEXP, 128, 768])
    moe_u = din("moe_u", [2, NEXP, D, DFF] if not _small else [2, NEXP, 128, 768])
    moe_d = din("moe_d", [2, NEXP, DFF, D] if not _small else [2, NEXP, 128, 768])
    c_ident = din("c_ident", [128, 128])
    c_bones = din("c_bones", [128, 128])
    c_perm = din("c_perm", [128, 128])
    c_uf = din("c_uf", [128, 128])
    c_ub = din("c_ub", [128, 128])
    c_negf = din("c_negf", [128, 512])
    c_negb = din("c_negb", [128, 512])
    c_ropeC = din("c_ropeC", [128, L])
    c_ropeS = din("c_ropeS", [128, L])

    outT = nc.dram_tensor("outT", [D, L], F32, kind="ExternalOutput").ap()

    R = dscr("R", [D, NT], F32)
    GU = dscr("GU", [256, NT], BF16)
    GV = dscr("GV", [NT, 256], BF16)
    QT = dscr("QT", [512, NT], BF16)
    KT = dscr("KT", [256, NT], BF16)
    V = dscr("V", [NT, 128], BF16)
    ZS = dscr("ZS", [NT, 256], BF16)
    XBC = dscr("XBC", [768, NT], BF16)
    DT = dscr("DT", [NT, 16], F32)
    MIX = dscr("MIX", [D, NT], BF16)
    HT = dscr("HT", [D, NT], BF16)
    COMBT = dscr("COMBT", [NEXP, NT], F32)

    _uniq = [0]

    def sb(st, name, shape, dt):
        _uniq[0] += 1
        return st.enter_context(nc.sbuf_tensor("%s_u%d" % (name, _uniq[0]), list(shape), dt))

    PS = [stack.enter_context(nc.psum_tensor("ps%d" % i, [128, 512], F32)) for i in range(8)]

    ident_bf = sb(stack, "ident_bf", [128, 128], BF16)
    ident_f = sb(stack, "ident_f", [128, 128], F32)
    ones_bf = sb(stack, "ones_bf", [128, 128], BF16)
    bones_bf = sb(stack, "bones_bf", [128, 128], BF16)
    perm_bf = sb(stack, "perm_bf", [128, 128], BF16)
    uf_bf = sb(stack, "uf_bf", [128, 128], BF16)
    ub_bf = sb(stack, "ub_bf", [128, 128], BF16)
    ones_f = sb(stack, "ones_f", [128, 64], F32)
    eps_t = sb(stack, "eps_t", [128, 1], F32)
    one_t = sb(stack, "one_t", [128, 1], F32)
    mods = sb(stack, "mods", [128, DEPTH, 48, 2], F32)
    G1 = sb(stack, "G1", [128, DEPTH, 8, 2], F32)
    G2 = sb(stack, "G2", [128, DEPTH, 8, 2], F32)
    g1s = sb(stack, "g1s", [128, DEPTH, 8], F32)
    g2s = sb(stack, "g2s", [128, DEPTH, 8], F32)
    bms = sb(stack, "bms", [128, DEPTH, 48], F32)
    css = sb(stack, "css", [128, 8, 2], F32)
    qgs = sb(stack, "qgs", [128, DEPTH], F32)
    kgs = sb(stack, "kgs", [128, DEPTH], F32)
    sinkE = sb(stack, "sinkE", [128, DEPTH, 2, 2], F32)
    cws = sb(stack, "cws", [128, DEPTH, 6, 5], F32)
    cbs = sb(stack, "cbs", [128, DEPTH, 6], F32)

    sp, pool = "sp", "pool"
    kb.dma(pool, ident_bf[:], c_ident, writes=["ident_bf"], semkey="c0")
    kb.dma(sp, ident_f[:], c_ident, writes=["ident_f"], semkey="c1")
    kb.dma(pool, bones_bf[:], c_bones, writes=["bones_bf"], semkey="c2")
    kb.dma(pool, perm_bf[:], c_perm, writes=["perm_bf"], semkey="c3")
    kb.dma(pool, uf_bf[:], c_uf, writes=["uf_bf"], semkey="c4")
    kb.dma(pool, ub_bf[:], c_ub, writes=["ub_bf"], semkey="c5")
    kb.dma(sp, g1s[:], g1T, writes=["g1s"], semkey="c6")
    kb.dma(sp, g2s[:], g2T, writes=["g2s"], semkey="c7")
    kb.dma(sp, bms[:], bmodT, writes=["bms"], semkey="c8")
    kb.dma(sp, css[:], cs_in, writes=["css"], semkey="c9")
    kb.dma(sp, qgs[:], qgT, writes=["qgs"], semkey="c10")
    kb.dma(sp, kgs[:], kgT, writes=["kgs"], semkey="c11")
    kb.dma(sp, sinkE[:], sinkL, writes=["sinkE"], semkey="c12")
    kb.dma(sp, cws[:], convw, writes=["cws"], semkey="c13")
    kb.dma(sp, cbs[:], convb, writes=["cbs"], semkey="c14")
    kb.op("dve", lambda: nc.vector.memset(ones_bf[:], 1.0), writes=["ones_bf"])
    kb.op("dve", lambda: nc.vector.memset(ones_f[:], 1.0), writes=["ones_f"])
    kb.op("dve", lambda: nc.vector.memset(eps_t[:], EPS), writes=["eps_t"])
    kb.op("dve", lambda: nc.vector.memset(one_t[:], 1.0), writes=["one_t"])
    kb.op("act", lambda: nc.scalar.activation(out=css[:], in_=css[:], func=AF.Silu), reads=["css"], writes=["css"])
    kb.op("act", lambda: nc.scalar.activation(out=sinkE[:], in_=sinkE[:], func=AF.Exp), reads=["sinkE"], writes=["sinkE"])

    with contextlib.ExitStack() as st:
        wm = [sb(st, "wm%d" % i, [128, 8, 512], F32) for i in range(2)]
        it = 0
        for l in range(nlayers):
            for gidx in range(12):
                s = it % 2
                it += 1
                kb.dma(sp, wm[s][:], w_mod[l].rearrange("(k p) n -> p k n", p=128)[:, :, gidx * 512:(gidx + 1) * 512],
                       writes=[("wm", s)], semkey=("wm", s))
                for jj in range(4):
                    j = gidx * 4 + jj
                    for k in range(8):
                        kb.op("pe", lambda k=k, jj=jj, j=j, s=s: nc.tensor.matmul(
                            PS[0][:, j * 2:j * 2 + 2], lhsT=wm[s][:, k, jj * 128:(jj + 1) * 128], rhs=css[:, k, :],
                            start=(k == 0), stop=(k == 7)), reads=[("wm", s), "css"], writes=["ps0"])
            kb.op("dve", lambda l=l: nc.vector.tensor_tensor(
                out=mods[:, l, :, :], in0=PS[0][:, 0:96].rearrange("p (a b) -> p a b", b=2),
                in1=bms[:, l, :].unsqueeze(2).broadcast_to([128, 48, 2]), op=ALU.add),
                reads=["ps0", "bms"], writes=["mods"])
            for (Gt, gs, off) in ((G1, g1s, 8), (G2, g2s, 32)):
                kb.op("dve", lambda Gt=Gt, off=off, l=l: nc.vector.tensor_scalar(
                    out=Gt[:, l, :, :], in0=mods[:, l, off:off + 8, :], scalar1=1.0, scalar2=None, op0=ALU.add),
                    reads=["mods"], writes=["G"])
                kb.op("dve", lambda Gt=Gt, gs=gs, l=l: nc.vector.tensor_tensor(
                    out=Gt[:, l, :, :], in0=Gt[:, l, :, :], in1=gs[:, l, :].unsqueeze(2).broadcast_to([128, 8, 2]),
                    op=ALU.mult), reads=["G", "g1s", "g2s"], writes=["G"])
        kb.barrier()

    def rmsnorm_block(st_tiles, xt, T, Gt, l, shift_off, which, out_tile, out_key, hf=None, xk="xt"):
        sq, rstd, tmp = st_tiles
        for k in range(8):
            s = k % 2
            kb.op("act", lambda k=k, s=s: nc.scalar.activation(out=sq[s][:, :T], in_=xt[:, k, :T], func=AF.Square),
                  reads=[xk], writes=[("sq", s)])
            kb.op("pe", lambda k=k, s=s: nc.tensor.matmul(PS[7][:, :T], lhsT=ones_bf[:], rhs=sq[s][:, :T],
                                                          start=(k == 0), stop=(k == 7)),
                  reads=[("sq", s), "ones_bf"], writes=["ps7"])
        kb.op("act", lambda: nc.scalar.activation(out=rstd[:, :T], in_=PS[7][:, :T], func=AF.Sqrt,
                                                  bias=eps_t[:], scale=1.0 / D), reads=["ps7", "eps_t"], writes=["rstd"])
        kb.op("dve", lambda: nc.vector.reciprocal(out=rstd[:, :T], in_=rstd[:, :T]), reads=["rstd"], writes=["rstd"])
        for k in range(8):
            s = k % 2
            kb.op("dve", lambda k=k, s=s: nc.vector.scalar_tensor_tensor(
                out=tmp[s][:, :T], in0=xt[:, k, :T], scalar=Gt[:, l, k, which:which + 1], in1=rstd[:, :T],
                op0=ALU.mult, op1=ALU.mult), reads=[xk, "G", "rstd"], writes=[("tmp", s)])
            if hf is None:
                kb.op("act", lambda k=k, s=s: nc.scalar.activation(
                    out=out_tile[:, k, :T], in_=tmp[s][:, :T], func=AF.Identity,
                    bias=mods[:, l, shift_off + k, which:which + 1], scale=1.0),
                    reads=[("tmp", s), "mods"], writes=[out_key])
            else:
                kb.op("act", lambda k=k, s=s: nc.scalar.activation(
                    out=hf[:, k, :T], in_=tmp[s][:, :T], func=AF.Identity,
                    bias=mods[:, l, shift_off + k, which:which + 1], scale=1.0),
                    reads=[("tmp", s), "mods"], writes=["hf"])
                kb.op("pool", lambda k=k: nc.gpsimd.tensor_copy(out=out_tile[:, k, :T], in_=hf[:, k, :T]),
                      reads=["hf"], writes=[out_key])

    for l in range(nlayers):
        need_ctx = l < DEPTH - 1
        is_moe = (l % 2 == 1)
        jl = l // 2
        xsrc = xT0 if l == 0 else R

        with contextlib.ExitStack() as st:
            Wfm = sb(st, "Wfm", [128, 8, 1792], BF16)
            Wtm = sb(st, "Wtm", [128, 8, 648], BF16)
            xts = [sb(st, "xt%d" % i, [128, 8, 512], F32) for i in range(2)]
            hTs = [sb(st, "hT%d" % i, [128, 8, 512], BF16) for i in range(2)]
            sq = [sb(st, "sq%d" % i, [128, 512], BF16) for i in range(2)]
            tmp = [sb(st, "tmp%d" % i, [128, 512], F32) for i in range(2)]
            rstd = sb(st, "rstd", [128, 512], F32)
            stgF = [sb(st, "stgF%d" % i, [128, 14, 512], BF16) for i in range(2)]
            stgGV = [sb(st, "stgGV%d" % i, [128, 4, 256], BF16) for i in range(2)]
            stgV = [sb(st, "stgV%d" % i, [128, 4, 128], BF16) for i in range(2)]
            stgZ = [sb(st, "stgZ%d" % i, [128, 4, 256], BF16) for i in range(2)]
            stgDT = [sb(st, "stgDT%d" % i, [128, 4, 16], F32) for i in range(2)]
            ropeC = sb(st, "ropeC", [128, L], F32)
            ropeS = sb(st, "ropeS", [128, L], F32)
            gvB = sb(st, "gvB", [128, 256], F32)
            dtbB = sb(st, "dtbB", [128, 8], F32)
            aB = sb(st, "aB", [128, 8], F32)
            qsq = [sb(st, "qsq%d" % i, [128, 512], BF16) for i in range(2)]
            qrn = [sb(st, "qrn%d" % i, [128, 512], F32) for i in range(2)]
            qn = [sb(st, "qn%d" % i, [128, 512], BF16) for i in range(2)]
            qt1 = [sb(st, "qt1%d" % i, [128, 512], F32) for i in range(2)]
            qt2 = [sb(st, "qt2%d" % i, [128, 512], F32) for i in range(2)]
            gl = [sb(st, "gl%d" % i, [128, 256], F32) for i in range(2)]
            gsq = [sb(st, "gsq%d" % i, [128, 256], F32) for i in range(2)]
            gss = [sb(st, "gss%d" % i, [128, 4], F32) for i in range(2)]
            dtx = [sb(st, "dtx%d" % i, [128, 8], F32) for i in range(2)]

            wv = w_in[l].rearrange("(k p) n -> p k n", p=128)
            kb.dma(pool, Wfm[:, :, 0:256], wv[:, :, 0:256], writes=["Wfm"], semkey="wf0")
            kb.dma(pool, Wfm[:, :, 256:768], wv[:, :, 512:1024], writes=["Wfm"], semkey="wf1")
            for kvh in range(2):
                for dup in range(2):
                    c0 = 768 + kvh * 128 + dup * 64
                    kb.dma(pool, Wfm[:, :, c0:c0 + 64], wv[:, :, 1024 + kvh * 64:1024 + (kvh + 1) * 64],
                           writes=["Wfm"], semkey="wf2_%d%d" % (kvh, dup))
            kb.dma(pool, Wfm[:, :, 1024:1792], wv[:, :, 1536:2304], writes=["Wfm"], semkey="wf3")
            kb.dma(pool, Wtm[:, :, 0:256], wv[:, :, 256:512], writes=["Wtm"], semkey="wt0")
            kb.dma(pool, Wtm[:, :, 256:384], wv[:, :, 1152:1280], writes=["Wtm"], semkey="wt1")
            kb.dma(pool, Wtm[:, :, 384:392], wv[:, :, 2304:2312], writes=["Wtm"], semkey="wt2")
            kb.dma(pool, Wtm[:, :, 392:648], wv[:, :, 1280:1536], writes=["Wtm"], semkey="wt3")
            kb.dma(sp, ropeC[:], c_ropeC, writes=["ropeC"], semkey="rc")
            kb.dma(sp, ropeS[:], c_ropeS, writes=["ropeS"], semkey="rs")
            kb.dma(sp, gvB[:], gvg[l].partition_broadcast(128), writes=["gvB"], semkey="gvB")
            kb.dma(sp, dtbB[:], dtb[l].partition_broadcast(128), writes=["dtbB"], semkey="dtbB")
            kb.dma(sp, aB[:], alog[l].partition_broadcast(128), writes=["aB"], semkey="aB")
            kb.op("act", lambda: nc.scalar.activation(out=aB[:], in_=aB[:], func=AF.Exp), reads=["aB"], writes=["aB"])
            kb.op("dve", lambda: nc.vector.tensor_scalar(out=aB[:], in0=aB[:], scalar1=-1.0, scalar2=None, op0=ALU.mult),
                  reads=["aB"], writes=["aB"])

            bank_rot = [0]

            def next_bank():
                b = bank_rot[0] % 5
                bank_rot[0] += 1
                return b

            def load1(bi):
                t0, T, isctx = BLOCKS[bi]
                s = bi % 2
                kb.dma(sp, xts[s][:, :, :T], xsrc.rearrange("(k p) t -> p k t", p=128)[:, :, t0:t0 + T],
                       writes=[("xt", s)], semkey=("xt", s))

            def norm1(bi):
                t0, T, isctx = BLOCKS[bi]
                s = bi % 2
                which = 1 if isctx else 0
                xt, hT = xts[s], hTs[s]
                for k in range(8):
                    s2 = k % 2
                    kb.op("act", lambda k=k, s2=s2: nc.scalar.activation(out=sq[s2][:, :T], in_=xt[:, k, :T], func=AF.Square),
                          reads=[("xt", s)], writes=[("sq", s2)])
                    kb.op("pe", lambda k=k, s2=s2: nc.tensor.matmul(PS[7][:, :T], lhsT=ones_bf[:], rhs=sq[s2][:, :T],
                                                                    start=(k == 0), stop=(k == 7)),
                          reads=[("sq", s2), "ones_bf"], writes=["ps7"])
                kb.op("act", lambda: nc.scalar.activation(out=rstd[:, :T], in_=PS[7][:, :T], func=AF.Sqrt,
                                                          bias=eps_t[:], scale=1.0 / D),
                      reads=["ps7", "eps_t"], writes=["rstd"])
                kb.op("dve", lambda: nc.vector.reciprocal(out=rstd[:, :T], in_=rstd[:, :T]), reads=["rstd"], writes=["rstd"])
                for k in range(8):
                    s2 = k % 2
                    kb.op("dve", lambda k=k, s2=s2: nc.vector.scalar_tensor_tensor(
                        out=tmp[s2][:, :T], in0=xt[:, k, :T], scalar=G1[:, l, k, which:which + 1], in1=rstd[:, :T],
                        op0=ALU.mult, op1=ALU.mult), reads=[("xt", s), "G", "rstd"], writes=[("tmp", s2)])
                    kb.op("act", lambda k=k, s2=s2: nc.scalar.activation(
                        out=hT[:, k, :T], in_=tmp[s2][:, :T], func=AF.Identity,
                        bias=mods[:, l, k, which:which + 1], scale=1.0),
                        reads=[("tmp", s2), "mods"], writes=[("hT", s)])

            load1(0)
            load1(1)
            norm1(0)
            for bi, (t0, T, isctx) in enumerate(BLOCKS):
                s = bi % 2
                which = 1 if isctx else 0
                xt, hT = xts[s], hTs[s]
                if bi + 2 < len(BLOCKS):
                    load1(bi + 2)
                sF = stgF[s]
                stageB, stageC = {}, {}

                def chunkA(ci, T=T, hT=hT, s=s, sF=sF, isctx=isctx, t0=t0):
                    b = next_bank()
                    pk = "ps%d" % b
                    for k in range(8):
                        kb.op("pe", lambda k=k: nc.tensor.matmul(
                            PS[b][:, :T], lhsT=Wfm[:, k, ci * 128:(ci + 1) * 128], rhs=hT[:, k, :T],
                            start=(k == 0), stop=(k == 7)), reads=["Wfm", ("hT", s)], writes=[pk])
                    ok = ("stgF", s, ci)
                    if ci < 2:
                        kb.op("act", lambda: nc.scalar.activation(out=sF[:, ci, :T], in_=PS[b][:, :T], func=AF.Gelu_apprx_tanh),
                              reads=[pk], writes=[ok])
                        return
                    if ci >= 8:
                        kb.op("dve", lambda: nc.vector.tensor_copy(out=sF[:, ci, :T], in_=PS[b][:, :T]), reads=[pk], writes=[ok])
                        return
                    qs = ci % 2
                    gcol = qgs if ci < 6 else kgs
                    kb.op("act", lambda: nc.scalar.activation(out=qsq[qs][:, :T], in_=PS[b][:, :T], func=AF.Square),
                          reads=[pk], writes=[("qsq", qs)])

                    def B():
                        kb.op("pe", lambda: nc.tensor.matmul(PS[5][:, :T], lhsT=bones_bf[:], rhs=qsq[qs][:, :T], start=True, stop=True),
                              reads=[("qsq", qs), "bones_bf"], writes=["ps5"])
                        kb.op("act", lambda: nc.scalar.activation(out=qrn[qs][:, :T], in_=PS[5][:, :T], func=AF.Sqrt,
                                                                  bias=eps_t[:], scale=1.0 / 64),
                              reads=["ps5", "eps_t"], writes=[("qrn", qs)])
                        kb.op("dve", lambda: nc.vector.reciprocal(out=qrn[qs][:, :T], in_=qrn[qs][:, :T]),
                              reads=[("qrn", qs)], writes=[("qrn", qs)])
                        if isctx:
                            kb.op("dve", lambda: nc.vector.scalar_tensor_tensor(
                                out=sF[:, ci, :T], in0=PS[b][:, :T], scalar=gcol[:, l:l + 1], in1=qrn[qs][:, :T],
                                op0=ALU.mult, op1=ALU.mult), reads=[pk, ("qrn", qs), "qgs", "kgs"], writes=[ok])
                        else:
                            kb.op("dve", lambda: nc.vector.scalar_tensor_tensor(
                                out=qn[qs][:, :T], in0=PS[b][:, :T], scalar=gcol[:, l:l + 1], in1=qrn[qs][:, :T],
                                op0=ALU.mult, op1=ALU.mult), reads=[pk, ("qrn", qs), "qgs", "kgs"], writes=[("qn", qs)])

                    def C():
                        lt0 = t0 - CL
                        kb.op("pe", lambda: nc.tensor.matmul(PS[6][:, :T], lhsT=perm_bf[:], rhs=qn[qs][:, :T], start=True, stop=True),
                              reads=[("qn", qs), "perm_bf"], writes=["ps6"])
                        kb.op("pool", lambda: nc.gpsimd.tensor_tensor(
                            out=qt1[qs][:, :T], in0=qn[qs][:, :T], in1=ropeC[:, lt0:lt0 + T], op=ALU.mult),
                            reads=[("qn", qs), "ropeC"], writes=[("qt1", qs)])
                        kb.op("dve", lambda: nc.vector.tensor_tensor(
                            out=qt2[qs][:, :T], in0=PS[6][:, :T], in1=ropeS[:, lt0:lt0 + T], op=ALU.mult),
                            reads=["ps6", "ropeS"], writes=[("qt2", qs)])
                        kb.op("pool", lambda: nc.gpsimd.tensor_tensor(
                            out=sF[:, ci, :T], in0=qt1[qs][:, :T], in1=qt2[qs][:, :T], op=ALU.add),
                            reads=[("qt1", qs), ("qt2", qs)], writes=[ok])

                    stageB[ci] = B
                    if not isctx:
                        stageC[ci] = C

                for ci in range(16):
                    if ci < 14:
                        chunkA(ci)
                    if ci == 9 and bi + 1 < len(BLOCKS):
                        norm1(bi + 1)
                    if ci - 1 in stageB:
                        stageB.pop(ci - 1)()
                    if ci - 2 in stageC:
                        stageC.pop(ci - 2)()
                assert not stageB and not stageC
                kb.dma(sp, GU.rearrange("(c p) t -> p c t", p=128)[:, :, t0:t0 + T], sF[:, 0:2, :T],
                       reads=[("stgF", s, ci) for ci in (0, 1)], semkey=("oGU", s))
                kb.dma(sp, QT.rearrange("(c p) t -> p c t", p=128)[:, :, t0:t0 + T], sF[:, 2:6, :T],
                       reads=[("stgF", s, ci) for ci in (2, 3, 4, 5)], semkey=("oQT", s))
                kb.dma(sp, KT.rearrange("(c p) t -> p c t", p=128)[:, :, t0:t0 + T], sF[:, 6:8, :T],
                       reads=[("stgF", s, ci) for ci in (6, 7)], semkey=("oKT", s))
                kb.dma(sp, XBC.rearrange("(c p) t -> p c t", p=128)[:, :, t0:t0 + T], sF[:, 8:14, :T],
                       reads=[("stgF", s, ci) for ci in range(8, 14)], semkey=("oXB", s))

                nsub = T // 128
                for sub in range(nsub):
                    ss_ = sub % 2
                    ba = next_bank()
                    bb = next_bank()
                    pka, pkb = "ps%d" % ba, "ps%d" % bb
                    for k in range(8):
                        kb.op("pe", lambda k=k, sub=sub, ba=ba: nc.tensor.matmul(
                            PS[ba][:, 0:392], lhsT=hT[:, k, sub * 128:(sub + 1) * 128], rhs=Wtm[:, k, 0:392],
                            start=(k == 0), stop=(k == 7)), reads=["Wtm", ("hT", s)], writes=[pka])
                    for k in range(8):
                        kb.op("pe", lambda k=k, sub=sub, bb=bb: nc.tensor.matmul(
                            PS[bb][:, 0:256], lhsT=hT[:, k, sub * 128:(sub + 1) * 128], rhs=Wtm[:, k, 392:648],
                            start=(k == 0), stop=(k == 7)), reads=["Wtm", ("hT", s)], writes=[pkb])
                    kb.op("act", lambda ba=ba, ss_=ss_: nc.scalar.activation(out=gl[ss_][:], in_=PS[ba][:, 0:256],
                                                                             func=AF.Gelu_apprx_tanh),
                          reads=[pka], writes=[("gl", ss_)])
                    kb.op("pool", lambda ss_=ss_: nc.gpsimd.tensor_tensor(out=gsq[ss_][:], in0=gl[ss_][:], in1=gl[ss_][:], op=ALU.mult),
                          reads=[("gl", ss_)], writes=[("gsq", ss_)])
                    kb.op("dve", lambda ss_=ss_: nc.vector.tensor_reduce(
                        out=gss[ss_][:], in_=gsq[ss_][:].rearrange("p (a b) -> p a b", b=64), axis=AX.X, op=ALU.add),
                        reads=[("gsq", ss_)], writes=[("gss", ss_)])
                    kb.op("act", lambda ss_=ss_: nc.scalar.activation(out=gss[ss_][:], in_=gss[ss_][:], func=AF.Sqrt,
                                                                      bias=eps_t[:], scale=1.0 / 64),
                          reads=[("gss", ss_), "eps_t"], writes=[("gss", ss_)])
                    kb.op("dve", lambda ss_=ss_: nc.vector.reciprocal(out=gss[ss_][:], in_=gss[ss_][:]),
                          reads=[("gss", ss_)], writes=[("gss", ss_)])
                    kb.op("dve", lambda ss_=ss_: nc.vector.tensor_tensor(
                        out=gsq[ss_][:].rearrange("p (a b) -> p a b", b=64), in0=gl[ss_][:].rearrange("p (a b) -> p a b", b=64),
                        in1=gss[ss_][:].unsqueeze(2).broadcast_to([128, 4, 64]), op=ALU.mult),
                        reads=[("gl", ss_), ("gss", ss_)], writes=[("gsq", ss_)])
                    kb.op("pool", lambda ss_=ss_, sub=sub: nc.gpsimd.tensor_tensor(
                        out=stgGV[s][:, sub, :], in0=gsq[ss_][:], in1=gvB[:], op=ALU.mult),
                        reads=[("gsq", ss_), "gvB"], writes=[("stgGV", s)])
                    kb.op("act", lambda ba=ba, sub=sub: nc.scalar.copy(out=stgV[s][:, sub, :], in_=PS[ba][:, 256:384]),
                          reads=[pka], writes=[("stgV", s)])
                    kb.op("dve", lambda ba=ba, ss_=ss_: nc.vector.tensor_tensor(out=dtx[ss_][:], in0=PS[ba][:, 384:392], in1=dtbB[:],
                                                                               op=ALU.add),
                          reads=[pka, "dtbB"], writes=[("dtx", ss_)])
                    kb.op("act", lambda ss_=ss_: nc.scalar.activation(out=dtx[ss_][:], in_=dtx[ss_][:], func=AF.Exp),
                          reads=[("dtx", ss_)], writes=[("dtx", ss_)])
                    kb.op("act", lambda ss_=ss_, sub=sub: nc.scalar.activation(out=stgDT[s][:, sub, 0:8], in_=dtx[ss_][:], func=AF.Ln,
                                                                               bias=one_t[:], scale=1.0),
                          reads=[("dtx", ss_), "one_t"], writes=[("stgDT", s)])
                    kb.op("dve", lambda sub=sub: nc.vector.tensor_tensor(out=stgDT[s][:, sub, 8:16], in0=stgDT[s][:, sub, 0:8],
                                                                         in1=aB[:], op=ALU.mult),
                          reads=[("stgDT", s), "aB"], writes=[("stgDT", s)])
                    kb.op("act", lambda bb=bb, sub=sub: nc.scalar.activation(out=stgZ[s][:, sub, :], in_=PS[bb][:, 0:256], func=AF.Silu),
                          reads=[pkb], writes=[("stgZ", s)])
                kb.dma(sp, GV[t0:t0 + T, :].rearrange("(s p) c -> p s c", p=128), stgGV[s][:, 0:nsub, :],
                       reads=[("stgGV", s)], semkey=("oGV", s))
                kb.dma(sp, V[t0:t0 + T, :].rearrange("(s p) c -> p s c", p=128), stgV[s][:, 0:nsub, :],
                       reads=[("stgV", s)], semkey=("oV", s))
                kb.dma(sp, ZS[t0:t0 + T, :].rearrange("(s p) c -> p s c", p=128), stgZ[s][:, 0:nsub, :],
                       reads=[("stgZ", s)], semkey=("oZS", s))
                kb.dma(sp, DT[t0:t0 + T, :].rearrange("(s p) c -> p s c", p=128), stgDT[s][:, 0:nsub, :],
                       reads=[("stgDT", s)], semkey=("oDT", s))
            kb.barrier()

        with contextlib.ExitStack() as st:
            wsb = sb(st, "wsb", [128, 4, 128], BF16)
            bsr = sb(st, "bsr", [128, 2, 128], F32)
            gtmp = [sb(st, "gtmp%d" % i, [128, 2, 128], F32) for i in range(2)]
            guT = [sb(st, "guT%d" % i, [128, 2, 512], BF16) for i in range(2)]
            vh = [sb(st, "vh%d" % i, [128, 4, 256], BF16) for i in range(2)]
            stg = [sb(st, "gstg%d" % i, [128, 2, 512], BF16) for i in range(2)]
            kb.dma(pool, wsb[:], wsT[l].rearrange("h j i -> j h i"), writes=["wsb"], semkey="wsb")
            kb.dma(sp, bsr[:], gbs[:, l, :, :], writes=["bsr"], semkey="bsr")
            blks = [b for b in BLOCKS if (need_ctx or not b[2])]
            def load2a(bi):
                t0, T, isctx = blks[bi]
                s = bi % 2
                nsub = T // 128
                kb.dma(sp, guT[s][:, :, :T], GU.rearrange("(c p) t -> p c t", p=128)[:, :, t0:t0 + T],
                       writes=[("guT", s)], semkey=("guT", s))
                kb.dma(sp, vh[s][:, 0:nsub, :], GV[t0:t0 + T, :].rearrange("(s p) c -> p s c", p=128),
                       writes=[("vh", s)], semkey=("vh", s))

            load2a(0)
            for bi, (t0, T, isctx) in enumerate(blks):
                s = bi % 2
                nsub = T // 128
                if bi + 1 < len(blks):
                    load2a(bi + 1)
                for sub in range(nsub):
                    b = sub % 4
                    pk = "ps%d" % b
                    for j in range(2):
                        for hh in range(2):
                            h = 2 * j + hh
                            kb.op("pe", lambda b=b, j=j, hh=hh, h=h, sub=sub: nc.tensor.matmul(
                                PS[b][hh * 64:(hh + 1) * 64, j * 128:(j + 1) * 128], lhsT=vh[s][:, sub, h * 64:(h + 1) * 64],
                                rhs=wsb[:, h, :], start=True, stop=True), reads=[("vh", s), "wsb"], writes=[pk])
                    g2 = sub % 2
                    kb.op("dve", lambda b=b, g2=g2: nc.vector.tensor_tensor(
                        out=gtmp[g2][:], in0=PS[b][:, 0:256].rearrange("p (a b) -> p a b", b=128),
                        in1=bsr[:], op=ALU.add), reads=[pk, "bsr"], writes=[("gtmp", g2)])
                    kb.op("pool", lambda sub=sub, g2=g2: nc.gpsimd.tensor_tensor(
                        out=stg[s][:, :, sub * 128:(sub + 1) * 128], in0=gtmp[g2][:],
                        in1=guT[s][:, :, sub * 128:(sub + 1) * 128], op=ALU.mult),
                        reads=[("gtmp", g2), ("guT", s)], writes=[("gstg", s)])
                kb.dma(sp, MIX.rearrange("(c p) t -> p c t", p=128)[:, 0:2, t0:t0 + T], stg[s][:, :, :T],
                       reads=[("gstg", s)], semkey=("oMIXg", s))
            kb.barrier()

        with contextlib.ExitStack() as st:
            QTs = sb(st, "QTs", [128, 4, NT], BF16)
            KTz = sb(st, "KTz", [128, 2, 2, NT], BF16)
            Vs = sb(st, "Vs", [128, NTILE, 128], BF16)
            onesv = sb(st, "onesv", [128, 64], BF16)
            Es = [sb(st, "E%d" % i, [128, 512], BF16) for i in range(4)]
            den = [sb(st, "den%d" % i, [128, 256], F32) for i in range(2)]
            stg = [sb(st, "astg%d" % i, [128, 4, 512], BF16) for i in range(2)]
            kb.op("dve", lambda: nc.vector.memset(onesv[:], 1.0), writes=["onesv"])
            for c4 in range(4):
                kb.dma(sp, QTs[:, c4, :], QT[c4 * 128:(c4 + 1) * 128, :], writes=["QTs"], semkey=("QTs", c4))
            kb.op("pool", lambda: nc.gpsimd.memset(KTz[:], 0.0), writes=["KTs"])
            for c2 in range(2):
                for half in range(2):
                    kb.dma(sp, KTz[half * 64:(half + 1) * 64, c2, half, :],
                           KT[c2 * 128 + half * 64:c2 * 128 + (half + 1) * 64, :], writes=["KTs"], semkey=("KTs", c2, half))
            for c8 in range(0, NTILE, 6):
                c9 = min(NTILE, c8 + 6)
                kb.dma(sp, Vs[:, c8:c9, :], V[c8 * 128:c9 * 128, :].rearrange("(s p) c -> p s c", p=128),
                       writes=["Vs"], semkey=("Vs", c8))
            srot = [0]
            od = [0]
            blks = [b for b in BLOCKS if (need_ctx or not b[2])]
            for bi, (t0, T, isctx) in enumerate(blks):
                s = bi % 2
                nsub = T // 128
                for sub in range(nsub):
                    tq = t0 // 128 + sub
                    if isctx:
                        kts = [(0, None), (1, None)]
                    else:
                        n = tq - 2
                        kts = [(0, None), (1, None)]
                        if n > 0:
                            kts.append((tq - 1, ub_bf))
                        kts.append((tq, None))
                        if n < 31:
                            kts.append((tq + 1, uf_bf))
                    for kv in range(2):
                        ob = 4 + (od[0] % 2)
                        db = 6 + (od[0] % 2)
                        od[0] += 1
                        okey, dkey = "ps%d" % ob, "ps%d" % db
                        slots = {}

                        def emitS(i, kv=kv, tq=tq, kts=kts, slots=slots):
                            kt, msk = kts[i]
                            sl = srot[0] % 4
                            srot[0] += 1
                            slots[i] = sl
                            pk = "ps%d" % sl
                            for half in range(2):
                                kb.op("pe", lambda half=half, sl=sl, kt=kt: nc.tensor.matmul(
                                    PS[sl][:, half * 256:(half + 1) * 256].rearrange("p (a b) -> p a b", b=128),
                                    lhsT=KTz[:, kv, half, kt * 128:(kt + 1) * 128],
                                    rhs=QTs[:, 2 * kv:2 * kv + 2, tq * 128:(tq + 1) * 128],
                                    start=True, stop=True), reads=["KTs", "QTs"], writes=[pk])
                            kb.op("act", lambda sl=sl: nc.scalar.activation(out=Es[sl][:], in_=PS[sl][:], func=AF.Exp, scale=0.125),
                                  reads=[pk], writes=[("E", sl)])
                            if msk is not None:
                                kb.op("pool", lambda sl=sl, msk=msk: nc.gpsimd.tensor_tensor(
                                    out=Es[sl][:].rearrange("p (a b) -> p a b", b=128),
                                    in0=Es[sl][:].rearrange("p (a b) -> p a b", b=128),
                                    in1=msk[:].unsqueeze(1).broadcast_to([128, 4, 128]), op=ALU.mult),
                                    reads=[("E", sl), "uf_bf", "ub_bf"], writes=[("E", sl)])

                        def emitPV(i, kv=kv, kts=kts, slots=slots, ob=ob, db=db, okey=okey, dkey=dkey):
                            kt, _ = kts[i]
                            sl = slots[i]
                            first, last = (i == 0), (i == len(kts) - 1)
                            for half in range(2):
                                kb.op("pe", lambda half=half, sl=sl, kt=kt: nc.tensor.matmul(
                                    PS[ob][half * 64:(half + 1) * 64, 0:256], lhsT=Vs[:, kt, kv * 64:(kv + 1) * 64],
                                    rhs=Es[sl][:, half * 256:(half + 1) * 256], start=first, stop=last),
                                    reads=["Vs", ("E", sl)], writes=[okey])
                                kb.op("pe", lambda half=half, sl=sl: nc.tensor.matmul(
                                    PS[db][half * 64:(half + 1) * 64, 0:256], lhsT=onesv[:],
                                    rhs=Es[sl][:, half * 256:(half + 1) * 256], start=first, stop=last),
                                    reads=["onesv", ("E", sl)], writes=[dkey])

                        nk = len(kts)
                        emitS(0)
                        if nk > 1:
                            emitS(1)
                        for i in range(nk):
                            emitPV(i)
                            if i + 2 < nk:
                                emitS(i + 2)
                        dn = den[kv]
                        kb.op("dve", lambda db=db, dn=dn, kv=kv: nc.vector.tensor_tensor(
                            out=dn[:].rearrange("p (a b) -> p a b", b=128),
                            in0=PS[db][:, 0:256].rearrange("p (a b) -> p a b", b=128),
                            in1=sinkE[:, l, kv, :].unsqueeze(2).broadcast_to([128, 2, 128]), op=ALU.add),
                            reads=[dkey, "sinkE"], writes=[("den", kv)])
                        kb.op("dve", lambda dn=dn: nc.vector.reciprocal(out=dn[:], in_=dn[:]),
                              reads=[("den", kv)], writes=[("den", kv)])
                        kb.op("dve", lambda ob=ob, dn=dn, kv=kv, sub=sub: nc.vector.tensor_tensor(
                            out=stg[s][:, 2 * kv:2 * kv + 2, sub * 128:(sub + 1) * 128],
                            in0=PS[ob][:, 0:256].rearrange("p (a b) -> p a b", b=128),
                            in1=dn[:].rearrange("p (a b) -> p a b", b=128), op=ALU.mult),
                            reads=[okey, ("den", kv)], writes=[("astg", s)])
                kb.dma(sp, MIX.rearrange("(c p) t -> p c t", p=128)[:, 2:6, t0:t0 + T], stg[s][:, :, :T],
                       reads=[("astg", s)], semkey=("oMIXa", s))
            kb.barrier()

        with contextlib.ExitStack() as st:
            XC = sb(st, "XC", [128, 6, NT], BF16)
            Xtm = sb(st, "Xtm", [128, NTILE, 256], BF16)
            Btm = sb(st, "Btm", [128, NTILE, 256], BF16)
            Ytm = sb(st, "Ytm", [128, NTILE, 256], F32)
            DTs = sb(st, "DTs", [128, NTILE, 16], F32)
            ZSs = sb(st, "ZSs", [128, NTILE, 256], BF16)
            Sf = sb(st, "Sf", [128, 256], F32)
            Sb_ = sb(st, "Sb", [128, 256], BF16)
            negf = sb(st, "negf", [128, 512], BF16)
            negb = sb(st, "negb", [128, 512], BF16)
            dskB = sb(st, "dskB", [128, 256], F32)
            ngB = sb(st, "ngB", [128, 256], F32)
            XB = [sb(st, "XB%d" % i, [128, 6, 516], BF16) for i in range(2)]
            cacc = [sb(st, "cacc%d" % i, [128, 512], F32) for i in range(2)]
            rhsA = [sb(st, "rhsA%d" % i, [128, 512], BF16) for i in range(2)]
            adtb = [sb(st, "adtb%d" % i, [128, 4], BF16) for i in range(2)]
            col = [sb(st, "col%d" % i, [128, 4], F32) for i in range(2)]
            Dm = [sb(st, "Dm%d" % i, [128, 512], F32) for i in range(2)]
            Lm = [sb(st, "Lm%d" % i, [128, 512], BF16) for i in range(2)]
            MT = [sb(st, "MT%d" % i, [128, 512], BF16) for i in range(2)]
            sm = [sb(st, "sm%d" % i, [128, 16], F32) for i in range(2)]
            xw = [sb(st, "xw%d" % i, [128, 256], BF16) for i in range(2)]
            xdt = [sb(st, "xdt%d" % i, [128, 256], BF16) for i in range(2)]
            yt1 = [sb(st, "yt1%d" % i, [128, 256], F32) for i in range(2)]
            yt2 = [sb(st, "yt2%d" % i, [128, 256], F32) for i in range(2)]
            tS = sb(st, "tS", [128, 256], F32)
            Sf2 = sb(st, "Sf2", [128, 256], F32)
            Sb2 = sb(st, "Sb2", [128, 256], BF16)
            tS2 = sb(st, "tS2", [128, 256], F32)
            yz = [sb(st, "yz%d" % i, [128, 256], F32) for i in range(2)]
            yjunk = sb(st, "yjunk", [128, 256], F32)
            yss = [sb(st, "yss%d" % i, [128, 1], F32) for i in range(2)]
            yo = [sb(st, "yo%d" % i, [128, 256], BF16) for i in range(2)]
            stg = [sb(st, "sstg%d" % i, [128, 2, 512], BF16) for i in range(2)]

            kb.dma(pool, negf[:], c_negf, writes=["negf"], semkey="negf")
            kb.dma(pool, negb[:], c_negb, writes=["negb"], semkey="negb")
            kb.dma(sp, dskB[:], dskE[l].partition_broadcast(128), writes=["dskB"], semkey="dskB")
            kb.dma(sp, ngB[:], sng[l].partition_broadcast(128), writes=["ngB"], semkey="ngB")
            for c8 in range(0, NTILE, 6):
                c9 = min(NTILE, c8 + 6)
                kb.dma(sp, DTs[:, c8:c9, :], DT[c8 * 128:c9 * 128, :].rearrange("(s p) c -> p s c", p=128),
                       writes=["DTs"], semkey=("DTs", c8))
                kb.dma(sp, ZSs[:, c8:c9, :], ZS[c8 * 128:c9 * 128, :].rearrange("(s p) c -> p s c", p=128),
                       writes=["ZSs"], semkey=("ZSs", c8))

            def loadxb(bi):
                t0, T, isctx = BLOCKS[bi]
                s = bi % 2
                seg0, seg1 = (0, CL) if isctx else (CL, NT)
                lo, hi = max(t0 - 2, seg0), min(t0 + T + 2, seg1)
                xb = XB[s]
                wr = [("XB", s)]
                if lo > t0 - 2:
                    kb.op("pool", lambda xb=xb: nc.gpsimd.memset(xb[:, :, 0:2], 0.0), writes=wr)
                if hi < t0 + T + 2:
                    kb.op("pool", lambda xb=xb, T=T: nc.gpsimd.memset(xb[:, :, T + 2:T + 4], 0.0), writes=wr)
                kb.dma(sp, xb[:, :, lo - (t0 - 2):hi - (t0 - 2)], XBC.rearrange("(c p) t -> p c t", p=128)[:, :, lo:hi],
                       writes=wr, semkey=("XB", s))

            loadxb(0)
            for bi, (t0, T, isctx) in enumerate(BLOCKS):
                s = bi % 2
                xb = XB[s]
                wr = [("XB", s)]
                if bi + 1 < len(BLOCKS):
                    loadxb(bi + 1)
                for j in range(6):
                    a = cacc[j % 2]
                    ak = ("cacc", j % 2)
                    kb.op("dve", lambda j=j, a=a, xb=xb, T=T: nc.vector.tensor_scalar(
                        out=a[:, :T], in0=xb[:, j, 0:T], scalar1=cws[:, l, j, 0:1], scalar2=None, op0=ALU.mult),
                        reads=wr + ["cws"], writes=[ak])
                    for k in range(1, 5):
                        kb.op("dve", lambda j=j, a=a, xb=xb, T=T, k=k: nc.vector.scalar_tensor_tensor(
                            out=a[:, :T], in0=xb[:, j, k:k + T], scalar=cws[:, l, j, k:k + 1], in1=a[:, :T],
                            op0=ALU.mult, op1=ALU.add), reads=wr + ["cws", ak], writes=[ak])
                    kb.op("act", lambda j=j, a=a, T=T, t0=t0: nc.scalar.activation(
                        out=XC[:, j, t0:t0 + T], in_=a[:, :T], func=AF.Silu, bias=cbs[:, l, j:j + 1], scale=1.0),
                        reads=[ak, "cbs"], writes=[("XC", bi)])
            kb.barrier()

            for c in range(NTILE):
                b = 6 + (c % 2)
                pk = "ps%d" % b
                psb = PS[b][:].bitcast(BF16)
                for q4, j in enumerate((0, 1, 2, 3)):
                    kb.op("pe", lambda q4=q4, j=j, c=c, psb=psb: nc.tensor.transpose(
                        out=psb[:, q4 * 128:(q4 + 1) * 128], in_=XC[:, j, c * 128:(c + 1) * 128], identity=ident_bf[:]),
                        reads=["XC", "ident_bf"], writes=[pk])
                kb.op("act", lambda c=c, psb=psb: nc.scalar.copy(out=Xtm[:, c, :], in_=psb[:, 0:256]), reads=[pk], writes=[("Xtm", c)])
                kb.op("act", lambda c=c, psb=psb: nc.scalar.copy(out=Btm[:, c, :], in_=psb[:, 256:512]), reads=[pk], writes=[("Btm", c)])
                kb.op("pool", lambda c=c: nc.gpsimd.tensor_tensor(out=Ytm[:, c, :], in0=Xtm[:, c, :], in1=dskB[:], op=ALU.mult),
                      reads=[("Xtm", c), "dskB"], writes=[("Ytm", c)])

            kb.barrier()
            it = [0]
            Sfs, Sbs, tSs = [Sf, Sf2], [Sb_, Sb2], [tS, tS2]
            for d in range(2):
                kb.op("dve", lambda d=d: nc.vector.memset(Sfs[d][:], 0.0), writes=[("Sf", d)])
                kb.op("pool", lambda d=d: nc.gpsimd.memset(Sbs[d][:], 0.0), writes=[("Sb", d)])

            def scan_step(d, c):
                U = uf_bf if d == 0 else ub_bf
                NEG = negf if d == 0 else negb
                last = 127 if d == 0 else 0
                Sfd, Sbd, tSd = Sfs[d], Sbs[d], tSs[d]
                kSf, kSb, ktS = ("Sf", d), ("Sb", d), ("tS", d)
                b2, b5, by = (2, 5, 3) if d == 0 else (6, 7, 4)
                k2, k5, ky = "ps%d" % b2, "ps%d" % b5, "ps%d" % by
                want_y = need_ctx or c >= 2
                s = d
                rb = d
                rk = "ps%d" % rb
                dt4 = DTs[:, c, d * 4:(d + 1) * 4]
                adt4 = DTs[:, c, 8 + d * 4:12 + d * 4]
                smk = ("sm", s)
                tot = PS[rb][:].rearrange("p (a b) -> p a b", b=128)[:, :, last]
                kb.op("dve", lambda: nc.vector.tensor_tensor(
                    out=rhsA[s][:].rearrange("p (a b) -> p a b", b=128),
                    in0=U[:].unsqueeze(1).broadcast_to([128, 4, 128]),
                    in1=adt4.unsqueeze(2).broadcast_to([128, 4, 128]), op=ALU.mult),
                    reads=["DTs", "uf_bf", "ub_bf"], writes=[("rhsA", s)])
                kb.op("act", lambda: nc.scalar.copy(out=adtb[s][:], in_=adt4), reads=["DTs"], writes=[("adtb", s)])
                kb.op("pe", lambda: nc.tensor.matmul(PS[rb][:], lhsT=ones_bf[:], rhs=rhsA[s][:], start=True, stop=False),
                      reads=[("rhsA", s), "ones_bf"], writes=[rk])
                kb.op("pe", lambda: nc.tensor.matmul(PS[rb][:], lhsT=ident_bf[:], rhs=NEG[:], start=False, stop=True),
                      reads=["negf", "negb", "ident_bf"], writes=[rk])
                kb.op("pe", lambda: nc.tensor.matmul(PS[b2][:, 256:260], lhsT=U[:], rhs=adtb[s][:], start=True, stop=True),
                      reads=[("adtb", s), "uf_bf", "ub_bf"], writes=[k2])
                for g in range(2):
                    kb.op("pe", lambda g=g: nc.tensor.matmul(
                        PS[b2][:, g * 128:(g + 1) * 128], lhsT=XC[:, 2 + g, c * 128:(c + 1) * 128],
                        rhs=XC[:, 4 + g, c * 128:(c + 1) * 128], start=True, stop=True), reads=["XC"], writes=[k2])
                yield
                kb.op("act", lambda: nc.scalar.copy(out=col[s][:], in_=PS[b2][:, 256:260]), reads=[k2], writes=[("col", s)])
                kb.op("dve", lambda: nc.vector.tensor_tensor(
                    out=Dm[s][:].rearrange("p (a b) -> p a b", b=128), in0=PS[rb][:].rearrange("p (a b) -> p a b", b=128),
                    in1=col[s][:].unsqueeze(2).broadcast_to([128, 4, 128]), op=ALU.subtract),
                    reads=[rk, ("col", s)], writes=[("Dm", s)])
                kb.op("dve", lambda: nc.vector.tensor_tensor(out=sm[s][:, 0:4], in0=tot, in1=col[s][:], op=ALU.subtract),
                      reads=[rk, ("col", s)], writes=[smk])
                yield
                kb.op("act", lambda: nc.scalar.activation(out=Lm[s][:], in_=Dm[s][:], func=AF.Exp),
                      reads=[("Dm", s)], writes=[("Lm", s)])
                kb.op("act", lambda: nc.scalar.activation(out=sm[s][:, 0:4], in_=sm[s][:, 0:4], func=AF.Exp), reads=[smk], writes=[smk])
                kb.op("act", lambda: nc.scalar.activation(out=sm[s][:, 4:8], in_=col[s][:], func=AF.Exp), reads=[("col", s)], writes=[smk])
                kb.op("act", lambda: nc.scalar.activation(out=sm[s][:, 8:12], in_=tot, func=AF.Exp), reads=[rk], writes=[smk])
                yield
                kb.op("dve", lambda: nc.vector.tensor_tensor(
                    out=MT[s][:].rearrange("p (g h i) -> p g h i", g=2, h=2),
                    in0=Lm[s][:].rearrange("p (g h i) -> p g h i", g=2, h=2),
                    in1=PS[b2][:, 0:256].rearrange("p (g i) -> p g i", g=2).unsqueeze(2).broadcast_to([128, 2, 2, 128]),
                    op=ALU.mult), reads=[("Lm", s), k2], writes=[("MT", s)])
                kb.op("dve", lambda: nc.vector.tensor_tensor(out=sm[s][:, 0:4], in0=sm[s][:, 0:4], in1=dt4, op=ALU.mult),
                      reads=[smk, "DTs"], writes=[smk])
                kb.op("dve", lambda: nc.vector.tensor_tensor(
                    out=xw[s][:].rearrange("p (a b) -> p a b", b=64), in0=Xtm[:, c, :].rearrange("p (a b) -> p a b", b=64),
                    in1=sm[s][:, 0:4].unsqueeze(2).broadcast_to([128, 4, 64]), op=ALU.mult),
                    reads=[("Xtm", c), smk], writes=[("xw", s)])
                if want_y:
                    kb.op("pool", lambda: nc.gpsimd.tensor_tensor(
                        out=xdt[s][:].rearrange("p (a b) -> p a b", b=64), in0=Xtm[:, c, :].rearrange("p (a b) -> p a b", b=64),
                        in1=dt4.unsqueeze(2).broadcast_to([128, 4, 64]), op=ALU.mult),
                        reads=[("Xtm", c), "DTs"], writes=[("xdt", s)])
                yield
                if want_y:
                    for h in range(4):
                        kb.op("pe", lambda h=h: nc.tensor.matmul(
                            PS[by][:, h * 64:(h + 1) * 64], lhsT=MT[s][:, h * 128:(h + 1) * 128], rhs=xdt[s][:, h * 64:(h + 1) * 64],
                            start=True, stop=True), reads=[("MT", s), ("xdt", s)], writes=[ky])
                    for g in range(2):
                        kb.op("pe", lambda g=g: nc.tensor.matmul(
                            PS[by][:, 256 + g * 128:256 + (g + 1) * 128], lhsT=XC[:, 4 + g, c * 128:(c + 1) * 128],
                            rhs=Sbd[:, g * 128:(g + 1) * 128], start=True, stop=True), reads=["XC", kSb], writes=[ky])
                for g in range(2):
                    kb.op("pe", lambda g=g: nc.tensor.matmul(
                        PS[b5][:, g * 128:(g + 1) * 128], lhsT=Btm[:, c, g * 128:(g + 1) * 128],
                        rhs=xw[s][:, g * 128:(g + 1) * 128], start=True, stop=True), reads=[("Btm", c), ("xw", s)], writes=[k5])
                yield
                kb.op("dve", lambda: nc.vector.tensor_tensor(
                    out=tSd[:].rearrange("p (a b) -> p a b", b=64), in0=Sfd[:].rearrange("p (a b) -> p a b", b=64),
                    in1=sm[s][:, 8:12].unsqueeze(2).broadcast_to([128, 4, 64]), op=ALU.mult),
                    reads=[kSf, smk], writes=[ktS])
                kb.op("dve", lambda: nc.vector.tensor_tensor(out=Sfd[:], in0=PS[b5][:, 0:256], in1=tSd[:], op=ALU.add),
                      reads=[k5, ktS], writes=[kSf])
                kb.op("act", lambda: nc.scalar.copy(out=Sbd[:], in_=Sfd[:]), reads=[kSf], writes=[kSb])
                if want_y:
                    kb.op("dve", lambda: nc.vector.tensor_tensor(
                        out=yt1[s][:].rearrange("p (a b) -> p a b", b=64), in0=PS[by][:, 256:512].rearrange("p (a b) -> p a b", b=64),
                        in1=sm[s][:, 4:8].unsqueeze(2).broadcast_to([128, 4, 64]), op=ALU.mult),
                        reads=[ky, smk], writes=[("yt1", s)])
                    kb.op("dve", lambda: nc.vector.tensor_tensor(out=yt2[s][:], in0=PS[by][:, 0:256], in1=yt1[s][:], op=ALU.add),
                          reads=[ky, ("yt1", s)], writes=[("yt2", s)])
                    kb.op("pool", lambda: nc.gpsimd.tensor_tensor(out=Ytm[:, c, :], in0=Ytm[:, c, :], in1=yt2[s][:], op=ALU.add),
                          reads=[("yt2", s), ("Ytm", c)], writes=[("Ytm", c)])

            orders = [list(range(NTILE)), [1, 0] + list(range(NTILE - 1, 1, -1))]
            for i in range(NTILE):
                gens = [scan_step(d, orders[d][i]) for d in range(2)]
                alive = True
                while alive:
                    alive = False
                    for g_ in gens:
                        try:
                            next(g_)
                            alive = True
                        except StopIteration:
                            pass
            kb.barrier()
            blks = [b for b in BLOCKS if (need_ctx or not b[2])]
            for bi, (t0, T, isctx) in enumerate(blks):
                sg = bi % 2
                nsub = T // 128
                for sub in range(nsub):
                    c = t0 // 128 + sub
                    s = sub % 2
                    kb.op("pool", lambda c=c, s=s: nc.gpsimd.tensor_tensor(out=yz[s][:], in0=Ytm[:, c, :], in1=ZSs[:, c, :], op=ALU.mult),
                          reads=[("Ytm", c), "ZSs"], writes=[("yz", s)])
                    kb.op("act", lambda s=s: nc.scalar.activation(out=yjunk[:], in_=yz[s][:], func=AF.Square, accum_out=yss[s][:]),
                          reads=[("yz", s)], writes=["yjunk", ("yss", s)])
                    kb.op("act", lambda s=s: nc.scalar.activation(out=yss[s][:], in_=yss[s][:], func=AF.Sqrt, bias=eps_t[:], scale=1.0 / 256),
                          reads=[("yss", s), "eps_t"], writes=[("yss", s)])
                    kb.op("dve", lambda s=s: nc.vector.reciprocal(out=yss[s][:], in_=yss[s][:]), reads=[("yss", s)], writes=[("yss", s)])
                    kb.op("dve", lambda s=s: nc.vector.scalar_tensor_tensor(
                        out=yo[s][:], in0=yz[s][:], scalar=yss[s][:, 0:1], in1=ngB[:], op0=ALU.mult, op1=ALU.mult),
                        reads=[("yz", s), ("yss", s), "ngB"], writes=[("yo", s)])
                    b = 6 + s
                    pk = "ps%d" % b
                    psb = PS[b][:].bitcast(BF16)
                    for j in range(2):
                        kb.op("pe", lambda j=j, s=s, psb=psb: nc.tensor.transpose(
                            out=psb[:, j * 128:(j + 1) * 128], in_=yo[s][:, j * 128:(j + 1) * 128], identity=ident_bf[:]),
                            reads=[("yo", s), "ident_bf"], writes=[pk])
                    kb.op("act", lambda sub=sub, psb=psb, sg=sg: nc.scalar.copy(
                        out=stg[sg][:, :, sub * 128:(sub + 1) * 128], in_=psb[:, 0:256].rearrange("p (a b) -> p a b", b=128)),
                        reads=[pk], writes=[("sstg", sg)])
                kb.dma(sp, MIX.rearrange("(c p) t -> p c t", p=128)[:, 6:8, t0:t0 + T], stg[sg][:, :, :T],
                       reads=[("sstg", sg)], semkey=("oMIXs", sg))
            kb.barrier()

        with contextlib.ExitStack() as st:
            Wo = sb(st, "Wo", [128, 8, D], BF16)
            mix = [sb(st, "mix%d" % i, [128, 8, 512], BF16) for i in range(2)]
            xts = [sb(st, "xt%d" % i, [128, 8, 512], F32) for i in range(2)]
            hTs = [sb(st, "hTs%d" % i, [128, 8, 512], BF16) for i in range(2)]
            sq = [sb(st, "sq%d" % i, [128, 512], BF16) for i in range(2)]
            tmp = [sb(st, "tmp%d" % i, [128, 512], F32) for i in range(2)]
            rstd = sb(st, "rstd", [128, 512], F32)
            if is_moe:
                hf = sb(st, "hf", [128, 8, 512], F32)
                wr_ = sb(st, "wr", [128, 8, NEXP], F32)
                lg = [sb(st, "lg%d" % i, [128, 8], F32) for i in range(2)]
                mx = [sb(st, "mx%d" % i, [128, 8], F32) for i in range(2)]
                ee = [sb(st, "ee%d" % i, [128, 8], F32) for i in range(2)]
                mk = [sb(st, "mk%d" % i, [128, 8], F32) for i in range(2)]
                r2 = [sb(st, "r2%d" % i, [128, 1], F32) for i in range(2)]
                cmb = [sb(st, "cmb%d" % i, [128, 8], F32) for i in range(2)]
                cstg = [sb(st, "cstg%d" % i, [8, 512], F32) for i in range(2)]
                kb.dma(sp, wr_[:], moe_r[jl].rearrange("(k p) e -> p k e", p=128), writes=["wr"], semkey="wr")
            kb.dma(pool, Wo[:], w_out[l].rearrange("(k p) n -> p k n", p=128), writes=["Wo"], semkey="Wo")
            blks = [b for b in BLOCKS if (need_ctx or not b[2])]
            brot = [0]
            def load3(bi):
                t0, T, isctx = blks[bi]
                s = bi % 2
                kb.dma(sp, mix[s][:, :, :T], MIX.rearrange("(c p) t -> p c t", p=128)[:, :, t0:t0 + T],
                       writes=[("mix", s)], semkey=("mix", s))
                kb.dma(sp, xts[s][:, :, :T], xsrc.rearrange("(k p) t -> p k t", p=128)[:, :, t0:t0 + T],
                       writes=[("xt", s)], semkey=("xt3", s))

            load3(0)
            pending_router = []
            for bi, (t0, T, isctx) in enumerate(blks):
                s = bi % 2
                which = 1 if isctx else 0
                xt = xts[s]
                if bi + 1 < len(blks):
                    load3(bi + 1)
                for co in range(8):
                    b = brot[0] % 4
                    brot[0] += 1
                    pk = "ps%d" % b
                    for k in range(8):
                        kb.op("pe", lambda k=k, co=co, b=b: nc.tensor.matmul(
                            PS[b][:, :T], lhsT=Wo[:, k, co * 128:(co + 1) * 128], rhs=mix[s][:, k, :T],
                            start=(k == 0), stop=(k == 7)), reads=["Wo", ("mix", s)], writes=[pk])
                    kb.op("dve", lambda co=co, b=b: nc.vector.scalar_tensor_tensor(
                        out=xt[:, co, :T], in0=PS[b][:, :T], scalar=mods[:, l, 16 + co, which:which + 1], in1=xt[:, co, :T],
                        op0=ALU.mult, op1=ALU.add), reads=[pk, "mods", ("xt", s)], writes=[("xt", s)])
                kb.dma(sp, R.rearrange("(k p) t -> p k t", p=128)[:, :, t0:t0 + T], xt[:, :, :T],
                       reads=[("xt", s)], semkey=("oR3", s))
                if pending_router:
                    pending_router.pop()()
                rmsnorm_block((sq, rstd, tmp), xt, T, G2, l, 24, which, hTs[s], ("hTs", s), hf=(hf if is_moe else None),
                              xk=("xt", s))
                kb.dma(sp, HT.rearrange("(k p) t -> p k t", p=128)[:, :, t0:t0 + T], hTs[s][:, :, :T],
                       reads=[("hTs", s)], semkey=("oHT", s))
                if is_moe:
                    def router(T=T, t0=t0, s=s):
                        nsub = T // 128
                        for sub in range(nsub):
                            s2 = sub % 2
                            for k in range(8):
                                kb.op("pe", lambda k=k, sub=sub: nc.tensor.matmul(
                                    PS[4][:, 0:8], lhsT=hf[:, k, sub * 128:(sub + 1) * 128], rhs=wr_[:, k, :],
                                    start=(k == 0), stop=(k == 7)), reads=["hf", "wr"], writes=["ps4"])
                            kb.op("act", lambda s2=s2: nc.scalar.copy(out=lg[s2][:], in_=PS[4][:, 0:8]), reads=["ps4"], writes=[("lg", s2)])
                            kb.op("dve", lambda s2=s2: nc.vector.max(out=mx[s2][:], in_=lg[s2][:]), reads=[("lg", s2)], writes=[("mx", s2)])
                            kb.op("dve", lambda s2=s2: nc.vector.tensor_scalar(out=ee[s2][:], in0=lg[s2][:], scalar1=mx[s2][:, 0:1], scalar2=None,
                                                                              op0=ALU.subtract), reads=[("lg", s2), ("mx", s2)], writes=[("ee", s2)])
                            kb.op("act", lambda s2=s2: nc.scalar.activation(out=ee[s2][:], in_=ee[s2][:], func=AF.Exp), reads=[("ee", s2)], writes=[("ee", s2)])
                            kb.op("dve", lambda s2=s2: nc.vector.tensor_tensor(out=r2[s2][:], in0=mx[s2][:, 1:2], in1=mx[s2][:, 0:1], op=ALU.subtract),
                                  reads=[("mx", s2)], writes=[("r2", s2)])
                            kb.op("act", lambda s2=s2: nc.scalar.activation(out=r2[s2][:], in_=r2[s2][:], func=AF.Exp), reads=[("r2", s2)], writes=[("r2", s2)])
                            kb.op("dve", lambda s2=s2: nc.vector.tensor_scalar(out=r2[s2][:], in0=r2[s2][:], scalar1=1.0, scalar2=None, op0=ALU.add),
                                  reads=[("r2", s2)], writes=[("r2", s2)])
                            kb.op("dve", lambda s2=s2: nc.vector.reciprocal(out=r2[s2][:], in_=r2[s2][:]), reads=[("r2", s2)], writes=[("r2", s2)])
                            kb.op("dve", lambda s2=s2: nc.vector.tensor_scalar(out=mk[s2][:], in0=lg[s2][:], scalar1=mx[s2][:, 1:2], scalar2=None,
                                                                              op0=ALU.is_ge), reads=[("lg", s2), ("mx", s2)], writes=[("mk", s2)])
                            kb.op("dve", lambda s2=s2: nc.vector.scalar_tensor_tensor(
                                out=cmb[s2][:], in0=ee[s2][:], scalar=r2[s2][:, 0:1], in1=mk[s2][:], op0=ALU.mult, op1=ALU.mult),
                                reads=[("ee", s2), ("r2", s2), ("mk", s2)], writes=[("cmb", s2)])
                            kb.op("pe", lambda s2=s2, sub=sub: nc.tensor.transpose(
                                out=PS[5][0:8, sub * 128:(sub + 1) * 128], in_=cmb[s2][:], identity=ident_f[:]),
                                reads=[("cmb", s2), "ident_f"], writes=["ps5"])
                        kb.op("act", lambda s=s, T=T: nc.scalar.copy(out=cstg[s][:, :T], in_=PS[5][0:8, :T]), reads=["ps5"], writes=[("cstg", s)])
                        kb.dma(sp, COMBT[:, t0:t0 + T], cstg[s][:, :T], reads=[("cstg", s)], semkey=("oCB", s))
                    pending_router.append(router)
            if pending_router:
                pending_router.pop()()
            kb.barrier()

        with contextlib.ExitStack() as st:
            Wg = [sb(st, "Wg%d" % i, [128, 8, 768], BF16) for i in range(2)]
            Wu = [sb(st, "Wu%d" % i, [128, 8, 768], BF16) for i in range(2)]
            Wd = [sb(st, "Wd%d" % i, [128, 6, D], BF16) for i in range(2)]
            xts = [sb(st, "xt%d" % i, [128, 8, 512], F32) for i in range(2)]
            hTs = [sb(st, "hTs%d" % i, [128, 8, 512], BF16) for i in range(2)]
            aT = [sb(st, "aT%d" % i, [128, 6, 512], BF16) for i in range(2)]
            sgs = [sb(st, "sg%d" % i, [128, 512], F32) for i in range(2)]
            cb = [sb(st, "cb%d" % i, [128, 512], F32) for i in range(2)]
            t4 = [sb(st, "t4%d" % i, [128, 512], F32) for i in range(2)]
            blks = [b for b in BLOCKS if (need_ctx or not b[2])]
            passes = [(e, g) for e in range(NEXP if is_moe else 1) for g in range(4)]
            final_layer = (l == nlayers - 1)

            def load_w(pi):
                e, g = passes[pi]
                f0, nf = FGROUPS[g]
                s = pi % 2
                if is_moe:
                    gsrc, usrc, dsrc = moe_g[jl, e], moe_u[jl, e], moe_d[jl, e]
                else:
                    gsrc, usrc, dsrc = ffn_g[jl], ffn_u[jl], ffn_d[jl]
                kb.dma(pool, Wg[s][:, :, 0:nf * 128], gsrc.rearrange("(k p) f -> p k f", p=128)[:, :, f0 * 128:(f0 + nf) * 128],
                       writes=[("Wg", s)], semkey=("Wg", s))
                kb.dma(pool, Wu[s][:, :, 0:nf * 128], usrc.rearrange("(k p) f -> p k f", p=128)[:, :, f0 * 128:(f0 + nf) * 128],
                       writes=[("Wu", s)], semkey=("Wu", s))
                kb.dma(pool, Wd[s][:, 0:nf, :], dsrc[f0 * 128:(f0 + nf) * 128, :].rearrange("(c p) d -> p c d", p=128),
                       writes=[("Wd", s)], semkey=("Wd", s))

            iters = [(pi, bi) for pi in range(len(passes)) for bi in range(len(blks))]

            def load_act(n):
                pi, bi = iters[n]
                e = passes[pi][0]
                t0, T, isctx = blks[bi]
                s = n % 2
                kb.dma(sp, hTs[s][:, :, :T], HT.rearrange("(k p) t -> p k t", p=128)[:, :, t0:t0 + T],
                       writes=[("hT", s)], semkey=("hT4", s))
                kb.dma(sp, xts[s][:, :, :T], R.rearrange("(k p) t -> p k t", p=128)[:, :, t0:t0 + T],
                       reads=[("R", bi)], writes=[("xt", s)], semkey=("xt4", s))
                if is_moe:
                    kb.dma(sp, cb[s][:, :T], COMBT[e, t0:t0 + T].partition_broadcast(128), writes=[("cb", s)], semkey=("cb", s))

            load_w(0)
            load_act(0)
            it = 0
            gub = [0]
            dbk = [0]
            for pi, (e, g) in enumerate(passes):
                if pi + 1 < len(passes):
                    load_w(pi + 1)
                f0, nf = FGROUPS[g]
                ws = pi % 2
                last_pass = (pi == len(passes) - 1)
                for bi, (t0, T, isctx) in enumerate(blks):
                    s = it % 2
                    if it + 1 < len(iters):
                        load_act(it + 1)
                    it += 1
                    which = 1 if isctx else 0
                    xt, hT = xts[s], hTs[s]
                    for fc in range(nf):
                        bg = (gub[0] % 2) * 2
                        gub[0] += 1
                        bu = bg + 1
                        gk, uk = "ps%d" % bg, "ps%d" % bu
                        for k in range(8):
                            kb.op("pe", lambda k=k, fc=fc, bg=bg: nc.tensor.matmul(
                                PS[bg][:, :T], lhsT=Wg[ws][:, k, fc * 128:(fc + 1) * 128], rhs=hT[:, k, :T],
                                start=(k == 0), stop=(k == 7)), reads=[("Wg", ws), ("hT", s)], writes=[gk])
                        for k in range(8):
                            kb.op("pe", lambda k=k, fc=fc, bu=bu: nc.tensor.matmul(
                                PS[bu][:, :T], lhsT=Wu[ws][:, k, fc * 128:(fc + 1) * 128], rhs=hT[:, k, :T],
                                start=(k == 0), stop=(k == 7)), reads=[("Wu", ws), ("hT", s)], writes=[uk])
                        sgi = fc % 2
                        kb.op("act", lambda bg=bg, sgi=sgi: nc.scalar.activation(out=sgs[sgi][:, :T], in_=PS[bg][:, :T], func=AF.Silu),
                              reads=[gk], writes=[("sg", sgi)])
                        kb.op("dve", lambda bu=bu, sgi=sgi, fc=fc: nc.vector.tensor_tensor(
                            out=aT[s][:, fc, :T], in0=sgs[sgi][:, :T], in1=PS[bu][:, :T], op=ALU.mult),
                            reads=[uk, ("sg", sgi)], writes=[("aT", s)])
                    for co in range(8):
                        bd = 4 + (dbk[0] % 3)
                        dbk[0] += 1
                        dk = "ps%d" % bd
                        for fc in range(nf):
                            kb.op("pe", lambda fc=fc, co=co, bd=bd: nc.tensor.matmul(
                                PS[bd][:, :T], lhsT=Wd[ws][:, fc, co * 128:(co + 1) * 128], rhs=aT[s][:, fc, :T],
                                start=(fc == 0), stop=(fc == nf - 1)), reads=[("Wd", ws), ("aT", s)], writes=[dk])
                        if is_moe:
                            ti = co % 2
                            kb.op("dve", lambda co=co, bd=bd, ti=ti: nc.vector.scalar_tensor_tensor(
                                out=t4[ti][:, :T], in0=PS[bd][:, :T], scalar=mods[:, l, 40 + co, which:which + 1], in1=cb[s][:, :T],
                                op0=ALU.mult, op1=ALU.mult), reads=[dk, "mods", ("cb", s)], writes=[("t4", ti)])
                            kb.op("pool", lambda co=co, ti=ti: nc.gpsimd.tensor_tensor(
                                out=xt[:, co, :T], in0=xt[:, co, :T], in1=t4[ti][:, :T], op=ALU.add),
                                reads=[("t4", ti), ("xt", s)], writes=[("xt", s)])
                        else:
                            kb.op("dve", lambda co=co, bd=bd: nc.vector.scalar_tensor_tensor(
                                out=xt[:, co, :T], in0=PS[bd][:, :T], scalar=mods[:, l, 40 + co, which:which + 1], in1=xt[:, co, :T],
                                op0=ALU.mult, op1=ALU.add), reads=[dk, "mods", ("xt", s)], writes=[("xt", s)])
                    if last_pass and final_layer:
                        if not isctx:
                            kb.dma(sp, outT.rearrange("(k p) t -> p k t", p=128)[:, :, t0 - CL:t0 - CL + T], xt[:, :, :T],
                                   reads=[("xt", s)], semkey=("oR4", s))
                    else:
                        kb.dma(sp, R.rearrange("(k p) t -> p k t", p=128)[:, :, t0:t0 + T], xt[:, :, :T],
                               reads=[("xt", s)], writes=[("R", bi)], semkey=("oR4", s))
            kb.barrier()

    return nc


def _consts():
    c = {}
    c["c_ident"] = np.eye(128, dtype=np.float32)
    bo = np.zeros((128, 128), np.float32)
    bo[:64, :64] = 1
    bo[64:, 64:] = 1
    c["c_bones"] = bo
    P = np.zeros((128, 128), np.float32)
    for m in range(128):
        partner = m + 16 if (m % 32) < 16 else m - 16
        P[partner, m] = 1
    c["c_perm"] = P
    t = np.arange(128)
    uf = (t[:, None] <= t[None, :]).astype(np.float32)
    ub = (t[:, None] >= t[None, :]).astype(np.float32)
    c["c_uf"], c["c_ub"] = uf, ub
    c["c_negf"] = np.tile((uf - 1.0) * 30000.0, (1, 4)).astype(np.float32)
    c["c_negb"] = np.tile((ub - 1.0) * 30000.0, (1, 4)).astype(np.float32)
    pos = np.arange(L)
    row, colp = pos // 64, pos % 64
    freqs = (10000.0 ** (-np.arange(16, dtype=np.float32) / 16)).astype(np.float32)
    C = np.zeros((128, L), np.float32)
    S = np.zeros((128, L), np.float32)
    for p in range(128):
        dd = p % 64
        pp = row if dd < 32 else colp
        ang = pp.astype(np.float32) * freqs[dd % 16]
        C[p] = np.cos(ang)
        S[p] = np.sin(ang) * (-1.0 if (dd % 32) < 16 else 1.0)
    c["c_ropeC"], c["c_ropeS"] = C, S
    return c


def _prep_shared(inp):
    f = np.float32
    a = lambda v: np.ascontiguousarray(np.asarray(v, dtype=f))
    sh = {}
    sh["w_mod"] = a(inp["w_mod"])
    sh["bmodT"] = a(np.asarray(inp["b_mod"]).reshape(DEPTH, 48, 128).transpose(2, 0, 1))
    sh["g1T"] = a(np.asarray(inp["norm1_g"]).reshape(DEPTH, 8, 128).transpose(2, 0, 1))
    sh["g2T"] = a(np.asarray(inp["norm2_g"]).reshape(DEPTH, 8, 128).transpose(2, 0, 1))
    sh["w_in"] = a(inp["w_in"])
    sh["w_out"] = a(inp["w_out"])
    sh["gvg"] = a(inp["gm_v_g"])
    sh["wsT"] = a(np.asarray(inp["gm_ws"]).transpose(0, 1, 3, 2))
    p = np.arange(128)
    bsv = np.asarray(inp["gm_bs"])
    gb = np.zeros((128, DEPTH, 2, 128), f)
    for j in range(2):
        gb[:, :, j, :] = bsv[:, 2 * j + (p // 64), :].transpose(1, 0, 2)
    sh["gbs"] = gb
    sh["qgT"] = a(np.asarray(inp["att_q_g"])[:, p % 64].T)
    sh["kgT"] = a(np.asarray(inp["att_k_g"])[:, p % 64].T)
    sk = np.asarray(inp["att_sink"])
    sl = np.zeros((128, DEPTH, 2, 2), f)
    for kv in range(2):
        for ti in range(2):
            sl[:, :, kv, ti] = sk[:, 4 * kv + 2 * ti + (p // 64)].T
    sh["sinkL"] = sl
    sh["convw"] = a(np.asarray(inp["ssm_conv_w"]).reshape(DEPTH, 5, 6, 128).transpose(3, 0, 2, 1))
    sh["convb"] = a(np.asarray(inp["ssm_conv_b"]).reshape(DEPTH, 6, 128).transpose(2, 0, 1))
    sh["dtb"] = a(np.asarray(inp["ssm_dt_bias"]).reshape(DEPTH, 8))
    sh["alog"] = a(np.asarray(inp["ssm_a_log"]).reshape(DEPTH, 8))
    sh["dskE"] = a(np.repeat(np.asarray(inp["ssm_d"]), 64, axis=1))
    sh["sng"] = a(inp["ssm_norm_g"])
    sh["ffn_g"] = a(inp["ffn_w_gate"])
    sh["ffn_u"] = a(inp["ffn_w_up"])
    sh["ffn_d"] = a(inp["ffn_w_down"])
    sh["moe_r"] = a(inp["moe_router"])
    sh["moe_g"] = a(inp["moe_w_gate"])
    sh["moe_u"] = a(inp["moe_w_up"])
    sh["moe_d"] = a(inp["moe_w_down"])
    sh.update(_consts())
    return sh


def _prep_core(inp, b):
    f = np.float32
    x = np.asarray(inp["x"][b], dtype=f)
    ctx = np.asarray(inp["ctx"][b], dtype=f)
    xT0 = np.ascontiguousarray(np.concatenate([ctx.T, x.T], axis=1))
    c = np.asarray(inp["c"][b], dtype=f).reshape(8, 128).T
    cc = np.asarray(inp["c_ctx"], dtype=f).reshape(8, 128).T
    cs = np.ascontiguousarray(np.stack([c, cc], axis=-1))
    return {"xT0": xT0, "cs": cs}


_NC_CACHE = {}


def kernel(**inputs):
    if "nc" not in _NC_CACHE:
        _NC_CACHE["nc"] = build_program()
    nc = _NC_CACHE["nc"]
    sh = _prep_shared(inputs)
    in_maps = []
    for b in range(8):
        m = dict(sh)
        m.update(_prep_core(inputs, b))
        in_maps.append(m)
    res = run_bass_kernel_spmd(nc, in_maps, core_ids=list(range(8)))
    out = np.stack([np.ascontiguousarray(r["outT"].T) for r in res.results], axis=0)
    return out.astype(np.float32)
```

```python
import contextlib
import numpy as np
import concourse.bass as bass
import concourse.mybir as mybir
from concourse.bass_utils import run_bass_kernel_spmd

F32, BF16 = mybir.dt.float32, mybir.dt.bfloat16
AF = mybir.ActivationFunctionType
ALU = mybir.AluOpType
AX = mybir.AxisListType

D = 1024
L = 4096
CL = 256
NT = L + CL
NTILE = NT // 128
DEPTH = 4
DFF = 2816
NEXP = 8
EPS = 1e-6
FGROUPS = [(0, 6), (6, 6), (12, 5), (17, 5)]
BLOCKS = [(0, 256, True)] + [(256 + 512 * i, 512, False) for i in range(8)]


SIM_FRESH_POOL = False


class KB:
    def __init__(self, nc, stack):
        self.nc = nc
        self.stack = stack
        self.eng = {"pe": nc.tensor, "dve": nc.vector, "act": nc.scalar, "pool": nc.gpsimd, "sp": nc.sync}
        self.esem = {e: stack.enter_context(nc.semaphore("es_" + e)) for e in self.eng}
        self.ecnt = {e: 0 for e in self.eng}
        self.seen = {e: {} for e in self.eng}
        self.lw = {}
        self.rd = {}
        self.dsem = {}
        self.free = []
        self.nsem = 0
        self.dead = False

    def _wait(self, E, tok):
        sem, val, _ = tok
        sid = id(sem)
        if self.seen[E].get(sid, 0) >= val:
            return
        self.eng[E].wait_ge(sem, val)
        self.seen[E][sid] = val

    def _deps(self, E, reads, writes):
        for k in reads:
            w = self.lw.get(k)
            if w is not None:
                if w[2] == E and E == "pe":
                    continue
                self._wait(E, w)
        for k in writes:
            w = self.lw.get(k)
            if w is not None and w[2] != E:
                self._wait(E, w)
            for r in self.rd.get(k, {}).values():
                if r[2] != E:
                    self._wait(E, r)

    def _record(self, tok, reads, writes):
        for k in writes:
            self.lw[k] = tok
            self.rd[k] = {}
        for k in reads:
            self.rd.setdefault(k, {})[id(tok[0])] = tok

    def op(self, E, fn, reads=(), writes=()):
        if self.dead:
            return None
        self._deps(E, reads, writes)
        inst = fn()
        self.ecnt[E] += 1
        inst.then_inc(self.esem[E], 1)
        tok = (self.esem[E], self.ecnt[E], E)
        self._record(tok, reads, writes)
        return tok

    def dma(self, E, out, in_, reads=(), writes=(), semkey=None):
        if self.dead:
            return None
        if SIM_FRESH_POOL and E == "pool":
            self.nsem += 1
            semkey = ("__fresh", self.nsem)
            self.dsem[semkey] = [self.stack.enter_context(self.nc.semaphore("dp%d" % self.nsem)), 0]
        if semkey not in self.dsem:
            if self.free:
                self.dsem[semkey] = self.free.pop()
            else:
                self.nsem += 1
                self.dsem[semkey] = [self.stack.enter_context(self.nc.semaphore("ds%d" % self.nsem)), 0]
        ent = self.dsem[semkey]
        self._deps(E, reads, writes)
        if ent[1] > 0:
            self._wait(E, (ent[0], ent[1], "dma"))
        ent[1] += 16
        self.eng[E].dma_start(out=out, in_=in_).then_inc(ent[0], 16)
        tok = (ent[0], ent[1], "dma:" + str(semkey))
        self._record(tok, reads, writes)
        return tok

    def barrier(self):
        if self.dead:
            return
        for E in self.eng:
            for Fn in self.eng:
                if Fn != E and self.ecnt[Fn] > 0:
                    self._wait(E, (self.esem[Fn], self.ecnt[Fn], Fn))
            for ent in self.dsem.values():
                if ent[1] > 0:
                    self._wait(E, (ent[0], ent[1], "dma"))
        self.lw = {}
        self.rd = {}
        self.free.extend(v for k, v in self.dsem.items() if not (isinstance(k, tuple) and k and k[0] == "__fresh"))
        self.dsem = {}


class _Stop(Exception):
    pass


def build_program(nlayers=DEPTH, debug=False, stop=None):
    nc = bass.Bass("TRN2", target_bir_lowering=False)
    stack = contextlib.ExitStack()
    with stack:
        try:
            _emit(nc, stack, nlayers, debug, stop)
        except _Stop:
            pass
    return nc


def _emit(nc, stack, nlayers, debug, stop=None):
    kb = KB(nc, stack)
    _bar = kb.barrier
    _phase = [0]

    def barrier_named():
        _bar()
        _phase[0] += 1
        if stop is not None and _phase[0] >= stop:
            kb.dead = True

    kb.barrier = barrier_named

    def din(name, shape, dt=F32):
        return nc.dram_tensor(name, list(shape), dt, kind="ExternalInput").ap()

    def dscr(name, shape, dt):
        kind = "ExternalOutput" if debug else None
        if kind:
            return nc.dram_tensor(name, list(shape), dt, kind=kind).ap()
        return nc.dram_tensor(name, list(shape), dt).ap()

    xT0 = din("xT0", [D, NT])
    cs_in = din("cs", [128, 8, 2])
    w_mod = din("w_mod", [DEPTH, D, 6 * D])
    bmodT = din("bmodT", [128, DEPTH, 48])
    g1T = din("g1T", [128, DEPTH, 8])
    g2T = din("g2T", [128, DEPTH, 8])
    w_in = din("w_in", [DEPTH, D, 2312])
    w_out = din("w_out", [DEPTH, D, D])
    gvg = din("gvg", [DEPTH, 256])
    wsT = din("wsT", [DEPTH, 4, 128, 128])
    gbs = din("gbs", [128, DEPTH, 2, 128])
    qgT = din("qgT", [128, DEPTH])
    kgT = din("kgT", [128, DEPTH])
    sinkL = din("sinkL", [128, DEPTH, 2, 2])
    convw = din("convw", [128, DEPTH, 6, 5])
    convb = din("convb", [128, DEPTH, 6])
    dtb = din("dtb", [DEPTH, 8])
    alog = din("alog", [DEPTH, 8])
    dskE = din("dskE", [DEPTH, 256])
    sng = din("sng", [DEPTH, 256])
    ffn_g = din("ffn_g", [2, D, DFF])
    ffn_u = din("ffn_u", [2, D, DFF])
    ffn_d = din("ffn_d", [2, DFF, D])
    moe_r = din("moe_r", [2, D, NEXP])
    _small = nlayers < 2
    moe_g = din("moe_g", [2, NEXP, D, DFF] if not _small else [2, NEXP, 128, 768])
    moe_u = din("moe_u", [2, NEXP, D, DFF] if not _small else [2, NEXP, 128, 768])
    moe_d = din("moe_d", [2, NEXP, DFF, D] if not _small else [2, NEXP, 128, 768])
    c_ident = din("c_ident", [128, 128])
    c_bones = din("c_bones", [128, 128])
    c_perm = din("c_perm", [128, 128])
    c_uf = din("c_uf", [128, 128])
    c_ub = din("c_ub", [128, 128])
    c_negf = din("c_negf", [128, 512])
    c_negb = din("c_negb", [128, 512])
    c_ropeC = din("c_ropeC", [128, L])
    c_ropeS = din("c_ropeS", [128, L])

    outT = nc.dram_tensor("outT", [D, L], F32, kind="ExternalOutput").ap()

    R = dscr("R", [D, NT], F32)
    GU = dscr("GU", [256, NT], BF16)
    GV = dscr("GV", [NT, 256], BF16)
    QT = dscr("QT", [512, NT], BF16)
    KT = dscr("KT", [256, NT], BF16)
    V = dscr("V", [NT, 128], BF16)
    ZS = dscr("ZS", [NT, 256], BF16)
    XBC = dscr("XBC", [768, NT], BF16)
    DT = dscr("DT", [NT, 16], F32)
    MIX = dscr("MIX", [D, NT], BF16)
    HT = dscr("HT", [D, NT], BF16)
    COMBT = dscr("COMBT", [NEXP, NT], F32)

    _uniq = [0]

    def sb(st, name, shape, dt):
        _uniq[0] += 1
        return st.enter_context(nc.sbuf_tensor("%s_u%d" % (name, _uniq[0]), list(shape), dt))

    PS = [stack.enter_context(nc.psum_tensor("ps%d" % i, [128, 512], F32)) for i in range(8)]

    ident_bf = sb(stack, "ident_bf", [128, 128], BF16)
    ident_f = sb(stack, "ident_f", [128, 128], F32)
    ones_bf = sb(stack, "ones_bf", [128, 128], BF16)
    bones_bf = sb(stack, "bones_bf", [128, 128], BF16)
    perm_bf = sb(stack, "perm_bf", [128, 128], BF16)
    uf_bf = sb(stack, "uf_bf", [128, 128], BF16)
    ub_bf = sb(stack, "ub_bf", [128, 128], BF16)
    ones_f = sb(stack, "ones_f", [128, 64], F32)
    eps_t = sb(stack, "eps_t", [128, 1], F32)
    one_t = sb(stack, "one_t", [128, 1], F32)
    mods = sb(stack, "mods", [128, DEPTH, 48, 2], F32)
    G1 = sb(stack, "G1", [128, DEPTH, 8, 2], F32)
    G2 = sb(stack, "G2", [128, DEPTH, 8, 2], F32)
    g1s = sb(stack, "g1s", [128, DEPTH, 8], F32)
    g2s = sb(stack, "g2s", [128, DEPTH, 8], F32)
    bms = sb(stack, "bms", [128, DEPTH, 48], F32)
    css = sb(stack, "css", [128, 8, 2], F32)
    qgs = sb(stack, "qgs", [128, DEPTH], F32)
    kgs = sb(stack, "kgs", [128, DEPTH], F32)
    sinkE = sb(stack, "sinkE", [128, DEPTH, 2, 2], F32)
    cws = sb(stack, "cws", [128, DEPTH, 6, 5], F32)
    cbs = sb(stack, "cbs", [128, DEPTH, 6], F32)

    sp, pool = "sp", "pool"
    kb.dma(pool, ident_bf[:], c_ident, writes=["ident_bf"], semkey="c0")
    kb.dma(sp, ident_f[:], c_ident, writes=["ident_f"], semkey="c1")
    kb.dma(pool, bones_bf[:], c_bones, writes=["bones_bf"], semkey="c2")
    kb.dma(pool, perm_bf[:], c_perm, writes=["perm_bf"], semkey="c3")
    kb.dma(pool, uf_bf[:], c_uf, writes=["uf_bf"], semkey="c4")
    kb.dma(pool, ub_bf[:], c_ub, writes=["ub_bf"], semkey="c5")
    kb.dma(sp, g1s[:], g1T, writes=["g1s"], semkey="c6")
    kb.dma(sp, g2s[:], g2T, writes=["g2s"], semkey="c7")
    kb.dma(sp, bms[:], bmodT, writes=["bms"], semkey="c8")
    kb.dma(sp, css[:], cs_in, writes=["css"], semkey="c9")
    kb.dma(sp, qgs[:], qgT, writes=["qgs"], semkey="c10")
    kb.dma(sp, kgs[:], kgT, writes=["kgs"], semkey="c11")
    kb.dma(sp, sinkE[:], sinkL, writes=["sinkE"], semkey="c12")
    kb.dma(sp, cws[:], convw, writes=["cws"], semkey="c13")
    kb.dma(sp, cbs[:], convb, writes=["cbs"], semkey="c14")
    kb.op("dve", lambda: nc.vector.memset(ones_bf[:], 1.0), writes=["ones_bf"])
    kb.op("dve", lambda: nc.vector.memset(ones_f[:], 1.0), writes=["ones_f"])
    kb.op("dve", lambda: nc.vector.memset(eps_t[:], EPS), writes=["eps_t"])
    kb.op("dve", lambda: nc.vector.memset(one_t[:], 1.0), writes=["one_t"])
    kb.op("act", lambda: nc.scalar.activation(out=css[:], in_=css[:], func=AF.Silu), reads=["css"], writes=["css"])
    kb.op("act", lambda: nc.scalar.activation(out=sinkE[:], in_=sinkE[:], func=AF.Exp), reads=["sinkE"], writes=["sinkE"])

    with contextlib.ExitStack() as st:
        wm = [sb(st, "wm%d" % i, [128, 8, 512], F32) for i in range(2)]
        it = 0
        for l in range(nlayers):
            for gidx in range(12):
                s = it % 2
                it += 1
                kb.dma(sp, wm[s][:], w_mod[l].rearrange("(k p) n -> p k n", p=128)[:, :, gidx * 512:(gidx + 1) * 512],
                       writes=[("wm", s)], semkey=("wm", s))
                for jj in range(4):
                    j = gidx * 4 + jj
                    for k in range(8):
                        kb.op("pe", lambda k=k, jj=jj, j=j, s=s: nc.tensor.matmul(
                            PS[0][:, j * 2:j * 2 + 2], lhsT=wm[s][:, k, jj * 128:(jj + 1) * 128], rhs=css[:, k, :],
                            start=(k == 0), stop=(k == 7)), reads=[("wm", s), "css"], writes=["ps0"])
            kb.op("dve", lambda l=l: nc.vector.tensor_tensor(
                out=mods[:, l, :, :], in0=PS[0][:, 0:96].rearrange("p (a b) -> p a b", b=2),
                in1=bms[:, l, :].unsqueeze(2).broadcast_to([128, 48, 2]), op=ALU.add),
                reads=["ps0", "bms"], writes=["mods"])
            for (Gt, gs, off) in ((G1, g1s, 8), (G2, g2s, 32)):
                kb.op("dve", lambda Gt=Gt, off=off, l=l: nc.vector.tensor_scalar(
                    out=Gt[:, l, :, :], in0=mods[:, l, off:off + 8, :], scalar1=1.0, scalar2=None, op0=ALU.add),
                    reads=["mods"], writes=["G"])
                kb.op("dve", lambda Gt=Gt, gs=gs, l=l: nc.vector.tensor_tensor(
                    out=Gt[:, l, :, :], in0=Gt[:, l, :, :], in1=gs[:, l, :].unsqueeze(2).broadcast_to([128, 8, 2]),
                    op=ALU.mult), reads=["G", "g1s", "g2s"], writes=["G"])
        kb.barrier()

    def rmsnorm_block(st_tiles, xt, T, Gt, l, shift_off, which, out_tile, out_key, hf=None, xk="xt"):
        sq, rstd, tmp = st_tiles
        for k in range(8):
            s = k % 2
            kb.op("act", lambda k=k, s=s: nc.scalar.activation(out=sq[s][:, :T], in_=xt[:, k, :T], func=AF.Square),
                  reads=[xk], writes=[("sq", s)])
            kb.op("pe", lambda k=k, s=s: nc.tensor.matmul(PS[7][:, :T], lhsT=ones_bf[:], rhs=sq[s][:, :T],
                                                          start=(k == 0), stop=(k == 7)),
                  reads=[("sq", s), "ones_bf"], writes=["ps7"])
        kb.op("act", lambda: nc.scalar.activation(out=rstd[:, :T], in_=PS[7][:, :T], func=AF.Sqrt,
                                                  bias=eps_t[:], scale=1.0 / D), reads=["ps7", "eps_t"], writes=["rstd"])
        kb.op("dve", lambda: nc.vector.reciprocal(out=rstd[:, :T], in_=rstd[:, :T]), reads=["rstd"], writes=["rstd"])
        for k in range(8):
            s = k % 2
            kb.op("dve", lambda k=k, s=s: nc.vector.scalar_tensor_tensor(
                out=tmp[s][:, :T], in0=xt[:, k, :T], scalar=Gt[:, l, k, which:which + 1], in1=rstd[:, :T],
                op0=ALU.mult, op1=ALU.mult), reads=[xk, "G", "rstd"], writes=[("tmp", s)])
            if hf is None:
                kb.op("act", lambda k=k, s=s: nc.scalar.activation(
                    out=out_tile[:, k, :T], in_=tmp[s][:, :T], func=AF.Identity,
                    bias=mods[:, l, shift_off + k, which:which + 1], scale=1.0),
                    reads=[("tmp", s), "mods"], writes=[out_key])
            else:
                kb.op("act", lambda k=k, s=s: nc.scalar.activation(
                    out=hf[:, k, :T], in_=tmp[s][:, :T], func=AF.Identity,
                    bias=mods[:, l, shift_off + k, which:which + 1], scale=1.0),
                    reads=[("tmp", s), "mods"], writes=["hf"])
                kb.op("pool", lambda k=k: nc.gpsimd.tensor_copy(out=out_tile[:, k, :T], in_=hf[:, k, :T]),
                      reads=["hf"], writes=[out_key])

    for l in range(nlayers):
        need_ctx = l < DEPTH - 1
        is_moe = (l % 2 == 1)
        jl = l // 2
        xsrc = xT0 if l == 0 else R

        with contextlib.ExitStack() as st:
            Wfm = sb(st, "Wfm", [128, 8, 1792], BF16)
            Wtm = sb(st, "Wtm", [128, 8, 648], BF16)
            xts = [sb(st, "xt%d" % i, [128, 8, 512], F32) for i in range(2)]
            hTs = [sb(st, "hT%d" % i, [128, 8, 512], BF16) for i in range(2)]
            sq = [sb(st, "sq%d" % i, [128, 512], BF16) for i in range(2)]
            tmp = [sb(st, "tmp%d" % i, [128, 512], F32) for i in range(2)]
            rstd = sb(st, "rstd", [128, 512], F32)
            stgF = [sb(st, "stgF%d" % i, [128, 14, 512], BF16) for i in range(2)]
            stgGV = [sb(st, "stgGV%d" % i, [128, 4, 256], BF16) for i in range(2)]
            stgV = [sb(st, "stgV%d" % i, [128, 4, 128], BF16) for i in range(2)]
            stgZ = [sb(st, "stgZ%d" % i, [128, 4, 256], BF16) for i in range(2)]
            stgDT = [sb(st, "stgDT%d" % i, [128, 4, 16], F32) for i in range(2)]
            ropeC = sb(st, "ropeC", [128, L], F32)
            ropeS = sb(st, "ropeS", [128, L], F32)
            gvB = sb(st, "gvB", [128, 256], F32)
            dtbB = sb(st, "dtbB", [128, 8], F32)
            aB = sb(st, "aB", [128, 8], F32)
            qsq = [sb(st, "qsq%d" % i, [128, 512], BF16) for i in range(2)]
            qrn = [sb(st, "qrn%d" % i, [128, 512], F32) for i in range(2)]
            qn = [sb(st, "qn%d" % i, [128, 512], BF16) for i in range(2)]
            qt1 = [sb(st, "qt1%d" % i, [128, 512], F32) for i in range(2)]
            qt2 = [sb(st, "qt2%d" % i, [128, 512], F32) for i in range(2)]
            gl = [sb(st, "gl%d" % i, [128, 256], F32) for i in range(2)]
            gsq = [sb(st, "gsq%d" % i, [128, 256], F32) for i in range(2)]
            gss = [sb(st, "gss%d" % i, [128, 4], F32) for i in range(2)]
            dtx = [sb(st, "dtx%d" % i, [128, 8], F32) for i in range(2)]

            wv = w_in[l].rearrange("(k p) n -> p k n", p=128)
            kb.dma(pool, Wfm[:, :, 0:256], wv[:, :, 0:256], writes=["Wfm"], semkey="wf0")
            kb.dma(pool, Wfm[:, :, 256:768], wv[:, :, 512:1024], writes=["Wfm"], semkey="wf1")
            for kvh in range(2):
                for dup in range(2):
                    c0 = 768 + kvh * 128 + dup * 64
                    kb.dma(pool, Wfm[:, :, c0:c0 + 64], wv[:, :, 1024 + kvh * 64:1024 + (kvh + 1) * 64],
                           writes=["Wfm"], semkey="wf2_%d%d" % (kvh, dup))
            kb.dma(pool, Wfm[:, :, 1024:1792], wv[:, :, 1536:2304], writes=["Wfm"], semkey="wf3")
            kb.dma(pool, Wtm[:, :, 0:256], wv[:, :, 256:512], writes=["Wtm"], semkey="wt0")
            kb.dma(pool, Wtm[:, :, 256:384], wv[:, :, 1152:1280], writes=["Wtm"], semkey="wt1")
            kb.dma(pool, Wtm[:, :, 384:392], wv[:, :, 2304:2312], writes=["Wtm"], semkey="wt2")
            kb.dma(pool, Wtm[:, :, 392:648], wv[:, :, 1280:1536], writes=["Wtm"], semkey="wt3")
            kb.dma(sp, ropeC[:], c_ropeC, writes=["ropeC"], semkey="rc")
            kb.dma(sp, ropeS[:], c_ropeS, writes=["ropeS"], semkey="rs")
            kb.dma(sp, gvB[:], gvg[l].partition_broadcast(128), writes=["gvB"], semkey="gvB")
            kb.dma(sp, dtbB[:], dtb[l].partition_broadcast(128), writes=["dtbB"], semkey="dtbB")
            kb.dma(sp, aB[:], alog[l].partition_broadcast(128), writes=["aB"], semkey="aB")
            kb.op("act", lambda: nc.scalar.activation(out=aB[:], in_=aB[:], func=AF.Exp), reads=["aB"], writes=["aB"])
            kb.op("dve", lambda: nc.vector.tensor_scalar(out=aB[:], in0=aB[:], scalar1=-1.0, scalar2=None, op0=ALU.mult),
                  reads=["aB"], writes=["aB"])

            bank_rot = [0]

            def next_bank():
                b = bank_rot[0] % 5
                bank_rot[0] += 1
                return b

            def load1(bi):
                t0, T, isctx = BLOCKS[bi]
                s = bi % 2
                kb.dma(sp, xts[s][:, :, :T], xsrc.rearrange("(k p) t -> p k t", p=128)[:, :, t0:t0 + T],
                       writes=[("xt", s)], semkey=("xt", s))

            def norm1(bi):
                t0, T, isctx = BLOCKS[bi]
                s = bi % 2
                which = 1 if isctx else 0
                xt, hT = xts[s], hTs[s]
                for k in range(8):
                    s2 = k % 2
                    kb.op("pool", lambda k=k, s2=s2: nc.gpsimd.tensor_tensor(out=sq[s2][:, :T], in0=xt[:, k, :T], in1=xt[:, k, :T],
                                                                           op=ALU.mult),
                          reads=[("xt", s)], writes=[("sq", s2)])
                    kb.op("pe", lambda k=k, s2=s2: nc.tensor.matmul(PS[7][:, :T], lhsT=ones_bf[:], rhs=sq[s2][:, :T],
                                                                    start=(k == 0), stop=(k == 7)),
                          reads=[("sq", s2), "ones_bf"], writes=["ps7"])
                kb.op("act", lambda: nc.scalar.activation(out=rstd[:, :T], in_=PS[7][:, :T], func=AF.Sqrt,
                                                          bias=eps_t[:], scale=1.0 / D),
                      reads=["ps7", "eps_t"], writes=["rstd"])
                kb.op("dve", lambda: nc.vector.reciprocal(out=rstd[:, :T], in_=rstd[:, :T]), reads=["rstd"], writes=["rstd"])
                for k in range(8):
                    s2 = k % 2
                    kb.op("dve", lambda k=k, s2=s2: nc.vector.scalar_tensor_tensor(
                        out=tmp[s2][:, :T], in0=xt[:, k, :T], scalar=G1[:, l, k, which:which + 1], in1=rstd[:, :T],
                        op0=ALU.mult, op1=ALU.mult), reads=[("xt", s), "G", "rstd"], writes=[("tmp", s2)])
                    kb.op("act", lambda k=k, s2=s2: nc.scalar.activation(
                        out=hT[:, k, :T], in_=tmp[s2][:, :T], func=AF.Identity,
                        bias=mods[:, l, k, which:which + 1], scale=1.0),
                        reads=[("tmp", s2), "mods"], writes=[("hT", s)])

            load1(0)
            load1(1)
            norm1(0)
            for bi, (t0, T, isctx) in enumerate(BLOCKS):
                s = bi % 2
                which = 1 if isctx else 0
                xt, hT = xts[s], hTs[s]
                if bi + 2 < len(BLOCKS):
                    load1(bi + 2)
                sF = stgF[s]
                stageB, stageC = {}, {}

                def chunkA(ci, T=T, hT=hT, s=s, sF=sF, isctx=isctx, t0=t0):
                    b = next_bank()
                    pk = "ps%d" % b
                    for k in range(8):
                        kb.op("pe", lambda k=k: nc.tensor.matmul(
                            PS[b][:, :T], lhsT=Wfm[:, k, ci * 128:(ci + 1) * 128], rhs=hT[:, k, :T],
                            start=(k == 0), stop=(k == 7)), reads=["Wfm", ("hT", s)], writes=[pk])
                    ok = ("stgF", s, ci)
                    if ci < 2:
                        kb.op("act", lambda: nc.scalar.activation(out=sF[:, ci, :T], in_=PS[b][:, :T], func=AF.Gelu_apprx_tanh),
                              reads=[pk], writes=[ok])
                        return
                    if ci >= 8:
                        kb.op("dve", lambda: nc.vector.tensor_copy(out=sF[:, ci, :T], in_=PS[b][:, :T]), reads=[pk], writes=[ok])
                        return
                    qs = ci % 2
                    gcol = qgs if ci < 6 else kgs
                    kb.op("act", lambda: nc.scalar.activation(out=qsq[qs][:, :T], in_=PS[b][:, :T], func=AF.Square),
                          reads=[pk], writes=[("qsq", qs)])

                    def B():
                        kb.op("pe", lambda: nc.tensor.matmul(PS[5][:, :T], lhsT=bones_bf[:], rhs=qsq[qs][:, :T], start=True, stop=True),
                              reads=[("qsq", qs), "bones_bf"], writes=["ps5"])
                        kb.op("act", lambda: nc.scalar.activation(out=qrn[qs][:, :T], in_=PS[5][:, :T], func=AF.Sqrt,
                                                                  bias=eps_t[:], scale=1.0 / 64),
                              reads=["ps5", "eps_t"], writes=[("qrn", qs)])
                        kb.op("dve", lambda: nc.vector.reciprocal(out=qrn[qs][:, :T], in_=qrn[qs][:, :T]),
                              reads=[("qrn", qs)], writes=[("qrn", qs)])
                        if isctx:
                            kb.op("dve", lambda: nc.vector.scalar_tensor_tensor(
                                out=sF[:, ci, :T], in0=PS[b][:, :T], scalar=gcol[:, l:l + 1], in1=qrn[qs][:, :T],
                                op0=ALU.mult, op1=ALU.mult), reads=[pk, ("qrn", qs), "qgs", "kgs"], writes=[ok])
                        else:
                            kb.op("dve", lambda: nc.vector.scalar_tensor_tensor(
                                out=qn[qs][:, :T], in0=PS[b][:, :T], scalar=gcol[:, l:l + 1], in1=qrn[qs][:, :T],
                                op0=ALU.mult, op1=ALU.mult), reads=[pk, ("qrn", qs), "qgs", "kgs"], writes=[("qn", qs)])

                    def C():
                        lt0 = t0 - CL
                        kb.op("pe", lambda: nc.tensor.matmul(PS[6][:, :T], lhsT=perm_bf[:], rhs=qn[qs][:, :T], start=True, stop=True),
                              reads=[("qn", qs), "perm_bf"], writes=["ps6"])
                        kb.op("pool", lambda: nc.gpsimd.tensor_tensor(
                            out=qt1[qs][:, :T], in0=qn[qs][:, :T], in1=ropeC[:, lt0:lt0 + T], op=ALU.mult),
                            reads=[("qn", qs), "ropeC"], writes=[("qt1", qs)])
                        kb.op("dve", lambda: nc.vector.tensor_tensor(
                            out=qt2[qs][:, :T], in0=PS[6][:, :T], in1=ropeS[:, lt0:lt0 + T], op=ALU.mult),
                            reads=["ps6", "ropeS"], writes=[("qt2", qs)])
                        kb.op("pool", lambda: nc.gpsimd.tensor_tensor(
                            out=sF[:, ci, :T], in0=qt1[qs][:, :T], in1=qt2[qs][:, :T], op=ALU.add),
                            reads=[("qt1", qs), ("qt2", qs)], writes=[ok])

                    stageB[ci] = B
                    if not isctx:
                        stageC[ci] = C

                for ci in range(16):
                    if ci < 14:
                        chunkA(ci)
                    if ci == 9 and bi + 1 < len(BLOCKS):
                        norm1(bi + 1)
                    if ci - 1 in stageB:
                        stageB.pop(ci - 1)()
                    if ci - 2 in stageC:
                        stageC.pop(ci - 2)()
                assert not stageB and not stageC
                kb.dma(sp, GU.rearrange("(c p) t -> p c t", p=128)[:, :, t0:t0 + T], sF[:, 0:2, :T],
                       reads=[("stgF", s, ci) for ci in (0, 1)], semkey=("oGU", s))
                kb.dma(sp, QT.rearrange("(c p) t -> p c t", p=128)[:, :, t0:t0 + T], sF[:, 2:6, :T],
                       reads=[("stgF", s, ci) for ci in (2, 3, 4, 5)], semkey=("oQT", s))
                kb.dma(sp, KT.rearrange("(c p) t -> p c t", p=128)[:, :, t0:t0 + T], sF[:, 6:8, :T],
                       reads=[("stgF", s, ci) for ci in (6, 7)], semkey=("oKT", s))
                kb.dma(sp, XBC.rearrange("(c p) t -> p c t", p=128)[:, :, t0:t0 + T], sF[:, 8:14, :T],
                       reads=[("stgF", s, ci) for ci in range(8, 14)], semkey=("oXB", s))

                nsub = T // 128
                for sub in range(nsub):
                    ss_ = sub % 2
                    ba = next_bank()
                    bb = next_bank()
                    pka, pkb = "ps%d" % ba, "ps%d" % bb
                    for k in range(8):
                        kb.op("pe", lambda k=k, sub=sub, ba=ba: nc.tensor.matmul(
                            PS[ba][:, 0:392], lhsT=hT[:, k, sub * 128:(sub + 1) * 128], rhs=Wtm[:, k, 0:392],
                            start=(k == 0), stop=(k == 7)), reads=["Wtm", ("hT", s)], writes=[pka])
                    for k in range(8):
                        kb.op("pe", lambda k=k, sub=sub, bb=bb: nc.tensor.matmul(
                            PS[bb][:, 0:256], lhsT=hT[:, k, sub * 128:(sub + 1) * 128], rhs=Wtm[:, k, 392:648],
                            start=(k == 0), stop=(k == 7)), reads=["Wtm", ("hT", s)], writes=[pkb])
                    kb.op("act", lambda ba=ba, ss_=ss_: nc.scalar.activation(out=gl[ss_][:], in_=PS[ba][:, 0:256],
                                                                             func=AF.Gelu_apprx_tanh),
                          reads=[pka], writes=[("gl", ss_)])
                    kb.op("pool", lambda ss_=ss_: nc.gpsimd.tensor_tensor(out=gsq[ss_][:], in0=gl[ss_][:], in1=gl[ss_][:], op=ALU.mult),
                          reads=[("gl", ss_)], writes=[("gsq", ss_)])
                    kb.op("dve", lambda ss_=ss_: nc.vector.tensor_reduce(
                        out=gss[ss_][:], in_=gsq[ss_][:].rearrange("p (a b) -> p a b", b=64), axis=AX.X, op=ALU.add),
                        reads=[("gsq", ss_)], writes=[("gss", ss_)])
                    kb.op("act", lambda ss_=ss_: nc.scalar.activation(out=gss[ss_][:], in_=gss[ss_][:], func=AF.Sqrt,
                                                                      bias=eps_t[:], scale=1.0 / 64),
                          reads=[("gss", ss_), "eps_t"], writes=[("gss", ss_)])
                    kb.op("dve", lambda ss_=ss_: nc.vector.reciprocal(out=gss[ss_][:], in_=gss[ss_][:]),
                          reads=[("gss", ss_)], writes=[("gss", ss_)])
                    kb.op("dve", lambda ss_=ss_: nc.vector.tensor_tensor(
                        out=gsq[ss_][:].rearrange("p (a b) -> p a b", b=64), in0=gl[ss_][:].rearrange("p (a b) -> p a b", b=64),
                        in1=gss[ss_][:].unsqueeze(2).broadcast_to([128, 4, 64]), op=ALU.mult),
                        reads=[("gl", ss_), ("gss", ss_)], writes=[("gsq", ss_)])
                    kb.op("pool", lambda ss_=ss_, sub=sub: nc.gpsimd.tensor_tensor(
                        out=stgGV[s][:, sub, :], in0=gsq[ss_][:], in1=gvB[:], op=ALU.mult),
                        reads=[("gsq", ss_), "gvB"], writes=[("stgGV", s)])
                    kb.op("act", lambda ba=ba, sub=sub: nc.scalar.copy(out=stgV[s][:, sub, :], in_=PS[ba][:, 256:384]),
                          reads=[pka], writes=[("stgV", s)])
                    kb.op("dve", lambda ba=ba, ss_=ss_: nc.vector.tensor_tensor(out=dtx[ss_][:], in0=PS[ba][:, 384:392], in1=dtbB[:],
                                                                               op=ALU.add),
                          reads=[pka, "dtbB"], writes=[("dtx", ss_)])
                    kb.op("act", lambda ss_=ss_: nc.scalar.activation(out=dtx[ss_][:], in_=dtx[ss_][:], func=AF.Exp),
                          reads=[("dtx", ss_)], writes=[("dtx", ss_)])
                    kb.op("act", lambda ss_=ss_, sub=sub: nc.scalar.activation(out=stgDT[s][:, sub, 0:8], in_=dtx[ss_][:], func=AF.Ln,
                                                                               bias=one_t[:], scale=1.0),
                          reads=[("dtx", ss_), "one_t"], writes=[("stgDT", s)])
                    kb.op("dve", lambda sub=sub: nc.vector.tensor_tensor(out=stgDT[s][:, sub, 8:16], in0=stgDT[s][:, sub, 0:8],
                                                                         in1=aB[:], op=ALU.mult),
                          reads=[("stgDT", s), "aB"], writes=[("stgDT", s)])
                    kb.op("act", lambda bb=bb, sub=sub: nc.scalar.activation(out=stgZ[s][:, sub, :], in_=PS[bb][:, 0:256], func=AF.Silu),
                          reads=[pkb], writes=[("stgZ", s)])
                kb.dma(sp, GV[t0:t0 + T, :].rearrange("(s p) c -> p s c", p=128), stgGV[s][:, 0:nsub, :],
                       reads=[("stgGV", s)], semkey=("oGV", s))
                kb.dma(sp, V[t0:t0 + T, :].rearrange("(s p) c -> p s c", p=128), stgV[s][:, 0:nsub, :],
                       reads=[("stgV", s)], semkey=("oV", s))
                kb.dma(sp, ZS[t0:t0 + T, :].rearrange("(s p) c -> p s c", p=128), stgZ[s][:, 0:nsub, :],
                       reads=[("stgZ", s)], semkey=("oZS", s))
                kb.dma(sp, DT[t0:t0 + T, :].rearrange("(s p) c -> p s c", p=128), stgDT[s][:, 0:nsub, :],
                       reads=[("stgDT", s)], semkey=("oDT", s))
            kb.barrier()

        with contextlib.ExitStack() as st:
            wsb = sb(st, "wsb", [128, 4, 128], BF16)
            bsr = sb(st, "bsr", [128, 2, 128], F32)
            gtmp = [sb(st, "gtmp%d" % i, [128, 2, 128], F32) for i in range(2)]
            guT = [sb(st, "guT%d" % i, [128, 2, 512], BF16) for i in range(2)]
            vh = [sb(st, "vh%d" % i, [128, 4, 256], BF16) for i in range(2)]
            stg = [sb(st, "gstg%d" % i, [128, 2, 512], BF16) for i in range(2)]
            kb.dma(pool, wsb[:], wsT[l].rearrange("h j i -> j h i"), writes=["wsb"], semkey="wsb")
            kb.dma(sp, bsr[:], gbs[:, l, :, :], writes=["bsr"], semkey="bsr")
            blks = [b for b in BLOCKS if (need_ctx or not b[2])]
            def load2a(bi):
                t0, T, isctx = blks[bi]
                s = bi % 2
                nsub = T // 128
                kb.dma(sp, guT[s][:, :, :T], GU.rearrange("(c p) t -> p c t", p=128)[:, :, t0:t0 + T],
                       writes=[("guT", s)], semkey=("guT", s))
                kb.dma(sp, vh[s][:, 0:nsub, :], GV[t0:t0 + T, :].rearrange("(s p) c -> p s c", p=128),
                       writes=[("vh", s)], semkey=("vh", s))

            load2a(0)
            for bi, (t0, T, isctx) in enumerate(blks):
                s = bi % 2
                nsub = T // 128
                if bi + 1 < len(blks):
                    load2a(bi + 1)
                for sub in range(nsub):
                    b = sub % 4
                    pk = "ps%d" % b
                    for j in range(2):
                        for hh in range(2):
                            h = 2 * j + hh
                            kb.op("pe", lambda b=b, j=j, hh=hh, h=h, sub=sub: nc.tensor.matmul(
                                PS[b][hh * 64:(hh + 1) * 64, j * 128:(j + 1) * 128], lhsT=vh[s][:, sub, h * 64:(h + 1) * 64],
                                rhs=wsb[:, h, :], start=True, stop=True), reads=[("vh", s), "wsb"], writes=[pk])
                    g2 = sub % 2
                    kb.op("dve", lambda b=b, g2=g2: nc.vector.tensor_tensor(
                        out=gtmp[g2][:], in0=PS[b][:, 0:256].rearrange("p (a b) -> p a b", b=128),
                        in1=bsr[:], op=ALU.add), reads=[pk, "bsr"], writes=[("gtmp", g2)])
                    kb.op("pool", lambda sub=sub, g2=g2: nc.gpsimd.tensor_tensor(
                        out=stg[s][:, :, sub * 128:(sub + 1) * 128], in0=gtmp[g2][:],
                        in1=guT[s][:, :, sub * 128:(sub + 1) * 128], op=ALU.mult),
                        reads=[("gtmp", g2), ("guT", s)], writes=[("gstg", s)])
                kb.dma(sp, MIX.rearrange("(c p) t -> p c t", p=128)[:, 0:2, t0:t0 + T], stg[s][:, :, :T],
                       reads=[("gstg", s)], semkey=("oMIXg", s))
            kb.barrier()

        with contextlib.ExitStack() as st:
            QTs = sb(st, "QTs", [128, 4, NT], BF16)
            KTz = sb(st, "KTz", [128, 2, 2, NT], BF16)
            Vs = sb(st, "Vs", [128, NTILE, 128], BF16)
            onesv = sb(st, "onesv", [128, 64], BF16)
            Es = [sb(st, "E%d" % i, [128, 512], BF16) for i in range(4)]
            den = [sb(st, "den%d" % i, [128, 256], F32) for i in range(2)]
            stg = [sb(st, "astg%d" % i, [128, 4, 512], BF16) for i in range(2)]
            kb.op("dve", lambda: nc.vector.memset(onesv[:], 1.0), writes=["onesv"])
            for c4 in range(4):
                kb.dma(sp, QTs[:, c4, :], QT[c4 * 128:(c4 + 1) * 128, :], writes=["QTs"], semkey=("QTs", c4))
            kb.op("pool", lambda: nc.gpsimd.memset(KTz[:], 0.0), writes=["KTs"])
            for c2 in range(2):
                for half in range(2):
                    kb.dma(sp, KTz[half * 64:(half + 1) * 64, c2, half, :],
                           KT[c2 * 128 + half * 64:c2 * 128 + (half + 1) * 64, :], writes=["KTs"], semkey=("KTs", c2, half))
            for c8 in range(0, NTILE, 6):
                c9 = min(NTILE, c8 + 6)
                kb.dma(sp, Vs[:, c8:c9, :], V[c8 * 128:c9 * 128, :].rearrange("(s p) c -> p s c", p=128),
                       writes=["Vs"], semkey=("Vs", c8))
            srot = [0]
            od = [0]
            blks = [b for b in BLOCKS if (need_ctx or not b[2])]
            for bi, (t0, T, isctx) in enumerate(blks):
                s = bi % 2
                nsub = T // 128
                for sub in range(nsub):
                    tq = t0 // 128 + sub
                    if isctx:
                        kts = [(0, None), (1, None)]
                    else:
                        n = tq - 2
                        kts = [(0, None), (1, None)]
                        if n > 0:
                            kts.append((tq - 1, ub_bf))
                        kts.append((tq, None))
                        if n < 31:
                            kts.append((tq + 1, uf_bf))
                    for kv in range(2):
                        ob = 4 + (od[0] % 2)
                        db = 6 + (od[0] % 2)
                        od[0] += 1
                        okey, dkey = "ps%d" % ob, "ps%d" % db
                        slots = {}

                        def emitS(i, kv=kv, tq=tq, kts=kts, slots=slots):
                            kt, msk = kts[i]
                            sl = srot[0] % 4
                            srot[0] += 1
                            slots[i] = sl
                            pk = "ps%d" % sl
                            for half in range(2):
                                kb.op("pe", lambda half=half, sl=sl, kt=kt: nc.tensor.matmul(
                                    PS[sl][:, half * 256:(half + 1) * 256].rearrange("p (a b) -> p a b", b=128),
                                    lhsT=KTz[:, kv, half, kt * 128:(kt + 1) * 128],
                                    rhs=QTs[:, 2 * kv:2 * kv + 2, tq * 128:(tq + 1) * 128],
                                    start=True, stop=True), reads=["KTs", "QTs"], writes=[pk])
                            kb.op("act", lambda sl=sl: nc.scalar.activation(out=Es[sl][:], in_=PS[sl][:], func=AF.Exp, scale=0.125),
                                  reads=[pk], writes=[("E", sl)])
                            if msk is not None:
                                kb.op("pool", lambda sl=sl, msk=msk: nc.gpsimd.tensor_tensor(
                                    out=Es[sl][:].rearrange("p (a b) -> p a b", b=128),
                                    in0=Es[sl][:].rearrange("p (a b) -> p a b", b=128),
                                    in1=msk[:].unsqueeze(1).broadcast_to([128, 4, 128]), op=ALU.mult),
                                    reads=[("E", sl), "uf_bf", "ub_bf"], writes=[("E", sl)])

                        def emitPV(i, kv=kv, kts=kts, slots=slots, ob=ob, db=db, okey=okey, dkey=dkey):
                            kt, _ = kts[i]
                            sl = slots[i]
                            first, last = (i == 0), (i == len(kts) - 1)
                            for half in range(2):
                                kb.op("pe", lambda half=half, sl=sl, kt=kt: nc.tensor.matmul(
                                    PS[ob][half * 64:(half + 1) * 64, 0:256], lhsT=Vs[:, kt, kv * 64:(kv + 1) * 64],
                                    rhs=Es[sl][:, half * 256:(half + 1) * 256], start=first, stop=last),
                                    reads=["Vs", ("E", sl)], writes=[okey])
                                kb.op("pe", lambda half=half, sl=sl: nc.tensor.matmul(
                                    PS[db][half * 64:(half + 1) * 64, 0:256], lhsT=onesv[:],
                                    rhs=Es[sl][:, half * 256:(half + 1) * 256], start=first, stop=last),
                                    reads=["onesv", ("E", sl)], writes=[dkey])

                        nk = len(kts)
                        emitS(0)
                        if nk > 1:
                            emitS(1)
                        for i in range(nk):
                            emitPV(i)
                            if i + 2 < nk:
                                emitS(i + 2)
                        dn = den[kv]
                        kb.op("dve", lambda db=db, dn=dn, kv=kv: nc.vector.tensor_tensor(
                            out=dn[:].rearrange("p (a b) -> p a b", b=128),
                            in0=PS[db][:, 0:256].rearrange("p (a b) -> p a b", b=128),
                            in1=sinkE[:, l, kv, :].unsqueeze(2).broadcast_to([128, 2, 128]), op=ALU.add),
                            reads=[dkey, "sinkE"], writes=[("den", kv)])
                        kb.op("dve", lambda dn=dn: nc.vector.reciprocal(out=dn[:], in_=dn[:]),
                              reads=[("den", kv)], writes=[("den", kv)])
                        kb.op("dve", lambda ob=ob, dn=dn, kv=kv, sub=sub: nc.vector.tensor_tensor(
                            out=stg[s][:, 2 * kv:2 * kv + 2, sub * 128:(sub + 1) * 128],
                            in0=PS[ob][:, 0:256].rearrange("p (a b) -> p a b", b=128),
                            in1=dn[:].rearrange("p (a b) -> p a b", b=128), op=ALU.mult),
                            reads=[okey, ("den", kv)], writes=[("astg", s)])
                kb.dma(sp, MIX.rearrange("(c p) t -> p c t", p=128)[:, 2:6, t0:t0 + T], stg[s][:, :, :T],
                       reads=[("astg", s)], semkey=("oMIXa", s))
            kb.barrier()

        with contextlib.ExitStack() as st:
            XC = sb(st, "XC", [128, 6, NT], BF16)
            Xtm = sb(st, "Xtm", [128, NTILE, 256], BF16)
            Btm = sb(st, "Btm", [128, NTILE, 256], BF16)
            Ytm = sb(st, "Ytm", [128, NTILE, 256], F32)
            DTs = sb(st, "DTs", [128, NTILE, 16], F32)
            ZSs = sb(st, "ZSs", [128, NTILE, 256], BF16)
            Sf = sb(st, "Sf", [128, 256], F32)
            Sb_ = sb(st, "Sb", [128, 256], BF16)
            negf = sb(st, "negf", [128, 512], BF16)
            negb = sb(st, "negb", [128, 512], BF16)
            dskB = sb(st, "dskB", [128, 256], F32)
            ngB = sb(st, "ngB", [128, 256], F32)
            XB = [sb(st, "XB%d" % i, [128, 6, 516], BF16) for i in range(2)]
            cacc = [sb(st, "cacc%d" % i, [128, 512], F32) for i in range(2)]
            rhsA = [sb(st, "rhsA%d" % i, [128, 512], BF16) for i in range(2)]
            adtb = [sb(st, "adtb%d" % i, [128, 4], BF16) for i in range(2)]
            col = [sb(st, "col%d" % i, [128, 4], F32) for i in range(2)]
            Dm = [sb(st, "Dm%d" % i, [128, 512], F32) for i in range(2)]
            Lm = [sb(st, "Lm%d" % i, [128, 512], BF16) for i in range(2)]
            MT = [sb(st, "MT%d" % i, [128, 512], BF16) for i in range(2)]
            sm = [sb(st, "sm%d" % i, [128, 16], F32) for i in range(2)]
            xw = [sb(st, "xw%d" % i, [128, 256], BF16) for i in range(2)]
            xdt = [sb(st, "xdt%d" % i, [128, 256], BF16) for i in range(2)]
            yt1 = [sb(st, "yt1%d" % i, [128, 256], F32) for i in range(2)]
            yt2 = [sb(st, "yt2%d" % i, [128, 256], F32) for i in range(2)]
            tS = sb(st, "tS", [128, 256], F32)
            Sf2 = sb(st, "Sf2", [128, 256], F32)
            Sb2 = sb(st, "Sb2", [128, 256], BF16)
            tS2 = sb(st, "tS2", [128, 256], F32)
            yz = [sb(st, "yz%d" % i, [128, 256], F32) for i in range(2)]
            yjunk = sb(st, "yjunk", [128, 256], F32)
            yss = [sb(st, "yss%d" % i, [128, 1], F32) for i in range(2)]
            yo = [sb(st, "yo%d" % i, [128, 256], BF16) for i in range(2)]
            stg = [sb(st, "sstg%d" % i, [128, 2, 512], BF16) for i in range(2)]

            kb.dma(pool, negf[:], c_negf, writes=["negf"], semkey="negf")
            kb.dma(pool, negb[:], c_negb, writes=["negb"], semkey="negb")
            kb.dma(sp, dskB[:], dskE[l].partition_broadcast(128), writes=["dskB"], semkey="dskB")
            kb.dma(sp, ngB[:], sng[l].partition_broadcast(128), writes=["ngB"], semkey="ngB")
            for c8 in range(0, NTILE, 6):
                c9 = min(NTILE, c8 + 6)
                kb.dma(sp, DTs[:, c8:c9, :], DT[c8 * 128:c9 * 128, :].rearrange("(s p) c -> p s c", p=128),
                       writes=["DTs"], semkey=("DTs", c8))
                kb.dma(sp, ZSs[:, c8:c9, :], ZS[c8 * 128:c9 * 128, :].rearrange("(s p) c -> p s c", p=128),
                       writes=["ZSs"], semkey=("ZSs", c8))

            def loadxb(bi):
                t0, T, isctx = BLOCKS[bi]
                s = bi % 2
                seg0, seg1 = (0, CL) if isctx else (CL, NT)
                lo, hi = max(t0 - 2, seg0), min(t0 + T + 2, seg1)
                xb = XB[s]
                wr = [("XB", s)]
                if lo > t0 - 2:
                    kb.op("pool", lambda xb=xb: nc.gpsimd.memset(xb[:, :, 0:2], 0.0), writes=wr)
                if hi < t0 + T + 2:
                    kb.op("pool", lambda xb=xb, T=T: nc.gpsimd.memset(xb[:, :, T + 2:T + 4], 0.0), writes=wr)
                kb.dma(sp, xb[:, :, lo - (t0 - 2):hi - (t0 - 2)], XBC.rearrange("(c p) t -> p c t", p=128)[:, :, lo:hi],
                       writes=wr, semkey=("XB", s))

            Dg = sb(st, "Dg", [128, 6, 5, 128], BF16)
            for j in range(6):
                for k in range(5):
                    kb.op("dve", lambda j=j, k=k: nc.vector.tensor_scalar(
                        out=Dg[:, j, k, :], in0=ident_f[:], scalar1=cws[:, l, j, k:k + 1], scalar2=None, op0=ALU.mult),
                        reads=["ident_f", "cws"], writes=["Dg"])
            cbank = [0]
            loadxb(0)
            for bi, (t0, T, isctx) in enumerate(BLOCKS):
                s = bi % 2
                xb = XB[s]
                wr = [("XB", s)]
                if bi + 1 < len(BLOCKS):
                    loadxb(bi + 1)
                for j in range(6):
                    b = cbank[0] % 6
                    cbank[0] += 1
                    pk = "ps%d" % b
                    for k in range(5):
                        kb.op("pe", lambda j=j, k=k, b=b, xb=xb, T=T: nc.tensor.matmul(
                            PS[b][:, :T], lhsT=Dg[:, j, k, :], rhs=xb[:, j, k:k + T], start=(k == 0), stop=(k == 4)),
                            reads=wr + ["Dg"], writes=[pk])
                    kb.op("act", lambda j=j, b=b, T=T, t0=t0: nc.scalar.activation(
                        out=XC[:, j, t0:t0 + T], in_=PS[b][:, :T], func=AF.Silu, bias=cbs[:, l, j:j + 1], scale=1.0),
                        reads=[pk, "cbs"], writes=[("XC", bi)])
            kb.barrier()

            for c in range(NTILE):
                b = 6 + (c % 2)
                pk = "ps%d" % b
                psb = PS[b][:].bitcast(BF16)
                for q4, j in enumerate((0, 1, 2, 3)):
                    kb.op("pe", lambda q4=q4, j=j, c=c, psb=psb: nc.tensor.transpose(
                        out=psb[:, q4 * 128:(q4 + 1) * 128], in_=XC[:, j, c * 128:(c + 1) * 128], identity=ident_bf[:]),
                        reads=["XC", "ident_bf"], writes=[pk])
                kb.op("act", lambda c=c, psb=psb: nc.scalar.copy(out=Xtm[:, c, :], in_=psb[:, 0:256]), reads=[pk], writes=[("Xtm", c)])
                kb.op("act", lambda c=c, psb=psb: nc.scalar.copy(out=Btm[:, c, :], in_=psb[:, 256:512]), reads=[pk], writes=[("Btm", c)])
                kb.op("pool", lambda c=c: nc.gpsimd.tensor_tensor(out=Ytm[:, c, :], in0=Xtm[:, c, :], in1=dskB[:], op=ALU.mult),
                      reads=[("Xtm", c), "dskB"], writes=[("Ytm", c)])

            kb.barrier()
            it = [0]
            Sfs, Sbs, tSs = [Sf, Sf2], [Sb_, Sb2], [tS, tS2]
            for d in range(2):
                kb.op("dve", lambda d=d: nc.vector.memset(Sfs[d][:], 0.0), writes=[("Sf", d)])
                kb.op("pool", lambda d=d: nc.gpsimd.memset(Sbs[d][:], 0.0), writes=[("Sb", d)])

            def scan_step(d, c):
                U = uf_bf if d == 0 else ub_bf
                NEG = negf if d == 0 else negb
                last = 127 if d == 0 else 0
                Sfd, Sbd, tSd = Sfs[d], Sbs[d], tSs[d]
                kSf, kSb, ktS = ("Sf", d), ("Sb", d), ("tS", d)
                b2, b5, by = (2, 5, 3) if d == 0 else (6, 7, 4)
                k2, k5, ky = "ps%d" % b2, "ps%d" % b5, "ps%d" % by
                want_y = need_ctx or c >= 2
                s = d
                rb = d
                rk = "ps%d" % rb
                dt4 = DTs[:, c, d * 4:(d + 1) * 4]
                adt4 = DTs[:, c, 8 + d * 4:12 + d * 4]
                smk = ("sm", s)
                tot = PS[rb][:].rearrange("p (a b) -> p a b", b=128)[:, :, last]
                kb.op("pool", lambda: nc.gpsimd.tensor_tensor(
                    out=rhsA[s][:].rearrange("p (a b) -> p a b", b=128),
                    in0=U[:].unsqueeze(1).broadcast_to([128, 4, 128]),
                    in1=adt4.unsqueeze(2).broadcast_to([128, 4, 128]), op=ALU.mult),
                    reads=["DTs", "uf_bf", "ub_bf"], writes=[("rhsA", s)])
                kb.op("act", lambda: nc.scalar.copy(out=adtb[s][:], in_=adt4), reads=["DTs"], writes=[("adtb", s)])
                kb.op("pe", lambda: nc.tensor.matmul(PS[rb][:], lhsT=ones_bf[:], rhs=rhsA[s][:], start=True, stop=False),
                      reads=[("rhsA", s), "ones_bf"], writes=[rk])
                kb.op("pe", lambda: nc.tensor.matmul(PS[rb][:], lhsT=ident_bf[:], rhs=NEG[:], start=False, stop=True),
                      reads=["negf", "negb", "ident_bf"], writes=[rk])
                kb.op("pe", lambda: nc.tensor.matmul(PS[b2][:, 256:260], lhsT=U[:], rhs=adtb[s][:], start=True, stop=True),
                      reads=[("adtb", s), "uf_bf", "ub_bf"], writes=[k2])
                for g in range(2):
                    kb.op("pe", lambda g=g: nc.tensor.matmul(
                        PS[b2][:, g * 128:(g + 1) * 128], lhsT=XC[:, 2 + g, c * 128:(c + 1) * 128],
                        rhs=XC[:, 4 + g, c * 128:(c + 1) * 128], start=True, stop=True), reads=["XC"], writes=[k2])
                yield
                kb.op("act", lambda: nc.scalar.copy(out=col[s][:], in_=PS[b2][:, 256:260]), reads=[k2], writes=[("col", s)])
                kb.op("dve", lambda: nc.vector.tensor_tensor(
                    out=Dm[s][:].rearrange("p (a b) -> p a b", b=128), in0=PS[rb][:].rearrange("p (a b) -> p a b", b=128),
                    in1=col[s][:].unsqueeze(2).broadcast_to([128, 4, 128]), op=ALU.subtract),
                    reads=[rk, ("col", s)], writes=[("Dm", s)])
                kb.op("dve", lambda: nc.vector.tensor_tensor(out=sm[s][:, 0:4], in0=tot, in1=col[s][:], op=ALU.subtract),
                      reads=[rk, ("col", s)], writes=[smk])
                yield
                kb.op("act", lambda: nc.scalar.activation(out=Lm[s][:], in_=Dm[s][:], func=AF.Exp),
                      reads=[("Dm", s)], writes=[("Lm", s)])
                kb.op("act", lambda: nc.scalar.activation(out=sm[s][:, 0:4], in_=sm[s][:, 0:4], func=AF.Exp), reads=[smk], writes=[smk])
                kb.op("act", lambda: nc.scalar.activation(out=sm[s][:, 4:8], in_=col[s][:], func=AF.Exp), reads=[("col", s)], writes=[smk])
                kb.op("act", lambda: nc.scalar.activation(out=sm[s][:, 8:12], in_=tot, func=AF.Exp), reads=[rk], writes=[smk])
                yield
                kb.op("dve", lambda: nc.vector.tensor_tensor(
                    out=MT[s][:].rearrange("p (g h i) -> p g h i", g=2, h=2),
                    in0=Lm[s][:].rearrange("p (g h i) -> p g h i", g=2, h=2),
                    in1=PS[b2][:, 0:256].rearrange("p (g i) -> p g i", g=2).unsqueeze(2).broadcast_to([128, 2, 2, 128]),
                    op=ALU.mult), reads=[("Lm", s), k2], writes=[("MT", s)])
                kb.op("dve", lambda: nc.vector.tensor_tensor(out=sm[s][:, 0:4], in0=sm[s][:, 0:4], in1=dt4, op=ALU.mult),
                      reads=[smk, "DTs"], writes=[smk])
                kb.op("dve", lambda: nc.vector.tensor_tensor(
                    out=xw[s][:].rearrange("p (a b) -> p a b", b=64), in0=Xtm[:, c, :].rearrange("p (a b) -> p a b", b=64),
                    in1=sm[s][:, 0:4].unsqueeze(2).broadcast_to([128, 4, 64]), op=ALU.mult),
                    reads=[("Xtm", c), smk], writes=[("xw", s)])
                if want_y:
                    kb.op("pool", lambda: nc.gpsimd.tensor_tensor(
                        out=xdt[s][:].rearrange("p (a b) -> p a b", b=64), in0=Xtm[:, c, :].rearrange("p (a b) -> p a b", b=64),
                        in1=dt4.unsqueeze(2).broadcast_to([128, 4, 64]), op=ALU.mult),
                        reads=[("Xtm", c), "DTs"], writes=[("xdt", s)])
                yield
                if want_y:
                    for h in range(4):
                        kb.op("pe", lambda h=h: nc.tensor.matmul(
                            PS[by][:, h * 64:(h + 1) * 64], lhsT=MT[s][:, h * 128:(h + 1) * 128], rhs=xdt[s][:, h * 64:(h + 1) * 64],
                            start=True, stop=True), reads=[("MT", s), ("xdt", s)], writes=[ky])
                    for g in range(2):
                        kb.op("pe", lambda g=g: nc.tensor.matmul(
                            PS[by][:, 256 + g * 128:256 + (g + 1) * 128], lhsT=XC[:, 4 + g, c * 128:(c + 1) * 128],
                            rhs=Sbd[:, g * 128:(g + 1) * 128], start=True, stop=True), reads=["XC", kSb], writes=[ky])
                for g in range(2):
                    kb.op("pe", lambda g=g: nc.tensor.matmul(
                        PS[b5][:, g * 128:(g + 1) * 128], lhsT=Btm[:, c, g * 128:(g + 1) * 128],
                        rhs=xw[s][:, g * 128:(g + 1) * 128], start=True, stop=True), reads=[("Btm", c), ("xw", s)], writes=[k5])
                yield
                kb.op("pool", lambda: nc.gpsimd.tensor_tensor(
                    out=tSd[:].rearrange("p (a b) -> p a b", b=64), in0=Sfd[:].rearrange("p (a b) -> p a b", b=64),
                    in1=sm[s][:, 8:12].unsqueeze(2).broadcast_to([128, 4, 64]), op=ALU.mult),
                    reads=[kSf, smk], writes=[ktS])
                kb.op("dve", lambda: nc.vector.tensor_tensor(out=Sfd[:], in0=PS[b5][:, 0:256], in1=tSd[:], op=ALU.add),
                      reads=[k5, ktS], writes=[kSf])
                kb.op("act", lambda: nc.scalar.copy(out=Sbd[:], in_=Sfd[:]), reads=[kSf], writes=[kSb])
                if want_y:
                    kb.op("dve", lambda: nc.vector.tensor_tensor(
                        out=yt1[s][:].rearrange("p (a b) -> p a b", b=64), in0=PS[by][:, 256:512].rearrange("p (a b) -> p a b", b=64),
                        in1=sm[s][:, 4:8].unsqueeze(2).broadcast_to([128, 4, 64]), op=ALU.mult),
                        reads=[ky, smk], writes=[("yt1", s)])
                    kb.op("dve", lambda: nc.vector.tensor_tensor(out=yt2[s][:], in0=PS[by][:, 0:256], in1=yt1[s][:], op=ALU.add),
                          reads=[ky, ("yt1", s)], writes=[("yt2", s)])
                    kb.op("pool", lambda: nc.gpsimd.tensor_tensor(out=Ytm[:, c, :], in0=Ytm[:, c, :], in1=yt2[s][:], op=ALU.add),
                          reads=[("yt2", s), ("Ytm", c)], writes=[("Ytm", c)])

            orders = [list(range(NTILE)), [1, 0] + list(range(NTILE - 1, 1, -1))]
            for i in range(NTILE):
                gens = [scan_step(d, orders[d][i]) for d in range(2)]
                alive = True
                while alive:
                    alive = False
                    for g_ in gens:
                        try:
                            next(g_)
                            alive = True
                        except StopIteration:
                            pass
            kb.barrier()
            blks = [b for b in BLOCKS if (need_ctx or not b[2])]
            for bi, (t0, T, isctx) in enumerate(blks):
                sg = bi % 2
                nsub = T // 128
                for sub in range(nsub):
                    c = t0 // 128 + sub
                    s = sub % 2
                    kb.op("pool", lambda c=c, s=s: nc.gpsimd.tensor_tensor(out=yz[s][:], in0=Ytm[:, c, :], in1=ZSs[:, c, :], op=ALU.mult),
                          reads=[("Ytm", c), "ZSs"], writes=[("yz", s)])
                    kb.op("act", lambda s=s: nc.scalar.activation(out=yjunk[:], in_=yz[s][:], func=AF.Square, accum_out=yss[s][:]),
                          reads=[("yz", s)], writes=["yjunk", ("yss", s)])
                    kb.op("act", lambda s=s: nc.scalar.activation(out=yss[s][:], in_=yss[s][:], func=AF.Sqrt, bias=eps_t[:], scale=1.0 / 256),
                          reads=[("yss", s), "eps_t"], writes=[("yss", s)])
                    kb.op("dve", lambda s=s: nc.vector.reciprocal(out=yss[s][:], in_=yss[s][:]), reads=[("yss", s)], writes=[("yss", s)])
                    kb.op("dve", lambda s=s: nc.vector.scalar_tensor_tensor(
                        out=yo[s][:], in0=yz[s][:], scalar=yss[s][:, 0:1], in1=ngB[:], op0=ALU.mult, op1=ALU.mult),
                        reads=[("yz", s), ("yss", s), "ngB"], writes=[("yo", s)])
                    b = 6 + s
                    pk = "ps%d" % b
                    psb = PS[b][:].bitcast(BF16)
                    for j in range(2):
                        kb.op("pe", lambda j=j, s=s, psb=psb: nc.tensor.transpose(
                            out=psb[:, j * 128:(j + 1) * 128], in_=yo[s][:, j * 128:(j + 1) * 128], identity=ident_bf[:]),
                            reads=[("yo", s), "ident_bf"], writes=[pk])
                    kb.op("act", lambda sub=sub, psb=psb, sg=sg: nc.scalar.copy(
                        out=stg[sg][:, :, sub * 128:(sub + 1) * 128], in_=psb[:, 0:256].rearrange("p (a b) -> p a b", b=128)),
                        reads=[pk], writes=[("sstg", sg)])
                kb.dma(sp, MIX.rearrange("(c p) t -> p c t", p=128)[:, 6:8, t0:t0 + T], stg[sg][:, :, :T],
                       reads=[("sstg", sg)], semkey=("oMIXs", sg))
            kb.barrier()

        with contextlib.ExitStack() as st:
            Wo = sb(st, "Wo", [128, 8, D], BF16)
            mix = [sb(st, "mix%d" % i, [128, 8, 512], BF16) for i in range(2)]
            xts = [sb(st, "xt%d" % i, [128, 8, 512], F32) for i in range(2)]
            hTs = [sb(st, "hTs%d" % i, [128, 8, 512], BF16) for i in range(2)]
            sq = [sb(st, "sq%d" % i, [128, 512], BF16) for i in range(2)]
            tmp = [sb(st, "tmp%d" % i, [128, 512], F32) for i in range(2)]
            rstd = sb(st, "rstd", [128, 512], F32)
            if is_moe:
                hf = sb(st, "hf", [128, 8, 512], F32)
                wr_ = sb(st, "wr", [128, 8, NEXP], F32)
                lg = [sb(st, "lg%d" % i, [128, 8], F32) for i in range(2)]
                mx = [sb(st, "mx%d" % i, [128, 8], F32) for i in range(2)]
                ee = [sb(st, "ee%d" % i, [128, 8], F32) for i in range(2)]
                mk = [sb(st, "mk%d" % i, [128, 8], F32) for i in range(2)]
                r2 = [sb(st, "r2%d" % i, [128, 1], F32) for i in range(2)]
                cmb = [sb(st, "cmb%d" % i, [128, 8], F32) for i in range(2)]
                cstg = [sb(st, "cstg%d" % i, [8, 512], F32) for i in range(2)]
                kb.dma(sp, wr_[:], moe_r[jl].rearrange("(k p) e -> p k e", p=128), writes=["wr"], semkey="wr")
            kb.dma(pool, Wo[:], w_out[l].rearrange("(k p) n -> p k n", p=128), writes=["Wo"], semkey="Wo")
            blks = [b for b in BLOCKS if (need_ctx or not b[2])]
            brot = [0]
            def load3(bi):
                t0, T, isctx = blks[bi]
                s = bi % 2
                kb.dma(sp, mix[s][:, :, :T], MIX.rearrange("(c p) t -> p c t", p=128)[:, :, t0:t0 + T],
                       writes=[("mix", s)], semkey=("mix", s))
                kb.dma(sp, xts[s][:, :, :T], xsrc.rearrange("(k p) t -> p k t", p=128)[:, :, t0:t0 + T],
                       writes=[("xt", s)], semkey=("xt3", s))

            load3(0)
            pending_router = []
            for bi, (t0, T, isctx) in enumerate(blks):
                s = bi % 2
                which = 1 if isctx else 0
                xt = xts[s]
                if bi + 1 < len(blks):
                    load3(bi + 1)
                for co in range(8):
                    b = brot[0] % 4
                    brot[0] += 1
                    pk = "ps%d" % b
                    for k in range(8):
                        kb.op("pe", lambda k=k, co=co, b=b: nc.tensor.matmul(
                            PS[b][:, :T], lhsT=Wo[:, k, co * 128:(co + 1) * 128], rhs=mix[s][:, k, :T],
                            start=(k == 0), stop=(k == 7)), reads=["Wo", ("mix", s)], writes=[pk])
                    kb.op("dve", lambda co=co, b=b: nc.vector.scalar_tensor_tensor(
                        out=xt[:, co, :T], in0=PS[b][:, :T], scalar=mods[:, l, 16 + co, which:which + 1], in1=xt[:, co, :T],
                        op0=ALU.mult, op1=ALU.add), reads=[pk, "mods", ("xt", s)], writes=[("xt", s)])
                kb.dma(sp, R.rearrange("(k p) t -> p k t", p=128)[:, :, t0:t0 + T], xt[:, :, :T],
                       reads=[("xt", s)], semkey=("oR3", s))
                if pending_router:
                    pending_router.pop()()
                rmsnorm_block((sq, rstd, tmp), xt, T, G2, l, 24, which, hTs[s], ("hTs", s), hf=(hf if is_moe else None),
                              xk=("xt", s))
                kb.dma(sp, HT.rearrange("(k p) t -> p k t", p=128)[:, :, t0:t0 + T], hTs[s][:, :, :T],
                       reads=[("hTs", s)], semkey=("oHT", s))
                if is_moe:
                    def router(T=T, t0=t0, s=s):
                        nsub = T // 128
                        for sub in range(nsub):
                            s2 = sub % 2
                            for k in range(8):
                                kb.op("pe", lambda k=k, sub=sub: nc.tensor.matmul(
                                    PS[4][:, 0:8], lhsT=hf[:, k, sub * 128:(sub + 1) * 128], rhs=wr_[:, k, :],
                                    start=(k == 0), stop=(k == 7)), reads=["hf", "wr"], writes=["ps4"])
                            kb.op("act", lambda s2=s2: nc.scalar.copy(out=lg[s2][:], in_=PS[4][:, 0:8]), reads=["ps4"], writes=[("lg", s2)])
                            kb.op("dve", lambda s2=s2: nc.vector.max(out=mx[s2][:], in_=lg[s2][:]), reads=[("lg", s2)], writes=[("mx", s2)])
                            kb.op("dve", lambda s2=s2: nc.vector.tensor_scalar(out=ee[s2][:], in0=lg[s2][:], scalar1=mx[s2][:, 0:1], scalar2=None,
                                                                              op0=ALU.subtract), reads=[("lg", s2), ("mx", s2)], writes=[("ee", s2)])
                            kb.op("act", lambda s2=s2: nc.scalar.activation(out=ee[s2][:], in_=ee[s2][:], func=AF.Exp), reads=[("ee", s2)], writes=[("ee", s2)])
                            kb.op("dve", lambda s2=s2: nc.vector.tensor_tensor(out=r2[s2][:], in0=mx[s2][:, 1:2], in1=mx[s2][:, 0:1], op=ALU.subtract),
                                  reads=[("mx", s2)], writes=[("r2", s2)])
                            kb.op("act", lambda s2=s2: nc.scalar.activation(out=r2[s2][:], in_=r2[s2][:], func=AF.Exp), reads=[("r2", s2)], writes=[("r2", s2)])
                            kb.op("dve", lambda s2=s2: nc.vector.tensor_scalar(out=r2[s2][:], in0=r2[s2][:], scalar1=1.0, scalar2=None, op0=ALU.add),
                                  reads=[("r2", s2)], writes=[("r2", s2)])
                            kb.op("dve", lambda s2=s2: nc.vector.reciprocal(out=r2[s2][:], in_=r2[s2][:]), reads=[("r2", s2)], writes=[("r2", s2)])
                            kb.op("dve", lambda s2=s2: nc.vector.tensor_scalar(out=mk[s2][:], in0=lg[s2][:], scalar1=mx[s2][:, 1:2], scalar2=None,
                                                                              op0=ALU.is_ge), reads=[("lg", s2), ("mx", s2)], writes=[("mk", s2)])
                            kb.op("dve", lambda s2=s2: nc.vector.scalar_tensor_tensor(
                                out=cmb[s2][:], in0=ee[s2][:], scalar=r2[s2][:, 0:1], in1=mk[s2][:], op0=ALU.mult, op1=ALU.mult),
                                reads=[("ee", s2), ("r2", s2), ("mk", s2)], writes=[("cmb", s2)])
                            kb.op("pe", lambda s2=s2, sub=sub: nc.tensor.transpose(
                                out=PS[5][0:8, sub * 128:(sub + 1) * 128], in_=cmb[s2][:], identity=ident_f[:]),
                                reads=[("cmb", s2), "ident_f"], writes=["ps5"])
                        kb.op("act", lambda s=s, T=T: nc.scalar.copy(out=cstg[s][:, :T], in_=PS[5][0:8, :T]), reads=["ps5"], writes=[("cstg", s)])
                        kb.dma(sp, COMBT[:, t0:t0 + T], cstg[s][:, :T], reads=[("cstg", s)], semkey=("oCB", s))
                    pending_router.append(router)
            if pending_router:
                pending_router.pop()()
            kb.barrier()

        with contextlib.ExitStack() as st:
            Wg = [sb(st, "Wg%d" % i, [128, 8, 768], BF16) for i in range(2)]
            Wu = [sb(st, "Wu%d" % i, [128, 8, 768], BF16) for i in range(2)]
            Wd = [sb(st, "Wd%d" % i, [128, 6, D], BF16) for i in range(2)]
            xts = [sb(st, "xt%d" % i, [128, 8, 512], F32) for i in range(2)]
            hTs = [sb(st, "hTs%d" % i, [128, 8, 512], BF16) for i in range(2)]
            aT = [sb(st, "aT%d" % i, [128, 6, 512], BF16) for i in range(2)]
            sgs = [sb(st, "sg%d" % i, [128, 512], F32) for i in range(2)]
            cb = [sb(st, "cb%d" % i, [128, 512], F32) for i in range(2)]
            t4 = [sb(st, "t4%d" % i, [128, 512], F32) for i in range(2)]
            blks = [b for b in BLOCKS if (need_ctx or not b[2])]
            passes = [(e, g) for e in range(NEXP if is_moe else 1) for g in range(4)]
            final_layer = (l == nlayers - 1)

            def load_w(pi):
                e, g = passes[pi]
                f0, nf = FGROUPS[g]
                s = pi % 2
                if is_moe:
                    gsrc, usrc, dsrc = moe_g[jl, e], moe_u[jl, e], moe_d[jl, e]
                else:
                    gsrc, usrc, dsrc = ffn_g[jl], ffn_u[jl], ffn_d[jl]
                kb.dma(pool, Wg[s][:, :, 0:nf * 128], gsrc.rearrange("(k p) f -> p k f", p=128)[:, :, f0 * 128:(f0 + nf) * 128],
                       writes=[("Wg", s)], semkey=("Wg", s))
                kb.dma(pool, Wu[s][:, :, 0:nf * 128], usrc.rearrange("(k p) f -> p k f", p=128)[:, :, f0 * 128:(f0 + nf) * 128],
                       writes=[("Wu", s)], semkey=("Wu", s))
                kb.dma(pool, Wd[s][:, 0:nf, :], dsrc[f0 * 128:(f0 + nf) * 128, :].rearrange("(c p) d -> p c d", p=128),
                       writes=[("Wd", s)], semkey=("Wd", s))

            iters = [(pi, bi) for pi in range(len(passes)) for bi in range(len(blks))]

            def load_act(n):
                pi, bi = iters[n]
                e = passes[pi][0]
                t0, T, isctx = blks[bi]
                s = n % 2
                kb.dma(sp, hTs[s][:, :, :T], HT.rearrange("(k p) t -> p k t", p=128)[:, :, t0:t0 + T],
                       writes=[("hT", s)], semkey=("hT4", s))
                kb.dma(sp, xts[s][:, :, :T], R.rearrange("(k p) t -> p k t", p=128)[:, :, t0:t0 + T],
                       reads=[("R", bi)], writes=[("xt", s)], semkey=("xt4", s))
                if is_moe:
                    kb.dma(sp, cb[s][:, :T], COMBT[e, t0:t0 + T].partition_broadcast(128), writes=[("cb", s)], semkey=("cb", s))

            load_w(0)
            load_act(0)
            it = 0
            gub = [0]
            dbk = [0]
            for pi, (e, g) in enumerate(passes):
                if pi + 1 < len(passes):
                    load_w(pi + 1)
                f0, nf = FGROUPS[g]
                ws = pi % 2
                last_pass = (pi == len(passes) - 1)
                for bi, (t0, T, isctx) in enumerate(blks):
                    s = it % 2
                    if it + 1 < len(iters):
                        load_act(it + 1)
                    it += 1
                    which = 1 if isctx else 0
                    xt, hT = xts[s], hTs[s]
                    for fc in range(nf):
                        bg = (gub[0] % 2) * 2
                        gub[0] += 1
                        bu = bg + 1
                        gk, uk = "ps%d" % bg, "ps%d" % bu
                        for k in range(8):
                            kb.op("pe", lambda k=k, fc=fc, bg=bg: nc.tensor.matmul(
                                PS[bg][:, :T], lhsT=Wg[ws][:, k, fc * 128:(fc + 1) * 128], rhs=hT[:, k, :T],
                                start=(k == 0), stop=(k == 7)), reads=[("Wg", ws), ("hT", s)], writes=[gk])
                        for k in range(8):
                            kb.op("pe", lambda k=k, fc=fc, bu=bu: nc.tensor.matmul(
                                PS[bu][:, :T], lhsT=Wu[ws][:, k, fc * 128:(fc + 1) * 128], rhs=hT[:, k, :T],
                                start=(k == 0), stop=(k == 7)), reads=[("Wu", ws), ("hT", s)], writes=[uk])
                        sgi = fc % 2
                        kb.op("act", lambda bg=bg, sgi=sgi: nc.scalar.activation(out=sgs[sgi][:, :T], in_=PS[bg][:, :T], func=AF.Silu),
                              reads=[gk], writes=[("sg", sgi)])
                        kb.op("dve", lambda bu=bu, sgi=sgi, fc=fc: nc.vector.tensor_tensor(
                            out=aT[s][:, fc, :T], in0=sgs[sgi][:, :T], in1=PS[bu][:, :T], op=ALU.mult),
                            reads=[uk, ("sg", sgi)], writes=[("aT", s)])
                    for co in range(8):
                        bd = 4 + (dbk[0] % 3)
                        dbk[0] += 1
                        dk = "ps%d" % bd
                        for fc in range(nf):
                            kb.op("pe", lambda fc=fc, co=co, bd=bd: nc.tensor.matmul(
                                PS[bd][:, :T], lhsT=Wd[ws][:, fc, co * 128:(co + 1) * 128], rhs=aT[s][:, fc, :T],
                                start=(fc == 0), stop=(fc == nf - 1)), reads=[("Wd", ws), ("aT", s)], writes=[dk])
                        if is_moe:
                            ti = co % 2
                            kb.op("dve", lambda co=co, bd=bd, ti=ti: nc.vector.scalar_tensor_tensor(
                                out=t4[ti][:, :T], in0=PS[bd][:, :T], scalar=mods[:, l, 40 + co, which:which + 1], in1=cb[s][:, :T],
                                op0=ALU.mult, op1=ALU.mult), reads=[dk, "mods", ("cb", s)], writes=[("t4", ti)])
                            kb.op("pool", lambda co=co, ti=ti: nc.gpsimd.tensor_tensor(
                                out=xt[:, co, :T], in0=xt[:, co, :T], in1=t4[ti][:, :T], op=ALU.add),
                                reads=[("t4", ti), ("xt", s)], writes=[("xt", s)])
                        else:
                            kb.op("dve", lambda co=co, bd=bd: nc.vector.scalar_tensor_tensor(
                                out=xt[:, co, :T], in0=PS[bd][:, :T], scalar=mods[:, l, 40 + co, which:which + 1], in1=xt[:, co, :T],
                                op0=ALU.mult, op1=ALU.add), reads=[dk, "mods", ("xt", s)], writes=[("xt", s)])
                    if last_pass and final_layer:
                        if not isctx:
                            kb.dma(sp, outT.rearrange("(k p) t -> p k t", p=128)[:, :, t0 - CL:t0 - CL + T], xt[:, :, :T],
                                   reads=[("xt", s)], semkey=("oR4", s))
                    else:
                        kb.dma(sp, R.rearrange("(k p) t -> p k t", p=128)[:, :, t0:t0 + T], xt[:, :, :T],
                               reads=[("xt", s)], writes=[("R", bi)], semkey=("oR4", s))
            kb.barrier()

    return nc


def _consts():
    c = {}
    c["c_ident"] = np.eye(128, dtype=np.float32)
    bo = np.zeros((128, 128), np.float32)
    bo[:64, :64] = 1
    bo[64:, 64:] = 1
    c["c_bones"] = bo
    P = np.zeros((128, 128), np.float32)
    for m in range(128):
        partner = m + 16 if (m % 32) < 16 else m - 16
        P[partner, m] = 1
    c["c_perm"] = P
    t = np.arange(128)
    uf = (t[:, None] <= t[None, :]).astype(np.float32)
    ub = (t[:, None] >= t[None, :]).astype(np.float32)
    c["c_uf"], c["c_ub"] = uf, ub
    c["c_negf"] = np.tile((uf - 1.0) * 30000.0, (1, 4)).astype(np.float32)
    c["c_negb"] = np.tile((ub - 1.0) * 30000.0, (1, 4)).astype(np.float32)
    pos = np.arange(L)
    row, colp = pos // 64, pos % 64
    freqs = (10000.0 ** (-np.arange(16, dtype=np.float32) / 16)).astype(np.float32)
    C = np.zeros((128, L), np.float32)
    S = np.zeros((128, L), np.float32)
    for p in range(128):
        dd = p % 64
        pp = row if dd < 32 else colp
        ang = pp.astype(np.float32) * freqs[dd % 16]
        C[p] = np.cos(ang)
        S[p] = np.sin(ang) * (-1.0 if (dd % 32) < 16 else 1.0)
    c["c_ropeC"], c["c_ropeS"] = C, S
    return c


def _prep_shared(inp):
    f = np.float32
    a = lambda v: np.ascontiguousarray(np.asarray(v, dtype=f))
    sh = {}
    sh["w_mod"] = a(inp["w_mod"])
    sh["bmodT"] = a(np.asarray(inp["b_mod"]).reshape(DEPTH, 48, 128).transpose(2, 0, 1))
    sh["g1T"] = a(np.asarray(inp["norm1_g"]).reshape(DEPTH, 8, 128).transpose(2, 0, 1))
    sh["g2T"] = a(np.asarray(inp["norm2_g"]).reshape(DEPTH, 8, 128).transpose(2, 0, 1))
    sh["w_in"] = a(inp["w_in"])
    sh["w_out"] = a(inp["w_out"])
    sh["gvg"] = a(inp["gm_v_g"])
    sh["wsT"] = a(np.asarray(inp["gm_ws"]).transpose(0, 1, 3, 2))
    p = np.arange(128)
    bsv = np.asarray(inp["gm_bs"])
    gb = np.zeros((128, DEPTH, 2, 128), f)
    for j in range(2):
        gb[:, :, j, :] = bsv[:, 2 * j + (p // 64), :].transpose(1, 0, 2)
    sh["gbs"] = gb
    sh["qgT"] = a(np.asarray(inp["att_q_g"])[:, p % 64].T)
    sh["kgT"] = a(np.asarray(inp["att_k_g"])[:, p % 64].T)
    sk = np.asarray(inp["att_sink"])
    sl = np.zeros((128, DEPTH, 2, 2), f)
    for kv in range(2):
        for ti in range(2):
            sl[:, :, kv, ti] = sk[:, 4 * kv + 2 * ti + (p // 64)].T
    sh["sinkL"] = sl
    sh["convw"] = a(np.asarray(inp["ssm_conv_w"]).reshape(DEPTH, 5, 6, 128).transpose(3, 0, 2, 1))
    sh["convb"] = a(np.asarray(inp["ssm_conv_b"]).reshape(DEPTH, 6, 128).transpose(2, 0, 1))
    sh["dtb"] = a(np.asarray(inp["ssm_dt_bias"]).reshape(DEPTH, 8))
    sh["alog"] = a(np.asarray(inp["ssm_a_log"]).reshape(DEPTH, 8))
    sh["dskE"] = a(np.repeat(np.asarray(inp["ssm_d"]), 64, axis=1))
    sh["sng"] = a(inp["ssm_norm_g"])
    sh["ffn_g"] = a(inp["ffn_w_gate"])
    sh["ffn_u"] = a(inp["ffn_w_up"])
    sh["ffn_d"] = a(inp["ffn_w_down"])
    sh["moe_r"] = a(inp["moe_router"])
    sh["moe_g"] = a(inp["moe_w_gate"])
    sh["moe_u"] = a(inp["moe_w_up"])
    sh["moe_d"] = a(inp["moe_w_down"])
    sh.update(_consts())
    return sh


def _prep_core(inp, b):
    f = np.float32
    x = np.asarray(inp["x"][b], dtype=f)
    ctx = np.asarray(inp["ctx"][b], dtype=f)
    xT0 = np.ascontiguousarray(np.concatenate([ctx.T, x.T], axis=1))
    c = np.asarray(inp["c"][b], dtype=f).reshape(8, 128).T
    cc = np.asarray(inp["c_ctx"], dtype=f).reshape(8, 128).T
    cs = np.ascontiguousarray(np.stack([c, cc], axis=-1))
    return {"xT0": xT0, "cs": cs}


_NC_CACHE = {}


def kernel(**inputs):
    if "nc" not in _NC_CACHE:
        _NC_CACHE["nc"] = build_program()
    nc = _NC_CACHE["nc"]
    sh = _prep_shared(inputs)
    in_maps = []
    for b in range(8):
        m = dict(sh)
        m.update(_prep_core(inputs, b))
        in_maps.append(m)
    res = run_bass_kernel_spmd(nc, in_maps, core_ids=list(range(8)))
    out = np.stack([np.ascontiguousarray(r["outT"].T) for r in res.results], axis=0)
    return out.astype(np.float32)
```
